# Optimizing a Trainium2 kernel written in Bass

```python
import math
import jax
import jax.numpy as jnp
from jax import lax
import numpy as np

D_MODEL = 1024
BATCH = 8
SEQ = 4096
DEPTH = 4

GRID_W = 64
CTX_LEN = 256
N_MIXERS = 4
GROUP_W = D_MODEL // N_MIXERS
HEAD_DIM = 64
N_HEADS = GROUP_W // HEAD_DIM
CONV_W = 4
CONV_PAD = (CONV_W // 2, CONV_W - 1 - CONV_W // 2)
LRU_C = 8.0
RWKV_LORA_W = 32
RWKV_LORA_A = 32
RWKV_LORA_G = 64
RWKV_GN_EPS = 64e-5
CHUNK = 64
D_FF = 2816
N_EXPERTS = 8
TOP_K = 2
D_FF_EXPERT = 1408
MOE_BLOCK = 256
N_DENSE = (DEPTH + 1) // 2
N_MOE = DEPTH // 2
EPS = 1e-6

A_COLS = 2 * GROUP_W
B_COLS = 3 * GROUP_W + 2 * RWKV_LORA_W + 2 * RWKV_LORA_A + RWKV_LORA_G
C_COLS = 4 * GROUP_W + 4 * N_HEADS
D_COLS = 4 * GROUP_W + 4 * N_HEADS
OFF_B = A_COLS
OFF_C = OFF_B + B_COLS
OFF_D = OFF_C + C_COLS
IN_COLS = OFF_D + D_COLS

kernel_name = 'hybrid_group_diffusion_trunk'


def rms_norm(x, g):
    xf = x.astype(jnp.float32)
    y = xf * lax.rsqrt(jnp.mean(xf * xf, axis=-1, keepdims=True) + EPS)
    return (y * g.astype(jnp.float32)).astype(x.dtype)


def heads(t):
    return t.reshape(*t.shape[:-1], N_HEADS, HEAD_DIM)


def merge_heads(t):
    return t.reshape(*t.shape[:-2], GROUP_W)


def head_rms_norm(t, g):
    return merge_heads(rms_norm(heads(t), heads(g)))


def l2_normalize(t):
    t = t.astype(jnp.float32)
    return t * lax.rsqrt(jnp.sum(t * t, axis=-1, keepdims=True) + EPS)


def centred_dwconv(t, w):
    return lax.conv_general_dilated(t, w[:, None, :].astype(t.dtype), (1,), [CONV_PAD],
                                    dimension_numbers=('NWC', 'WIO', 'NWC'),
                                    feature_group_count=t.shape[-1])


def token_shift_bidir(t, mu):
    prev = jnp.pad(t[:, :-1], ((0, 0), (1, 0), (0, 0)))
    nxt = jnp.pad(t[:, 1:], ((0, 0), (0, 1), (0, 0)))
    return t + mu[0] * (prev - t) + mu[1] * (nxt - t)


def bidir_stack(t):
    return jnp.stack([t, jnp.flip(t, axis=1)], axis=0)


def flip_dir1(t):
    return jnp.stack([t[0], jnp.flip(t[1], axis=1)], axis=0)


def bidir_merge(t):
    return t[0] + jnp.flip(t[1], axis=1)


def raster_to_colmajor(t, rows):
    b, s, ch = t.shape
    return t.reshape(b, rows, GRID_W, ch).transpose(0, 2, 1, 3).reshape(b, s, ch)


def colmajor_to_raster(t, rows):
    b, s, ch = t.shape
    return t.reshape(b, GRID_W, rows, ch).transpose(0, 2, 1, 3).reshape(b, s, ch)


def to_chunks(t):
    nd, bsz, s = t.shape[:3]
    t = t.reshape(nd, bsz, s // CHUNK, CHUNK, *t.shape[3:])
    return jnp.moveaxis(jnp.moveaxis(t, 4, 3), 2, 0)


def from_chunks(t):
    nc, nd, bsz, h, l, d = t.shape
    return jnp.swapaxes(jnp.moveaxis(t, 0, 2), 3, 4).reshape(nd, bsz, nc * l, h, d)


def linear_scan(a, b, h0):
    b = b.at[:, :, 0].add(a[:, :, 0] * h0)

    def combine(lhs, rhs):
        a_l, b_l = lhs
        a_r, b_r = rhs
        return a_l * a_r, a_r * b_l + b_r

    return lax.associative_scan(combine, (a, b), axis=2)[1]


def rglru_mixer(pc, pl, conv_w, conv_b, w_a, b_a, w_x, b_x, lam, ctx_out):
    f32 = jnp.float32

    def gates(u):
        ud = bidir_stack(u.astype(f32))
        uh = heads(ud)
        r = jax.nn.sigmoid(merge_heads(jnp.einsum('dbshi,dhij->dbshj', uh, w_a)) + b_a[:, None, None])
        i = jax.nn.sigmoid(merge_heads(jnp.einsum('dbshi,dhij->dbshj', uh, w_x)) + b_x[:, None, None])
        log_a = -LRU_C * r * jax.nn.softplus(-lam)[:, None, None]
        return jnp.exp(log_a), jnp.sqrt(-jnp.expm1(2.0 * log_a)) * (i * ud)

    a_c, b_c = gates(centred_dwconv(pc[..., :GROUP_W], conv_w) + conv_b)
    h_c = linear_scan(a_c, b_c, jnp.zeros_like(b_c[:, :, 0]))
    a_l, b_l = gates(centred_dwconv(pl[..., :GROUP_W], conv_w) + conv_b)
    h_l = linear_scan(a_l, b_l, h_c[:, :, -1])
    y_l = (bidir_merge(h_l) * jax.nn.gelu(pl[..., GROUP_W:].astype(f32))).astype(pl.dtype)
    y_c = (bidir_merge(h_c) * jax.nn.gelu(pc[..., GROUP_W:].astype(f32))).astype(pc.dtype) if ctx_out else None
    return y_c, y_l


def rwkv7_scan(r, w, kk, a, k, v, s0):
    xs = tuple(jnp.moveaxis(t, 2, 0) for t in (r, w, kk, a, k, v))

    def step(s, inp):
        r_t, w_t, kk_t, a_t, k_t, v_t = inp
        sk = jnp.einsum('...vk,...k->...v', s, kk_t)
        s = (s * w_t[..., None, :] - sk[..., :, None] * (kk_t * a_t)[..., None, :]
             + v_t[..., :, None] * k_t[..., None, :])
        return s, jnp.einsum('...vk,...k->...v', s, r_t)

    s_final, ys = lax.scan(step, s0, xs)
    return jnp.moveaxis(ys, 0, 2), s_final


def rwkv7_mixer(pc, pl, mu, w_up, w0, a_up, a0, g_up, k_k, k_a, r_k, ln_g, ln_b, ctx_out):
    f32 = jnp.float32
    G = GROUP_W

    def prepare(p):
        p = token_shift_bidir(p.astype(f32), mu)
        bsz, s = p.shape[:2]
        r, k, v = p[..., :G], p[..., G:2 * G], p[..., 2 * G:3 * G]
        off = 3 * G
        xw = p[..., off:off + 2 * RWKV_LORA_W].reshape(bsz, s, 2, RWKV_LORA_W)
        off += 2 * RWKV_LORA_W
        xa = p[..., off:off + 2 * RWKV_LORA_A].reshape(bsz, s, 2, RWKV_LORA_A)
        xg = p[..., off + 2 * RWKV_LORA_A:]
        log_w = -math.exp(-0.5) * jax.nn.sigmoid(
            jnp.einsum('bsdr,drc->dbsc', jnp.tanh(xw), w_up) + w0[:, None, None])
        a = jax.nn.sigmoid(jnp.einsum('bsdr,drc->dbsc', xa, a_up) + a0[:, None, None])
        kk = l2_normalize(heads(k * k_k))
        k_mod = k * (1.0 + (a - 1.0) * k_a)
        bonus = jnp.sum(jnp.sum(heads(r * k_mod * r_k), axis=-1, keepdims=True), axis=0) * heads(v)
        g = jax.nn.sigmoid(xg) @ g_up
        ins = (bidir_stack(heads(r)), flip_dir1(heads(jnp.exp(log_w))), bidir_stack(kk),
               flip_dir1(heads(a)), flip_dir1(heads(k_mod)), bidir_stack(heads(v)))
        return ins, bonus, g

    def finish(ys, bonus, g):
        y = bidir_merge(ys)
        mean = jnp.mean(y, axis=-1, keepdims=True)
        var = jnp.mean(jnp.square(y - mean), axis=-1, keepdims=True)
        y = merge_heads((y - mean) * lax.rsqrt(var + RWKV_GN_EPS)) * ln_g + ln_b
        return (y + merge_heads(bonus)) * g

    ins_c, bonus_c, g_c = prepare(pc)
    ins_l, bonus_l, g_l = prepare(pl)
    bsz = pc.shape[0]
    s0 = jnp.zeros((2, bsz, N_HEADS, HEAD_DIM, HEAD_DIM), f32)
    ys_c, s_c = rwkv7_scan(*ins_c, s0)
    ys_l, _ = rwkv7_scan(*ins_l, s_c)
    y_l = finish(ys_l, bonus_l, g_l).astype(pl.dtype)
    y_c = finish(ys_c, bonus_c, g_c).astype(pc.dtype) if ctx_out else None
    return y_c, y_l


def mlstm_chunked(q, k, v, log_i, log_f, state):
    qc, kc, vc = to_chunks(q), to_chunks(k), to_chunks(v)
    li, lf = to_chunks(log_i), to_chunks(log_f)
    causal = jnp.tril(jnp.ones((CHUNK, CHUNK), dtype=bool))
    b = jnp.cumsum(lf, axis=-1)
    log_d = jnp.where(causal, b[..., :, None] - b[..., None, :] + li[..., None, :], -jnp.inf)
    m_intra = jnp.max(log_d, axis=-1)
    b_last = b[..., -1]
    log_end = b_last[..., None] - b + li
    qk = jnp.einsum('...ld,...sd->...ls', qc, kc)

    def step(carry, inp):
        mem, nrm, m = carry
        q_, k_, v_, qk_, ld_, mi_, b_, bl_, le_ = inp
        m_inter = b_ + m[..., None]
        m_t = jnp.maximum(m_inter, mi_)
        w_inter = jnp.exp(m_inter - m_t)
        p = jnp.exp(ld_ - m_t[..., None]) * qk_
        num = (jnp.einsum('...ls,...sd->...ld', p, v_)
               + w_inter[..., None] * jnp.einsum('...vk,...lk->...lv', mem, q_))
        den = jnp.sum(p, axis=-1) + w_inter * jnp.einsum('...k,...lk->...l', nrm, q_)
        h = num / jnp.maximum(jnp.abs(den), jnp.exp(-m_t))[..., None]
        m_new = jnp.maximum(bl_ + m, jnp.max(le_, axis=-1))
        w_prev = jnp.exp(bl_ + m - m_new)
        w_new = jnp.exp(le_ - m_new[..., None])
        mem = w_prev[..., None, None] * mem + jnp.einsum('...l,...lv,...lk->...vk', w_new, v_, k_)
        nrm = w_prev[..., None] * nrm + jnp.einsum('...l,...lk->...k', w_new, k_)
        return (mem, nrm, m_new), h

    state, hs = lax.scan(step, state, (qc, kc, vc, qk, log_d, m_intra, b, b_last, log_end))
    return from_chunks(hs), state


def mlstm_mixer(pc, pl, i_b, f_b, norm_g, rows, ctx_out):
    f32 = jnp.float32
    G = GROUP_W

    def prepare(p):
        p = p.astype(f32)
        bsz, s = p.shape[:2]
        q, k, v, o = p[..., :G], p[..., G:2 * G], p[..., 2 * G:3 * G], p[..., 3 * G:4 * G]
        gates = jnp.moveaxis(p[..., 4 * G:].reshape(bsz, s, 4, N_HEADS), 2, 0)
        log_i = gates[:2] + i_b[:, None, None]
        log_f = jax.nn.log_sigmoid(gates[2:] + f_b[:, None, None])
        ins = (bidir_stack(heads(q)), bidir_stack(heads(k) / math.sqrt(HEAD_DIM)),
               bidir_stack(heads(v)), flip_dir1(log_i), flip_dir1(log_f))
        return ins, o

    def finish(hs, o):
        return head_rms_norm(merge_heads(bidir_merge(hs)), norm_g) * jax.nn.sigmoid(o)

    ins_c, o_c = prepare(pc)
    ins_l, o_l = prepare(raster_to_colmajor(pl, rows))
    bsz = pc.shape[0]
    state0 = (jnp.zeros((2, bsz, N_HEADS, HEAD_DIM, HEAD_DIM), f32),
              jnp.zeros((2, bsz, N_HEADS, HEAD_DIM), f32),
              jnp.zeros((2, bsz, N_HEADS), f32))
    h_c, st_c = mlstm_chunked(*ins_c, state0)
    h_l, _ = mlstm_chunked(*ins_l, st_c)
    y_l = colmajor_to_raster(finish(h_l, o_l), rows).astype(pl.dtype)
    y_c = finish(h_c, o_c).astype(pc.dtype) if ctx_out else None
    return y_c, y_l


def gdn_chunked(q, k, v, log_alpha, beta, state):
    qc, kc, vc = to_chunks(q), to_chunks(k), to_chunks(v)
    la, bt = to_chunks(log_alpha), to_chunks(beta)
    incl = jnp.tril(jnp.ones((CHUNK, CHUNK), dtype=bool))
    strict = jnp.tril(jnp.ones((CHUNK, CHUNK), dtype=bool), k=-1)
    gam = jnp.cumsum(la, axis=-1)
    decay = jnp.exp(jnp.where(incl, gam[..., :, None] - gam[..., None, :], -jnp.inf))
    k_beta = kc * bt[..., None]
    lower = jnp.where(strict, jnp.einsum('...id,...jd->...ij', k_beta, kc) * decay, 0.0)
    rhs = jnp.concatenate([vc * bt[..., None], k_beta * jnp.exp(gam)[..., None]], axis=-1)
    sol = lax.linalg.triangular_solve(lower + jnp.eye(CHUNK, dtype=lower.dtype), rhs,
                                      left_side=True, lower=True, unit_diagonal=True)
    u, w = sol[..., :HEAD_DIM], sol[..., HEAD_DIM:]
    qk = jnp.einsum('...id,...jd->...ij', qc, kc) * decay
    q_dec = qc * jnp.exp(gam)[..., None]
    k_dec = kc * jnp.exp(gam[..., -1:] - gam)[..., None]
    g_last = jnp.exp(gam[..., -1])

    def step(s, inp):
        u_, w_, qk_, qd_, kd_, gl_ = inp
        v_new = u_ - jnp.einsum('...lk,...kv->...lv', w_, s)
        o = jnp.einsum('...lk,...kv->...lv', qd_, s) + jnp.einsum('...ls,...sv->...lv', qk_, v_new)
        s = gl_[..., None, None] * s + jnp.einsum('...lk,...lv->...kv', kd_, v_new)
        return s, o

    state, os_ = lax.scan(step, state, (u, w, qk, q_dec, k_dec, g_last))
    return from_chunks(os_), state


def gdn_mixer(pc, pl, conv_w, a_log, dt_bias, norm_g, rows, ctx_out):
    f32 = jnp.float32
    G = GROUP_W

    def prepare(p):
        bsz, s = p.shape[:2]
        qkv = jax.nn.silu(centred_dwconv(p[..., :3 * G], conv_w)).astype(f32)
        q = l2_normalize(heads(qkv[..., :G])) * HEAD_DIM ** -0.5
        k = l2_normalize(heads(qkv[..., G:2 * G]))
        v = heads(qkv[..., 2 * G:])
        ab = jnp.moveaxis(p[..., 4 * G:].astype(f32).reshape(bsz, s, 4, N_HEADS), 2, 0)
        log_alpha = -jnp.exp(a_log)[:, None, None] * jax.nn.softplus(ab[:2] + dt_bias[:, None, None])
        beta = jax.nn.sigmoid(ab[2:])
        ins = (bidir_stack(q), bidir_stack(k), bidir_stack(v), flip_dir1(log_alpha), flip_dir1(beta))
        return ins, p[..., 3 * G:4 * G]

    def finish(os_, gate):
        return head_rms_norm(merge_heads(bidir_merge(os_)), norm_g) * jax.nn.silu(gate.astype(f32))

    ins_c, gate_c = prepare(pc)
    ins_l, gate_l = prepare(raster_to_colmajor(pl, rows))
    bsz = pc.shape[0]
    s0 = jnp.zeros((2, bsz, N_HEADS, HEAD_DIM, HEAD_DIM), f32)
    o_c, s_c = gdn_chunked(*ins_c, s0)
    o_l, _ = gdn_chunked(*ins_l, s_c)
    y_l = colmajor_to_raster(finish(o_l, gate_l), rows).astype(pl.dtype)
    y_c = finish(o_c, gate_c).astype(pc.dtype) if ctx_out else None
    return y_c, y_l


def swiglu(h, w_gate, w_up, w_down):
    return (jax.nn.silu(h @ w_gate) * (h @ w_up)) @ w_down


def moe_swiglu(h, router, w_gate, w_up, w_down):
    t, d = h.shape
    logits = (h @ router).astype(jnp.float32)
    top_logits, top_idx = lax.top_k(logits, TOP_K)
    gate = jax.nn.softmax(top_logits, axis=-1)
    tk = t * TOP_K
    flat_e = top_idx.reshape(tk)
    flat_tok = jnp.arange(tk, dtype=jnp.int32) // TOP_K
    order = jnp.argsort(flat_e)
    se, stok, sw = flat_e[order], flat_tok[order], gate.reshape(tk)[order]
    counts = jnp.zeros((N_EXPERTS,), jnp.int32).at[flat_e].add(1)
    start = jnp.cumsum(counts) - counts
    padded = (counts + MOE_BLOCK - 1) // MOE_BLOCK * MOE_BLOCK
    pad_start = jnp.cumsum(padded) - padded
    pad_end = pad_start + padded
    pos = pad_start[se] + jnp.arange(tk, dtype=jnp.int32) - start[se]
    n_blocks = -(-tk // MOE_BLOCK) + N_EXPERTS
    buf_tok = jnp.full((n_blocks * MOE_BLOCK,), t, jnp.int32).at[pos].set(stok)
    buf_w = jnp.zeros((n_blocks * MOE_BLOCK,), h.dtype).at[pos].set(sw.astype(h.dtype))
    block_e = jnp.clip(jnp.searchsorted(pad_end, jnp.arange(n_blocks, dtype=jnp.int32) * MOE_BLOCK,
                                        side='right'), 0, N_EXPERTS - 1)
    h_pad = jnp.concatenate([h, jnp.zeros((1, d), h.dtype)], axis=0)

    def expert_block(args):
        tok, e = args
        xb = h_pad[tok]
        return (jax.nn.silu(xb @ w_gate[e]) * (xb @ w_up[e])) @ w_down[e]

    yb = lax.map(expert_block, (buf_tok.reshape(n_blocks, MOE_BLOCK), block_e))
    y = jnp.zeros((t + 1, d), h.dtype).at[buf_tok].add(yb.reshape(-1, d) * buf_w[:, None])
    return y[:t]


def setup_inputs(seed: int = 0) -> dict:
    key = jax.random.key(seed)
    keys = iter(jax.random.split(key, 64))

    def nrm(shape, scale):
        return scale * jax.random.normal(next(keys), shape, jnp.float32)

    def unif(shape, lo, hi):
        return jax.random.uniform(next(keys), shape, jnp.float32, lo, hi)

    G, H = GROUP_W, N_HEADS
    lru_p = unif((DEPTH, 2, G), 0.9, 0.999) ** (1.0 / LRU_C)
    dt = jnp.exp(unif((DEPTH, 2, H), math.log(1e-3), math.log(1e-1)))
    return {
        'x': nrm((BATCH, SEQ, D_MODEL), 1.0),
        'c': nrm((BATCH, D_MODEL), 1.0),
        'ctx': nrm((BATCH, CTX_LEN, D_MODEL), 1.0),
        'c_ctx': nrm((D_MODEL,), 1.0),
        'mod_w': nrm((DEPTH, D_MODEL, 6 * D_MODEL), 0.5 * D_MODEL ** -0.5),
        'mod_b': nrm((DEPTH, 6 * D_MODEL), 0.02),
        'norm_mix_g': 1.0 + nrm((DEPTH, D_MODEL), 0.02),
        'norm_ffn_g': 1.0 + nrm((DEPTH, D_MODEL), 0.02),
        'w_in': nrm((DEPTH, D_MODEL, IN_COLS), D_MODEL ** -0.5),
        'w_out': nrm((DEPTH, D_MODEL, D_MODEL), D_MODEL ** -0.5),
        'lru_conv_w': nrm((DEPTH, CONV_W, G), CONV_W ** -0.5),
        'lru_conv_b': nrm((DEPTH, G), 0.02),
        'lru_w_a': nrm((DEPTH, 2, H, HEAD_DIM, HEAD_DIM), HEAD_DIM ** -0.5),
        'lru_b_a': nrm((DEPTH, 2, G), 0.02),
        'lru_w_x': nrm((DEPTH, 2, H, HEAD_DIM, HEAD_DIM), HEAD_DIM ** -0.5),
        'lru_b_x': nrm((DEPTH, 2, G), 0.02),
        'lru_lambda': jnp.log(lru_p) - jnp.log1p(-lru_p),
        'rwkv_mu': unif((DEPTH, 2, B_COLS), 0.0, 0.5),
        'rwkv_w_up': nrm((DEPTH, 2, RWKV_LORA_W, G), 0.1),
        'rwkv_w0': unif((DEPTH, 2, G), -3.0, 2.0),
        'rwkv_a_up': nrm((DEPTH, 2, RWKV_LORA_A, G), 0.1),
        'rwkv_a0': nrm((DEPTH, 2, G), 0.1),
        'rwkv_g_up': nrm((DEPTH, RWKV_LORA_G, G), RWKV_LORA_G ** -0.5),
        'rwkv_k_k': 0.85 + nrm((DEPTH, G), 0.02),
        'rwkv_k_a': 1.0 + nrm((DEPTH, G), 0.02),
        'rwkv_r_k': nrm((DEPTH, G), 0.1),
        'rwkv_ln_g': 1.0 + nrm((DEPTH, G), 0.02),
        'rwkv_ln_b': nrm((DEPTH, G), 0.02),
        'mlstm_i_b': nrm((DEPTH, 2, H), 0.1),
        'mlstm_f_b': jnp.linspace(3.0, 6.0, H, dtype=jnp.float32) + nrm((DEPTH, 2, H), 0.05),
        'mlstm_norm_g': 1.0 + nrm((DEPTH, G), 0.02),
        'gdn_conv_w': nrm((DEPTH, CONV_W, 3 * G), CONV_W ** -0.5),
        'gdn_a_log': jnp.log(unif((DEPTH, 2, H), 1.0, 16.0)),
        'gdn_dt_bias': dt + jnp.log(-jnp.expm1(-dt)),
        'gdn_norm_g': 1.0 + nrm((DEPTH, G), 0.02),
        'ffn_w_gate': nrm((N_DENSE, D_MODEL, D_FF), D_MODEL ** -0.5),
        'ffn_w_up': nrm((N_DENSE, D_MODEL, D_FF), D_MODEL ** -0.5),
        'ffn_w_down': nrm((N_DENSE, D_FF, D_MODEL), D_FF ** -0.5),
        'moe_router': nrm((N_MOE, D_MODEL, N_EXPERTS), D_MODEL ** -0.5),
        'moe_w_gate': nrm((N_MOE, N_EXPERTS, D_MODEL, D_FF_EXPERT), D_MODEL ** -0.5),
        'moe_w_up': nrm((N_MOE, N_EXPERTS, D_MODEL, D_FF_EXPERT), D_MODEL ** -0.5),
        'moe_w_down': nrm((N_MOE, N_EXPERTS, D_FF_EXPERT, D_MODEL), D_FF_EXPERT ** -0.5),
        'final_norm_g': 1.0 + nrm((D_MODEL,), 0.02),
    }


def reference(x, c, ctx, c_ctx, mod_w, mod_b, norm_mix_g, norm_ffn_g, w_in, w_out,
              lru_conv_w, lru_conv_b, lru_w_a, lru_b_a, lru_w_x, lru_b_x, lru_lambda,
              rwkv_mu, rwkv_w_up, rwkv_w0, rwkv_a_up, rwkv_a0, rwkv_g_up, rwkv_k_k, rwkv_k_a,
              rwkv_r_k, rwkv_ln_g, rwkv_ln_b, mlstm_i_b, mlstm_f_b, mlstm_norm_g,
              gdn_conv_w, gdn_a_log, gdn_dt_bias, gdn_norm_g,
              ffn_w_gate, ffn_w_up, ffn_w_down, moe_router, moe_w_gate, moe_w_up, moe_w_down,
              final_norm_g):
    rows = x.shape[1] // GRID_W
    silu_c = jax.nn.silu(c)
    silu_cc = jax.nn.silu(c_ctx)
    xc = ctx
    for l in range(DEPTH):
        last = l == DEPTH - 1
        mod = silu_c @ mod_w[l] + mod_b[l]
        mod_c = silu_cc @ mod_w[l] + mod_b[l]
        sh1, sc1, gt1, sh2, sc2, gt2 = [m[:, None] for m in jnp.split(mod, 6, axis=-1)]
        csh1, csc1, cgt1, csh2, csc2, cgt2 = jnp.split(mod_c, 6, axis=-1)
        h = rms_norm(x, norm_mix_g[l]) * (1.0 + sc1) + sh1
        hc = rms_norm(xc, norm_mix_g[l]) * (1.0 + csc1) + csh1
        p = h @ w_in[l]
        pc = hc @ w_in[l]
        ya_c, ya = rglru_mixer(pc[..., :OFF_B], p[..., :OFF_B], lru_conv_w[l], lru_conv_b[l],
                               lru_w_a[l], lru_b_a[l], lru_w_x[l], lru_b_x[l], lru_lambda[l], not last)
        yb_c, yb = rwkv7_mixer(pc[..., OFF_B:OFF_C], p[..., OFF_B:OFF_C], rwkv_mu[l], rwkv_w_up[l],
                               rwkv_w0[l], rwkv_a_up[l], rwkv_a0[l], rwkv_g_up[l], rwkv_k_k[l],
                               rwkv_k_a[l], rwkv_r_k[l], rwkv_ln_g[l], rwkv_ln_b[l], not last)
        yc_c, yc = mlstm_mixer(pc[..., OFF_C:OFF_D], p[..., OFF_C:OFF_D], mlstm_i_b[l], mlstm_f_b[l],
                               mlstm_norm_g[l], rows, not last)
        yd_c, yd = gdn_mixer(pc[..., OFF_D:], p[..., OFF_D:], gdn_conv_w[l], gdn_a_log[l],
                             gdn_dt_bias[l], gdn_norm_g[l], rows, not last)
        x = x + gt1 * (jnp.concatenate([ya, yb, yc, yd], axis=-1) @ w_out[l])
        if not last:
            xc = xc + cgt1 * (jnp.concatenate([ya_c, yb_c, yc_c, yd_c], axis=-1) @ w_out[l])
        h2 = rms_norm(x, norm_ffn_g[l]) * (1.0 + sc2) + sh2
        tokens = h2.reshape(-1, D_MODEL)
        n_lat = tokens.shape[0]
        if not last:
            hc2 = rms_norm(xc, norm_ffn_g[l]) * (1.0 + csc2) + csh2
            tokens = jnp.concatenate([tokens, hc2.reshape(-1, D_MODEL)], axis=0)
        if l % 2 == 0:
            y = swiglu(tokens, ffn_w_gate[l // 2], ffn_w_up[l // 2], ffn_w_down[l // 2])
        else:
            y = moe_swiglu(tokens, moe_router[l // 2], moe_w_gate[l // 2], moe_w_up[l // 2],
                           moe_w_down[l // 2])
        x = x + gt2 * y[:n_lat].reshape(x.shape)
        if not last:
            xc = xc + cgt2 * y[n_lat:].reshape(xc.shape)
    return rms_norm(x, final_norm_g)
```

```python
import math
import numpy as np
import concourse.bass as bass
import concourse.mybir as mybir
from concourse.bass_utils import run_bass_kernel_spmd
from contextlib import ExitStack

F32 = mybir.dt.float32
AF = mybir.ActivationFunctionType
ALU = mybir.AluOpType
AX = mybir.AxisListType

D_MODEL = 1024
SEQ = 4096
CTX = 256
T = SEQ + CTX
NT = T // 128
G = 256
IN_COLS = 3552
OFF_B = 512
OFF_C = OFF_B + 960
OFF_D = OFF_C + 1040
D_FF = 2816
D_FFE = 1408
EPS = 1e-6

ENGS = ['pe', 'dve', 'act', 'pool', 'sp']
SAME_SYNC = {'pe': False, 'dve': True, 'act': True, 'pool': True, 'sp': True}

def _box(ap):
    t = ap.tensor
    name = t.name
    dims = ap.ap
    off = int(ap.offset)
    if str(ap.space) in ('SB', 'PSUM', 'SBUF'):
        row = dims[0][0] if dims[0][0] > 0 else 1
        p0 = off // row
        f0 = off % row
        p1 = p0 + dims[0][1]
        lo = hi = f0
        for st, cnt in dims[1:]:
            if st >= 0:
                hi += st * (cnt - 1)
            else:
                lo += st * (cnt - 1)
        return (name, p0, p1, lo, hi + 1)
    lo = hi = off
    for st, cnt in dims:
        if st >= 0:
            hi += st * (cnt - 1)
        else:
            lo += st * (cnt - 1)
    return (name, 0, 1, lo, hi + 1)


class Sched:
    def __init__(self, nc, es, n_dma=8):
        self.nc = nc
        self.es = es
        self.eng = dict(pe=nc.tensor, dve=nc.vector, act=nc.scalar, pool=nc.gpsimd, sp=nc.sync)
        self.sem = {}
        self.cnt = {}
        self.unit = {}
        for e in ENGS:
            self.sem[e] = es.enter_context(nc.semaphore('s_' + e))
            self.cnt[e] = 0
            self.unit[e] = 1
        self.n_dma = n_dma
        self.dma_rr = {}
        for q in ('sp', 'act', 'pool'):
            self.dma_rr[q] = 0
            for i in range(n_dma):
                c = ('dma', q, i)
                self.sem[c] = es.enter_context(nc.semaphore('d_%s%d' % (q, i)))
                self.cnt[c] = 0
                self.unit[c] = 16
        self.seen = {e: {} for e in ENGS}
        self.recs = {}
        self.nins = 0

    def _need(self, reads, writes):
        need = {}
        for aps, isw in ((reads, False), (writes, True)):
            for ap in aps:
                name, p0, p1, f0, f1 = _box(ap)
                if not isw and name.startswith('psb'):
                    isw, p0, p1, f0, f1 = True, 0, 128, 0, 1 << 30
                for r in self.recs.get(name, ()):
                    if r[0] < p1 and p0 < r[1] and r[2] < f1 and f0 < r[3]:
                        if isw or r[6]:
                            c, v = r[4], r[5]
                            if need.get(c, 0) < v:
                                need[c] = v
        return need

    def _record(self, reads, writes, clock, val):
        for aps, isw in ((reads, False), (writes, True)):
            for ap in aps:
                name, p0, p1, f0, f1 = _box(ap)
                if not isw and name.startswith('psb'):
                    isw, p0, p1, f0, f1 = True, 0, 128, 0, 1 << 30
                lst = self.recs.setdefault(name, [])
                if isw:
                    lst[:] = [r for r in lst if not (p0 <= r[0] and r[1] <= p1 and f0 <= r[2] and r[3] <= f1)]
                else:
                    lst[:] = [r for r in lst if not (r[4] == clock and not r[6] and p0 <= r[0] and r[1] <= p1 and f0 <= r[2] and r[3] <= f1)]
                lst.append((p0, p1, f0, f1, clock, val, isw))
                if len(lst) > 48:
                    self._prune(lst)

    def _prune(self, lst):
        def stale(r):
            c, v = r[4], r[5]
            for e in ENGS:
                if e == c and not SAME_SYNC[e]:
                    continue
                if self.seen[e].get(c, 0) < v:
                    return False
            return True
        lst[:] = [r for r in lst if not stale(r)]

    def _waits(self, e, need):
        eo = self.eng[e]
        for c, v in need.items():
            if c == e and not SAME_SYNC[e]:
                continue
            if self.seen[e].get(c, 0) >= v:
                continue
            eo.wait_ge(self.sem[c], v * self.unit[c])
            self.seen[e][c] = v

    def op(self, e, fn, reads, writes):
        need = self._need(reads, writes)
        self._waits(e, need)
        ins = fn(self.eng[e])
        self.cnt[e] += 1
        ins.then_inc(self.sem[e], 1)
        self._record(reads, writes, e, self.cnt[e])
        self.nins += 1
        return ins

    def dma(self, out, in_, q='sp', **kw):
        need = self._need([in_], [out])
        k = self.dma_rr[q]
        self.dma_rr[q] = (k + 1) % self.n_dma
        c = ('dma', q, k)
        if self.cnt[c] > 0:
            need[c] = max(need.get(c, 0), self.cnt[c])
        self._waits(q, need)
        ins = self.eng[q].dma_start(out=out, in_=in_, **kw)
        self.cnt[c] += 1
        ins.then_inc(self.sem[c], 16)
        self._record([in_], [out], c, self.cnt[c])
        self.nins += 1

    def barrier(self):
        for e in ENGS:
            need = {c: v for c, v in self.cnt.items() if v > 0 and c != e}
            self._waits(e, need)
        self.recs = {}

    def finish(self):
        need = {c: v for c, v in self.cnt.items() if v > 0 and c != 'sp'}
        self._waits('sp', need)

    def mm(self, out, lhsT, rhs, start=True, stop=True):
        self.op('pe', lambda e: e.matmul(out, lhsT, rhs, start=start, stop=stop), [lhsT, rhs] + ([] if start else [out]), [out])

    def tr(self, out, in_, ident):
        self.op('pe', lambda e: e.transpose(out, in_, ident), [in_, ident], [out])

    def act(self, out, in_, func, bias=None, scale=None, accum_out=None):
        kw = {}
        rd = [in_]
        wr = [out]
        if bias is not None:
            kw['bias'] = bias
            if not isinstance(bias, (int, float)):
                rd.append(bias)
        if scale is not None:
            kw['scale'] = scale
            if not isinstance(scale, (int, float)):
                rd.append(scale)
        if accum_out is not None:
            kw['accum_out'] = accum_out
            wr.append(accum_out)
        self.op('act', lambda e: e.activation(out, in_, func, **kw), rd, wr)

    def tt(self, out, in0, in1, op, e='dve'):
        self.op(e, lambda en: en.tensor_tensor(out, in0, in1, op), [in0, in1], [out])

    def ts(self, out, in0, s1, s2, op0, op1=None, e='dve', accum_out=None):
        rd = [in0]
        for s in (s1, s2):
            if s is not None and not isinstance(s, (int, float)):
                rd.append(s)
        wr = [out] + ([accum_out] if accum_out is not None else [])
        kw = {}
        if op1 is not None:
            kw['op1'] = op1
        if accum_out is not None:
            kw['accum_out'] = accum_out
        self.op(e, lambda en: en.tensor_scalar(out, in0, s1, s2, op0, **kw), rd, wr)

    def stt(self, out, in0, scalar, in1, op0, op1, e='dve'):
        rd = [in0, in1]
        if not isinstance(scalar, (int, float)):
            rd.append(scalar)
        self.op(e, lambda en: en.scalar_tensor_tensor(out, in0, scalar, in1, op0, op1), rd, [out])

    def copy(self, out, in_, e='dve'):
        if e == 'act':
            self.op(e, lambda en: en.copy(out, in_), [in_], [out])
        else:
            self.op(e, lambda en: en.tensor_copy(out, in_), [in_], [out])

    def memset(self, ap, val, e='dve'):
        self.op(e, lambda en: en.memset(ap, val), [], [ap])

    def reduce(self, out, in_, op, axis=None, e='dve'):
        axis = axis or AX.X
        self.op(e, lambda en: en.tensor_reduce(out, in_, axis, op), [in_], [out])

    def scan(self, out, d0, d1, init, op0, op1):
        rd = [d0, d1]
        if not isinstance(init, (int, float)):
            rd.append(init)
        self.op('dve', lambda en: en.tensor_tensor_scan(out, d0, d1, init, op0, op1), rd, [out])

    def recip(self, out, in_):
        self.op('dve', lambda en: en.reciprocal(out, in_), [in_], [out])


class Ctx:
    def __init__(self, nc, S, W, D, ps, ident):
        self.nc, self.S, self.W, self.D, self.ps, self.ident = nc, S, W, D, ps, ident
        self.uid = 0
        self.psi = 0

    def sb(self, es, shape, dt=F32, name=None):
        self.uid += 1
        return es.enter_context(self.nc.sbuf_tensor("%s_%d" % (name or "t", self.uid), list(shape), dt))

    def bank(self):
        b = self.ps[self.psi % 8]
        self.psi += 1
        return b


def bc_rows(ap, n):
    return ap.to_broadcast([n, ap.shape[1]])


def stage_mod(K, l):
    S, W, D = K.S, K.W, K.D
    with ExitStack() as es:
        cc = K.sb(es, [128, 8, 2])
        S.dma(cc[:], W['cc'])
        S.act(cc[:], cc[:], AF.Silu)
        mb = K.sb(es, [2, 6144])
        S.dma(mb[:], bc_rows(W['mod_b'][l:l + 1, :], 2))
        mo = K.sb(es, [2, 6144])
        wts = [K.sb(es, [128, 8, 512]) for _ in range(2)]
        wv = W['mod_w'][l].rearrange("(k p) c -> p k c", p=128)
        for n in range(12):
            wt = wts[n % 2]
            S.dma(wt[:], wv[:, :, n * 512:(n + 1) * 512], q=('sp' if n % 2 == 0 else 'pool'))
            ps = K.bank()
            for k in range(8):
                S.mm(ps[0:2, :], cc[:, k, :], wt[:, k, :], start=(k == 0), stop=(k == 7))
            S.tt(mo[:, n * 512:(n + 1) * 512], ps[0:2, :], mb[:, n * 512:(n + 1) * 512], ALU.add)
        S.dma(D['mod'], mo[:])
    S.barrier()


def load_mod_tiles(K, es, l, which, gname):
    S, W, D = K.S, K.W, K.D
    base = 3072 * which
    outs = []
    gt = K.sb(es, [128, 1024])
    S.dma(gt[:], bc_rows(W[gname][l:l + 1, :], 128))
    for seg in range(2):
        sh = K.sb(es, [128, 1024])
        sc = K.sb(es, [128, 1024])
        S.dma(sh[:], bc_rows(D['mod'][seg:seg + 1, base:base + 1024], 128), q='pool')
        S.dma(sc[:], bc_rows(D['mod'][seg:seg + 1, base + 1024:base + 2048], 128), q='pool')
        S.stt(sc[:], sc[:], 1.0, gt[:], ALU.add, ALU.mult)
        outs += [sc, sh]
    return outs


def norm_mod_T(K, es_tmp, xt, Gt, SHt, hT_dst, tmp):
    S = K.S
    junk, ss, h = tmp
    S.act(junk[:], xt[:], AF.Square, accum_out=ss[:, 0:1])
    S.ts(ss[:, 1:2], ss[:, 0:1], 1.0 / D_MODEL, EPS, ALU.mult, ALU.add)
    S.act(ss[:, 2:3], ss[:, 1:2], AF.Sqrt)
    S.recip(ss[:, 3:4], ss[:, 2:3])
    S.stt(h[:], xt[:], ss[:, 3:4], Gt[:], ALU.mult, ALU.mult)
    S.tt(h[:], h[:], SHt[:], ALU.add, e='pool')
    for b in range(2):
        ps = K.bank()
        for j in range(4):
            k = b * 4 + j
            S.tr(ps[:, j * 128:(j + 1) * 128], h[:, k * 128:(k + 1) * 128], K.ident[:])
        src = ps[:].rearrange("p (j t) -> p j t", j=4)
        if b == 0:
            S.copy(hT_dst[:, 0:4, :], src, e='dve')
        else:
            S.copy(hT_dst[:, 4:8, :], src, e='act')


def stage_inproj(K, l):
    S, W, D = K.S, K.W, K.D
    HALF = T // 2
    wv = W['w_in'][l].rearrange("(k p) c -> p k c", p=128)
    with ExitStack() as es:
        GL, SHL, GC, SHC = load_mod_tiles(K, es, l, 0, 'norm_mix_g')
        hT = K.sb(es, [128, 8, HALF])
        xts = [K.sb(es, [128, 1024]) for _ in range(2)]
        tmp = (K.sb(es, [128, 1024]), K.sb(es, [128, 4]), K.sb(es, [128, 1024]))
        wts = [K.sb(es, [128, 8, 128]) for _ in range(3)]
        ots = [K.sb(es, [128, HALF]) for _ in range(2)]
        for half in range(2):
            for ti in range(17):
                t = half * 17 + ti
                xt = xts[ti % 2]
                S.dma(xt[:], D['xres'][t * 128:(t + 1) * 128, :])
                isctx = t < 2
                norm_mod_T(K, es, xt, GC if isctx else GL, SHC if isctx else SHL,
                           hT[:, :, ti * 128:(ti + 1) * 128], tmp)
            for cchunk in range(28):
                c0 = cchunk * 128
                cw = min(128, IN_COLS - c0)
                wt = wts[cchunk % 3]
                S.dma(wt[:, :, :cw], wv[:, :, c0:c0 + cw], q=('sp' if cchunk % 2 == 0 else 'pool'))
                ot = ots[cchunk % 2]
                for si, (n0, nw) in enumerate([(0, 512), (512, 512), (1024, 512), (1536, 512), (2048, 128)]):
                    ps = K.bank()
                    for k in range(8):
                        S.mm(ps[:cw, :nw], wt[:, k, :cw], hT[:, k, n0:n0 + nw], start=(k == 0), stop=(k == 7))
                    S.copy(ot[:cw, n0:n0 + nw], ps[:cw, :nw], e=('dve' if si % 2 == 0 else 'act'))
                S.dma(D['pT'][c0:c0 + cw, half * HALF:(half + 1) * HALF], ot[:cw, :], q='act')
    S.barrier()


def stage_outproj(K, l, last):
    S, W, D = K.S, K.W, K.D
    wv = W['w_out'][l].rearrange("(k p) c -> p k c", p=128)
    with ExitStack() as es:
        wo = K.sb(es, [128, 8, 1024])
        S.dma(wo[:, 0:4, :], wv[:, 0:4, :])
        S.dma(wo[:, 4:8, :], wv[:, 4:8, :], q='pool')
        gts = []
        for seg in range(2):
            g = K.sb(es, [128, 1024])
            S.dma(g[:], bc_rows(D['mod'][seg:seg + 1, 2048:3072], 128))
            gts.append(g)
        yts = [K.sb(es, [128, 1024]) for _ in range(2)]
        xts = [K.sb(es, [128, 1024]) for _ in range(2)]
        yTs = [K.sb(es, [128, 8, 128]) for _ in range(2)]
        for t in range(2 if last else 0, NT):
            yt, xt, yT = yts[t % 2], xts[t % 2], yTs[t % 2]
            S.dma(yt[:], D['y'][t * 128:(t + 1) * 128, :])
            S.dma(xt[:], D['xres'][t * 128:(t + 1) * 128, :], q='pool')
            for b in range(2):
                ps = K.bank()
                for j in range(4):
                    k = b * 4 + j
                    S.tr(ps[:, j * 128:(j + 1) * 128], yt[:, k * 128:(k + 1) * 128], K.ident[:])
                S.copy(yT[:, b * 4:(b + 1) * 4, :], ps[:].rearrange("p (j t) -> p j t", j=4), e=('dve' if b == 0 else 'act'))
            gt = gts[0] if t >= 2 else gts[1]
            for n in range(2):
                ps = K.bank()
                for k in range(8):
                    S.mm(ps[:, :], yT[:, k, :], wo[:, k, n * 512:(n + 1) * 512], start=(k == 0), stop=(k == 7))
                S.tt(yt[:, n * 512:(n + 1) * 512], ps[:, :], gt[:, n * 512:(n + 1) * 512], ALU.mult)
                S.tt(xt[:, n * 512:(n + 1) * 512], xt[:, n * 512:(n + 1) * 512], yt[:, n * 512:(n + 1) * 512], ALU.add, e='pool')
            S.dma(D['xres'][t * 128:(t + 1) * 128, :], xt[:], q='act')
    S.barrier()


def stage_ffn(K, l, last):
    S, W, D = K.S, K.W, K.D
    dense = (l % 2 == 0)
    li = l // 2
    if dense:
        E, NF = 1, D_FF // 128
        wg_v = [W['ffn_w_gate'][li].rearrange("(k p) c -> p k c", p=128)]
        wu_v = [W['ffn_w_up'][li].rearrange("(k p) c -> p k c", p=128)]
        wd_v = [W['ffn_w_down'][li].rearrange("(f p) c -> p f c", p=128)]
    else:
        E, NF = 8, D_FFE // 128
        wg_v = [W['moe_w_gate'][li, e].rearrange("(k p) c -> p k c", p=128) for e in range(8)]
        wu_v = [W['moe_w_up'][li, e].rearrange("(k p) c -> p k c", p=128) for e in range(8)]
        wd_v = [W['moe_w_down'][li, e].rearrange("(f p) c -> p f c", p=128) for e in range(8)]
    blocks = ([] if last else [(0, 2)]) + [(2 + 4 * i, 4) for i in range(8)]
    with ExitStack() as es:
        gn = K.sb(es, [128, 1024])
        S.dma(gn[:], bc_rows(W['norm_ffn_g'][l:l + 1, :], 128))
        G2, SH2, GT2 = K.sb(es, [128, 1024]), K.sb(es, [128, 1024]), K.sb(es, [128, 1024])
        xblk = K.sb(es, [128, 4, 1024])
        h2T = K.sb(es, [128, 8, 512])
        actT = K.sb(es, [128, NF, 512])
        yT = K.sb(es, [128, 8, 512])
        tmp = (K.sb(es, [128, 1024]), K.sb(es, [128, 4]), K.sb(es, [128, 1024]))
        sgs = [K.sb(es, [128, 512]) for _ in range(2)]
        wgs = [K.sb(es, [128, 8, 128]) for _ in range(2)]
        wus = [K.sb(es, [128, 8, 128]) for _ in range(2)]
        wds = [K.sb(es, [128, NF, 128]) for _ in range(2)]
        if not dense:
            Gbc = K.sb(es, [128, 8, 512])
            rt = K.sb(es, [128, 8, 8])
            S.dma(rt[:], W['moe_router'][li].rearrange("(k p) e -> p k e", p=128))
            gateT = K.sb(es, [8, 512])
            sel = K.sb(es, [8, 8, 128])
            S.dma(sel[:], W['c_sel8'])
            gsm = K.sb(es, [128, 64])
        cur_seg = None
        wi = 0
        for (t0, nt) in blocks:
            NB = nt * 128
            seg = 1 if t0 < 2 else 0
            if seg != cur_seg:
                cur_seg = seg
                S.dma(SH2[:], bc_rows(D['mod'][seg:seg + 1, 3072:4096], 128), q='pool')
                S.dma(G2[:], bc_rows(D['mod'][seg:seg + 1, 4096:5120], 128), q='pool')
                S.dma(GT2[:], bc_rows(D['mod'][seg:seg + 1, 5120:6144], 128), q='pool')
                S.stt(G2[:], G2[:], 1.0, gn[:], ALU.add, ALU.mult)
            for ti in range(nt):
                t = t0 + ti
                S.dma(xblk[:, ti, :], D['xres'][t * 128:(t + 1) * 128, :])
                norm_mod_T(K, es, xblk[:, ti, :], G2, SH2, h2T[:, :, ti * 128:(ti + 1) * 128], tmp)
            if not dense:
                for ti in range(nt):
                    ps = K.bank()
                    for k in range(8):
                        S.mm(ps[:, 0:8], h2T[:, k, ti * 128:(ti + 1) * 128], rt[:, k, :], start=(k == 0), stop=(k == 7))
                    lg, eq, l2, ex = gsm[:, 0:8], gsm[:, 8:16], gsm[:, 16:24], gsm[:, 24:32]
                    m1, m2, nm1, sm, rs = gsm[:, 32:33], gsm[:, 33:34], gsm[:, 34:35], gsm[:, 35:36], gsm[:, 36:37]
                    gate = gsm[:, 40:48]
                    S.copy(lg, ps[:, 0:8])
                    S.reduce(m1, lg, ALU.max)
                    S.ts(eq, lg, m1, None, ALU.is_equal)
                    S.stt(l2, eq, -1e30, lg, ALU.mult, ALU.add)
                    S.reduce(m2, l2, ALU.max)
                    S.ts(eq, lg, m2, None, ALU.is_ge)
                    S.ts(nm1, m1, -1.0, None, ALU.mult)
                    S.act(ex, lg, AF.Exp, bias=nm1)
                    S.tt(ex, ex, eq, ALU.mult)
                    S.reduce(sm, ex, ALU.add)
                    S.recip(rs, sm)
                    S.ts(gate, ex, rs, None, ALU.mult)
                    ps2 = K.bank()
                    S.tr(ps2[0:8, 0:128], gate, K.ident[:])
                    S.copy(gateT[:, ti * 128:(ti + 1) * 128], ps2[0:8, 0:128])
                for e in range(8):
                    ps = K.bank()
                    S.mm(ps[:, :NB], sel[:, e, :], gateT[:, :NB])
                    S.copy(Gbc[:, e, :NB], ps[:, :NB], e='act')
            for e in range(E):
                for f in range(NF):
                    wg, wu = wgs[wi % 2], wus[wi % 2]
                    sg = sgs[wi % 2]
                    wi += 1
                    S.dma(wg[:], wg_v[e][:, :, f * 128:(f + 1) * 128], q='sp')
                    S.dma(wu[:], wu_v[e][:, :, f * 128:(f + 1) * 128], q='pool')
                    psg, psu = K.bank(), K.bank()
                    for k in range(8):
                        S.mm(psg[:, :NB], wg[:, k, :], h2T[:, k, :NB], start=(k == 0), stop=(k == 7))
                    for k in range(8):
                        S.mm(psu[:, :NB], wu[:, k, :], h2T[:, k, :NB], start=(k == 0), stop=(k == 7))
                    S.act(sg[:, :NB], psg[:, :NB], AF.Silu)
                    S.tt(actT[:, f, :NB], sg[:, :NB], psu[:, :NB], ALU.mult)
                    if not dense:
                        S.tt(actT[:, f, :NB], actT[:, f, :NB], Gbc[:, e, :NB], ALU.mult, e='pool')
                for cchunk in range(8):
                    wd = wds[cchunk % 2]
                    S.dma(wd[:], wd_v[e][:, :, cchunk * 128:(cchunk + 1) * 128], q='act')
                    ps = K.bank()
                    for f in range(NF):
                        S.mm(ps[:, :NB], wd[:, f, :], actT[:, f, :NB], start=(f == 0), stop=(f == NF - 1))
                    if e == 0:
                        S.copy(yT[:, cchunk, :NB], ps[:, :NB], e='act')
                    else:
                        S.tt(yT[:, cchunk, :NB], yT[:, cchunk, :NB], ps[:, :NB], ALU.add)
            for ti in range(nt):
                t = t0 + ti
                yt = tmp[0]
                for b in range(2):
                    ps = K.bank()
                    for j in range(4):
                        S.tr(ps[:, j * 128:(j + 1) * 128], yT[:, b * 4 + j, ti * 128:(ti + 1) * 128], K.ident[:])
                    S.tt(yt[:, b * 512:(b + 1) * 512], ps[:, :], GT2[:, b * 512:(b + 1) * 512], ALU.mult)
                S.tt(xblk[:, ti, :], xblk[:, ti, :], yt[:], ALU.add, e='pool')
                S.dma(D['xres'][t * 128:(t + 1) * 128, :], xblk[:, ti, :], q='act')
    S.barrier()


def stage_final(K):
    S, W, D = K.S, K.W, K.D
    with ExitStack() as es:
        g = K.sb(es, [128, 1024])
        S.dma(g[:], bc_rows(W['final_norm_g'], 128))
        xts = [K.sb(es, [128, 1024]) for _ in range(2)]
        ots = [K.sb(es, [128, 1024]) for _ in range(2)]
        junk = K.sb(es, [128, 1024])
        sss = [K.sb(es, [128, 4]) for _ in range(2)]
        for t in range(2, NT):
            xt, ot, ss = xts[t % 2], ots[t % 2], sss[t % 2]
            S.dma(xt[:], D['xres'][t * 128:(t + 1) * 128, :])
            S.act(junk[:], xt[:], AF.Square, accum_out=ss[:, 0:1])
            S.ts(ss[:, 1:2], ss[:, 0:1], 1.0 / D_MODEL, EPS, ALU.mult, ALU.add)
            S.act(ss[:, 2:3], ss[:, 1:2], AF.Sqrt)
            S.recip(ss[:, 3:4], ss[:, 2:3])
            S.stt(ot[:], xt[:], ss[:, 3:4], g[:], ALU.mult, ALU.mult)
            S.dma(K.out[(t - 2) * 128:(t - 1) * 128, :], ot[:], q='pool')
    S.barrier()


def mix_identity(K, l):
    S, D = K.S, K.D
    with ExitStack() as es:
        pts = [K.sb(es, [128, 8, 128]) for _ in range(2)]
        yts = [K.sb(es, [128, 1024]) for _ in range(2)]
        pv = D['pT'][0:1024, :].rearrange("(k p) t -> p k t", p=128)
        for t in range(NT):
            pt, yt = pts[t % 2], yts[t % 2]
            S.dma(pt[:], pv[:, :, t * 128:(t + 1) * 128])
            for b in range(2):
                ps = K.bank()
                for j in range(4):
                    S.tr(ps[:, j * 128:(j + 1) * 128], pt[:, b * 4 + j, :], K.ident[:])
                S.copy(yt[:, b * 512:(b + 1) * 512], ps[:, :], e=('dve' if b == 0 else 'act'))
            S.dma(D['y'][t * 128:(t + 1) * 128, :], yt[:], q='pool')
    S.barrier()


def to_token_major(K, es, src, ncols_tile, dst_cols, bufs):
    S, D = K.S, K.D
    gi = 0
    for t0 in range(0, NT, 4):
        n = min(4, NT - t0)
        ps = K.bank()
        for j in range(n):
            S.tr(ps[:, j * 128:(j + 1) * 128], src[:, (t0 + j) * 128:(t0 + j + 1) * 128], K.ident[:])
        yb = bufs[gi % 2]
        S.copy(yb[:, :n * 128], ps[:, :n * 128], e=('dve' if gi % 2 == 0 else 'act'))
        dst = D['y'][t0 * 128:(t0 + n) * 128, dst_cols[0]:dst_cols[1]].rearrange("(j p) c -> p j c", p=128)
        S.dma(dst, yb[:, :n * 128].rearrange("p (j c) -> p j c", j=n), q=('sp' if gi % 2 == 0 else 'pool'))
        gi += 1


def mixer_a(K, l, last):
    S, W, D = K.S, K.W, K.D
    SEGS = [(0, CTX), (CTX, T)]
    with ExitStack() as es0:
        ybufs = [K.sb(es0, [128, 512]) for _ in range(2)]
        for ct in range(2):
            with ExitStack() as es:
                pk = K.sb(es, [128, 16])
                S.dma(pk[:, 0:11], W['pk_lru'][l, ct])
                S.act(pk[:, 11:13], pk[:, 9:11], AF.Exp, scale=-1.0)
                S.act(pk[:, 11:13], pk[:, 11:13], AF.Ln, bias=1.0)
                S.ts(pk[:, 13:15], pk[:, 11:13], -16.0, None, ALU.mult)
                S.ts(pk[:, 11:13], pk[:, 11:13], -8.0, None, ALU.mult)
                wbd = K.sb(es, [128, 4, 128])
                S.memset(wbd[:], 0.0)
                for d in range(2):
                    for wi, wn in enumerate(('lru_w_a', 'lru_w_x')):
                        for hl in range(2):
                            S.dma(wbd[hl * 64:(hl + 1) * 64, d * 2 + wi, hl * 64:(hl + 1) * 64], W[wn][l, d, ct * 2 + hl], q='pool')
                xb = K.sb(es, [128, T])
                u = K.sb(es, [128, T])
                gt = K.sb(es, [128, T])
                ra = K.sb(es, [128, T])
                ib = K.sb(es, [128, T])
                h0 = K.sb(es, [128, T])
                h1 = K.sb(es, [128, T])
                S.dma(xb[:], D['pT'][ct * 128:(ct + 1) * 128, :])
                S.dma(gt[:], D['pT'][256 + ct * 128:256 + (ct + 1) * 128, :], q='pool')
                S.ts(u[:], xb[:], pk[:, 2:3], pk[:, 4:5], ALU.mult, ALU.add)
                for (a, b) in SEGS:
                    for j, s in ((0, -2), (1, -1), (3, 1)):
                        lo, hi = max(a, a - s), min(b, b - s)
                        S.stt(u[:, lo:hi], xb[:, lo + s:hi + s], pk[:, j:j + 1], u[:, lo:hi], ALU.mult, ALU.add)
                S.tt(xb[:], gt[:], gt[:], ALU.mult, e='pool')
                S.ts(xb[:], xb[:], 0.044715, 1.0, ALU.mult, ALU.add, e='pool')
                S.tt(xb[:], xb[:], gt[:], ALU.mult, e='pool')
                S.act(xb[:], xb[:], AF.Sigmoid, scale=1.5957691216057308)
                S.tt(gt[:], gt[:], xb[:], ALU.mult, e='pool')
                for d in range(2):
                    blocks = [(n0, min(512, T - n0)) for n0 in range(0, T, 512)]
                    for wi, dst, bcol in ((0, ra, 5 + d), (1, ib, 7 + d)):
                        for (n0, nw) in blocks:
                            ps = K.bank()
                            S.mm(ps[:, :nw], wbd[:, d * 2 + wi, :], u[:, n0:n0 + nw])
                            S.act(dst[:, n0:n0 + nw], ps[:, :nw], AF.Sigmoid, bias=pk[:, bcol:bcol + 1])
                    hd = h0 if d == 0 else h1
                    S.tt(ib[:], ib[:], u[:], ALU.mult, e='pool')
                    S.act(hd[:], ra[:], AF.Exp, scale=pk[:, 13 + d:14 + d])
                    S.ts(hd[:], hd[:], -1.0, 1.0, ALU.mult, ALU.add)
                    S.act(hd[:], hd[:], AF.Sqrt)
                    S.tt(ib[:], ib[:], hd[:], ALU.mult)
                    S.act(ra[:], ra[:], AF.Exp, scale=pk[:, 11 + d:12 + d])
                    if d == 0:
                        S.scan(h0[:], ra[:], ib[:], 0.0, ALU.mult, ALU.add)
                    else:
                        S.scan(h1[:, 0:CTX][:, ::-1], ra[:, 0:CTX][:, ::-1], ib[:, 0:CTX][:, ::-1], 0.0, ALU.mult, ALU.add)
                        S.scan(h1[:, CTX:T][:, ::-1], ra[:, CTX:T][:, ::-1], ib[:, CTX:T][:, ::-1], h1[:, 0:1], ALU.mult, ALU.add)
                S.tt(h0[:], h0[:], h1[:], ALU.add, e='pool')
                S.tt(h0[:], h0[:], gt[:], ALU.mult)
                to_token_major(K, es, h0, 128, (ct * 128, (ct + 1) * 128), ybufs)
            S.barrier()
    S.barrier()


def chunk_cols(n):
    if n < 4:
        return slice(n * 64, (n + 1) * 64)
    c = n - 4
    return slice(CTX + c, T, 64)


NCH = T // 64
DBG = {}


def to_traversal(S, dst, src, e='dve'):
    S.copy(dst[:, 0:CTX], src[:, 0:CTX], e=e)
    S.copy(dst[:, CTX:T].rearrange("p (c r) -> p c r", r=64), src[:, CTX:T].rearrange("p (r c) -> p c r", c=64), e=e)


def scan_order(d):
    if d == 0:
        return list(range(NCH))
    return [3, 2, 1, 0] + list(range(NCH - 1, 3, -1))


def mixer_c(K, l, last):
    S, W, D = K.S, K.W, K.D
    base = OFF_C
    NQ = 6
    with ExitStack() as es0:
        tokcol = K.sb(es0, [64, NCH, NQ * 8])
        masks = K.sb(es0, [64, 2, 64])
        S.dma(masks[:], W['c_masks'])
        ngt = K.sb(es0, [64, 256])
        S.dma(ngt[:], bc_rows(W['mlstm_norm_g'][l:l + 1, :], 64))
        with ExitStack() as es:
            stack = K.sb(es, [NQ * 8, T])
            pk = K.sb(es, [4, 8])
            S.dma(pk[:, 0:4], W['pk_ml'][l])
            S.ts(pk[:, 4:8], pk[:, 0:4], -1.0, None, ALU.mult)
            rst = K.sb(es, [4, T])
            nbg = K.sb(es, [4, T])
            graw = K.sb(es, [4, T])
            li = K.sb(es, [4, T])
            lf = K.sb(es, [4, T])
            bb = K.sb(es, [4, T])
            gg = K.sb(es, [4, T])
            cm = K.sb(es, [4, T])
            mx = K.sb(es, [4, T])
            tmp = graw
            sm = K.sb(es, [4, 8, NCH])
            v3 = lambda t: t[:, :].rearrange("p (n i) -> p n i", i=64)
            bcn = lambda a: a.unsqueeze(2).to_broadcast([4, NCH, 64])
            for d in range(2):
                rv = (lambda a: a) if d == 0 else (lambda a: a[:, ::-1])
                lastidx = 63 if d == 0 else 0
                S.dma(rst[:], W['c_rst'][:, d, :], q='pool')
                S.ts(nbg[:], rst[:], 1e30, -1e30, ALU.mult, ALU.add)
                S.dma(graw[:], D['pT'][base + 1024 + d * 4: base + 1024 + d * 4 + 4, :])
                to_traversal(S, li, graw)
                S.ts(li[:], li[:], pk[:, d:d + 1], None, ALU.add)
                S.dma(graw[:], D['pT'][base + 1024 + 8 + d * 4: base + 1024 + 8 + d * 4 + 4, :])
                to_traversal(S, lf, graw)
                S.act(lf[:], lf[:], AF.Exp, scale=-1.0, bias=pk[:, 6 + d:7 + d])
                S.act(lf[:], lf[:], AF.Ln, bias=1.0)
                S.ts(lf[:], lf[:], -1.0, None, ALU.mult)
                S.scan(rv(bb[:, :]), rv(rst[:, :]), rv(lf[:, :]), 0.0, ALU.mult, ALU.add)
                S.tt(gg[:], li[:], bb[:], ALU.subtract)
                S.scan(rv(cm[:, :]), rv(nbg[:, :]), rv(gg[:, :]), 0.0, ALU.add, ALU.max)
                bL, cmL = v3(bb)[:, :, lastidx], v3(cm)[:, :, lastidx]
                d1t, mm_, mprev, e4, t5 = sm[:, 0, :], sm[:, 1, :], sm[:, 2, :], sm[:, 3, :], sm[:, 4, :]
                S.tt(d1t, bL, cmL, ALU.add)
                if d == 0:
                    S.scan(mm_, bL, d1t, 0.0, ALU.add, ALU.max)
                    S.memset(mprev[:, 0:1], 0.0)
                    S.copy(mprev[:, 1:NCH], mm_[:, 0:NCH - 1])
                else:
                    S.scan(mm_[:, 0:4][:, ::-1], bL[:, 0:4][:, ::-1], d1t[:, 0:4][:, ::-1], 0.0, ALU.add, ALU.max)
                    S.scan(mm_[:, 4:NCH][:, ::-1], bL[:, 4:NCH][:, ::-1], d1t[:, 4:NCH][:, ::-1], mm_[:, 0:1], ALU.add, ALU.max)
                    S.copy(mprev[:, 0:3], mm_[:, 1:4])
                    S.memset(mprev[:, 3:4], 0.0)
                    S.copy(mprev[:, 4:NCH - 1], mm_[:, 5:NCH])
                    S.copy(mprev[:, NCH - 1:NCH], mm_[:, 0:1])
                S.tt(v3(mx), v3(cm), bcn(mprev), ALU.max)
                def put(q, src):
                    r0 = q * 8 + d * 4
                    S.dma(stack[r0:r0 + 4, :], src, q='pool')
                S.act(tmp[:], mx[:], AF.Exp, scale=-1.0)
                put(0, tmp[:])
                S.tt(v3(tmp), bcn(mprev), v3(mx), ALU.subtract)
                S.act(tmp[:], tmp[:], AF.Exp)
                put(1, tmp[:])
                S.tt(tmp[:], bb[:], mx[:], ALU.add)
                S.act(tmp[:], tmp[:], AF.Exp, scale=-1.0)
                put(2, tmp[:])
                S.act(tmp[:], gg[:], AF.Exp)
                put(3, tmp[:])
                S.tt(e4, bL, mprev, ALU.add)
                S.tt(e4, e4, mm_, ALU.subtract)
                S.act(e4, e4, AF.Exp)
                S.copy(v3(tmp), bcn(e4))
                put(4, tmp[:])
                S.tt(t5, bL, mm_, ALU.subtract)
                S.tt(v3(tmp), v3(gg), bcn(t5), ALU.add)
                S.act(tmp[:], tmp[:], AF.Exp)
                put(5, tmp[:])
            NR = NQ * 8
            for n0 in range(0, NCH, 8):
                nn = min(8, NCH - n0)
                ps = K.bank()
                for j in range(nn):
                    S.tr(ps[0:64, j * NR:(j + 1) * NR], stack[0:NR, (n0 + j) * 64:(n0 + j + 1) * 64], K.ident[0:NR, 0:NR])
                S.copy(tokcol[:, n0:n0 + nn, :], ps[0:64, 0:nn * NR].rearrange("p (j c) -> p j c", c=NR))
        S.barrier()
        for h in range(4):
            with ExitStack() as es:
                qT = K.sb(es, [64, T])
                kT = K.sb(es, [64, T])
                vT = K.sb(es, [64, T])
                ktok = K.sb(es, [64, NCH, 64])
                vtok = K.sb(es, [64, NCH, 65])
                hacc = K.sb(es, [64, NCH, 64])
                osig = K.sb(es, [64, NCH, 64])
                S.dma(qT[:], D['pT'][base + h * 64: base + (h + 1) * 64, :])
                S.dma(kT[:], D['pT'][base + 256 + h * 64: base + 256 + (h + 1) * 64, :], q='pool')
                S.dma(vT[:], D['pT'][base + 512 + h * 64: base + 512 + (h + 1) * 64, :], q='act')
                S.ts(kT[:], kT[:], 0.125, None, ALU.mult, e='pool')
                S.memset(vtok[:, :, 64:65], 1.0)
                for n0 in range(0, NCH, 8):
                    nn = min(8, NCH - n0)
                    ps1, ps2 = K.bank(), K.bank()
                    for j in range(nn):
                        cs = chunk_cols(n0 + j)
                        S.tr(ps1[0:64, j * 64:(j + 1) * 64], kT[:, cs], K.ident[0:64, 0:64])
                        S.tr(ps2[0:64, j * 64:(j + 1) * 64], vT[:, cs], K.ident[0:64, 0:64])
                    S.copy(ktok[:, n0:n0 + nn, :], ps1[0:64, 0:nn * 64].rearrange("p (j c) -> p j c", c=64), e='act')
                    S.copy(vtok[:, n0:n0 + nn, 0:64], ps2[0:64, 0:nn * 64].rearrange("p (j c) -> p j c", c=64))
                S.dma(vT[:], D['pT'][base + 768 + h * 64: base + 768 + (h + 1) * 64, :], q='act')
                for d in range(2):
                    col = lambda q: (lambda n: tokcol[:, n, q * 8 + d * 4 + h: q * 8 + d * 4 + h + 1])
                    c1, c2, c3, eg, c4, wn = [col(q) for q in range(6)]
                    Cs = [K.sb(es, [64, 65]) for _ in range(2)]
                    S.memset(Cs[0][:], 0.0)
                    pts = [K.sb(es, [64, 64]) for _ in range(2)]
                    tts = [K.sb(es, [64, 65]) for _ in range(2)]
                    vws = [K.sb(es, [64, 65]) for _ in range(2)]
                    dns = [K.sb(es, [64, 2]) for _ in range(2)]
                    for si, n in enumerate(scan_order(d)):
                        cs = chunk_cols(n)
                        Cc, Cn = Cs[si % 2], Cs[(si + 1) % 2]
                        pt, tot, vw, dn = pts[si % 2], tts[si % 2], vws[si % 2], dns[si % 2]
                        ps_s, ps_o, ps_i, ps_c = K.bank(), K.bank(), K.bank(), K.bank()
                        S.mm(ps_s[0:64, 0:64], kT[:, cs], qT[:, cs])
                        S.stt(pt[:], ps_s[0:64, 0:64], eg(n), masks[:, d, :], ALU.mult, ALU.mult)
                        S.mm(ps_o[0:64, 0:65], pt[:], vtok[:, n, :])
                        S.mm(ps_i[0:64, 0:65], qT[:, cs], Cc[:])
                        S.ts(tot[:], ps_o[0:64, 0:65], c1(n), None, ALU.mult)
                        S.stt(tot[:], ps_i[0:64, 0:65], c2(n), tot[:], ALU.mult, ALU.add)
                        S.act(dn[:, 0:1], tot[:, 64:65], AF.Abs)
                        S.ts(dn[:, 0:1], dn[:, 0:1], c3(n), None, ALU.max)
                        S.recip(dn[:, 1:2], dn[:, 0:1])
                        if d == 0:
                            S.ts(hacc[:, n, :], tot[:, 0:64], dn[:, 1:2], None, ALU.mult)
                        else:
                            S.stt(hacc[:, n, :], tot[:, 0:64], dn[:, 1:2], hacc[:, n, :], ALU.mult, ALU.add)
                        S.ts(vw[:], vtok[:, n, :], wn(n), None, ALU.mult, e='pool')
                        S.mm(ps_c[0:64, 0:65], ktok[:, n, :], vw[:])
                        S.stt(Cn[:], Cc[:], c4(n), ps_c[0:64, 0:65], ALU.mult, ALU.add)
                for n0 in range(0, NCH, 8):
                    nn = min(8, NCH - n0)
                    ps1 = K.bank()
                    for j in range(nn):
                        S.tr(ps1[0:64, j * 64:(j + 1) * 64], vT[:, chunk_cols(n0 + j)], K.ident[0:64, 0:64])
                    S.act(osig[:, n0:n0 + nn, :], ps1[0:64, 0:nn * 64].rearrange("p (j c) -> p j c", c=64), AF.Sigmoid)
                sq = K.sb(es, [64, NCH, 64])
                ssq = K.sb(es, [64, NCH, 4])
                S.tt(sq[:], hacc[:], hacc[:], ALU.mult, e='pool')
                S.reduce(ssq[:, :, 0], sq[:], ALU.add)
                S.ts(ssq[:, :, 1], ssq[:, :, 0], 1.0 / 64, EPS, ALU.mult, ALU.add)
                S.act(ssq[:, :, 2], ssq[:, :, 1], AF.Sqrt)
                S.recip(ssq[:, :, 3], ssq[:, :, 2])
                S.tt(hacc[:], hacc[:], ssq[:, :, 3].unsqueeze(2).to_broadcast([64, NCH, 64]), ALU.mult)
                S.tt(hacc[:], hacc[:], ngt[:, h * 64:(h + 1) * 64].unsqueeze(1).to_broadcast([64, NCH, 64]), ALU.mult, e='pool')
                S.tt(hacc[:], hacc[:], osig[:], ALU.mult)
                c0 = 512 + h * 64
                S.dma(D['y'][0:CTX, c0:c0 + 64].rearrange("(n i) c -> i n c", i=64), hacc[:, 0:4, :])
                yv = D['y'][CTX:T, c0:c0 + 64].rearrange("(r c) ch -> r c ch", c=64)
                for g4 in range(4):
                    S.dma(yv[:, g4 * 16:(g4 + 1) * 16, :], hacc[:, 4 + g4 * 16:4 + (g4 + 1) * 16, :], q=('sp', 'pool')[g4 % 2])
            S.barrier()
    S.barrier()


def dwconv_trav(S, out, x, wcol, bias=None):
    if bias is None:
        S.ts(out[:], x[:], wcol(2), None, ALU.mult)
    else:
        S.ts(out[:], x[:], wcol(2), bias, ALU.mult, ALU.add)
    for (a, b) in ((0, CTX), (CTX, T)):
        for j, s in ((0, -2), (1, -1), (3, 1)):
            lo, hi = max(a, a - s), min(b, b - s)
            S.stt(out[:, lo:hi], x[:, lo + s:hi + s], wcol(j), out[:, lo:hi], ALU.mult, ALU.add)


def neumann_inverse(K, S, P, PT, B, BT, tmps):
    B2s, B2Ts = tmps
    cb, cbt = B, BT
    for m in range(1, 6):
        lastm = (m == 5)
        ps1 = K.bank()
        S.mm(ps1[:, 0:128], cbt[:], cb[:])
        nb = B2s[m % 2]
        S.copy(nb[:], ps1[:, 0:128], e='act')
        if not lastm:
            ps2 = K.bank()
            S.mm(ps2[:, 0:128], cb[:], cbt[:])
            nbt = B2Ts[m % 2]
            S.copy(nbt[:], ps2[:, 0:128], e='dve')
        ps3 = K.bank()
        S.mm(ps3[:, 0:128], PT[:], nb[:])
        if not lastm:
            ps4 = K.bank()
            S.mm(ps4[:, 0:128], nb[:], PT[:])
        S.tt(P[:], P[:], ps3[:, 0:128], ALU.add)
        if not lastm:
            S.tt(PT[:], PT[:], ps4[:, 0:128], ALU.add)
            cb, cbt = nb, nbt


def mixer_d(K, l, last):
    S, W, D = K.S, K.W, K.D
    base = OFF_D
    NQ = 6
    NR = NQ * 8
    NP = NCH // 2
    with ExitStack() as es0:
        tok64 = K.sb(es0, [64, NCH, NR])
        tokP = K.sb(es0, [128, NP, NR])
        stack = K.sb(es0, [NR, T])
        masks = K.sb(es0, [64, 2, 64])
        S.dma(masks[:], W['c_masks'])
        m128 = K.sb(es0, [128, 4, 128])
        S.dma(m128[:], W['c_m128'])
        sel = K.sb(es0, [8, 8, 128])
        S.dma(sel[:], W['c_sel8'])
        ngt = K.sb(es0, [64, 256])
        S.dma(ngt[:], bc_rows(W['gdn_norm_g'][l:l + 1, :], 64))
        with ExitStack() as es:
            pk = K.sb(es, [4, 8])
            S.dma(pk[:, 0:4], W['pk_gd'][l])
            S.act(pk[:, 4:6], pk[:, 0:2], AF.Exp)
            S.ts(pk[:, 4:6], pk[:, 4:6], -1.0, None, ALU.mult)
            rst = K.sb(es, [4, T])
            graw = K.sb(es, [4, T])
            la = K.sb(es, [4, T])
            bt = K.sb(es, [4, T])
            gam = K.sb(es, [4, T])
            tmp = K.sb(es, [4, T])
            sm = K.sb(es, [4, 4, NCH])
            v3 = lambda t: t[:, :].rearrange("p (n i) -> p n i", i=64)
            bcn = lambda a: a.unsqueeze(2).to_broadcast([4, NCH, 64])
            for d in range(2):
                rv = (lambda a: a) if d == 0 else (lambda a: a[:, ::-1])
                lastidx = 63 if d == 0 else 0
                S.dma(rst[:], W['c_rst'][:, d, :], q='pool')
                S.dma(graw[:], D['pT'][base + 1024 + d * 4: base + 1024 + d * 4 + 4, :])
                to_traversal(S, la, graw)
                S.act(la[:], la[:], AF.Exp, bias=pk[:, 2 + d:3 + d])
                S.act(la[:], la[:], AF.Ln, bias=1.0)
                S.ts(la[:], la[:], pk[:, 4 + d:5 + d], None, ALU.mult)
                S.dma(graw[:], D['pT'][base + 1024 + 8 + d * 4: base + 1024 + 8 + d * 4 + 4, :])
                to_traversal(S, bt, graw)
                S.act(bt[:], bt[:], AF.Sigmoid)
                S.scan(rv(gam[:, :]), rv(rst[:, :]), rv(la[:, :]), 0.0, ALU.mult, ALU.add)
                gL = v3(gam)[:, :, lastidx]
                def put(q, src):
                    r0 = q * 8 + d * 4
                    S.dma(stack[r0:r0 + 4, :], src, q='pool')
                put(0, gam[:])
                put(1, bt[:])
                S.act(tmp[:], gam[:], AF.Exp)
                S.tt(tmp[:], tmp[:], bt[:], ALU.mult)
                put(2, tmp[:])
                S.tt(v3(tmp), bcn(gL), v3(gam), ALU.subtract)
                S.act(tmp[:], tmp[:], AF.Exp)
                put(3, tmp[:])
                S.act(sm[:, 0, :], gL, AF.Exp)
                S.copy(v3(tmp), bcn(sm[:, 0, :]))
                put(4, tmp[:])
                S.ts(tmp[:], bt[:], -1.0, None, ALU.mult)
                put(5, tmp[:])
            for n0 in range(0, NCH, 8):
                nn = min(8, NCH - n0)
                ps = K.bank()
                for j in range(nn):
                    S.tr(ps[0:64, j * NR:(j + 1) * NR], stack[0:NR, (n0 + j) * 64:(n0 + j + 1) * 64], K.ident[0:NR, 0:NR])
                S.copy(tok64[:, n0:n0 + nn, :], ps[0:64, 0:nn * NR].rearrange("p (j c) -> p j c", c=NR))
            for n0 in range(0, NP, 8):
                nn = min(8, NP - n0)
                ps = K.bank()
                for j in range(nn):
                    S.tr(ps[:, j * NR:(j + 1) * NR], stack[0:NR, (n0 + j) * 128:(n0 + j + 1) * 128], K.ident[0:NR, 0:NR])
                S.copy(tokP[:, n0:n0 + nn, :], ps[:, 0:nn * NR].rearrange("p (j c) -> p j c", c=NR), e='act')
        S.barrier()
        if DBG.get('d_stop') == 1:
            return
        for h in range(DBG.get('d_heads', 4)):
            with ExitStack() as es:
                raw = K.sb(es, [64, T])
                trv = K.sb(es, [64, T])
                qT = K.sb(es, [64, T])
                kT = K.sb(es, [64, T])
                vT = K.sb(es, [64, T])
                ktok = K.sb(es, [64, NCH, 64])
                kP = K.sb(es, [128, NP, 64])
                vP = K.sb(es, [128, NP, 64])
                hacc = K.sb(es, [64, NCH, 64])
                cw = K.sb(es, [64, 3, 4])
                ones = K.sb(es, [64, 64])
                S.memset(ones[:], 1.0)
                S.dma(cw[:], W['pk_gdc'][l, h])
                for gi, dst in enumerate((qT, kT, vT)):
                    S.dma(raw[:], D['pT'][base + gi * 256 + h * 64: base + gi * 256 + (h + 1) * 64, :])
                    to_traversal(S, trv, raw, e='pool')
                    dwconv_trav(S, dst, trv, lambda j: cw[:, gi, j:j + 1])
                    S.act(dst[:], dst[:], AF.Silu)
                    if gi < 2:
                        S.tt(trv[:], dst[:], dst[:], ALU.mult, e='pool')
                        for n0 in range(0, T, 512):
                            nw = min(512, T - n0)
                            ps = K.bank()
                            S.mm(ps[0:64, :nw], ones[:], trv[:, n0:n0 + nw])
                            S.ts(raw[:, n0:n0 + nw], ps[0:64, :nw], EPS, None, ALU.add)
                        S.act(raw[:], raw[:], AF.Sqrt)
                        S.recip(raw[:], raw[:])
                        if gi == 0:
                            S.stt(dst[:], dst[:], 0.125, raw[:], ALU.mult, ALU.mult)
                        else:
                            S.tt(dst[:], dst[:], raw[:], ALU.mult)
                for n0 in range(0, NCH, 8):
                    nn = min(8, NCH - n0)
                    ps1 = K.bank()
                    for j in range(nn):
                        S.tr(ps1[0:64, j * 64:(j + 1) * 64], kT[:, (n0 + j) * 64:(n0 + j + 1) * 64], K.ident[0:64, 0:64])
                    S.copy(ktok[:, n0:n0 + nn, :], ps1[0:64, 0:nn * 64].rearrange("p (j c) -> p j c", c=64), e='act')
                for n0 in range(0, NP, 8):
                    nn = min(8, NP - n0)
                    ps1, ps2 = K.bank(), K.bank()
                    for j in range(nn):
                        S.tr(ps1[:, j * 64:(j + 1) * 64], kT[:, (n0 + j) * 128:(n0 + j + 1) * 128], K.ident[0:64, 0:64])
                        S.tr(ps2[:, j * 64:(j + 1) * 64], vT[:, (n0 + j) * 128:(n0 + j + 1) * 128], K.ident[0:64, 0:64])
                    S.copy(kP[:, n0:n0 + nn, :], ps1[:, 0:nn * 64].rearrange("p (j c) -> p j c", c=64), e='act')
                    S.copy(vP[:, n0:n0 + nn, :], ps2[:, 0:nn * 64].rearrange("p (j c) -> p j c", c=64))
                if DBG.get('d_stop') == 2:
                    S.barrier()
                    return
                S.dma(raw[:], D['pT'][base + 768 + h * 64: base + 768 + (h + 1) * 64, :])
                kdec = trv
                kdec3 = kdec[:, :].rearrange("p (n c) -> p n c", c=64)
                GBs = [K.sb(es, [128, 128]) for _ in range(2)]
                decL = [K.sb(es, [128, 128]) for _ in range(2)]
                Bm = [K.sb(es, [128, 128]) for _ in range(2)]
                BTm = [K.sb(es, [128, 128]) for _ in range(2)]
                Pm = [K.sb(es, [128, 128]) for _ in range(2)]
                PTm = [K.sb(es, [128, 128]) for _ in range(2)]
                B2s = [K.sb(es, [128, 128]) for _ in range(2)]
                B2Ts = [K.sb(es, [128, 128]) for _ in range(2)]
                rU = [K.sb(es, [128, 64]) for _ in range(2)]
                rW = [K.sb(es, [128, 64]) for _ in range(2)]
                wTs = [K.sb(es, [64, 128]) for _ in range(2)]
                us = [K.sb(es, [64, 2, 64]) for _ in range(2)]
                qkTs = [K.sb(es, [64, 2, 64]) for _ in range(2)]
                qds = [K.sb(es, [64, 128]) for _ in range(2)]
                vns = [K.sb(es, [64, 64]) for _ in range(2)]
                Ss = [K.sb(es, [64, 64]) for _ in range(2)]
                for d in range(2):
                    cP = lambda q, pi: tokP[:, pi, q * 8 + d * 4 + h: q * 8 + d * 4 + h + 1]
                    c64 = lambda q, n: tok64[:, n, q * 8 + d * 4 + h: q * 8 + d * 4 + h + 1]
                    S.tt(kdec3, ktok[:], tok64[:, :, 3 * 8 + d * 4 + h].unsqueeze(2).to_broadcast([64, NCH, 64]), ALU.mult, e='pool')
                    S.memset(Ss[0][:], 0.0)
                    si = 0
                    order = scan_order(d)
                    pairs = [order[i] // 2 for i in range(0, NCH, 2)]
                    for pidx, pi in enumerate(pairs):
                        if pidx >= DBG.get('d_pairs', 99):
                            break
                        b = pidx % 2
                        GB, dL, Bc, BTc, P, PT = GBs[b], decL[b], Bm[b], BTm[b], Pm[b], PTm[b]
                        tk = slice(pi * 128, (pi + 1) * 128)
                        ps = K.bank()
                        S.mm(ps[:, 0:128], sel[:, d * 4 + h, :], stack[0:8, tk])
                        S.copy(GB[:], ps[:, 0:128], e='act')
                        S.ts(dL[:], GB[:], cP(0, pi), 0.0, ALU.subtract, ALU.max)
                        S.act(dL[:], dL[:], AF.Exp, scale=-1.0)
                        S.tt(dL[:], dL[:], m128[:, 3 - d, :], ALU.mult, e='pool')
                        if DBG.get('d_stop') == 5:
                            continue
                        ps = K.bank()
                        S.mm(ps[:, 0:128], kT[:, tk], kT[:, tk])
                        S.stt(BTc[:], ps[:, 0:128], cP(5, pi), dL[:], ALU.mult, ALU.mult)
                        if DBG.get('d_stop') == 6:
                            continue
                        ps = K.bank()
                        S.tr(ps[:, 0:128], BTc[:], K.ident[:])
                        S.copy(Bc[:], ps[:, 0:128], e='act')
                        if DBG.get('d_stop') == 7:
                            continue
                        S.tt(P[:], Bc[:], K.ident[:], ALU.add)
                        if DBG.get('d_stop') == 8:
                            continue
                        S.tt(PT[:], BTc[:], K.ident[:], ALU.add, e='pool')
                        if DBG.get('d_stop') == 3:
                            continue
                        neumann_inverse(K, S, P, PT, Bc, BTc, (B2s, B2Ts))
                        if DBG.get('d_stop') == 4:
                            continue
                        S.ts(rU[b][:], vP[:, pi, :], cP(1, pi), None, ALU.mult, e='pool')
                        S.ts(rW[b][:], kP[:, pi, :], cP(2, pi), None, ALU.mult, e='pool')
                        ps = K.bank()
                        S.mm(ps[0:64, 0:128], rW[b][:], P[:])
                        S.copy(wTs[b][:], ps[0:64, 0:128], e='act')
                        ps = K.bank()
                        for c in range(2):
                            S.mm(ps[0:64, c * 64:(c + 1) * 64], P[:, c * 64:(c + 1) * 64], rU[b][:])
                        S.copy(us[b][:], ps[0:64, 0:128].rearrange("p (c v) -> p c v", c=2))
                        S.act(qds[b][:], GB[0:64, :], AF.Exp)
                        S.tt(qds[b][:], qds[b][:], qT[:, tk], ALU.mult, e='pool')
                        for c in range(2):
                            n = pi * 2 + c
                            ck = slice(n * 64, (n + 1) * 64)
                            ps = K.bank()
                            S.mm(ps[0:64, 0:64], kT[:, ck], qT[:, ck])
                            dt_ = vns[c]
                            S.ts(qkTs[b][:, c, :], GB[0:64, c * 64:(c + 1) * 64], c64(0, n), 0.0, ALU.subtract, ALU.min)
                            S.act(qkTs[b][:, c, :], qkTs[b][:, c, :], AF.Exp)
                            S.tt(qkTs[b][:, c, :], qkTs[b][:, c, :], masks[:, d, :], ALU.mult, e='pool')
                            S.tt(qkTs[b][:, c, :], qkTs[b][:, c, :], ps[0:64, 0:64], ALU.mult)
                        for c in ((0, 1) if d == 0 else (1, 0)):
                            n = pi * 2 + c
                            Sc, Sn = Ss[si % 2], Ss[(si + 1) % 2]
                            vn = vns[si % 2]
                            si += 1
                            ps1 = K.bank()
                            S.mm(ps1[0:64, 0:64], wTs[b][:, c * 64:(c + 1) * 64], Sc[:])
                            S.tt(vn[:], us[b][:, c, :], ps1[0:64, 0:64], ALU.subtract)
                            ps2 = K.bank()
                            S.mm(ps2[0:64, 0:64], qds[b][:, c * 64:(c + 1) * 64], Sc[:], start=True, stop=False)
                            S.mm(ps2[0:64, 0:64], qkTs[b][:, c, :], vn[:], start=False, stop=True)
                            if d == 0:
                                S.copy(hacc[:, n, :], ps2[0:64, 0:64], e='act')
                            else:
                                S.tt(hacc[:, n, :], hacc[:, n, :], ps2[0:64, 0:64], ALU.add)
                            ps3 = K.bank()
                            S.mm(ps3[0:64, 0:64], kdec3[:, n, :], vn[:])
                            S.stt(Sn[:], Sc[:], c64(4, n), ps3[0:64, 0:64], ALU.mult, ALU.add)
                osig = kT[:, :].rearrange("p (n c) -> p n c", c=64)
                for n0 in range(0, NCH, 8):
                    nn = min(8, NCH - n0)
                    ps1 = K.bank()
                    for j in range(nn):
                        S.tr(ps1[0:64, j * 64:(j + 1) * 64], raw[:, chunk_cols(n0 + j)], K.ident[0:64, 0:64])
                    S.act(osig[:, n0:n0 + nn, :], ps1[0:64, 0:nn * 64].rearrange("p (j c) -> p j c", c=64), AF.Silu)
                sq = qT[:, :].rearrange("p (n c) -> p n c", c=64)
                ssq = K.sb(es, [64, NCH, 4])
                S.tt(sq, hacc[:], hacc[:], ALU.mult, e='pool')
                S.reduce(ssq[:, :, 0], sq, ALU.add)
                S.ts(ssq[:, :, 1], ssq[:, :, 0], 1.0 / 64, EPS, ALU.mult, ALU.add)
                S.act(ssq[:, :, 2], ssq[:, :, 1], AF.Sqrt)
                S.recip(ssq[:, :, 3], ssq[:, :, 2])
                S.tt(hacc[:], hacc[:], ssq[:, :, 3].unsqueeze(2).to_broadcast([64, NCH, 64]), ALU.mult)
                S.tt(hacc[:], hacc[:], ngt[:, h * 64:(h + 1) * 64].unsqueeze(1).to_broadcast([64, NCH, 64]), ALU.mult, e='pool')
                S.tt(hacc[:], hacc[:], osig, ALU.mult)
                c0 = 768 + h * 64
                S.dma(D['y'][0:CTX, c0:c0 + 64].rearrange("(n i) c -> i n c", i=64), hacc[:, 0:4, :])
                yv = D['y'][CTX:T, c0:c0 + 64].rearrange("(r c) ch -> r c ch", c=64)
                for g4 in range(4):
                    S.dma(yv[:, g4 * 16:(g4 + 1) * 16, :], hacc[:, 4 + g4 * 16:4 + (g4 + 1) * 16, :], q=('sp', 'pool')[g4 % 2])
            S.barrier()
    S.barrier()


def shift_T(S, out, x, mu, np_):
    S.ts(out[0:np_, :], x[0:np_, :], mu[0:np_, 2:3], None, ALU.mult)
    for (a, b) in ((0, CTX), (CTX, T)):
        S.stt(out[0:np_, a + 1:b], x[0:np_, a:b - 1], mu[0:np_, 0:1], out[0:np_, a + 1:b], ALU.mult, ALU.add)
        S.stt(out[0:np_, a:b - 1], x[0:np_, a + 1:b], mu[0:np_, 1:2], out[0:np_, a:b - 1], ALU.mult, ALU.add)


def load_mu(S, W, l, mu, row0, np_):
    S.dma(mu[0:np_, 0:2], W['pk_mu'][l, row0:row0 + np_, :], q='pool')
    S.ts(mu[0:np_, 2:3], mu[0:np_, 0:1], -1.0, 1.0, ALU.mult, ALU.add)
    S.tt(mu[0:np_, 2:3], mu[0:np_, 2:3], mu[0:np_, 1:2], ALU.subtract)


def mixer_b(K, l, last):
    S, W, D = K.S, K.W, K.D
    base = OFF_B
    NP = NCH // 2
    with ExitStack() as es0:
        masks = K.sb(es0, [64, 2, 64])
        S.dma(masks[:], W['c_masks'])
        m128 = K.sb(es0, [128, 4, 128])
        S.dma(m128[:], W['c_m128'])
        ones = K.sb(es0, [64, 64])
        S.memset(ones[:], 1.0)
        for h in range(DBG.get('b_heads', 4)):
            with ExitStack() as es:
                rT, kT, kkT = K.sb(es, [64, T]), K.sb(es, [64, T]), K.sb(es, [64, T])
                Lb = K.sb(es, [64, T])
                bh, ch, kh, rh = K.sb(es, [64, T]), K.sb(es, [64, T]), K.sb(es, [64, T]), K.sb(es, [64, T])
                Vtok = K.sb(es, [64, NCH, 64])
                Vpair = K.sb(es, [128, NP, 64])
                hacc = K.sb(es, [64, NCH, 64])
                pk = K.sb(es, [64, 8])
                S.dma(pk[:, 0:7], W['pk_rw'][l, h])
                mu = K.sb(es, [64, 3])
                wup = K.sb(es, [32, 2, 64])
                aup = K.sb(es, [32, 2, 64])
                gup = K.sb(es, [64, 64])
                for d in range(2):
                    S.dma(wup[:, d, :], W['rwkv_w_up'][l, d][:, h * 64:(h + 1) * 64], q='pool')
                    S.dma(aup[:, d, :], W['rwkv_a_up'][l, d][:, h * 64:(h + 1) * 64], q='pool')
                S.dma(gup[:], W['rwkv_g_up'][l][:, h * 64:(h + 1) * 64], q='pool')
                lng = K.sb(es, [64, 2, 64])
                S.dma(lng[:, 0, :], bc_rows(W['rwkv_ln_g'][l:l + 1, h * 64:(h + 1) * 64], 64))
                S.dma(lng[:, 1, :], bc_rows(W['rwkv_ln_b'][l:l + 1, h * 64:(h + 1) * 64], 64))
                bon = K.sb(es, [64, NCH])
                GLc = K.sb(es, [64, NCH])
                for gi, dst in enumerate((rT, kT, Lb)):
                    r0 = gi * 256 + h * 64
                    load_mu(S, W, l, mu, r0, 64)
                    S.dma(ch[:], D['pT'][base + r0: base + r0 + 64, :])
                    shift_T(S, dst, ch, mu, 64)
                for n0 in range(0, NCH, 8):
                    nn = min(8, NCH - n0)
                    ps1 = K.bank()
                    for j in range(nn):
                        S.tr(ps1[0:64, j * 64:(j + 1) * 64], Lb[:, (n0 + j) * 64:(n0 + j + 1) * 64], K.ident[0:64, 0:64])
                    S.copy(Vtok[:, n0:n0 + nn, :], ps1[0:64, 0:nn * 64].rearrange("p (j c) -> p j c", c=64), e='act')
                for n0 in range(0, NP, 8):
                    nn = min(8, NP - n0)
                    ps2 = K.bank()
                    for j in range(nn):
                        S.tr(ps2[:, j * 64:(j + 1) * 64], Lb[:, (n0 + j) * 128:(n0 + j + 1) * 128], K.ident[0:64, 0:64])
                    S.copy(Vpair[:, n0:n0 + nn, :], ps2[:, 0:nn * 64].rearrange("p (j c) -> p j c", c=64))
                S.ts(kkT[:], kT[:], pk[:, 0:1], None, ALU.mult)
                S.tt(ch[:], kkT[:], kkT[:], ALU.mult, e='pool')
                for n0 in range(0, T, 512):
                    nw = min(512, T - n0)
                    ps = K.bank()
                    S.mm(ps[0:64, :nw], ones[:], ch[:, n0:n0 + nw])
                    S.ts(bh[:, n0:n0 + nw], ps[0:64, :nw], EPS, None, ALU.add)
                S.act(bh[:], bh[:], AF.Sqrt)
                S.recip(bh[:], bh[:])
                S.tt(kkT[:], kkT[:], bh[:], ALU.mult)
                Bm = [K.sb(es, [128, 128]) for _ in range(2)]
                BTm = [K.sb(es, [128, 128]) for _ in range(2)]
                Pm = [K.sb(es, [128, 128]) for _ in range(2)]
                PTm = [K.sb(es, [128, 128]) for _ in range(2)]
                B2s = [K.sb(es, [128, 128]) for _ in range(2)]
                B2Ts = [K.sb(es, [128, 128]) for _ in range(2)]
                AkTs = [K.sb(es, [128, 128]) for _ in range(2)]
                AVs = [K.sb(es, [128, 64]) for _ in range(2)]
                Cps = [K.sb(es, [128, 64]) for _ in range(2)]
                Kts = [K.sb(es, [64, 2, 64]) for _ in range(2)]
                NBts = [K.sb(es, [64, 2, 64]) for _ in range(2)]
                WcTs = [K.sb(es, [64, 128]) for _ in range(2)]
                us = [K.sb(es, [64, 2, 64]) for _ in range(2)]
                QKs = [K.sb(es, [64, 2, 64]) for _ in range(2)]
                NQBs = [K.sb(es, [64, 2, 64]) for _ in range(2)]
                zns = [K.sb(es, [64, 64]) for _ in range(2)]
                Ms = [K.sb(es, [64, 64]) for _ in range(2)]
                mts = [K.sb(es, [64, 64]) for _ in range(2)]
                for d in range(2):
                    lastidx = 63 if d == 0 else 0
                    r0 = 768 + d * 32
                    load_mu(S, W, l, mu, r0, 32)
                    S.dma(kh[0:32, :], D['pT'][base + r0: base + r0 + 32, :])
                    shift_T(S, bh, kh, mu, 32)
                    S.act(bh[0:32, :], bh[0:32, :], AF.Tanh)
                    for n0 in range(0, T, 512):
                        nw = min(512, T - n0)
                        ps = K.bank()
                        S.mm(ps[0:64, :nw], wup[:, d, :], bh[0:32, n0:n0 + nw])
                        S.act(Lb[:, n0:n0 + nw], ps[0:64, :nw], AF.Sigmoid, bias=pk[:, 3 + d:4 + d])
                    S.ts(Lb[:], Lb[:], -math.exp(-0.5), None, ALU.mult)
                    for n in range(NCH):
                        ck = slice(n * 64, (n + 1) * 64)
                        if d == 0:
                            S.scan(rh[:, ck], ones[:, :], Lb[:, ck], 0.0, ALU.mult, ALU.add)
                        else:
                            S.scan(rh[:, ck][:, ::-1], ones[:, :], Lb[:, ck][:, ::-1], 0.0, ALU.mult, ALU.add)
                    S.act(GLc[:], rh[:, :].rearrange("p (n i) -> p n i", i=64)[:, :, lastidx], AF.Exp)
                    S.tt(ch[:], rh[:], Lb[:], ALU.subtract, e='pool')
                    S.act(ch[:], ch[:], AF.Exp)
                    S.tt(ch[:], ch[:], kkT[:], ALU.mult, e='pool')
                    r0 = 832 + d * 32
                    load_mu(S, W, l, mu, r0, 32)
                    S.dma(Lb[0:32, :], D['pT'][base + r0: base + r0 + 32, :])
                    shift_T(S, bh, Lb, mu, 32)
                    for n0 in range(0, T, 512):
                        nw = min(512, T - n0)
                        ps = K.bank()
                        S.mm(ps[0:64, :nw], aup[:, d, :], bh[0:32, n0:n0 + nw])
                        S.act(kh[:, n0:n0 + nw], ps[0:64, :nw], AF.Sigmoid, bias=pk[:, 5 + d:6 + d])
                    S.act(bh[:], rh[:], AF.Exp, scale=-1.0)
                    S.tt(bh[:], bh[:], kkT[:], ALU.mult)
                    S.tt(bh[:], bh[:], kh[:], ALU.mult, e='pool')
                    S.ts(kh[:], kh[:], -1.0, pk[:, 1:2], ALU.add, ALU.mult)
                    S.stt(kh[:], kh[:], 1.0, kT[:], ALU.add, ALU.mult)
                    S.stt(Lb[:], kh[:], pk[:, 2:3], rT[:], ALU.mult, ALU.mult)
                    ps = K.bank()
                    for n in range(NCH):
                        S.mm(ps[0:64, n:n + 1], Lb[:, n * 64:(n + 1) * 64], ones[:, 0:1])
                    if d == 0:
                        S.copy(bon[:], ps[0:64, 0:NCH])
                    else:
                        S.tt(bon[:], bon[:], ps[0:64, 0:NCH], ALU.add)
                    S.act(Lb[:], rh[:], AF.Exp, scale=-1.0)
                    S.tt(kh[:], kh[:], Lb[:], ALU.mult, e='pool')
                    S.act(rh[:], rh[:], AF.Exp)
                    S.tt(rh[:], rh[:], rT[:], ALU.mult)
                    S.memset(Ms[0][:], 0.0)
                    si = 0
                    order = scan_order(d)
                    pairs = [order[i] // 2 for i in range(0, NCH, 2)]
                    for pidx, pi in enumerate(pairs):
                        if pidx >= DBG.get('b_pairs', 99):
                            break
                        b = pidx % 2
                        Bc, BTc, P, PT = Bm[b], BTm[b], Pm[b], PTm[b]
                        tk = slice(pi * 128, (pi + 1) * 128)
                        ps = K.bank()
                        S.mm(ps[:, 0:128], bh[:, tk], ch[:, tk])
                        S.stt(Bc[:], ps[:, 0:128], -1.0, m128[:, 2 + d, :], ALU.mult, ALU.mult)
                        ps = K.bank()
                        S.mm(ps[:, 0:128], ch[:, tk], bh[:, tk])
                        S.stt(BTc[:], ps[:, 0:128], -1.0, m128[:, 3 - d, :], ALU.mult, ALU.mult)
                        S.tt(P[:], Bc[:], K.ident[:], ALU.add, e='pool')
                        S.tt(PT[:], BTc[:], K.ident[:], ALU.add, e='pool')
                        neumann_inverse(K, S, P, PT, Bc, BTc, (B2s, B2Ts))
                        ps = K.bank()
                        S.mm(ps[:, 0:128], kh[:, tk], ch[:, tk])
                        S.tt(AkTs[b][:], ps[:, 0:128], m128[:, 2 + d, :], ALU.mult)
                        ps = K.bank()
                        S.mm(ps[:, 0:64], AkTs[b][:], Vpair[:, pi, :])
                        S.copy(AVs[b][:], ps[:, 0:64], e='act')
                        ps = K.bank()
                        S.tr(ps[:, 0:64], ch[:, tk], K.ident[0:64, 0:64])
                        S.copy(Cps[b][:], ps[:, 0:64], e='act')
                        ps = K.bank()
                        for c in range(2):
                            ck = slice(pi * 128 + c * 64, pi * 128 + (c + 1) * 64)
                            S.tr(ps[0:64, c * 64:(c + 1) * 64], kh[:, ck], K.ident[0:64, 0:64])
                            S.tr(ps[0:64, 128 + c * 64:128 + (c + 1) * 64], bh[:, ck], K.ident[0:64, 0:64])
                        S.copy(Kts[b][:], ps[0:64, 0:128].rearrange("p (c k) -> p c k", c=2), e='act')
                        S.ts(NBts[b][:], ps[0:64, 128:256].rearrange("p (c k) -> p c k", c=2), -1.0, None, ALU.mult)
                        ps = K.bank()
                        S.mm(ps[0:64, 0:128], Cps[b][:], P[:])
                        S.copy(WcTs[b][:], ps[0:64, 0:128], e='act')
                        ps = K.bank()
                        for c in range(2):
                            S.mm(ps[0:64, c * 64:(c + 1) * 64], P[:, c * 64:(c + 1) * 64], AVs[b][:])
                        S.copy(us[b][:], ps[0:64, 0:128].rearrange("p (c v) -> p c v", c=2))
                        ps = K.bank()
                        for c in range(2):
                            ck = slice(pi * 128 + c * 64, pi * 128 + (c + 1) * 64)
                            S.mm(ps[0:64, c * 64:(c + 1) * 64], kh[:, ck], rh[:, ck])
                            S.mm(ps[0:64, 128 + c * 64:128 + (c + 1) * 64], bh[:, ck], rh[:, ck])
                        for c in range(2):
                            S.tt(QKs[b][:, c, :], ps[0:64, c * 64:(c + 1) * 64], masks[:, d, :], ALU.mult)
                            S.stt(NQBs[b][:, c, :], ps[0:64, 128 + c * 64:128 + (c + 1) * 64], -1.0, masks[:, d, :], ALU.mult, ALU.mult)
                        for c in ((0, 1) if d == 0 else (1, 0)):
                            n = pi * 2 + c
                            ck = slice(n * 64, (n + 1) * 64)
                            Mc, Mn = Ms[si % 2], Ms[(si + 1) % 2]
                            zn, mt = zns[si % 2], mts[si % 2]
                            si += 1
                            ps1 = K.bank()
                            S.mm(ps1[0:64, 0:64], WcTs[b][:, c * 64:(c + 1) * 64], Mc[:])
                            S.tt(zn[:], us[b][:, c, :], ps1[0:64, 0:64], ALU.add)
                            ps2 = K.bank()
                            S.mm(ps2[0:64, 0:64], rh[:, ck], Mc[:], start=True, stop=False)
                            S.mm(ps2[0:64, 0:64], QKs[b][:, c, :], Vtok[:, n, :], start=False, stop=False)
                            S.mm(ps2[0:64, 0:64], NQBs[b][:, c, :], zn[:], start=False, stop=True)
                            if d == 0:
                                S.copy(hacc[:, n, :], ps2[0:64, 0:64], e='act')
                            else:
                                S.tt(hacc[:, n, :], hacc[:, n, :], ps2[0:64, 0:64], ALU.add)
                            ps3 = K.bank()
                            S.mm(ps3[0:64, 0:64], Kts[b][:, c, :], Vtok[:, n, :], start=True, stop=False)
                            S.mm(ps3[0:64, 0:64], NBts[b][:, c, :], zn[:], start=False, stop=True)
                            S.ts(mt[:], ps3[0:64, 0:64], GLc[:, n:n + 1], None, ALU.mult)
                            S.stt(Mn[:], Mc[:], GLc[:, n:n + 1], mt[:], ALU.mult, ALU.add)
                gtok = kh[:, :].rearrange("p (n c) -> p n c", c=64)
                load_mu(S, W, l, mu, 896, 64)
                S.dma(rh[:], D['pT'][base + 896: base + 960, :])
                shift_T(S, bh, rh, mu, 64)
                S.act(bh[:], bh[:], AF.Sigmoid)
                for n0 in range(0, NCH, 8):
                    nn = min(8, NCH - n0)
                    ps = K.bank()
                    for j in range(nn):
                        S.mm(ps[0:64, j * 64:(j + 1) * 64], bh[:, (n0 + j) * 64:(n0 + j + 1) * 64], gup[:])
                    S.copy(gtok[:, n0:n0 + nn, :], ps[0:64, 0:nn * 64].rearrange("p (j c) -> p j c", c=64), e='act')
                st = K.sb(es, [64, NCH, 4])
                sq = ch[:, :].rearrange("p (n c) -> p n c", c=64)
                bc3 = lambda a: a.unsqueeze(2).to_broadcast([64, NCH, 64])
                S.reduce(st[:, :, 0], hacc[:], ALU.add)
                S.ts(st[:, :, 0], st[:, :, 0], 1.0 / 64, None, ALU.mult)
                S.tt(hacc[:], hacc[:], bc3(st[:, :, 0]), ALU.subtract)
                S.tt(sq, hacc[:], hacc[:], ALU.mult, e='pool')
                S.reduce(st[:, :, 1], sq, ALU.add)
                S.ts(st[:, :, 1], st[:, :, 1], 1.0 / 64, 64e-5, ALU.mult, ALU.add)
                S.act(st[:, :, 2], st[:, :, 1], AF.Sqrt)
                S.recip(st[:, :, 3], st[:, :, 2])
                S.tt(hacc[:], hacc[:], bc3(st[:, :, 3]), ALU.mult)
                S.tt(hacc[:], hacc[:], lng[:, 0, :].unsqueeze(1).to_broadcast([64, NCH, 64]), ALU.mult, e='pool')
                S.tt(hacc[:], hacc[:], lng[:, 1, :].unsqueeze(1).to_broadcast([64, NCH, 64]), ALU.add, e='pool')
                S.tt(sq, Vtok[:], bc3(bon[:, :]), ALU.mult)
                S.tt(hacc[:], hacc[:], sq, ALU.add, e='pool')
                S.tt(hacc[:], hacc[:], gtok, ALU.mult)
                c0 = 256 + h * 64
                yv = D['y'][:, c0:c0 + 64].rearrange("(n i) c -> i n c", i=64)
                for g4 in range(4):
                    S.dma(yv[:, g4 * 17:(g4 + 1) * 17, :], hacc[:, g4 * 17:(g4 + 1) * 17, :], q=('sp', 'pool')[g4 % 2])
            S.barrier()
    S.barrier()


W_SHAPES = {
    'xin': [T, 1024], 'cc': [128, 8, 2],
    'mod_w': [4, 1024, 6144], 'mod_b': [4, 6144], 'norm_mix_g': [4, 1024], 'norm_ffn_g': [4, 1024],
    'w_in': [4, 1024, IN_COLS], 'w_out': [4, 1024, 1024],
    'lru_conv_w': [4, 4, 256], 'lru_conv_b': [4, 256], 'lru_w_a': [4, 2, 4, 64, 64], 'lru_b_a': [4, 2, 256],
    'lru_w_x': [4, 2, 4, 64, 64], 'lru_b_x': [4, 2, 256], 'lru_lambda': [4, 2, 256],
    'rwkv_mu': [4, 2, 960], 'rwkv_w_up': [4, 2, 32, 256], 'rwkv_w0': [4, 2, 256], 'rwkv_a_up': [4, 2, 32, 256],
    'rwkv_a0': [4, 2, 256], 'rwkv_g_up': [4, 64, 256], 'rwkv_k_k': [4, 256], 'rwkv_k_a': [4, 256],
    'rwkv_r_k': [4, 256], 'rwkv_ln_g': [4, 256], 'rwkv_ln_b': [4, 256],
    'mlstm_i_b': [4, 2, 4], 'mlstm_f_b': [4, 2, 4], 'mlstm_norm_g': [4, 256],
    'gdn_conv_w': [4, 4, 768], 'gdn_a_log': [4, 2, 4], 'gdn_dt_bias': [4, 2, 4], 'gdn_norm_g': [4, 256],
    'ffn_w_gate': [2, 1024, D_FF], 'ffn_w_up': [2, 1024, D_FF], 'ffn_w_down': [2, D_FF, 1024],
    'moe_router': [2, 1024, 8], 'moe_w_gate': [2, 8, 1024, D_FFE], 'moe_w_up': [2, 8, 1024, D_FFE],
    'moe_w_down': [2, 8, D_FFE, 1024], 'final_norm_g': [1, 1024],
    'c_ident': [128, 128], 'c_sel8': [8, 8, 128],
    'pk_lru': [4, 2, 128, 11], 'pk_ml': [4, 4, 4],
    'c_masks': [64, 2, 64], 'c_rst': [4, 2, T], 'c_m128': [128, 4, 128],
    'pk_gd': [4, 4, 4], 'pk_gdc': [4, 4, 64, 3, 4], 'pk_mu': [4, 960, 2], 'pk_rw': [4, 4, 64, 7],
}


def make_consts():
    c = {}
    c['c_ident'] = np.eye(128, dtype=np.float32)
    s = np.zeros((8, 8, 128), np.float32)
    for e in range(8):
        s[e, e, :] = 1.0
    c['c_sel8'] = s
    jj, ii = np.meshgrid(np.arange(64), np.arange(64), indexing='ij')
    c['c_masks'] = np.ascontiguousarray(np.stack([(ii >= jj), (ii <= jj)], axis=1).astype(np.float32))
    ja, ia = np.meshgrid(np.arange(128), np.arange(128), indexing='ij')
    same = (ja // 64) == (ia // 64)
    c['c_m128'] = np.ascontiguousarray(np.stack([same & (ia >= ja), same & (ia <= ja), same & (ia > ja), same & (ia < ja)], axis=1).astype(np.float32))
    idx = np.arange(T) % 64
    r = np.stack([(idx != 0), (idx != 63)], axis=0).astype(np.float32)
    c['c_rst'] = np.ascontiguousarray(np.broadcast_to(r[None], (4, 2, T)))
    return c


def build(layers=(0, 1, 2, 3), mixers=None, final=True, dbg=()):
    nc = bass.Bass("TRN2", target_bir_lowering=False)
    W = {n: nc.dram_tensor(n, sh, F32, kind="ExternalInput").ap() for n, sh in W_SHAPES.items()}
    out = nc.dram_tensor('out', [SEQ, 1024], F32, kind="ExternalOutput").ap()
    D = {}
    for n, sh in {'xres': [T, 1024], 'pT': [IN_COLS, T], 'y': [T, 1024], 'mod': [2, 6144]}.items():
        kind = "ExternalOutput" if n in dbg else "Internal"
        D[n] = nc.dram_tensor('d_' + n, sh, F32, kind=kind).ap()
    with ExitStack() as es:
        S = Sched(nc, es)
        ps = [es.enter_context(nc.psum_tensor("psb%d" % i, [128, 512], F32)) for i in range(8)]
        ident = es.enter_context(nc.sbuf_tensor("ident", [128, 128], F32))
        K = Ctx(nc, S, W, D, ps, ident)
        K.out = out
        S.dma(ident[:], W['c_ident'])
        for t in range(NT):
            S.dma(D['xres'][t * 128:(t + 1) * 128, :], W['xin'][t * 128:(t + 1) * 128, :], q=('sp', 'pool', 'act')[t % 3])
        S.barrier()
        for l in layers:
            last = (l == 3)
            stage_mod(K, l)
            stage_inproj(K, l)
            if mixers is None:
                mix_identity(K, l)
            else:
                for m in mixers:
                    m(K, l, last)
            stage_outproj(K, l, last)
            stage_ffn(K, l, last)
        if final:
            stage_final(K)
        S.finish()
    K.S = S
    return nc, S


def make_packs(inputs):
    f = lambda n: np.asarray(inputs[n], dtype=np.float32)
    pk = {}
    cols = [f('lru_conv_w')[:, j, :] for j in range(4)] + [f('lru_conv_b')]
    cols += [f('lru_b_a')[:, 0], f('lru_b_a')[:, 1], f('lru_b_x')[:, 0], f('lru_b_x')[:, 1], f('lru_lambda')[:, 0], f('lru_lambda')[:, 1]]
    a = np.stack(cols, axis=-1)
    pk['pk_lru'] = np.ascontiguousarray(a.reshape(4, 2, 128, 11))
    pk['pk_gd'] = np.ascontiguousarray(np.concatenate([f('gdn_a_log'), f('gdn_dt_bias')], axis=1).transpose(0, 2, 1))
    pk['pk_gdc'] = np.ascontiguousarray(f('gdn_conv_w').reshape(4, 4, 3, 4, 64).transpose(0, 3, 4, 2, 1))
    pk['pk_mu'] = np.ascontiguousarray(f('rwkv_mu').transpose(0, 2, 1))
    cols = [f('rwkv_k_k'), f('rwkv_k_a'), f('rwkv_r_k'), f('rwkv_w0')[:, 0], f('rwkv_w0')[:, 1], f('rwkv_a0')[:, 0], f('rwkv_a0')[:, 1]]
    pk['pk_rw'] = np.ascontiguousarray(np.stack(cols, axis=-1).reshape(4, 4, 64, 7))
    pk['pk_ml'] = np.ascontiguousarray(np.concatenate([f('mlstm_i_b'), f('mlstm_f_b')], axis=1).transpose(0, 2, 1))
    return pk


def host_inputs(inputs, b):
    m = {}
    m['xin'] = np.ascontiguousarray(np.concatenate([inputs['ctx'][b], inputs['x'][b]], axis=0))
    cc = np.stack([np.asarray(inputs['c'][b]).reshape(8, 128).T, np.asarray(inputs['c_ctx']).reshape(8, 128).T], axis=-1)
    m['cc'] = np.ascontiguousarray(cc.astype(np.float32))
    for n in W_SHAPES:
        if n in m or n.startswith('c_') or n.startswith('pk_'):
            continue
        m[n] = np.ascontiguousarray(np.asarray(inputs[n], dtype=np.float32).reshape(W_SHAPES[n]))
    m.update(make_consts())
    m.update(make_packs(inputs))
    return m


def build_test(L, mixers):
    nc = bass.Bass("TRN2", target_bir_lowering=False)
    W = {n: nc.dram_tensor(n, sh, F32, kind="ExternalInput").ap() for n, sh in W_SHAPES.items()}
    D = {}
    for n, sh in {'xres': [T, 1024], 'pT': [IN_COLS, T], 'y': [T, 1024], 'mod': [2, 6144]}.items():
        kind = "ExternalOutput" if n in ('pT', 'y') else "Internal"
        D[n] = nc.dram_tensor('d_' + n, sh, F32, kind=kind).ap()
    add_scratch(nc, D)
    with ExitStack() as es:
        S = Sched(nc, es)
        ps = [es.enter_context(nc.psum_tensor("psb%d" % i, [128, 512], F32)) for i in range(8)]
        ident = es.enter_context(nc.sbuf_tensor("ident", [128, 128], F32))
        K = Ctx(nc, S, W, D, ps, ident)
        S.dma(ident[:], W['c_ident'])
        for t in range(NT):
            S.dma(D['xres'][t * 128:(t + 1) * 128, :], W['xin'][t * 128:(t + 1) * 128, :], q=('sp', 'pool', 'act')[t % 3])
        S.barrier()
        stage_mod(K, L)
        stage_inproj(K, L)
        for m in mixers:
            m(K, L, False)
        S.finish()
    return nc, S


def add_scratch(nc, D):
    pass


def kernel(**inputs):
    nc, S = build(layers=(0, 1, 2, 3), mixers=[mixer_a, mixer_b, mixer_c, mixer_d], final=True)
    in_maps = [host_inputs(inputs, b) for b in range(8)]
    res = run_bass_kernel_spmd(nc, in_maps, core_ids=list(range(8)))
    return np.stack([np.asarray(r['out'], dtype=np.float32) for r in res.results], axis=0)
```

```python
import math
import numpy as np
import concourse.bass as bass
import concourse.mybir as mybir
from concourse.bass_utils import run_bass_kernel_spmd
from contextlib import ExitStack

F32 = mybir.dt.float32
F32R = mybir.dt.float32r
FAST_MM = True
AF = mybir.ActivationFunctionType
ALU = mybir.AluOpType
AX = mybir.AxisListType

D_MODEL = 1024
SEQ = 4096
CTX = 256
T = SEQ + CTX
NT = T // 128
G = 256
IN_COLS = 3552
OFF_B = 512
OFF_C = OFF_B + 960
OFF_D = OFF_C + 1040
D_FF = 2816
D_FFE = 1408
EPS = 1e-6

ENGS = ['pe', 'dve', 'act', 'pool', 'sp']
SAME_SYNC = {'pe': False, 'dve': True, 'act': True, 'pool': True, 'sp': True}

def _box(ap):
    t = ap.tensor
    name = t.name
    dims = ap.ap
    off = int(ap.offset)
    if str(ap.space) in ('SB', 'PSUM', 'SBUF'):
        row = dims[0][0] if dims[0][0] > 0 else 1
        p0 = off // row
        f0 = off % row
        p1 = p0 + dims[0][1]
        lo = hi = f0
        for st, cnt in dims[1:]:
            if st >= 0:
                hi += st * (cnt - 1)
            else:
                lo += st * (cnt - 1)
        return (name, p0, p1, lo, hi + 1)
    lo = hi = off
    for st, cnt in dims:
        if st >= 0:
            hi += st * (cnt - 1)
        else:
            lo += st * (cnt - 1)
    return (name, 0, 1, lo, hi + 1)


class Sched:
    def __init__(self, nc, es, n_dma=8):
        self.nc = nc
        self.es = es
        self.eng = dict(pe=nc.tensor, dve=nc.vector, act=nc.scalar, pool=nc.gpsimd, sp=nc.sync)
        self.sem = {}
        self.cnt = {}
        self.unit = {}
        for e in ENGS:
            self.sem[e] = es.enter_context(nc.semaphore('s_' + e))
            self.cnt[e] = 0
            self.unit[e] = 1
        self.n_dma = n_dma
        self.dma_rr = {}
        for q in ('sp', 'act', 'pool'):
            self.dma_rr[q] = 0
            for i in range(n_dma):
                c = ('dma', q, i)
                self.sem[c] = es.enter_context(nc.semaphore('d_%s%d' % (q, i)))
                self.cnt[c] = 0
                self.unit[c] = 16
        self.seen = {e: {} for e in ENGS}
        self.recs = {}
        self.nins = 0

    def _need(self, reads, writes):
        need = {}
        for aps, isw in ((reads, False), (writes, True)):
            for ap in aps:
                name, p0, p1, f0, f1 = _box(ap)
                if not isw and name.startswith('psb'):
                    isw, p0, p1, f0, f1 = True, 0, 128, 0, 1 << 30
                for r in self.recs.get(name, ()):
                    if r[0] < p1 and p0 < r[1] and r[2] < f1 and f0 < r[3]:
                        if isw or r[6]:
                            c, v = r[4], r[5]
                            if need.get(c, 0) < v:
                                need[c] = v
        return need

    def _record(self, reads, writes, clock, val):
        for aps, isw in ((reads, False), (writes, True)):
            for ap in aps:
                name, p0, p1, f0, f1 = _box(ap)
                if not isw and name.startswith('psb'):
                    isw, p0, p1, f0, f1 = True, 0, 128, 0, 1 << 30
                lst = self.recs.setdefault(name, [])
                if isw:
                    lst[:] = [r for r in lst if not (p0 <= r[0] and r[1] <= p1 and f0 <= r[2] and r[3] <= f1)]
                else:
                    lst[:] = [r for r in lst if not (r[4] == clock and not r[6] and p0 <= r[0] and r[1] <= p1 and f0 <= r[2] and r[3] <= f1)]
                lst.append((p0, p1, f0, f1, clock, val, isw))
                if len(lst) > 48:
                    self._prune(lst)

    def _prune(self, lst):
        def stale(r):
            c, v = r[4], r[5]
            for e in ENGS:
                if e == c and not SAME_SYNC[e]:
                    continue
                if self.seen[e].get(c, 0) < v:
                    return False
            return True
        lst[:] = [r for r in lst if not stale(r)]

    def _waits(self, e, need):
        eo = self.eng[e]
        for c, v in need.items():
            if c == e and not SAME_SYNC[e]:
                continue
            if self.seen[e].get(c, 0) >= v:
                continue
            eo.wait_ge(self.sem[c], v * self.unit[c])
            self.seen[e][c] = v

    def op(self, e, fn, reads, writes):
        need = self._need(reads, writes)
        self._waits(e, need)
        ins = fn(self.eng[e])
        self.cnt[e] += 1
        ins.then_inc(self.sem[e], 1)
        self._record(reads, writes, e, self.cnt[e])
        self.nins += 1
        return ins

    def dma(self, out, in_, q='sp', **kw):
        need = self._need([in_], [out])
        k = self.dma_rr[q]
        self.dma_rr[q] = (k + 1) % self.n_dma
        c = ('dma', q, k)
        if self.cnt[c] > 0:
            need[c] = max(need.get(c, 0), self.cnt[c])
        self._waits(q, need)
        ins = self.eng[q].dma_start(out=out, in_=in_, **kw)
        self.cnt[c] += 1
        ins.then_inc(self.sem[c], 16)
        self._record([in_], [out], c, self.cnt[c])
        self.nins += 1

    def barrier(self):
        for e in ENGS:
            need = {c: v for c, v in self.cnt.items() if v > 0 and c != e}
            self._waits(e, need)
        self.recs = {}

    def finish(self):
        need = {c: v for c, v in self.cnt.items() if v > 0 and c != 'sp'}
        self._waits('sp', need)

    def mm(self, out, lhsT, rhs, start=True, stop=True, fast=False):
        if fast and FAST_MM:
            lhsT, rhs = lhsT.bitcast(F32R), rhs.bitcast(F32R)
        self.op('pe', lambda e: e.matmul(out, lhsT, rhs, start=start, stop=stop), [lhsT, rhs] + ([] if start else [out]), [out])

    def tr(self, out, in_, ident):
        self.op('pe', lambda e: e.transpose(out, in_, ident), [in_, ident], [out])

    def act(self, out, in_, func, bias=None, scale=None, accum_out=None):
        kw = {}
        rd = [in_]
        wr = [out]
        if bias is not None:
            kw['bias'] = bias
            if not isinstance(bias, (int, float)):
                rd.append(bias)
        if scale is not None:
            kw['scale'] = scale
            if not isinstance(scale, (int, float)):
                rd.append(scale)
        if accum_out is not None:
            kw['accum_out'] = accum_out
            wr.append(accum_out)
        self.op('act', lambda e: e.activation(out, in_, func, **kw), rd, wr)

    def tt(self, out, in0, in1, op, e='dve'):
        self.op(e, lambda en: en.tensor_tensor(out, in0, in1, op), [in0, in1], [out])

    def ts(self, out, in0, s1, s2, op0, op1=None, e='dve', accum_out=None):
        rd = [in0]
        for s in (s1, s2):
            if s is not None and not isinstance(s, (int, float)):
                rd.append(s)
        wr = [out] + ([accum_out] if accum_out is not None else [])
        kw = {}
        if op1 is not None:
            kw['op1'] = op1
        if accum_out is not None:
            kw['accum_out'] = accum_out
        self.op(e, lambda en: en.tensor_scalar(out, in0, s1, s2, op0, **kw), rd, wr)

    def stt(self, out, in0, scalar, in1, op0, op1, e='dve'):
        rd = [in0, in1]
        if not isinstance(scalar, (int, float)):
            rd.append(scalar)
        self.op(e, lambda en: en.scalar_tensor_tensor(out, in0, scalar, in1, op0, op1), rd, [out])

    def copy(self, out, in_, e='dve'):
        if e == 'act':
            self.op(e, lambda en: en.copy(out, in_), [in_], [out])
        else:
            self.op(e, lambda en: en.tensor_copy(out, in_), [in_], [out])

    def memset(self, ap, val, e='dve'):
        self.op(e, lambda en: en.memset(ap, val), [], [ap])

    def reduce(self, out, in_, op, axis=None, e='dve'):
        axis = axis or AX.X
        self.op(e, lambda en: en.tensor_reduce(out, in_, axis, op), [in_], [out])

    def scan(self, out, d0, d1, init, op0, op1):
        rd = [d0, d1]
        if not isinstance(init, (int, float)):
            rd.append(init)
        self.op('dve', lambda en: en.tensor_tensor_scan(out, d0, d1, init, op0, op1), rd, [out])

    def recip(self, out, in_):
        self.op('dve', lambda en: en.reciprocal(out, in_), [in_], [out])


class Ctx:
    def __init__(self, nc, S, W, D, ps, ident):
        self.nc, self.S, self.W, self.D, self.ps, self.ident = nc, S, W, D, ps, ident
        self.uid = 0
        self.psi = 0

    def sb(self, es, shape, dt=F32, name=None):
        self.uid += 1
        return es.enter_context(self.nc.sbuf_tensor("%s_%d" % (name or "t", self.uid), list(shape), dt))

    def bank(self):
        b = self.ps[self.psi % 8]
        self.psi += 1
        return b


def r32(ap):
    return ap.bitcast(F32R) if FAST_MM else ap


def bc_rows(ap, n):
    return ap.to_broadcast([n, ap.shape[1]])


def stage_mod(K, l):
    S, W, D = K.S, K.W, K.D
    with ExitStack() as es:
        cc = K.sb(es, [128, 8, 2])
        S.dma(cc[:], W['cc'])
        S.act(cc[:], cc[:], AF.Silu)
        mb = K.sb(es, [2, 6144])
        S.dma(mb[:], bc_rows(W['mod_b'][l:l + 1, :], 2))
        mo = K.sb(es, [2, 6144])
        wts = [K.sb(es, [128, 8, 512]) for _ in range(2)]
        wv = W['mod_w'][l].rearrange("(k p) c -> p k c", p=128)
        for n in range(12):
            wt = wts[n % 2]
            S.dma(wt[:], wv[:, :, n * 512:(n + 1) * 512], q=('sp' if n % 2 == 0 else 'pool'))
            ps = K.bank()
            for k in range(8):
                S.mm(ps[0:2, :], cc[:, k, :], wt[:, k, :], start=(k == 0), stop=(k == 7))
            S.tt(mo[:, n * 512:(n + 1) * 512], ps[0:2, :], mb[:, n * 512:(n + 1) * 512], ALU.add)
        S.dma(D['mod'], mo[:])
    S.barrier()


def load_mod_tiles(K, es, l, which, gname):
    S, W, D = K.S, K.W, K.D
    base = 3072 * which
    outs = []
    gt = K.sb(es, [128, 1024])
    S.dma(gt[:], bc_rows(W[gname][l:l + 1, :], 128))
    for seg in range(2):
        sh = K.sb(es, [128, 1024])
        sc = K.sb(es, [128, 1024])
        S.dma(sh[:], bc_rows(D['mod'][seg:seg + 1, base:base + 1024], 128), q='pool')
        S.dma(sc[:], bc_rows(D['mod'][seg:seg + 1, base + 1024:base + 2048], 128), q='pool')
        S.stt(sc[:], sc[:], 1.0, gt[:], ALU.add, ALU.mult)
        outs += [sc, sh]
    return outs


def norm_mod_T(K, es_tmp, xt, Gt, SHt, hT_dst, tmp):
    S = K.S
    junk, ss, h = tmp
    S.act(junk[:], xt[:], AF.Square, accum_out=ss[:, 0:1])
    S.ts(ss[:, 1:2], ss[:, 0:1], 1.0 / D_MODEL, EPS, ALU.mult, ALU.add)
    S.act(ss[:, 2:3], ss[:, 1:2], AF.Sqrt)
    S.recip(ss[:, 3:4], ss[:, 2:3])
    S.stt(h[:], xt[:], ss[:, 3:4], Gt[:], ALU.mult, ALU.mult)
    S.tt(h[:], h[:], SHt[:], ALU.add, e='pool')
    for b in range(2):
        ps = K.bank()
        for j in range(4):
            k = b * 4 + j
            S.tr(ps[:, j * 128:(j + 1) * 128], h[:, k * 128:(k + 1) * 128], K.ident[:])
        src = ps[:].rearrange("p (j t) -> p j t", j=4)
        if b == 0:
            S.copy(r32(hT_dst[:, 0:4, :]), src, e='dve')
        else:
            S.copy(r32(hT_dst[:, 4:8, :]), src, e='act')


def stage_inproj(K, l):
    S, W, D = K.S, K.W, K.D
    HALF = T // 2
    wv = W['w_in'][l].rearrange("(k p) c -> p k c", p=128)
    with ExitStack() as es:
        GL, SHL, GC, SHC = load_mod_tiles(K, es, l, 0, 'norm_mix_g')
        hT = K.sb(es, [128, 8, HALF])
        xts = [K.sb(es, [128, 1024]) for _ in range(2)]
        tmp = (K.sb(es, [128, 1024]), K.sb(es, [128, 4]), K.sb(es, [128, 1024]))
        wts = [K.sb(es, [128, 8, 128]) for _ in range(3)]
        ots = [K.sb(es, [128, HALF]) for _ in range(2)]
        for half in range(2):
            for ti in range(17):
                t = half * 17 + ti
                xt = xts[ti % 2]
                S.dma(xt[:], D['xres'][t * 128:(t + 1) * 128, :])
                isctx = t < 2
                norm_mod_T(K, es, xt, GC if isctx else GL, SHC if isctx else SHL,
                           hT[:, :, ti * 128:(ti + 1) * 128], tmp)
            for cchunk in range(28):
                c0 = cchunk * 128
                cw = min(128, IN_COLS - c0)
                wt = wts[cchunk % 3]
                S.dma(r32(wt[:, :, :cw]), wv[:, :, c0:c0 + cw], q='pool')
                ot = ots[cchunk % 2]
                for si, (n0, nw) in enumerate([(0, 512), (512, 512), (1024, 512), (1536, 512), (2048, 128)]):
                    ps = K.bank()
                    for k in range(8):
                        S.mm(ps[:cw, :nw], wt[:, k, :cw], hT[:, k, n0:n0 + nw], start=(k == 0), stop=(k == 7), fast=True)
                    S.copy(ot[:cw, n0:n0 + nw], ps[:cw, :nw], e=('dve' if si % 2 == 0 else 'act'))
                S.dma(D['pT'][c0:c0 + cw, half * HALF:(half + 1) * HALF], ot[:cw, :], q='act')
    S.barrier()


def stage_outproj(K, l, last):
    S, W, D = K.S, K.W, K.D
    wv = W['w_out'][l].rearrange("(k p) c -> p k c", p=128)
    with ExitStack() as es:
        wo = K.sb(es, [128, 8, 1024])
        S.dma(r32(wo[:, 0:4, :]), wv[:, 0:4, :], q='pool')
        S.dma(r32(wo[:, 4:8, :]), wv[:, 4:8, :], q='pool')
        gts = []
        for seg in range(2):
            g = K.sb(es, [128, 1024])
            S.dma(g[:], bc_rows(D['mod'][seg:seg + 1, 2048:3072], 128))
            gts.append(g)
        yts = [K.sb(es, [128, 1024]) for _ in range(2)]
        xts = [K.sb(es, [128, 1024]) for _ in range(2)]
        yTs = [K.sb(es, [128, 8, 128]) for _ in range(2)]
        for t in range(2 if last else 0, NT):
            yt, xt, yT = yts[t % 2], xts[t % 2], yTs[t % 2]
            S.dma(yt[:], D['y'][t * 128:(t + 1) * 128, :])
            S.dma(xt[:], D['xres'][t * 128:(t + 1) * 128, :], q='pool')
            for b in range(2):
                ps = K.bank()
                for j in range(4):
                    k = b * 4 + j
                    S.tr(ps[:, j * 128:(j + 1) * 128], yt[:, k * 128:(k + 1) * 128], K.ident[:])
                S.copy(r32(yT[:, b * 4:(b + 1) * 4, :]), ps[:].rearrange("p (j t) -> p j t", j=4), e=('dve' if b == 0 else 'act'))
            gt = gts[0] if t >= 2 else gts[1]
            for n in range(2):
                ps = K.bank()
                for k in range(8):
                    S.mm(ps[:, :], yT[:, k, :], wo[:, k, n * 512:(n + 1) * 512], start=(k == 0), stop=(k == 7), fast=True)
                S.tt(yt[:, n * 512:(n + 1) * 512], ps[:, :], gt[:, n * 512:(n + 1) * 512], ALU.mult)
                S.tt(xt[:, n * 512:(n + 1) * 512], xt[:, n * 512:(n + 1) * 512], yt[:, n * 512:(n + 1) * 512], ALU.add, e='pool')
            S.dma(D['xres'][t * 128:(t + 1) * 128, :], xt[:], q='act')
    S.barrier()


def stage_ffn(K, l, last):
    S, W, D = K.S, K.W, K.D
    dense = (l % 2 == 0)
    li = l // 2
    if dense:
        E, NF = 1, D_FF // 128
        wg_v = [W['ffn_w_gate'][li].rearrange("(k p) c -> p k c", p=128)]
        wu_v = [W['ffn_w_up'][li].rearrange("(k p) c -> p k c", p=128)]
        wd_v = [W['ffn_w_down'][li].rearrange("(f p) c -> p f c", p=128)]
    else:
        E, NF = 8, D_FFE // 128
        wg_v = [W['moe_w_gate'][li, e].rearrange("(k p) c -> p k c", p=128) for e in range(8)]
        wu_v = [W['moe_w_up'][li, e].rearrange("(k p) c -> p k c", p=128) for e in range(8)]
        wd_v = [W['moe_w_down'][li, e].rearrange("(f p) c -> p f c", p=128) for e in range(8)]
    blocks = ([] if last else [(0, 2)]) + [(2 + 4 * i, 4) for i in range(8)]
    with ExitStack() as es:
        gn = K.sb(es, [128, 1024])
        S.dma(gn[:], bc_rows(W['norm_ffn_g'][l:l + 1, :], 128))
        G2, SH2, GT2 = K.sb(es, [128, 1024]), K.sb(es, [128, 1024]), K.sb(es, [128, 1024])
        xblk = K.sb(es, [128, 4, 1024])
        h2T = K.sb(es, [128, 8, 512])
        actT = K.sb(es, [128, NF, 512])
        yT = K.sb(es, [128, 8, 512])
        tmp = (K.sb(es, [128, 1024]), K.sb(es, [128, 4]), K.sb(es, [128, 1024]))
        sgs = [K.sb(es, [128, 512]) for _ in range(2)]
        wgs = [K.sb(es, [128, 8, 128]) for _ in range(2)]
        wus = [K.sb(es, [128, 8, 128]) for _ in range(2)]
        wds = [K.sb(es, [128, NF, 128]) for _ in range(2)]
        if not dense:
            Gbc = K.sb(es, [128, 8, 512])
            rt = K.sb(es, [128, 8, 8])
            S.dma(rt[:], W['moe_router'][li].rearrange("(k p) e -> p k e", p=128))
            gateT = K.sb(es, [8, 512])
            sel = K.sb(es, [8, 8, 128])
            S.dma(sel[:], W['c_sel8'])
            gsm = K.sb(es, [128, 64])
        cur_seg = None
        wi = 0
        for (t0, nt) in blocks:
            NB = nt * 128
            seg = 1 if t0 < 2 else 0
            if seg != cur_seg:
                cur_seg = seg
                S.dma(SH2[:], bc_rows(D['mod'][seg:seg + 1, 3072:4096], 128), q='pool')
                S.dma(G2[:], bc_rows(D['mod'][seg:seg + 1, 4096:5120], 128), q='pool')
                S.dma(GT2[:], bc_rows(D['mod'][seg:seg + 1, 5120:6144], 128), q='pool')
                S.stt(G2[:], G2[:], 1.0, gn[:], ALU.add, ALU.mult)
            for ti in range(nt):
                t = t0 + ti
                S.dma(xblk[:, ti, :], D['xres'][t * 128:(t + 1) * 128, :])
                norm_mod_T(K, es, xblk[:, ti, :], G2, SH2, h2T[:, :, ti * 128:(ti + 1) * 128], tmp)
            if not dense:
                for ti in range(nt):
                    ps = K.bank()
                    for k in range(8):
                        S.mm(ps[:, 0:8], h2T[:, k, ti * 128:(ti + 1) * 128], rt[:, k, :], start=(k == 0), stop=(k == 7))
                    lg, eq, l2, ex = gsm[:, 0:8], gsm[:, 8:16], gsm[:, 16:24], gsm[:, 24:32]
                    m1, m2, nm1, sm, rs = gsm[:, 32:33], gsm[:, 33:34], gsm[:, 34:35], gsm[:, 35:36], gsm[:, 36:37]
                    gate = gsm[:, 40:48]
                    S.copy(lg, ps[:, 0:8])
                    S.reduce(m1, lg, ALU.max)
                    S.ts(eq, lg, m1, None, ALU.is_equal)
                    S.stt(l2, eq, -1e30, lg, ALU.mult, ALU.add)
                    S.reduce(m2, l2, ALU.max)
                    S.ts(eq, lg, m2, None, ALU.is_ge)
                    S.ts(nm1, m1, -1.0, None, ALU.mult)
                    S.act(ex, lg, AF.Exp, bias=nm1)
                    S.tt(ex, ex, eq, ALU.mult)
                    S.reduce(sm, ex, ALU.add)
                    S.recip(rs, sm)
                    S.ts(gate, ex, rs, None, ALU.mult)
                    ps2 = K.bank()
                    S.tr(ps2[0:8, 0:128], gate, K.ident[:])
                    S.copy(gateT[:, ti * 128:(ti + 1) * 128], ps2[0:8, 0:128])
                for e in range(8):
                    ps = K.bank()
                    S.mm(ps[:, :NB], sel[:, e, :], gateT[:, :NB])
                    S.copy(Gbc[:, e, :NB], ps[:, :NB], e='act')
            for e in range(E):
                for f in range(NF):
                    wg, wu = wgs[wi % 2], wus[wi % 2]
                    sg = sgs[wi % 2]
                    wi += 1
                    S.dma(r32(wg[:]), wg_v[e][:, :, f * 128:(f + 1) * 128], q='pool')
                    S.dma(r32(wu[:]), wu_v[e][:, :, f * 128:(f + 1) * 128], q='pool')
                    psg, psu = K.bank(), K.bank()
                    for k in range(8):
                        S.mm(psg[:, :NB], wg[:, k, :], h2T[:, k, :NB], start=(k == 0), stop=(k == 7), fast=True)
                    for k in range(8):
                        S.mm(psu[:, :NB], wu[:, k, :], h2T[:, k, :NB], start=(k == 0), stop=(k == 7), fast=True)
                    S.act(sg[:, :NB], psg[:, :NB], AF.Silu)
                    S.tt(r32(actT[:, f, :NB]), sg[:, :NB], psu[:, :NB], ALU.mult)
                    if not dense:
                        S.tt(r32(actT[:, f, :NB]), actT[:, f, :NB], Gbc[:, e, :NB], ALU.mult, e='pool')
                for cchunk in range(8):
                    wd = wds[cchunk % 2]
                    S.dma(r32(wd[:]), wd_v[e][:, :, cchunk * 128:(cchunk + 1) * 128], q='pool')
                    ps = K.bank()
                    for f in range(NF):
                        S.mm(ps[:, :NB], wd[:, f, :], actT[:, f, :NB], start=(f == 0), stop=(f == NF - 1), fast=True)
                    if e == 0:
                        S.copy(yT[:, cchunk, :NB], ps[:, :NB], e='act')
                    else:
                        S.tt(yT[:, cchunk, :NB], yT[:, cchunk, :NB], ps[:, :NB], ALU.add)
            for ti in range(nt):
                t = t0 + ti
                yt = tmp[0]
                for b in range(2):
                    ps = K.bank()
                    for j in range(4):
                        S.tr(ps[:, j * 128:(j + 1) * 128], yT[:, b * 4 + j, ti * 128:(ti + 1) * 128], K.ident[:])
                    S.tt(yt[:, b * 512:(b + 1) * 512], ps[:, :], GT2[:, b * 512:(b + 1) * 512], ALU.mult)
                S.tt(xblk[:, ti, :], xblk[:, ti, :], yt[:], ALU.add, e='pool')
                S.dma(D['xres'][t * 128:(t + 1) * 128, :], xblk[:, ti, :], q='act')
    S.barrier()


def stage_final(K):
    S, W, D = K.S, K.W, K.D
    with ExitStack() as es:
        g = K.sb(es, [128, 1024])
        S.dma(g[:], bc_rows(W['final_norm_g'], 128))
        xts = [K.sb(es, [128, 1024]) for _ in range(2)]
        ots = [K.sb(es, [128, 1024]) for _ in range(2)]
        junk = K.sb(es, [128, 1024])
        sss = [K.sb(es, [128, 4]) for _ in range(2)]
        for t in range(2, NT):
            xt, ot, ss = xts[t % 2], ots[t % 2], sss[t % 2]
            S.dma(xt[:], D['xres'][t * 128:(t + 1) * 128, :])
            S.act(junk[:], xt[:], AF.Square, accum_out=ss[:, 0:1])
            S.ts(ss[:, 1:2], ss[:, 0:1], 1.0 / D_MODEL, EPS, ALU.mult, ALU.add)
            S.act(ss[:, 2:3], ss[:, 1:2], AF.Sqrt)
            S.recip(ss[:, 3:4], ss[:, 2:3])
            S.stt(ot[:], xt[:], ss[:, 3:4], g[:], ALU.mult, ALU.mult)
            S.dma(K.out[(t - 2) * 128:(t - 1) * 128, :], ot[:], q='pool')
    S.barrier()


def mix_identity(K, l):
    S, D = K.S, K.D
    with ExitStack() as es:
        pts = [K.sb(es, [128, 8, 128]) for _ in range(2)]
        yts = [K.sb(es, [128, 1024]) for _ in range(2)]
        pv = D['pT'][0:1024, :].rearrange("(k p) t -> p k t", p=128)
        for t in range(NT):
            pt, yt = pts[t % 2], yts[t % 2]
            S.dma(pt[:], pv[:, :, t * 128:(t + 1) * 128])
            for b in range(2):
                ps = K.bank()
                for j in range(4):
                    S.tr(ps[:, j * 128:(j + 1) * 128], pt[:, b * 4 + j, :], K.ident[:])
                S.copy(yt[:, b * 512:(b + 1) * 512], ps[:, :], e=('dve' if b == 0 else 'act'))
            S.dma(D['y'][t * 128:(t + 1) * 128, :], yt[:], q='pool')
    S.barrier()


def to_token_major(K, es, src, ncols_tile, dst_cols, bufs):
    S, D = K.S, K.D
    gi = 0
    for t0 in range(0, NT, 4):
        n = min(4, NT - t0)
        ps = K.bank()
        for j in range(n):
            S.tr(ps[:, j * 128:(j + 1) * 128], src[:, (t0 + j) * 128:(t0 + j + 1) * 128], K.ident[:])
        yb = bufs[gi % 2]
        S.copy(yb[:, :n * 128], ps[:, :n * 128], e=('dve' if gi % 2 == 0 else 'act'))
        dst = D['y'][t0 * 128:(t0 + n) * 128, dst_cols[0]:dst_cols[1]].rearrange("(j p) c -> p j c", p=128)
        S.dma(dst, yb[:, :n * 128].rearrange("p (j c) -> p j c", j=n), q=('sp' if gi % 2 == 0 else 'pool'))
        gi += 1


def mixer_a(K, l, last):
    S, W, D = K.S, K.W, K.D
    SEGS = [(0, CTX), (CTX, T)]
    with ExitStack() as es0:
        ybufs = [K.sb(es0, [128, 512]) for _ in range(2)]
        for ct in range(2):
            with ExitStack() as es:
                pk = K.sb(es, [128, 16])
                S.dma(pk[:, 0:11], W['pk_lru'][l, ct])
                S.act(pk[:, 11:13], pk[:, 9:11], AF.Exp, scale=-1.0)
                S.act(pk[:, 11:13], pk[:, 11:13], AF.Ln, bias=1.0)
                S.ts(pk[:, 13:15], pk[:, 11:13], -16.0, None, ALU.mult)
                S.ts(pk[:, 11:13], pk[:, 11:13], -8.0, None, ALU.mult)
                wbd = K.sb(es, [128, 4, 128])
                S.memset(wbd[:], 0.0)
                for d in range(2):
                    for wi, wn in enumerate(('lru_w_a', 'lru_w_x')):
                        for hl in range(2):
                            S.dma(wbd[hl * 64:(hl + 1) * 64, d * 2 + wi, hl * 64:(hl + 1) * 64], W[wn][l, d, ct * 2 + hl], q='pool')
                xb = K.sb(es, [128, T])
                u = K.sb(es, [128, T])
                gt = K.sb(es, [128, T])
                ra = K.sb(es, [128, T])
                ib = K.sb(es, [128, T])
                h0 = K.sb(es, [128, T])
                h1 = K.sb(es, [128, T])
                S.dma(xb[:], D['pT'][ct * 128:(ct + 1) * 128, :])
                S.dma(gt[:], D['pT'][256 + ct * 128:256 + (ct + 1) * 128, :], q='pool')
                S.ts(u[:], xb[:], pk[:, 2:3], pk[:, 4:5], ALU.mult, ALU.add)
                for (a, b) in SEGS:
                    for j, s in ((0, -2), (1, -1), (3, 1)):
                        lo, hi = max(a, a - s), min(b, b - s)
                        S.stt(u[:, lo:hi], xb[:, lo + s:hi + s], pk[:, j:j + 1], u[:, lo:hi], ALU.mult, ALU.add)
                S.tt(xb[:], gt[:], gt[:], ALU.mult, e='pool')
                S.ts(xb[:], xb[:], 0.044715, 1.0, ALU.mult, ALU.add, e='pool')
                S.tt(xb[:], xb[:], gt[:], ALU.mult, e='pool')
                S.act(xb[:], xb[:], AF.Sigmoid, scale=1.5957691216057308)
                S.tt(gt[:], gt[:], xb[:], ALU.mult, e='pool')
                for d in range(2):
                    blocks = [(n0, min(512, T - n0)) for n0 in range(0, T, 512)]
                    for wi, dst, bcol in ((0, ra, 5 + d), (1, ib, 7 + d)):
                        for (n0, nw) in blocks:
                            ps = K.bank()
                            S.mm(ps[:, :nw], wbd[:, d * 2 + wi, :], u[:, n0:n0 + nw])
                            S.act(dst[:, n0:n0 + nw], ps[:, :nw], AF.Sigmoid, bias=pk[:, bcol:bcol + 1])
                    hd = h0 if d == 0 else h1
                    S.tt(ib[:], ib[:], u[:], ALU.mult, e='pool')
                    S.act(hd[:], ra[:], AF.Exp, scale=pk[:, 13 + d:14 + d])
                    S.ts(hd[:], hd[:], -1.0, 1.0, ALU.mult, ALU.add)
                    S.act(hd[:], hd[:], AF.Sqrt)
                    S.tt(ib[:], ib[:], hd[:], ALU.mult)
                    S.act(ra[:], ra[:], AF.Exp, scale=pk[:, 11 + d:12 + d])
                    if d == 0:
                        S.scan(h0[:], ra[:], ib[:], 0.0, ALU.mult, ALU.add)
                    else:
                        S.scan(h1[:, 0:CTX][:, ::-1], ra[:, 0:CTX][:, ::-1], ib[:, 0:CTX][:, ::-1], 0.0, ALU.mult, ALU.add)
                        S.scan(h1[:, CTX:T][:, ::-1], ra[:, CTX:T][:, ::-1], ib[:, CTX:T][:, ::-1], h1[:, 0:1], ALU.mult, ALU.add)
                S.tt(h0[:], h0[:], h1[:], ALU.add, e='pool')
                S.tt(h0[:], h0[:], gt[:], ALU.mult)
                to_token_major(K, es, h0, 128, (ct * 128, (ct + 1) * 128), ybufs)
            S.barrier()
    S.barrier()


def chunk_cols(n):
    if n < 4:
        return slice(n * 64, (n + 1) * 64)
    c = n - 4
    return slice(CTX + c, T, 64)


NCH = T // 64
DBG = {}


def to_traversal(S, dst, src, e='dve'):
    S.copy(dst[:, 0:CTX], src[:, 0:CTX], e=e)
    S.copy(dst[:, CTX:T].rearrange("p (c r) -> p c r", r=64), src[:, CTX:T].rearrange("p (r c) -> p c r", c=64), e=e)


def scan_order(d):
    if d == 0:
        return list(range(NCH))
    return [3, 2, 1, 0] + list(range(NCH - 1, 3, -1))


def mixer_c(K, l, last):
    S, W, D = K.S, K.W, K.D
    base = OFF_C
    NQ = 6
    with ExitStack() as es0:
        tokcol = K.sb(es0, [64, NCH, NQ * 8])
        masks = K.sb(es0, [64, 2, 64])
        S.dma(masks[:], W['c_masks'])
        ngt = K.sb(es0, [64, 256])
        S.dma(ngt[:], bc_rows(W['mlstm_norm_g'][l:l + 1, :], 64))
        with ExitStack() as es:
            stack = K.sb(es, [NQ * 8, T])
            pk = K.sb(es, [4, 8])
            S.dma(pk[:, 0:4], W['pk_ml'][l])
            S.ts(pk[:, 4:8], pk[:, 0:4], -1.0, None, ALU.mult)
            rst = K.sb(es, [4, T])
            nbg = K.sb(es, [4, T])
            graw = K.sb(es, [4, T])
            li = K.sb(es, [4, T])
            lf = K.sb(es, [4, T])
            bb = K.sb(es, [4, T])
            gg = K.sb(es, [4, T])
            cm = K.sb(es, [4, T])
            mx = K.sb(es, [4, T])
            tmp = graw
            sm = K.sb(es, [4, 8, NCH])
            v3 = lambda t: t[:, :].rearrange("p (n i) -> p n i", i=64)
            bcn = lambda a: a.unsqueeze(2).to_broadcast([4, NCH, 64])
            for d in range(2):
                rv = (lambda a: a) if d == 0 else (lambda a: a[:, ::-1])
                lastidx = 63 if d == 0 else 0
                S.dma(rst[:], W['c_rst'][:, d, :], q='pool')
                S.ts(nbg[:], rst[:], 1e30, -1e30, ALU.mult, ALU.add)
                S.dma(graw[:], D['pT'][base + 1024 + d * 4: base + 1024 + d * 4 + 4, :])
                to_traversal(S, li, graw)
                S.ts(li[:], li[:], pk[:, d:d + 1], None, ALU.add)
                S.dma(graw[:], D['pT'][base + 1024 + 8 + d * 4: base + 1024 + 8 + d * 4 + 4, :])
                to_traversal(S, lf, graw)
                S.act(lf[:], lf[:], AF.Exp, scale=-1.0, bias=pk[:, 6 + d:7 + d])
                S.act(lf[:], lf[:], AF.Ln, bias=1.0)
                S.ts(lf[:], lf[:], -1.0, None, ALU.mult)
                S.scan(rv(bb[:, :]), rv(rst[:, :]), rv(lf[:, :]), 0.0, ALU.mult, ALU.add)
                S.tt(gg[:], li[:], bb[:], ALU.subtract)
                S.scan(rv(cm[:, :]), rv(nbg[:, :]), rv(gg[:, :]), 0.0, ALU.add, ALU.max)
                bL, cmL = v3(bb)[:, :, lastidx], v3(cm)[:, :, lastidx]
                d1t, mm_, mprev, e4, t5 = sm[:, 0, :], sm[:, 1, :], sm[:, 2, :], sm[:, 3, :], sm[:, 4, :]
                S.tt(d1t, bL, cmL, ALU.add)
                if d == 0:
                    S.scan(mm_, bL, d1t, 0.0, ALU.add, ALU.max)
                    S.memset(mprev[:, 0:1], 0.0)
                    S.copy(mprev[:, 1:NCH], mm_[:, 0:NCH - 1])
                else:
                    S.scan(mm_[:, 0:4][:, ::-1], bL[:, 0:4][:, ::-1], d1t[:, 0:4][:, ::-1], 0.0, ALU.add, ALU.max)
                    S.scan(mm_[:, 4:NCH][:, ::-1], bL[:, 4:NCH][:, ::-1], d1t[:, 4:NCH][:, ::-1], mm_[:, 0:1], ALU.add, ALU.max)
                    S.copy(mprev[:, 0:3], mm_[:, 1:4])
                    S.memset(mprev[:, 3:4], 0.0)
                    S.copy(mprev[:, 4:NCH - 1], mm_[:, 5:NCH])
                    S.copy(mprev[:, NCH - 1:NCH], mm_[:, 0:1])
                S.tt(v3(mx), v3(cm), bcn(mprev), ALU.max)
                def put(q, src):
                    r0 = q * 8 + d * 4
                    S.dma(stack[r0:r0 + 4, :], src, q='pool')
                S.act(tmp[:], mx[:], AF.Exp, scale=-1.0)
                put(0, tmp[:])
                S.tt(v3(tmp), bcn(mprev), v3(mx), ALU.subtract)
                S.act(tmp[:], tmp[:], AF.Exp)
                put(1, tmp[:])
                S.tt(tmp[:], bb[:], mx[:], ALU.add)
                S.act(tmp[:], tmp[:], AF.Exp, scale=-1.0)
                put(2, tmp[:])
                S.act(tmp[:], gg[:], AF.Exp)
                put(3, tmp[:])
                S.tt(e4, bL, mprev, ALU.add)
                S.tt(e4, e4, mm_, ALU.subtract)
                S.act(e4, e4, AF.Exp)
                S.copy(v3(tmp), bcn(e4))
                put(4, tmp[:])
                S.tt(t5, bL, mm_, ALU.subtract)
                S.tt(v3(tmp), v3(gg), bcn(t5), ALU.add)
                S.act(tmp[:], tmp[:], AF.Exp)
                put(5, tmp[:])
            NR = NQ * 8
            for n0 in range(0, NCH, 8):
                nn = min(8, NCH - n0)
                ps = K.bank()
                for j in range(nn):
                    S.tr(ps[0:64, j * NR:(j + 1) * NR], stack[0:NR, (n0 + j) * 64:(n0 + j + 1) * 64], K.ident[0:NR, 0:NR])
                S.copy(tokcol[:, n0:n0 + nn, :], ps[0:64, 0:nn * NR].rearrange("p (j c) -> p j c", c=NR))
        S.barrier()
        for h in range(4):
            with ExitStack() as es:
                qT = K.sb(es, [64, T])
                kT = K.sb(es, [64, T])
                vT = K.sb(es, [64, T])
                ktok = K.sb(es, [64, NCH, 64])
                vtok = K.sb(es, [64, NCH, 65])
                hacc = K.sb(es, [64, NCH, 64])
                osig = K.sb(es, [64, NCH, 64])
                S.dma(qT[:], D['pT'][base + h * 64: base + (h + 1) * 64, :])
                S.dma(kT[:], D['pT'][base + 256 + h * 64: base + 256 + (h + 1) * 64, :], q='pool')
                S.dma(vT[:], D['pT'][base + 512 + h * 64: base + 512 + (h + 1) * 64, :], q='act')
                S.ts(kT[:], kT[:], 0.125, None, ALU.mult, e='pool')
                S.memset(vtok[:, :, 64:65], 1.0)
                for n0 in range(0, NCH, 8):
                    nn = min(8, NCH - n0)
                    ps1, ps2 = K.bank(), K.bank()
                    for j in range(nn):
                        cs = chunk_cols(n0 + j)
                        S.tr(ps1[0:64, j * 64:(j + 1) * 64], kT[:, cs], K.ident[0:64, 0:64])
                        S.tr(ps2[0:64, j * 64:(j + 1) * 64], vT[:, cs], K.ident[0:64, 0:64])
                    S.copy(ktok[:, n0:n0 + nn, :], ps1[0:64, 0:nn * 64].rearrange("p (j c) -> p j c", c=64), e='act')
                    S.copy(vtok[:, n0:n0 + nn, 0:64], ps2[0:64, 0:nn * 64].rearrange("p (j c) -> p j c", c=64))
                S.dma(vT[:], D['pT'][base + 768 + h * 64: base + 768 + (h + 1) * 64, :], q='act')
                for d in range(2):
                    col = lambda q: (lambda n: tokcol[:, n, q * 8 + d * 4 + h: q * 8 + d * 4 + h + 1])
                    c1, c2, c3, eg, c4, wn = [col(q) for q in range(6)]
                    Cs = [K.sb(es, [64, 65]) for _ in range(2)]
                    S.memset(Cs[0][:], 0.0)
                    pts = [K.sb(es, [64, 64]) for _ in range(2)]
                    tts = [K.sb(es, [64, 65]) for _ in range(2)]
                    vws = [K.sb(es, [64, 65]) for _ in range(2)]
                    dns = [K.sb(es, [64, 2]) for _ in range(2)]
                    for si, n in enumerate(scan_order(d)):
                        cs = chunk_cols(n)
                        Cc, Cn = Cs[si % 2], Cs[(si + 1) % 2]
                        pt, tot, vw, dn = pts[si % 2], tts[si % 2], vws[si % 2], dns[si % 2]
                        ps_s, ps_o, ps_i, ps_c = K.bank(), K.bank(), K.bank(), K.bank()
                        S.mm(ps_s[0:64, 0:64], kT[:, cs], qT[:, cs])
                        S.stt(pt[:], ps_s[0:64, 0:64], eg(n), masks[:, d, :], ALU.mult, ALU.mult)
                        S.mm(ps_o[0:64, 0:65], pt[:], vtok[:, n, :])
                        S.mm(ps_i[0:64, 0:65], qT[:, cs], Cc[:])
                        S.ts(tot[:], ps_o[0:64, 0:65], c1(n), None, ALU.mult)
                        S.stt(tot[:], ps_i[0:64, 0:65], c2(n), tot[:], ALU.mult, ALU.add)
                        S.act(dn[:, 0:1], tot[:, 64:65], AF.Abs)
                        S.ts(dn[:, 0:1], dn[:, 0:1], c3(n), None, ALU.max)
                        S.recip(dn[:, 1:2], dn[:, 0:1])
                        if d == 0:
                            S.ts(hacc[:, n, :], tot[:, 0:64], dn[:, 1:2], None, ALU.mult)
                        else:
                            S.stt(hacc[:, n, :], tot[:, 0:64], dn[:, 1:2], hacc[:, n, :], ALU.mult, ALU.add)
                        S.ts(vw[:], vtok[:, n, :], wn(n), None, ALU.mult, e='pool')
                        S.mm(ps_c[0:64, 0:65], ktok[:, n, :], vw[:])
                        S.stt(Cn[:], Cc[:], c4(n), ps_c[0:64, 0:65], ALU.mult, ALU.add)
                for n0 in range(0, NCH, 8):
                    nn = min(8, NCH - n0)
                    ps1 = K.bank()
                    for j in range(nn):
                        S.tr(ps1[0:64, j * 64:(j + 1) * 64], vT[:, chunk_cols(n0 + j)], K.ident[0:64, 0:64])
                    S.act(osig[:, n0:n0 + nn, :], ps1[0:64, 0:nn * 64].rearrange("p (j c) -> p j c", c=64), AF.Sigmoid)
                sq = K.sb(es, [64, NCH, 64])
                ssq = K.sb(es, [64, NCH, 4])
                S.tt(sq[:], hacc[:], hacc[:], ALU.mult, e='pool')
                S.reduce(ssq[:, :, 0], sq[:], ALU.add)
                S.ts(ssq[:, :, 1], ssq[:, :, 0], 1.0 / 64, EPS, ALU.mult, ALU.add)
                S.act(ssq[:, :, 2], ssq[:, :, 1], AF.Sqrt)
                S.recip(ssq[:, :, 3], ssq[:, :, 2])
                S.tt(hacc[:], hacc[:], ssq[:, :, 3].unsqueeze(2).to_broadcast([64, NCH, 64]), ALU.mult)
                S.tt(hacc[:], hacc[:], ngt[:, h * 64:(h + 1) * 64].unsqueeze(1).to_broadcast([64, NCH, 64]), ALU.mult, e='pool')
                S.tt(hacc[:], hacc[:], osig[:], ALU.mult)
                c0 = 512 + h * 64
                S.dma(D['y'][0:CTX, c0:c0 + 64].rearrange("(n i) c -> i n c", i=64), hacc[:, 0:4, :])
                yv = D['y'][CTX:T, c0:c0 + 64].rearrange("(r c) ch -> r c ch", c=64)
                for g4 in range(4):
                    S.dma(yv[:, g4 * 16:(g4 + 1) * 16, :], hacc[:, 4 + g4 * 16:4 + (g4 + 1) * 16, :], q=('sp', 'pool')[g4 % 2])
            S.barrier()
    S.barrier()


def dwconv_trav(S, out, x, wcol, bias=None):
    if bias is None:
        S.ts(out[:], x[:], wcol(2), None, ALU.mult)
    else:
        S.ts(out[:], x[:], wcol(2), bias, ALU.mult, ALU.add)
    for (a, b) in ((0, CTX), (CTX, T)):
        for j, s in ((0, -2), (1, -1), (3, 1)):
            lo, hi = max(a, a - s), min(b, b - s)
            S.stt(out[:, lo:hi], x[:, lo + s:hi + s], wcol(j), out[:, lo:hi], ALU.mult, ALU.add)


def neumann_inverse(K, S, P, PT, B, BT, tmps):
    B2s, B2Ts = tmps
    cb, cbt = B, BT
    for m in range(1, 6):
        lastm = (m == 5)
        ps1 = K.bank()
        S.mm(ps1[:, 0:128], cbt[:], cb[:])
        nb = B2s[m % 2]
        S.copy(nb[:], ps1[:, 0:128], e='act')
        if not lastm:
            ps2 = K.bank()
            S.mm(ps2[:, 0:128], cb[:], cbt[:])
            nbt = B2Ts[m % 2]
            S.copy(nbt[:], ps2[:, 0:128], e='dve')
        ps3 = K.bank()
        S.mm(ps3[:, 0:128], PT[:], nb[:])
        if not lastm:
            ps4 = K.bank()
            S.mm(ps4[:, 0:128], nb[:], PT[:])
        S.tt(P[:], P[:], ps3[:, 0:128], ALU.add)
        if not lastm:
            S.tt(PT[:], PT[:], ps4[:, 0:128], ALU.add)
            cb, cbt = nb, nbt


def mixer_d(K, l, last):
    S, W, D = K.S, K.W, K.D
    base = OFF_D
    NQ = 6
    NR = NQ * 8
    NP = NCH // 2
    with ExitStack() as es0:
        tok64 = K.sb(es0, [64, NCH, NR])
        tokP = K.sb(es0, [128, NP, NR])
        stack = K.sb(es0, [NR, T])
        masks = K.sb(es0, [64, 2, 64])
        S.dma(masks[:], W['c_masks'])
        m128 = K.sb(es0, [128, 4, 128])
        S.dma(m128[:], W['c_m128'])
        sel = K.sb(es0, [8, 8, 128])
        S.dma(sel[:], W['c_sel8'])
        ngt = K.sb(es0, [64, 256])
        S.dma(ngt[:], bc_rows(W['gdn_norm_g'][l:l + 1, :], 64))
        with ExitStack() as es:
            pk = K.sb(es, [4, 8])
            S.dma(pk[:, 0:4], W['pk_gd'][l])
            S.act(pk[:, 4:6], pk[:, 0:2], AF.Exp)
            S.ts(pk[:, 4:6], pk[:, 4:6], -1.0, None, ALU.mult)
            rst = K.sb(es, [4, T])
            graw = K.sb(es, [4, T])
            la = K.sb(es, [4, T])
            bt = K.sb(es, [4, T])
            gam = K.sb(es, [4, T])
            tmp = K.sb(es, [4, T])
            sm = K.sb(es, [4, 4, NCH])
            v3 = lambda t: t[:, :].rearrange("p (n i) -> p n i", i=64)
            bcn = lambda a: a.unsqueeze(2).to_broadcast([4, NCH, 64])
            for d in range(2):
                rv = (lambda a: a) if d == 0 else (lambda a: a[:, ::-1])
                lastidx = 63 if d == 0 else 0
                S.dma(rst[:], W['c_rst'][:, d, :], q='pool')
                S.dma(graw[:], D['pT'][base + 1024 + d * 4: base + 1024 + d * 4 + 4, :])
                to_traversal(S, la, graw)
                S.act(la[:], la[:], AF.Exp, bias=pk[:, 2 + d:3 + d])
                S.act(la[:], la[:], AF.Ln, bias=1.0)
                S.ts(la[:], la[:], pk[:, 4 + d:5 + d], None, ALU.mult)
                S.dma(graw[:], D['pT'][base + 1024 + 8 + d * 4: base + 1024 + 8 + d * 4 + 4, :])
                to_traversal(S, bt, graw)
                S.act(bt[:], bt[:], AF.Sigmoid)
                S.scan(rv(gam[:, :]), rv(rst[:, :]), rv(la[:, :]), 0.0, ALU.mult, ALU.add)
                gL = v3(gam)[:, :, lastidx]
                def put(q, src):
                    r0 = q * 8 + d * 4
                    S.dma(stack[r0:r0 + 4, :], src, q='pool')
                put(0, gam[:])
                put(1, bt[:])
                S.act(tmp[:], gam[:], AF.Exp)
                S.tt(tmp[:], tmp[:], bt[:], ALU.mult)
                put(2, tmp[:])
                S.tt(v3(tmp), bcn(gL), v3(gam), ALU.subtract)
                S.act(tmp[:], tmp[:], AF.Exp)
                put(3, tmp[:])
                S.act(sm[:, 0, :], gL, AF.Exp)
                S.copy(v3(tmp), bcn(sm[:, 0, :]))
                put(4, tmp[:])
                S.ts(tmp[:], bt[:], -1.0, None, ALU.mult)
                put(5, tmp[:])
            for n0 in range(0, NCH, 8):
                nn = min(8, NCH - n0)
                ps = K.bank()
                for j in range(nn):
                    S.tr(ps[0:64, j * NR:(j + 1) * NR], stack[0:NR, (n0 + j) * 64:(n0 + j + 1) * 64], K.ident[0:NR, 0:NR])
                S.copy(tok64[:, n0:n0 + nn, :], ps[0:64, 0:nn * NR].rearrange("p (j c) -> p j c", c=NR))
            for n0 in range(0, NP, 8):
                nn = min(8, NP - n0)
                ps = K.bank()
                for j in range(nn):
                    S.tr(ps[:, j * NR:(j + 1) * NR], stack[0:NR, (n0 + j) * 128:(n0 + j + 1) * 128], K.ident[0:NR, 0:NR])
                S.copy(tokP[:, n0:n0 + nn, :], ps[:, 0:nn * NR].rearrange("p (j c) -> p j c", c=NR), e='act')
        S.barrier()
        if DBG.get('d_stop') == 1:
            return
        for h in range(DBG.get('d_heads', 4)):
            with ExitStack() as es:
                raw = K.sb(es, [64, T])
                trv = K.sb(es, [64, T])
                qT = K.sb(es, [64, T])
                kT = K.sb(es, [64, T])
                vT = K.sb(es, [64, T])
                ktok = K.sb(es, [64, NCH, 64])
                kP = K.sb(es, [128, NP, 64])
                vP = K.sb(es, [128, NP, 64])
                hacc = K.sb(es, [64, NCH, 64])
                cw = K.sb(es, [64, 3, 4])
                ones = K.sb(es, [64, 64])
                S.memset(ones[:], 1.0)
                S.dma(cw[:], W['pk_gdc'][l, h])
                for gi, dst in enumerate((qT, kT, vT)):
                    S.dma(raw[:], D['pT'][base + gi * 256 + h * 64: base + gi * 256 + (h + 1) * 64, :])
                    to_traversal(S, trv, raw, e='pool')
                    dwconv_trav(S, dst, trv, lambda j: cw[:, gi, j:j + 1])
                    S.act(dst[:], dst[:], AF.Silu)
                    if gi < 2:
                        S.tt(trv[:], dst[:], dst[:], ALU.mult, e='pool')
                        for n0 in range(0, T, 512):
                            nw = min(512, T - n0)
                            ps = K.bank()
                            S.mm(ps[0:64, :nw], ones[:], trv[:, n0:n0 + nw])
                            S.ts(raw[:, n0:n0 + nw], ps[0:64, :nw], EPS, None, ALU.add)
                        S.act(raw[:], raw[:], AF.Sqrt)
                        S.recip(raw[:], raw[:])
                        if gi == 0:
                            S.stt(dst[:], dst[:], 0.125, raw[:], ALU.mult, ALU.mult)
                        else:
                            S.tt(dst[:], dst[:], raw[:], ALU.mult)
                for n0 in range(0, NCH, 8):
                    nn = min(8, NCH - n0)
                    ps1 = K.bank()
                    for j in range(nn):
                        S.tr(ps1[0:64, j * 64:(j + 1) * 64], kT[:, (n0 + j) * 64:(n0 + j + 1) * 64], K.ident[0:64, 0:64])
                    S.copy(ktok[:, n0:n0 + nn, :], ps1[0:64, 0:nn * 64].rearrange("p (j c) -> p j c", c=64), e='act')
                for n0 in range(0, NP, 8):
                    nn = min(8, NP - n0)
                    ps1, ps2 = K.bank(), K.bank()
                    for j in range(nn):
                        S.tr(ps1[:, j * 64:(j + 1) * 64], kT[:, (n0 + j) * 128:(n0 + j + 1) * 128], K.ident[0:64, 0:64])
                        S.tr(ps2[:, j * 64:(j + 1) * 64], vT[:, (n0 + j) * 128:(n0 + j + 1) * 128], K.ident[0:64, 0:64])
                    S.copy(kP[:, n0:n0 + nn, :], ps1[:, 0:nn * 64].rearrange("p (j c) -> p j c", c=64), e='act')
                    S.copy(vP[:, n0:n0 + nn, :], ps2[:, 0:nn * 64].rearrange("p (j c) -> p j c", c=64))
                if DBG.get('d_stop') == 2:
                    S.barrier()
                    return
                S.dma(raw[:], D['pT'][base + 768 + h * 64: base + 768 + (h + 1) * 64, :])
                kdec = trv
                kdec3 = kdec[:, :].rearrange("p (n c) -> p n c", c=64)
                GBs = [K.sb(es, [128, 128]) for _ in range(2)]
                decL = [K.sb(es, [128, 128]) for _ in range(2)]
                Bm = [K.sb(es, [128, 128]) for _ in range(2)]
                BTm = [K.sb(es, [128, 128]) for _ in range(2)]
                Pm = [K.sb(es, [128, 128]) for _ in range(2)]
                PTm = [K.sb(es, [128, 128]) for _ in range(2)]
                B2s = [K.sb(es, [128, 128]) for _ in range(2)]
                B2Ts = [K.sb(es, [128, 128]) for _ in range(2)]
                rU = [K.sb(es, [128, 64]) for _ in range(2)]
                rW = [K.sb(es, [128, 64]) for _ in range(2)]
                wTs = [K.sb(es, [64, 128]) for _ in range(2)]
                us = [K.sb(es, [64, 2, 64]) for _ in range(2)]
                qkTs = [K.sb(es, [64, 2, 64]) for _ in range(2)]
                qds = [K.sb(es, [64, 128]) for _ in range(2)]
                vns = [K.sb(es, [64, 64]) for _ in range(2)]
                Ss = [K.sb(es, [64, 64]) for _ in range(2)]
                for d in range(2):
                    cP = lambda q, pi: tokP[:, pi, q * 8 + d * 4 + h: q * 8 + d * 4 + h + 1]
                    c64 = lambda q, n: tok64[:, n, q * 8 + d * 4 + h: q * 8 + d * 4 + h + 1]
                    S.tt(kdec3, ktok[:], tok64[:, :, 3 * 8 + d * 4 + h].unsqueeze(2).to_broadcast([64, NCH, 64]), ALU.mult, e='pool')
                    S.memset(Ss[0][:], 0.0)
                    si = 0
                    order = scan_order(d)
                    pairs = [order[i] // 2 for i in range(0, NCH, 2)]
                    for pidx, pi in enumerate(pairs):
                        if pidx >= DBG.get('d_pairs', 99):
                            break
                        b = pidx % 2
                        GB, dL, Bc, BTc, P, PT = GBs[b], decL[b], Bm[b], BTm[b], Pm[b], PTm[b]
                        tk = slice(pi * 128, (pi + 1) * 128)
                        ps = K.bank()
                        S.mm(ps[:, 0:128], sel[:, d * 4 + h, :], stack[0:8, tk])
                        S.copy(GB[:], ps[:, 0:128], e='act')
                        S.ts(dL[:], GB[:], cP(0, pi), 0.0, ALU.subtract, ALU.max)
                        S.act(dL[:], dL[:], AF.Exp, scale=-1.0)
                        S.tt(dL[:], dL[:], m128[:, 3 - d, :], ALU.mult, e='pool')
                        if DBG.get('d_stop') == 5:
                            continue
                        ps = K.bank()
                        S.mm(ps[:, 0:128], kT[:, tk], kT[:, tk])
                        S.stt(BTc[:], ps[:, 0:128], cP(5, pi), dL[:], ALU.mult, ALU.mult)
                        if DBG.get('d_stop') == 6:
                            continue
                        ps = K.bank()
                        S.tr(ps[:, 0:128], BTc[:], K.ident[:])
                        S.copy(Bc[:], ps[:, 0:128], e='act')
                        if DBG.get('d_stop') == 7:
                            continue
                        S.tt(P[:], Bc[:], K.ident[:], ALU.add)
                        if DBG.get('d_stop') == 8:
                            continue
                        S.tt(PT[:], BTc[:], K.ident[:], ALU.add, e='pool')
                        if DBG.get('d_stop') == 3:
                            continue
                        neumann_inverse(K, S, P, PT, Bc, BTc, (B2s, B2Ts))
                        if DBG.get('d_stop') == 4:
                            continue
                        S.ts(rU[b][:], vP[:, pi, :], cP(1, pi), None, ALU.mult, e='pool')
                        S.ts(rW[b][:], kP[:, pi, :], cP(2, pi), None, ALU.mult, e='pool')
                        ps = K.bank()
                        S.mm(ps[0:64, 0:128], rW[b][:], P[:])
                        S.copy(wTs[b][:], ps[0:64, 0:128], e='act')
                        ps = K.bank()
                        for c in range(2):
                            S.mm(ps[0:64, c * 64:(c + 1) * 64], P[:, c * 64:(c + 1) * 64], rU[b][:])
                        S.copy(us[b][:], ps[0:64, 0:128].rearrange("p (c v) -> p c v", c=2))
                        S.act(qds[b][:], GB[0:64, :], AF.Exp)
                        S.tt(qds[b][:], qds[b][:], qT[:, tk], ALU.mult, e='pool')
                        for c in range(2):
                            n = pi * 2 + c
                            ck = slice(n * 64, (n + 1) * 64)
                            ps = K.bank()
                            S.mm(ps[0:64, 0:64], kT[:, ck], qT[:, ck])
                            dt_ = vns[c]
                            S.ts(qkTs[b][:, c, :], GB[0:64, c * 64:(c + 1) * 64], c64(0, n), 0.0, ALU.subtract, ALU.min)
                            S.act(qkTs[b][:, c, :], qkTs[b][:, c, :], AF.Exp)
                            S.tt(qkTs[b][:, c, :], qkTs[b][:, c, :], masks[:, d, :], ALU.mult, e='pool')
                            S.tt(qkTs[b][:, c, :], qkTs[b][:, c, :], ps[0:64, 0:64], ALU.mult)
                        for c in ((0, 1) if d == 0 else (1, 0)):
                            n = pi * 2 + c
                            Sc, Sn = Ss[si % 2], Ss[(si + 1) % 2]
                            vn = vns[si % 2]
                            si += 1
                            ps1 = K.bank()
                            S.mm(ps1[0:64, 0:64], wTs[b][:, c * 64:(c + 1) * 64], Sc[:])
                            S.tt(vn[:], us[b][:, c, :], ps1[0:64, 0:64], ALU.subtract)
                            ps2 = K.bank()
                            S.mm(ps2[0:64, 0:64], qds[b][:, c * 64:(c + 1) * 64], Sc[:], start=True, stop=False)
                            S.mm(ps2[0:64, 0:64], qkTs[b][:, c, :], vn[:], start=False, stop=True)
                            if d == 0:
                                S.copy(hacc[:, n, :], ps2[0:64, 0:64], e='act')
                            else:
                                S.tt(hacc[:, n, :], hacc[:, n, :], ps2[0:64, 0:64], ALU.add)
                            ps3 = K.bank()
                            S.mm(ps3[0:64, 0:64], kdec3[:, n, :], vn[:])
                            S.stt(Sn[:], Sc[:], c64(4, n), ps3[0:64, 0:64], ALU.mult, ALU.add)
                osig = kT[:, :].rearrange("p (n c) -> p n c", c=64)
                for n0 in range(0, NCH, 8):
                    nn = min(8, NCH - n0)
                    ps1 = K.bank()
                    for j in range(nn):
                        S.tr(ps1[0:64, j * 64:(j + 1) * 64], raw[:, chunk_cols(n0 + j)], K.ident[0:64, 0:64])
                    S.act(osig[:, n0:n0 + nn, :], ps1[0:64, 0:nn * 64].rearrange("p (j c) -> p j c", c=64), AF.Silu)
                sq = qT[:, :].rearrange("p (n c) -> p n c", c=64)
                ssq = K.sb(es, [64, NCH, 4])
                S.tt(sq, hacc[:], hacc[:], ALU.mult, e='pool')
                S.reduce(ssq[:, :, 0], sq, ALU.add)
                S.ts(ssq[:, :, 1], ssq[:, :, 0], 1.0 / 64, EPS, ALU.mult, ALU.add)
                S.act(ssq[:, :, 2], ssq[:, :, 1], AF.Sqrt)
                S.recip(ssq[:, :, 3], ssq[:, :, 2])
                S.tt(hacc[:], hacc[:], ssq[:, :, 3].unsqueeze(2).to_broadcast([64, NCH, 64]), ALU.mult)
                S.tt(hacc[:], hacc[:], ngt[:, h * 64:(h + 1) * 64].unsqueeze(1).to_broadcast([64, NCH, 64]), ALU.mult, e='pool')
                S.tt(hacc[:], hacc[:], osig, ALU.mult)
                c0 = 768 + h * 64
                S.dma(D['y'][0:CTX, c0:c0 + 64].rearrange("(n i) c -> i n c", i=64), hacc[:, 0:4, :])
                yv = D['y'][CTX:T, c0:c0 + 64].rearrange("(r c) ch -> r c ch", c=64)
                for g4 in range(4):
                    S.dma(yv[:, g4 * 16:(g4 + 1) * 16, :], hacc[:, 4 + g4 * 16:4 + (g4 + 1) * 16, :], q=('sp', 'pool')[g4 % 2])
            S.barrier()
    S.barrier()


def shift_T(S, out, x, mu, np_):
    S.ts(out[0:np_, :], x[0:np_, :], mu[0:np_, 2:3], None, ALU.mult)
    for (a, b) in ((0, CTX), (CTX, T)):
        S.stt(out[0:np_, a + 1:b], x[0:np_, a:b - 1], mu[0:np_, 0:1], out[0:np_, a + 1:b], ALU.mult, ALU.add)
        S.stt(out[0:np_, a:b - 1], x[0:np_, a + 1:b], mu[0:np_, 1:2], out[0:np_, a:b - 1], ALU.mult, ALU.add)


def load_mu(S, W, l, mu, row0, np_):
    S.dma(mu[0:np_, 0:2], W['pk_mu'][l, row0:row0 + np_, :], q='pool')
    S.ts(mu[0:np_, 2:3], mu[0:np_, 0:1], -1.0, 1.0, ALU.mult, ALU.add)
    S.tt(mu[0:np_, 2:3], mu[0:np_, 2:3], mu[0:np_, 1:2], ALU.subtract)


def mixer_b(K, l, last):
    S, W, D = K.S, K.W, K.D
    base = OFF_B
    NP = NCH // 2
    with ExitStack() as es0:
        masks = K.sb(es0, [64, 2, 64])
        S.dma(masks[:], W['c_masks'])
        m128 = K.sb(es0, [128, 4, 128])
        S.dma(m128[:], W['c_m128'])
        ones = K.sb(es0, [64, 64])
        S.memset(ones[:], 1.0)
        for h in range(DBG.get('b_heads', 4)):
            with ExitStack() as es:
                rT, kT, kkT = K.sb(es, [64, T]), K.sb(es, [64, T]), K.sb(es, [64, T])
                Lb = K.sb(es, [64, T])
                bh, ch, kh, rh = K.sb(es, [64, T]), K.sb(es, [64, T]), K.sb(es, [64, T]), K.sb(es, [64, T])
                Vtok = K.sb(es, [64, NCH, 64])
                Vpair = K.sb(es, [128, NP, 64])
                hacc = K.sb(es, [64, NCH, 64])
                pk = K.sb(es, [64, 8])
                S.dma(pk[:, 0:7], W['pk_rw'][l, h])
                mu = K.sb(es, [64, 3])
                wup = K.sb(es, [32, 2, 64])
                aup = K.sb(es, [32, 2, 64])
                gup = K.sb(es, [64, 64])
                for d in range(2):
                    S.dma(wup[:, d, :], W['rwkv_w_up'][l, d][:, h * 64:(h + 1) * 64], q='pool')
                    S.dma(aup[:, d, :], W['rwkv_a_up'][l, d][:, h * 64:(h + 1) * 64], q='pool')
                S.dma(gup[:], W['rwkv_g_up'][l][:, h * 64:(h + 1) * 64], q='pool')
                lng = K.sb(es, [64, 2, 64])
                S.dma(lng[:, 0, :], bc_rows(W['rwkv_ln_g'][l:l + 1, h * 64:(h + 1) * 64], 64))
                S.dma(lng[:, 1, :], bc_rows(W['rwkv_ln_b'][l:l + 1, h * 64:(h + 1) * 64], 64))
                bon = K.sb(es, [64, NCH])
                GLc = K.sb(es, [64, NCH])
                for gi, dst in enumerate((rT, kT, Lb)):
                    r0 = gi * 256 + h * 64
                    load_mu(S, W, l, mu, r0, 64)
                    S.dma(ch[:], D['pT'][base + r0: base + r0 + 64, :])
                    shift_T(S, dst, ch, mu, 64)
                for n0 in range(0, NCH, 8):
                    nn = min(8, NCH - n0)
                    ps1 = K.bank()
                    for j in range(nn):
                        S.tr(ps1[0:64, j * 64:(j + 1) * 64], Lb[:, (n0 + j) * 64:(n0 + j + 1) * 64], K.ident[0:64, 0:64])
                    S.copy(Vtok[:, n0:n0 + nn, :], ps1[0:64, 0:nn * 64].rearrange("p (j c) -> p j c", c=64), e='act')
                for n0 in range(0, NP, 8):
                    nn = min(8, NP - n0)
                    ps2 = K.bank()
                    for j in range(nn):
                        S.tr(ps2[:, j * 64:(j + 1) * 64], Lb[:, (n0 + j) * 128:(n0 + j + 1) * 128], K.ident[0:64, 0:64])
                    S.copy(Vpair[:, n0:n0 + nn, :], ps2[:, 0:nn * 64].rearrange("p (j c) -> p j c", c=64))
                S.ts(kkT[:], kT[:], pk[:, 0:1], None, ALU.mult)
                S.tt(ch[:], kkT[:], kkT[:], ALU.mult, e='pool')
                for n0 in range(0, T, 512):
                    nw = min(512, T - n0)
                    ps = K.bank()
                    S.mm(ps[0:64, :nw], ones[:], ch[:, n0:n0 + nw])
                    S.ts(bh[:, n0:n0 + nw], ps[0:64, :nw], EPS, None, ALU.add)
                S.act(bh[:], bh[:], AF.Sqrt)
                S.recip(bh[:], bh[:])
                S.tt(kkT[:], kkT[:], bh[:], ALU.mult)
                Bm = [K.sb(es, [128, 128]) for _ in range(2)]
                BTm = [K.sb(es, [128, 128]) for _ in range(2)]
                Pm = [K.sb(es, [128, 128]) for _ in range(2)]
                PTm = [K.sb(es, [128, 128]) for _ in range(2)]
                B2s = [K.sb(es, [128, 128]) for _ in range(2)]
                B2Ts = [K.sb(es, [128, 128]) for _ in range(2)]
                AkTs = [K.sb(es, [128, 128]) for _ in range(2)]
                AVs = [K.sb(es, [128, 64]) for _ in range(2)]
                Cps = [K.sb(es, [128, 64]) for _ in range(2)]
                Kts = [K.sb(es, [64, 2, 64]) for _ in range(2)]
                NBts = [K.sb(es, [64, 2, 64]) for _ in range(2)]
                WcTs = [K.sb(es, [64, 128]) for _ in range(2)]
                us = [K.sb(es, [64, 2, 64]) for _ in range(2)]
                QKs = [K.sb(es, [64, 2, 64]) for _ in range(2)]
                NQBs = [K.sb(es, [64, 2, 64]) for _ in range(2)]
                zns = [K.sb(es, [64, 64]) for _ in range(2)]
                Ms = [K.sb(es, [64, 64]) for _ in range(2)]
                mts = [K.sb(es, [64, 64]) for _ in range(2)]
                for d in range(2):
                    lastidx = 63 if d == 0 else 0
                    r0 = 768 + d * 32
                    load_mu(S, W, l, mu, r0, 32)
                    S.dma(kh[0:32, :], D['pT'][base + r0: base + r0 + 32, :])
                    shift_T(S, bh, kh, mu, 32)
                    S.act(bh[0:32, :], bh[0:32, :], AF.Tanh)
                    for n0 in range(0, T, 512):
                        nw = min(512, T - n0)
                        ps = K.bank()
                        S.mm(ps[0:64, :nw], wup[:, d, :], bh[0:32, n0:n0 + nw])
                        S.act(Lb[:, n0:n0 + nw], ps[0:64, :nw], AF.Sigmoid, bias=pk[:, 3 + d:4 + d])
                    S.ts(Lb[:], Lb[:], -math.exp(-0.5), None, ALU.mult)
                    for n in range(NCH):
                        ck = slice(n * 64, (n + 1) * 64)
                        if d == 0:
                            S.scan(rh[:, ck], ones[:, :], Lb[:, ck], 0.0, ALU.mult, ALU.add)
                        else:
                            S.scan(rh[:, ck][:, ::-1], ones[:, :], Lb[:, ck][:, ::-1], 0.0, ALU.mult, ALU.add)
                    S.act(GLc[:], rh[:, :].rearrange("p (n i) -> p n i", i=64)[:, :, lastidx], AF.Exp)
                    S.tt(ch[:], rh[:], Lb[:], ALU.subtract, e='pool')
                    S.act(ch[:], ch[:], AF.Exp)
                    S.tt(ch[:], ch[:], kkT[:], ALU.mult, e='pool')
                    r0 = 832 + d * 32
                    load_mu(S, W, l, mu, r0, 32)
                    S.dma(Lb[0:32, :], D['pT'][base + r0: base + r0 + 32, :])
                    shift_T(S, bh, Lb, mu, 32)
                    for n0 in range(0, T, 512):
                        nw = min(512, T - n0)
                        ps = K.bank()
                        S.mm(ps[0:64, :nw], aup[:, d, :], bh[0:32, n0:n0 + nw])
                        S.act(kh[:, n0:n0 + nw], ps[0:64, :nw], AF.Sigmoid, bias=pk[:, 5 + d:6 + d])
                    S.act(bh[:], rh[:], AF.Exp, scale=-1.0)
                    S.tt(bh[:], bh[:], kkT[:], ALU.mult)
                    S.tt(bh[:], bh[:], kh[:], ALU.mult, e='pool')
                    S.ts(kh[:], kh[:], -1.0, pk[:, 1:2], ALU.add, ALU.mult)
                    S.stt(kh[:], kh[:], 1.0, kT[:], ALU.add, ALU.mult)
                    S.stt(Lb[:], kh[:], pk[:, 2:3], rT[:], ALU.mult, ALU.mult)
                    ps = K.bank()
                    for n in range(NCH):
                        S.mm(ps[0:64, n:n + 1], Lb[:, n * 64:(n + 1) * 64], ones[:, 0:1])
                    if d == 0:
                        S.copy(bon[:], ps[0:64, 0:NCH])
                    else:
                        S.tt(bon[:], bon[:], ps[0:64, 0:NCH], ALU.add)
                    S.act(Lb[:], rh[:], AF.Exp, scale=-1.0)
                    S.tt(kh[:], kh[:], Lb[:], ALU.mult, e='pool')
                    S.act(rh[:], rh[:], AF.Exp)
                    S.tt(rh[:], rh[:], rT[:], ALU.mult)
                    S.memset(Ms[0][:], 0.0)
                    si = 0
                    order = scan_order(d)
                    pairs = [order[i] // 2 for i in range(0, NCH, 2)]
                    for pidx, pi in enumerate(pairs):
                        if pidx >= DBG.get('b_pairs', 99):
                            break
                        b = pidx % 2
                        Bc, BTc, P, PT = Bm[b], BTm[b], Pm[b], PTm[b]
                        tk = slice(pi * 128, (pi + 1) * 128)
                        ps = K.bank()
                        S.mm(ps[:, 0:128], bh[:, tk], ch[:, tk])
                        S.stt(Bc[:], ps[:, 0:128], -1.0, m128[:, 2 + d, :], ALU.mult, ALU.mult)
                        ps = K.bank()
                        S.mm(ps[:, 0:128], ch[:, tk], bh[:, tk])
                        S.stt(BTc[:], ps[:, 0:128], -1.0, m128[:, 3 - d, :], ALU.mult, ALU.mult)
                        S.tt(P[:], Bc[:], K.ident[:], ALU.add, e='pool')
                        S.tt(PT[:], BTc[:], K.ident[:], ALU.add, e='pool')
                        neumann_inverse(K, S, P, PT, Bc, BTc, (B2s, B2Ts))
                        ps = K.bank()
                        S.mm(ps[:, 0:128], kh[:, tk], ch[:, tk])
                        S.tt(AkTs[b][:], ps[:, 0:128], m128[:, 2 + d, :], ALU.mult)
                        ps = K.bank()
                        S.mm(ps[:, 0:64], AkTs[b][:], Vpair[:, pi, :])
                        S.copy(AVs[b][:], ps[:, 0:64], e='act')
                        ps = K.bank()
                        S.tr(ps[:, 0:64], ch[:, tk], K.ident[0:64, 0:64])
                        S.copy(Cps[b][:], ps[:, 0:64], e='act')
                        ps = K.bank()
                        for c in range(2):
                            ck = slice(pi * 128 + c * 64, pi * 128 + (c + 1) * 64)
                            S.tr(ps[0:64, c * 64:(c + 1) * 64], kh[:, ck], K.ident[0:64, 0:64])
                            S.tr(ps[0:64, 128 + c * 64:128 + (c + 1) * 64], bh[:, ck], K.ident[0:64, 0:64])
                        S.copy(Kts[b][:], ps[0:64, 0:128].rearrange("p (c k) -> p c k", c=2), e='act')
                        S.ts(NBts[b][:], ps[0:64, 128:256].rearrange("p (c k) -> p c k", c=2), -1.0, None, ALU.mult)
                        ps = K.bank()
                        S.mm(ps[0:64, 0:128], Cps[b][:], P[:])
                        S.copy(WcTs[b][:], ps[0:64, 0:128], e='act')
                        ps = K.bank()
                        for c in range(2):
                            S.mm(ps[0:64, c * 64:(c + 1) * 64], P[:, c * 64:(c + 1) * 64], AVs[b][:])
                        S.copy(us[b][:], ps[0:64, 0:128].rearrange("p (c v) -> p c v", c=2))
                        ps = K.bank()
                        for c in range(2):
                            ck = slice(pi * 128 + c * 64, pi * 128 + (c + 1) * 64)
                            S.mm(ps[0:64, c * 64:(c + 1) * 64], kh[:, ck], rh[:, ck])
                            S.mm(ps[0:64, 128 + c * 64:128 + (c + 1) * 64], bh[:, ck], rh[:, ck])
                        for c in range(2):
                            S.tt(QKs[b][:, c, :], ps[0:64, c * 64:(c + 1) * 64], masks[:, d, :], ALU.mult)
                            S.stt(NQBs[b][:, c, :], ps[0:64, 128 + c * 64:128 + (c + 1) * 64], -1.0, masks[:, d, :], ALU.mult, ALU.mult)
                        for c in ((0, 1) if d == 0 else (1, 0)):
                            n = pi * 2 + c
                            ck = slice(n * 64, (n + 1) * 64)
                            Mc, Mn = Ms[si % 2], Ms[(si + 1) % 2]
                            zn, mt = zns[si % 2], mts[si % 2]
                            si += 1
                            ps1 = K.bank()
                            S.mm(ps1[0:64, 0:64], WcTs[b][:, c * 64:(c + 1) * 64], Mc[:])
                            S.tt(zn[:], us[b][:, c, :], ps1[0:64, 0:64], ALU.add)
                            ps2 = K.bank()
                            S.mm(ps2[0:64, 0:64], rh[:, ck], Mc[:], start=True, stop=False)
                            S.mm(ps2[0:64, 0:64], QKs[b][:, c, :], Vtok[:, n, :], start=False, stop=False)
                            S.mm(ps2[0:64, 0:64], NQBs[b][:, c, :], zn[:], start=False, stop=True)
                            if d == 0:
                                S.copy(hacc[:, n, :], ps2[0:64, 0:64], e='act')
                            else:
                                S.tt(hacc[:, n, :], hacc[:, n, :], ps2[0:64, 0:64], ALU.add)
                            ps3 = K.bank()
                            S.mm(ps3[0:64, 0:64], Kts[b][:, c, :], Vtok[:, n, :], start=True, stop=False)
                            S.mm(ps3[0:64, 0:64], NBts[b][:, c, :], zn[:], start=False, stop=True)
                            S.ts(mt[:], ps3[0:64, 0:64], GLc[:, n:n + 1], None, ALU.mult)
                            S.stt(Mn[:], Mc[:], GLc[:, n:n + 1], mt[:], ALU.mult, ALU.add)
                gtok = kh[:, :].rearrange("p (n c) -> p n c", c=64)
                load_mu(S, W, l, mu, 896, 64)
                S.dma(rh[:], D['pT'][base + 896: base + 960, :])
                shift_T(S, bh, rh, mu, 64)
                S.act(bh[:], bh[:], AF.Sigmoid)
                for n0 in range(0, NCH, 8):
                    nn = min(8, NCH - n0)
                    ps = K.bank()
                    for j in range(nn):
                        S.mm(ps[0:64, j * 64:(j + 1) * 64], bh[:, (n0 + j) * 64:(n0 + j + 1) * 64], gup[:])
                    S.copy(gtok[:, n0:n0 + nn, :], ps[0:64, 0:nn * 64].rearrange("p (j c) -> p j c", c=64), e='act')
                st = K.sb(es, [64, NCH, 4])
                sq = ch[:, :].rearrange("p (n c) -> p n c", c=64)
                bc3 = lambda a: a.unsqueeze(2).to_broadcast([64, NCH, 64])
                S.reduce(st[:, :, 0], hacc[:], ALU.add)
                S.ts(st[:, :, 0], st[:, :, 0], 1.0 / 64, None, ALU.mult)
                S.tt(hacc[:], hacc[:], bc3(st[:, :, 0]), ALU.subtract)
                S.tt(sq, hacc[:], hacc[:], ALU.mult, e='pool')
                S.reduce(st[:, :, 1], sq, ALU.add)
                S.ts(st[:, :, 1], st[:, :, 1], 1.0 / 64, 64e-5, ALU.mult, ALU.add)
                S.act(st[:, :, 2], st[:, :, 1], AF.Sqrt)
                S.recip(st[:, :, 3], st[:, :, 2])
                S.tt(hacc[:], hacc[:], bc3(st[:, :, 3]), ALU.mult)
                S.tt(hacc[:], hacc[:], lng[:, 0, :].unsqueeze(1).to_broadcast([64, NCH, 64]), ALU.mult, e='pool')
                S.tt(hacc[:], hacc[:], lng[:, 1, :].unsqueeze(1).to_broadcast([64, NCH, 64]), ALU.add, e='pool')
                S.tt(sq, Vtok[:], bc3(bon[:, :]), ALU.mult)
                S.tt(hacc[:], hacc[:], sq, ALU.add, e='pool')
                S.tt(hacc[:], hacc[:], gtok, ALU.mult)
                c0 = 256 + h * 64
                yv = D['y'][:, c0:c0 + 64].rearrange("(n i) c -> i n c", i=64)
                for g4 in range(4):
                    S.dma(yv[:, g4 * 17:(g4 + 1) * 17, :], hacc[:, g4 * 17:(g4 + 1) * 17, :], q=('sp', 'pool')[g4 % 2])
            S.barrier()
    S.barrier()


W_SHAPES = {
    'xin': [T, 1024], 'cc': [128, 8, 2],
    'mod_w': [4, 1024, 6144], 'mod_b': [4, 6144], 'norm_mix_g': [4, 1024], 'norm_ffn_g': [4, 1024],
    'w_in': [4, 1024, IN_COLS], 'w_out': [4, 1024, 1024],
    'lru_conv_w': [4, 4, 256], 'lru_conv_b': [4, 256], 'lru_w_a': [4, 2, 4, 64, 64], 'lru_b_a': [4, 2, 256],
    'lru_w_x': [4, 2, 4, 64, 64], 'lru_b_x': [4, 2, 256], 'lru_lambda': [4, 2, 256],
    'rwkv_mu': [4, 2, 960], 'rwkv_w_up': [4, 2, 32, 256], 'rwkv_w0': [4, 2, 256], 'rwkv_a_up': [4, 2, 32, 256],
    'rwkv_a0': [4, 2, 256], 'rwkv_g_up': [4, 64, 256], 'rwkv_k_k': [4, 256], 'rwkv_k_a': [4, 256],
    'rwkv_r_k': [4, 256], 'rwkv_ln_g': [4, 256], 'rwkv_ln_b': [4, 256],
    'mlstm_i_b': [4, 2, 4], 'mlstm_f_b': [4, 2, 4], 'mlstm_norm_g': [4, 256],
    'gdn_conv_w': [4, 4, 768], 'gdn_a_log': [4, 2, 4], 'gdn_dt_bias': [4, 2, 4], 'gdn_norm_g': [4, 256],
    'ffn_w_gate': [2, 1024, D_FF], 'ffn_w_up': [2, 1024, D_FF], 'ffn_w_down': [2, D_FF, 1024],
    'moe_router': [2, 1024, 8], 'moe_w_gate': [2, 8, 1024, D_FFE], 'moe_w_up': [2, 8, 1024, D_FFE],
    'moe_w_down': [2, 8, D_FFE, 1024], 'final_norm_g': [1, 1024],
    'c_ident': [128, 128], 'c_sel8': [8, 8, 128],
    'pk_lru': [4, 2, 128, 11], 'pk_ml': [4, 4, 4],
    'c_masks': [64, 2, 64], 'c_rst': [4, 2, T], 'c_m128': [128, 4, 128],
    'pk_gd': [4, 4, 4], 'pk_gdc': [4, 4, 64, 3, 4], 'pk_mu': [4, 960, 2], 'pk_rw': [4, 4, 64, 7],
}


def make_consts():
    c = {}
    c['c_ident'] = np.eye(128, dtype=np.float32)
    s = np.zeros((8, 8, 128), np.float32)
    for e in range(8):
        s[e, e, :] = 1.0
    c['c_sel8'] = s
    jj, ii = np.meshgrid(np.arange(64), np.arange(64), indexing='ij')
    c['c_masks'] = np.ascontiguousarray(np.stack([(ii >= jj), (ii <= jj)], axis=1).astype(np.float32))
    ja, ia = np.meshgrid(np.arange(128), np.arange(128), indexing='ij')
    same = (ja // 64) == (ia // 64)
    c['c_m128'] = np.ascontiguousarray(np.stack([same & (ia >= ja), same & (ia <= ja), same & (ia > ja), same & (ia < ja)], axis=1).astype(np.float32))
    idx = np.arange(T) % 64
    r = np.stack([(idx != 0), (idx != 63)], axis=0).astype(np.float32)
    c['c_rst'] = np.ascontiguousarray(np.broadcast_to(r[None], (4, 2, T)))
    return c


def build(layers=(0, 1, 2, 3), mixers=None, final=True, dbg=()):
    nc = bass.Bass("TRN2", target_bir_lowering=False)
    W = {n: nc.dram_tensor(n, sh, F32, kind="ExternalInput").ap() for n, sh in W_SHAPES.items()}
    out = nc.dram_tensor('out', [SEQ, 1024], F32, kind="ExternalOutput").ap()
    D = {}
    for n, sh in {'xres': [T, 1024], 'pT': [IN_COLS, T], 'y': [T, 1024], 'mod': [2, 6144]}.items():
        kind = "ExternalOutput" if n in dbg else "Internal"
        D[n] = nc.dram_tensor('d_' + n, sh, F32, kind=kind).ap()
    with ExitStack() as es:
        S = Sched(nc, es)
        ps = [es.enter_context(nc.psum_tensor("psb%d" % i, [128, 512], F32)) for i in range(8)]
        ident = es.enter_context(nc.sbuf_tensor("ident", [128, 128], F32))
        K = Ctx(nc, S, W, D, ps, ident)
        K.out = out
        S.dma(ident[:], W['c_ident'])
        for t in range(NT):
            S.dma(D['xres'][t * 128:(t + 1) * 128, :], W['xin'][t * 128:(t + 1) * 128, :], q=('sp', 'pool', 'act')[t % 3])
        S.barrier()
        for l in layers:
            last = (l == 3)
            stage_mod(K, l)
            stage_inproj(K, l)
            if mixers is None:
                mix_identity(K, l)
            else:
                for m in mixers:
                    m(K, l, last)
            stage_outproj(K, l, last)
            stage_ffn(K, l, last)
        if final:
            stage_final(K)
        S.finish()
    K.S = S
    return nc, S


def make_packs(inputs):
    f = lambda n: np.asarray(inputs[n], dtype=np.float32)
    pk = {}
    cols = [f('lru_conv_w')[:, j, :] for j in range(4)] + [f('lru_conv_b')]
    cols += [f('lru_b_a')[:, 0], f('lru_b_a')[:, 1], f('lru_b_x')[:, 0], f('lru_b_x')[:, 1], f('lru_lambda')[:, 0], f('lru_lambda')[:, 1]]
    a = np.stack(cols, axis=-1)
    pk['pk_lru'] = np.ascontiguousarray(a.reshape(4, 2, 128, 11))
    pk['pk_gd'] = np.ascontiguousarray(np.concatenate([f('gdn_a_log'), f('gdn_dt_bias')], axis=1).transpose(0, 2, 1))
    pk['pk_gdc'] = np.ascontiguousarray(f('gdn_conv_w').reshape(4, 4, 3, 4, 64).transpose(0, 3, 4, 2, 1))
    pk['pk_mu'] = np.ascontiguousarray(f('rwkv_mu').transpose(0, 2, 1))
    cols = [f('rwkv_k_k'), f('rwkv_k_a'), f('rwkv_r_k'), f('rwkv_w0')[:, 0], f('rwkv_w0')[:, 1], f('rwkv_a0')[:, 0], f('rwkv_a0')[:, 1]]
    pk['pk_rw'] = np.ascontiguousarray(np.stack(cols, axis=-1).reshape(4, 4, 64, 7))
    pk['pk_ml'] = np.ascontiguousarray(np.concatenate([f('mlstm_i_b'), f('mlstm_f_b')], axis=1).transpose(0, 2, 1))
    return pk


def host_inputs(inputs, b):
    m = {}
    m['xin'] = np.ascontiguousarray(np.concatenate([inputs['ctx'][b], inputs['x'][b]], axis=0))
    cc = np.stack([np.asarray(inputs['c'][b]).reshape(8, 128).T, np.asarray(inputs['c_ctx']).reshape(8, 128).T], axis=-1)
    m['cc'] = np.ascontiguousarray(cc.astype(np.float32))
    for n in W_SHAPES:
        if n in m or n.startswith('c_') or n.startswith('pk_'):
            continue
        m[n] = np.ascontiguousarray(np.asarray(inputs[n], dtype=np.float32).reshape(W_SHAPES[n]))
    m.update(make_consts())
    m.update(make_packs(inputs))
    return m


def build_test(L, mixers):
    nc = bass.Bass("TRN2", target_bir_lowering=False)
    W = {n: nc.dram_tensor(n, sh, F32, kind="ExternalInput").ap() for n, sh in W_SHAPES.items()}
    D = {}
    for n, sh in {'xres': [T, 1024], 'pT': [IN_COLS, T], 'y': [T, 1024], 'mod': [2, 6144]}.items():
        kind = "ExternalOutput" if n in ('pT', 'y') else "Internal"
        D[n] = nc.dram_tensor('d_' + n, sh, F32, kind=kind).ap()
    add_scratch(nc, D)
    with ExitStack() as es:
        S = Sched(nc, es)
        ps = [es.enter_context(nc.psum_tensor("psb%d" % i, [128, 512], F32)) for i in range(8)]
        ident = es.enter_context(nc.sbuf_tensor("ident", [128, 128], F32))
        K = Ctx(nc, S, W, D, ps, ident)
        S.dma(ident[:], W['c_ident'])
        for t in range(NT):
            S.dma(D['xres'][t * 128:(t + 1) * 128, :], W['xin'][t * 128:(t + 1) * 128, :], q=('sp', 'pool', 'act')[t % 3])
        S.barrier()
        stage_mod(K, L)
        stage_inproj(K, L)
        for m in mixers:
            m(K, L, False)
        S.finish()
    return nc, S


def add_scratch(nc, D):
    pass


def kernel(**inputs):
    nc, S = build(layers=(0, 1, 2, 3), mixers=[mixer_a, mixer_b, mixer_c, mixer_d], final=True)
    in_maps = [host_inputs(inputs, b) for b in range(8)]
    res = run_bass_kernel_spmd(nc, in_maps, core_ids=list(range(8)))
    return np.stack([np.asarray(r['out'], dtype=np.float32) for r in res.results], axis=0)
```

```python
import math
import numpy as np
import concourse.bass as bass
import concourse.mybir as mybir
from concourse.bass_utils import run_bass_kernel_spmd
from contextlib import ExitStack

F32 = mybir.dt.float32
F32R = mybir.dt.float32r
FAST_MM = True
AF = mybir.ActivationFunctionType
ALU = mybir.AluOpType
AX = mybir.AxisListType

D_MODEL = 1024
SEQ = 4096
CTX = 256
T = SEQ + CTX
NT = T // 128
G = 256
IN_COLS = 3552
OFF_B = 512
OFF_C = OFF_B + 960
OFF_D = OFF_C + 1040
D_FF = 2816
D_FFE = 1408
EPS = 1e-6

ENGS = ['pe', 'dve', 'act', 'pool', 'sp']
SAME_SYNC = {'pe': False, 'dve': True, 'act': True, 'pool': True, 'sp': True}

def _box(ap):
    t = ap.tensor
    name = t.name
    dims = ap.ap
    off = int(ap.offset)
    if str(ap.space) in ('SB', 'PSUM', 'SBUF'):
        row = dims[0][0] if dims[0][0] > 0 else 1
        p0 = off // row
        f0 = off % row
        p1 = p0 + dims[0][1]
        lo = hi = f0
        for st, cnt in dims[1:]:
            if st >= 0:
                hi += st * (cnt - 1)
            else:
                lo += st * (cnt - 1)
        return (name, p0, p1, lo, hi + 1)
    lo = hi = off
    for st, cnt in dims:
        if st >= 0:
            hi += st * (cnt - 1)
        else:
            lo += st * (cnt - 1)
    return (name, 0, 1, lo, hi + 1)


class Sched:
    def __init__(self, nc, es, n_dma=8):
        self.nc = nc
        self.es = es
        self.eng = dict(pe=nc.tensor, dve=nc.vector, act=nc.scalar, pool=nc.gpsimd, sp=nc.sync)
        self.sem = {}
        self.cnt = {}
        self.unit = {}
        for e in ENGS:
            self.sem[e] = es.enter_context(nc.semaphore('s_' + e))
            self.cnt[e] = 0
            self.unit[e] = 1
        self.n_dma = n_dma
        self.dma_rr = {}
        for q in ('sp', 'act', 'pool'):
            self.dma_rr[q] = 0
            for i in range(n_dma):
                c = ('dma', q, i)
                self.sem[c] = es.enter_context(nc.semaphore('d_%s%d' % (q, i)))
                self.cnt[c] = 0
                self.unit[c] = 16
        self.seen = {e: {} for e in ENGS}
        self.recs = {}
        self.nins = 0

    def _need(self, reads, writes):
        need = {}
        for aps, isw in ((reads, False), (writes, True)):
            for ap in aps:
                name, p0, p1, f0, f1 = _box(ap)
                if not isw and name.startswith('psb'):
                    isw, p0, p1, f0, f1 = True, 0, 128, 0, 1 << 30
                for r in self.recs.get(name, ()):
                    if r[0] < p1 and p0 < r[1] and r[2] < f1 and f0 < r[3]:
                        if isw or r[6]:
                            c, v = r[4], r[5]
                            if need.get(c, 0) < v:
                                need[c] = v
        return need

    def _record(self, reads, writes, clock, val):
        for aps, isw in ((reads, False), (writes, True)):
            for ap in aps:
                name, p0, p1, f0, f1 = _box(ap)
                if not isw and name.startswith('psb'):
                    isw, p0, p1, f0, f1 = True, 0, 128, 0, 1 << 30
                lst = self.recs.setdefault(name, [])
                if isw:
                    lst[:] = [r for r in lst if not (p0 <= r[0] and r[1] <= p1 and f0 <= r[2] and r[3] <= f1)]
                else:
                    lst[:] = [r for r in lst if not (r[4] == clock and not r[6] and p0 <= r[0] and r[1] <= p1 and f0 <= r[2] and r[3] <= f1)]
                lst.append((p0, p1, f0, f1, clock, val, isw))
                if len(lst) > 48:
                    self._prune(lst)

    def _prune(self, lst):
        def stale(r):
            c, v = r[4], r[5]
            for e in ENGS:
                if e == c and not SAME_SYNC[e]:
                    continue
                if self.seen[e].get(c, 0) < v:
                    return False
            return True
        lst[:] = [r for r in lst if not stale(r)]

    def _waits(self, e, need):
        eo = self.eng[e]
        for c, v in need.items():
            if c == e and not SAME_SYNC[e]:
                continue
            if self.seen[e].get(c, 0) >= v:
                continue
            eo.wait_ge(self.sem[c], v * self.unit[c])
            self.seen[e][c] = v

    def op(self, e, fn, reads, writes):
        need = self._need(reads, writes)
        self._waits(e, need)
        ins = fn(self.eng[e])
        self.cnt[e] += 1
        ins.then_inc(self.sem[e], 1)
        self._record(reads, writes, e, self.cnt[e])
        self.nins += 1
        return ins

    def dma(self, out, in_, q='sp', **kw):
        need = self._need([in_], [out])
        k = self.dma_rr[q]
        self.dma_rr[q] = (k + 1) % self.n_dma
        c = ('dma', q, k)
        if self.cnt[c] > 0:
            need[c] = max(need.get(c, 0), self.cnt[c])
        self._waits(q, need)
        ins = self.eng[q].dma_start(out=out, in_=in_, **kw)
        self.cnt[c] += 1
        ins.then_inc(self.sem[c], 16)
        self._record([in_], [out], c, self.cnt[c])
        self.nins += 1

    def barrier(self):
        for e in ENGS:
            need = {c: v for c, v in self.cnt.items() if v > 0 and c != e}
            self._waits(e, need)
        self.recs = {}

    def finish(self):
        need = {c: v for c, v in self.cnt.items() if v > 0 and c != 'sp'}
        self._waits('sp', need)

    def mm(self, out, lhsT, rhs, start=True, stop=True, fast=False):
        if fast and FAST_MM:
            lhsT, rhs = lhsT.bitcast(F32R), rhs.bitcast(F32R)
        self.op('pe', lambda e: e.matmul(out, lhsT, rhs, start=start, stop=stop), [lhsT, rhs] + ([] if start else [out]), [out])

    def tr(self, out, in_, ident):
        self.op('pe', lambda e: e.transpose(out, in_, ident), [in_, ident], [out])

    def act(self, out, in_, func, bias=None, scale=None, accum_out=None):
        kw = {}
        rd = [in_]
        wr = [out]
        if bias is not None:
            kw['bias'] = bias
            if not isinstance(bias, (int, float)):
                rd.append(bias)
        if scale is not None:
            kw['scale'] = scale
            if not isinstance(scale, (int, float)):
                rd.append(scale)
        if accum_out is not None:
            kw['accum_out'] = accum_out
            wr.append(accum_out)
        self.op('act', lambda e: e.activation(out, in_, func, **kw), rd, wr)

    def tt(self, out, in0, in1, op, e='dve'):
        self.op(e, lambda en: en.tensor_tensor(out, in0, in1, op), [in0, in1], [out])

    def ts(self, out, in0, s1, s2, op0, op1=None, e='dve', accum_out=None):
        rd = [in0]
        for s in (s1, s2):
            if s is not None and not isinstance(s, (int, float)):
                rd.append(s)
        wr = [out] + ([accum_out] if accum_out is not None else [])
        kw = {}
        if op1 is not None:
            kw['op1'] = op1
        if accum_out is not None:
            kw['accum_out'] = accum_out
        self.op(e, lambda en: en.tensor_scalar(out, in0, s1, s2, op0, **kw), rd, wr)

    def stt(self, out, in0, scalar, in1, op0, op1, e='dve'):
        rd = [in0, in1]
        if not isinstance(scalar, (int, float)):
            rd.append(scalar)
        self.op(e, lambda en: en.scalar_tensor_tensor(out, in0, scalar, in1, op0, op1), rd, [out])

    def copy(self, out, in_, e='dve'):
        if e == 'act':
            self.op(e, lambda en: en.copy(out, in_), [in_], [out])
        else:
            self.op(e, lambda en: en.tensor_copy(out, in_), [in_], [out])

    def memset(self, ap, val, e='dve'):
        self.op(e, lambda en: en.memset(ap, val), [], [ap])

    def reduce(self, out, in_, op, axis=None, e='dve'):
        axis = axis or AX.X
        self.op(e, lambda en: en.tensor_reduce(out, in_, axis, op), [in_], [out])

    def scan(self, out, d0, d1, init, op0, op1):
        rd = [d0, d1]
        if not isinstance(init, (int, float)):
            rd.append(init)
        self.op('dve', lambda en: en.tensor_tensor_scan(out, d0, d1, init, op0, op1), rd, [out])

    def recip(self, out, in_):
        self.op('dve', lambda en: en.reciprocal(out, in_), [in_], [out])


class Ctx:
    def __init__(self, nc, S, W, D, ps, ident):
        self.nc, self.S, self.W, self.D, self.ps, self.ident = nc, S, W, D, ps, ident
        self.uid = 0
        self.psi = 0

    def sb(self, es, shape, dt=F32, name=None):
        self.uid += 1
        return es.enter_context(self.nc.sbuf_tensor("%s_%d" % (name or "t", self.uid), list(shape), dt))

    def bank(self):
        b = self.ps[self.psi % 8]
        self.psi += 1
        return b


def r32(ap):
    return ap.bitcast(F32R) if FAST_MM else ap


def bc_rows(ap, n):
    return ap.to_broadcast([n, ap.shape[1]])


def stage_mod(K, l):
    S, W, D = K.S, K.W, K.D
    with ExitStack() as es:
        cc = K.sb(es, [128, 8, 2])
        S.dma(cc[:], W['cc'])
        S.act(cc[:], cc[:], AF.Silu)
        mb = K.sb(es, [2, 6144])
        S.dma(mb[:], bc_rows(W['mod_b'][l:l + 1, :], 2))
        mo = K.sb(es, [2, 6144])
        wts = [K.sb(es, [128, 8, 512]) for _ in range(2)]
        wv = W['mod_w'][l].rearrange("(k p) c -> p k c", p=128)
        for n in range(12):
            wt = wts[n % 2]
            S.dma(wt[:], wv[:, :, n * 512:(n + 1) * 512], q=('sp' if n % 2 == 0 else 'pool'))
            ps = K.bank()
            for k in range(8):
                S.mm(ps[0:2, :], cc[:, k, :], wt[:, k, :], start=(k == 0), stop=(k == 7))
            S.tt(mo[:, n * 512:(n + 1) * 512], ps[0:2, :], mb[:, n * 512:(n + 1) * 512], ALU.add)
        S.dma(D['mod'], mo[:])
    S.barrier()


def load_mod_tiles(K, es, l, which, gname):
    S, W, D = K.S, K.W, K.D
    base = 3072 * which
    outs = []
    gt = K.sb(es, [128, 1024])
    S.dma(gt[:], bc_rows(W[gname][l:l + 1, :], 128))
    for seg in range(2):
        sh = K.sb(es, [128, 1024])
        sc = K.sb(es, [128, 1024])
        S.dma(sh[:], bc_rows(D['mod'][seg:seg + 1, base:base + 1024], 128), q='pool')
        S.dma(sc[:], bc_rows(D['mod'][seg:seg + 1, base + 1024:base + 2048], 128), q='pool')
        S.stt(sc[:], sc[:], 1.0, gt[:], ALU.add, ALU.mult)
        outs += [sc, sh]
    return outs


def norm_mod_T(K, es_tmp, xt, Gt, SHt, hT_dst, tmp):
    S = K.S
    junk, ss, h = tmp
    S.act(junk[:], xt[:], AF.Square, accum_out=ss[:, 0:1])
    S.ts(ss[:, 1:2], ss[:, 0:1], 1.0 / D_MODEL, EPS, ALU.mult, ALU.add)
    S.act(ss[:, 2:3], ss[:, 1:2], AF.Sqrt)
    S.recip(ss[:, 3:4], ss[:, 2:3])
    S.stt(h[:], xt[:], ss[:, 3:4], Gt[:], ALU.mult, ALU.mult)
    S.tt(h[:], h[:], SHt[:], ALU.add, e='pool')
    for b in range(2):
        ps = K.bank()
        for j in range(4):
            k = b * 4 + j
            S.tr(ps[:, j * 128:(j + 1) * 128], h[:, k * 128:(k + 1) * 128], K.ident[:])
        src = ps[:].rearrange("p (j t) -> p j t", j=4)
        if b == 0:
            S.copy(r32(hT_dst[:, 0:4, :]), src, e='dve')
        else:
            S.copy(r32(hT_dst[:, 4:8, :]), src, e='act')


def stage_inproj(K, l):
    S, W, D = K.S, K.W, K.D
    HALF = T // 2
    wv = W['w_in'][l].rearrange("(k p) c -> p k c", p=128)
    with ExitStack() as es:
        GL, SHL, GC, SHC = load_mod_tiles(K, es, l, 0, 'norm_mix_g')
        hT = K.sb(es, [128, 8, HALF])
        xts = [K.sb(es, [128, 1024]) for _ in range(2)]
        tmp = (K.sb(es, [128, 1024]), K.sb(es, [128, 4]), K.sb(es, [128, 1024]))
        wts = [K.sb(es, [128, 8, 128]) for _ in range(3)]
        ots = [K.sb(es, [128, HALF]) for _ in range(2)]
        for half in range(2):
            for ti in range(17):
                t = half * 17 + ti
                xt = xts[ti % 2]
                S.dma(xt[:], D['xres'][t * 128:(t + 1) * 128, :])
                isctx = t < 2
                norm_mod_T(K, es, xt, GC if isctx else GL, SHC if isctx else SHL,
                           hT[:, :, ti * 128:(ti + 1) * 128], tmp)
            for cchunk in range(28):
                c0 = cchunk * 128
                cw = min(128, IN_COLS - c0)
                wt = wts[cchunk % 3]
                S.dma(r32(wt[:, :, :cw]), wv[:, :, c0:c0 + cw], q='pool')
                ot = ots[cchunk % 2]
                for si, (n0, nw) in enumerate([(0, 512), (512, 512), (1024, 512), (1536, 512), (2048, 128)]):
                    ps = K.bank()
                    for k in range(8):
                        S.mm(ps[:cw, :nw], wt[:, k, :cw], hT[:, k, n0:n0 + nw], start=(k == 0), stop=(k == 7), fast=True)
                    S.copy(ot[:cw, n0:n0 + nw], ps[:cw, :nw], e=('dve' if si % 2 == 0 else 'act'))
                S.dma(D['pT'][c0:c0 + cw, half * HALF:(half + 1) * HALF], ot[:cw, :], q='act')
    S.barrier()


def stage_outproj(K, l, last):
    S, W, D = K.S, K.W, K.D
    wv = W['w_out'][l].rearrange("(k p) c -> p k c", p=128)
    with ExitStack() as es:
        wo = K.sb(es, [128, 8, 1024])
        S.dma(r32(wo[:, 0:4, :]), wv[:, 0:4, :], q='pool')
        S.dma(r32(wo[:, 4:8, :]), wv[:, 4:8, :], q='pool')
        gts = []
        for seg in range(2):
            g = K.sb(es, [128, 1024])
            S.dma(g[:], bc_rows(D['mod'][seg:seg + 1, 2048:3072], 128))
            gts.append(g)
        yts = [K.sb(es, [128, 1024]) for _ in range(2)]
        xts = [K.sb(es, [128, 1024]) for _ in range(2)]
        yTs = [K.sb(es, [128, 8, 128]) for _ in range(2)]
        for t in range(2 if last else 0, NT):
            yt, xt, yT = yts[t % 2], xts[t % 2], yTs[t % 2]
            S.dma(yt[:], D['y'][t * 128:(t + 1) * 128, :])
            S.dma(xt[:], D['xres'][t * 128:(t + 1) * 128, :], q='pool')
            for b in range(2):
                ps = K.bank()
                for j in range(4):
                    k = b * 4 + j
                    S.tr(ps[:, j * 128:(j + 1) * 128], yt[:, k * 128:(k + 1) * 128], K.ident[:])
                S.copy(r32(yT[:, b * 4:(b + 1) * 4, :]), ps[:].rearrange("p (j t) -> p j t", j=4), e=('dve' if b == 0 else 'act'))
            gt = gts[0] if t >= 2 else gts[1]
            for n in range(2):
                ps = K.bank()
                for k in range(8):
                    S.mm(ps[:, :], yT[:, k, :], wo[:, k, n * 512:(n + 1) * 512], start=(k == 0), stop=(k == 7), fast=True)
                S.tt(yt[:, n * 512:(n + 1) * 512], ps[:, :], gt[:, n * 512:(n + 1) * 512], ALU.mult)
                S.tt(xt[:, n * 512:(n + 1) * 512], xt[:, n * 512:(n + 1) * 512], yt[:, n * 512:(n + 1) * 512], ALU.add, e='pool')
            S.dma(D['xres'][t * 128:(t + 1) * 128, :], xt[:], q='act')
    S.barrier()


def stage_ffn(K, l, last):
    S, W, D = K.S, K.W, K.D
    dense = (l % 2 == 0)
    li = l // 2
    if dense:
        E, NF = 1, D_FF // 128
        wg_v = [W['ffn_w_gate'][li].rearrange("(k p) c -> p k c", p=128)]
        wu_v = [W['ffn_w_up'][li].rearrange("(k p) c -> p k c", p=128)]
        wd_v = [W['ffn_w_down'][li].rearrange("(f p) c -> p f c", p=128)]
    else:
        E, NF = 8, D_FFE // 128
        wg_v = [W['moe_w_gate'][li, e].rearrange("(k p) c -> p k c", p=128) for e in range(8)]
        wu_v = [W['moe_w_up'][li, e].rearrange("(k p) c -> p k c", p=128) for e in range(8)]
        wd_v = [W['moe_w_down'][li, e].rearrange("(f p) c -> p f c", p=128) for e in range(8)]
    blocks = ([] if last else [(0, 2)]) + [(2 + 4 * i, 4) for i in range(8)]
    with ExitStack() as es:
        gn = K.sb(es, [128, 1024])
        S.dma(gn[:], bc_rows(W['norm_ffn_g'][l:l + 1, :], 128))
        G2, SH2, GT2 = K.sb(es, [128, 1024]), K.sb(es, [128, 1024]), K.sb(es, [128, 1024])
        xblk = K.sb(es, [128, 4, 1024])
        h2T = K.sb(es, [128, 8, 512])
        actT = K.sb(es, [128, NF, 512])
        yT = K.sb(es, [128, 8, 512])
        tmp = (K.sb(es, [128, 1024]), K.sb(es, [128, 4]), K.sb(es, [128, 1024]))
        sgs = [K.sb(es, [128, 512]) for _ in range(2)]
        wgs = [K.sb(es, [128, 8, 128]) for _ in range(2)]
        wus = [K.sb(es, [128, 8, 128]) for _ in range(2)]
        wds = [K.sb(es, [128, NF, 128]) for _ in range(2)]
        if not dense:
            Gbc = K.sb(es, [128, 8, 512])
            rt = K.sb(es, [128, 8, 8])
            S.dma(rt[:], W['moe_router'][li].rearrange("(k p) e -> p k e", p=128))
            gateT = K.sb(es, [8, 512])
            sel = K.sb(es, [8, 8, 128])
            S.dma(sel[:], W['c_sel8'])
            gsm = K.sb(es, [128, 64])
        cur_seg = None
        wi = 0
        for (t0, nt) in blocks:
            NB = nt * 128
            seg = 1 if t0 < 2 else 0
            if seg != cur_seg:
                cur_seg = seg
                S.dma(SH2[:], bc_rows(D['mod'][seg:seg + 1, 3072:4096], 128), q='pool')
                S.dma(G2[:], bc_rows(D['mod'][seg:seg + 1, 4096:5120], 128), q='pool')
                S.dma(GT2[:], bc_rows(D['mod'][seg:seg + 1, 5120:6144], 128), q='pool')
                S.stt(G2[:], G2[:], 1.0, gn[:], ALU.add, ALU.mult)
            for ti in range(nt):
                t = t0 + ti
                S.dma(xblk[:, ti, :], D['xres'][t * 128:(t + 1) * 128, :])
                norm_mod_T(K, es, xblk[:, ti, :], G2, SH2, h2T[:, :, ti * 128:(ti + 1) * 128], tmp)
            if not dense:
                for ti in range(nt):
                    ps = K.bank()
                    for k in range(8):
                        S.mm(ps[:, 0:8], h2T[:, k, ti * 128:(ti + 1) * 128], rt[:, k, :], start=(k == 0), stop=(k == 7))
                    lg, eq, l2, ex = gsm[:, 0:8], gsm[:, 8:16], gsm[:, 16:24], gsm[:, 24:32]
                    m1, m2, nm1, sm, rs = gsm[:, 32:33], gsm[:, 33:34], gsm[:, 34:35], gsm[:, 35:36], gsm[:, 36:37]
                    gate = gsm[:, 40:48]
                    S.copy(lg, ps[:, 0:8])
                    S.reduce(m1, lg, ALU.max)
                    S.ts(eq, lg, m1, None, ALU.is_equal)
                    S.stt(l2, eq, -1e30, lg, ALU.mult, ALU.add)
                    S.reduce(m2, l2, ALU.max)
                    S.ts(eq, lg, m2, None, ALU.is_ge)
                    S.ts(nm1, m1, -1.0, None, ALU.mult)
                    S.act(ex, lg, AF.Exp, bias=nm1)
                    S.tt(ex, ex, eq, ALU.mult)
                    S.reduce(sm, ex, ALU.add)
                    S.recip(rs, sm)
                    S.ts(gate, ex, rs, None, ALU.mult)
                    ps2 = K.bank()
                    S.tr(ps2[0:8, 0:128], gate, K.ident[:])
                    S.copy(gateT[:, ti * 128:(ti + 1) * 128], ps2[0:8, 0:128])
                for e in range(8):
                    ps = K.bank()
                    S.mm(ps[:, :NB], sel[:, e, :], gateT[:, :NB])
                    S.copy(Gbc[:, e, :NB], ps[:, :NB], e='act')
            for e in range(E):
                for f in range(NF):
                    wg, wu = wgs[wi % 2], wus[wi % 2]
                    sg = sgs[wi % 2]
                    wi += 1
                    S.dma(r32(wg[:]), wg_v[e][:, :, f * 128:(f + 1) * 128], q='pool')
                    S.dma(r32(wu[:]), wu_v[e][:, :, f * 128:(f + 1) * 128], q='pool')
                    psg, psu = K.bank(), K.bank()
                    for k in range(8):
                        S.mm(psg[:, :NB], wg[:, k, :], h2T[:, k, :NB], start=(k == 0), stop=(k == 7), fast=True)
                    for k in range(8):
                        S.mm(psu[:, :NB], wu[:, k, :], h2T[:, k, :NB], start=(k == 0), stop=(k == 7), fast=True)
                    S.act(sg[:, :NB], psg[:, :NB], AF.Silu)
                    S.tt(r32(actT[:, f, :NB]), sg[:, :NB], psu[:, :NB], ALU.mult)
                    if not dense:
                        S.tt(r32(actT[:, f, :NB]), actT[:, f, :NB], Gbc[:, e, :NB], ALU.mult, e='pool')
                for cchunk in range(8):
                    wd = wds[cchunk % 2]
                    S.dma(r32(wd[:]), wd_v[e][:, :, cchunk * 128:(cchunk + 1) * 128], q='pool')
                    ps = K.bank()
                    for f in range(NF):
                        S.mm(ps[:, :NB], wd[:, f, :], actT[:, f, :NB], start=(f == 0), stop=(f == NF - 1), fast=True)
                    if e == 0:
                        S.copy(yT[:, cchunk, :NB], ps[:, :NB], e='act')
                    else:
                        S.tt(yT[:, cchunk, :NB], yT[:, cchunk, :NB], ps[:, :NB], ALU.add)
            for ti in range(nt):
                t = t0 + ti
                yt = tmp[0]
                for b in range(2):
                    ps = K.bank()
                    for j in range(4):
                        S.tr(ps[:, j * 128:(j + 1) * 128], yT[:, b * 4 + j, ti * 128:(ti + 1) * 128], K.ident[:])
                    S.tt(yt[:, b * 512:(b + 1) * 512], ps[:, :], GT2[:, b * 512:(b + 1) * 512], ALU.mult)
                S.tt(xblk[:, ti, :], xblk[:, ti, :], yt[:], ALU.add, e='pool')
                S.dma(D['xres'][t * 128:(t + 1) * 128, :], xblk[:, ti, :], q='act')
    S.barrier()


def stage_final(K):
    S, W, D = K.S, K.W, K.D
    with ExitStack() as es:
        g = K.sb(es, [128, 1024])
        S.dma(g[:], bc_rows(W['final_norm_g'], 128))
        xts = [K.sb(es, [128, 1024]) for _ in range(2)]
        ots = [K.sb(es, [128, 1024]) for _ in range(2)]
        junk = K.sb(es, [128, 1024])
        sss = [K.sb(es, [128, 4]) for _ in range(2)]
        for t in range(2, NT):
            xt, ot, ss = xts[t % 2], ots[t % 2], sss[t % 2]
            S.dma(xt[:], D['xres'][t * 128:(t + 1) * 128, :])
            S.act(junk[:], xt[:], AF.Square, accum_out=ss[:, 0:1])
            S.ts(ss[:, 1:2], ss[:, 0:1], 1.0 / D_MODEL, EPS, ALU.mult, ALU.add)
            S.act(ss[:, 2:3], ss[:, 1:2], AF.Sqrt)
            S.recip(ss[:, 3:4], ss[:, 2:3])
            S.stt(ot[:], xt[:], ss[:, 3:4], g[:], ALU.mult, ALU.mult)
            S.dma(K.out[(t - 2) * 128:(t - 1) * 128, :], ot[:], q='pool')
    S.barrier()


def mix_identity(K, l):
    S, D = K.S, K.D
    with ExitStack() as es:
        pts = [K.sb(es, [128, 8, 128]) for _ in range(2)]
        yts = [K.sb(es, [128, 1024]) for _ in range(2)]
        pv = D['pT'][0:1024, :].rearrange("(k p) t -> p k t", p=128)
        for t in range(NT):
            pt, yt = pts[t % 2], yts[t % 2]
            S.dma(pt[:], pv[:, :, t * 128:(t + 1) * 128])
            for b in range(2):
                ps = K.bank()
                for j in range(4):
                    S.tr(ps[:, j * 128:(j + 1) * 128], pt[:, b * 4 + j, :], K.ident[:])
                S.copy(yt[:, b * 512:(b + 1) * 512], ps[:, :], e=('dve' if b == 0 else 'act'))
            S.dma(D['y'][t * 128:(t + 1) * 128, :], yt[:], q='pool')
    S.barrier()


def to_token_major(K, es, src, ncols_tile, dst_cols, bufs):
    S, D = K.S, K.D
    gi = 0
    for t0 in range(0, NT, 4):
        n = min(4, NT - t0)
        ps = K.bank()
        for j in range(n):
            S.tr(ps[:, j * 128:(j + 1) * 128], src[:, (t0 + j) * 128:(t0 + j + 1) * 128], K.ident[:])
        yb = bufs[gi % 2]
        S.copy(yb[:, :n * 128], ps[:, :n * 128], e=('dve' if gi % 2 == 0 else 'act'))
        dst = D['y'][t0 * 128:(t0 + n) * 128, dst_cols[0]:dst_cols[1]].rearrange("(j p) c -> p j c", p=128)
        S.dma(dst, yb[:, :n * 128].rearrange("p (j c) -> p j c", j=n), q=('sp' if gi % 2 == 0 else 'pool'))
        gi += 1


def mixer_a(K, l, last):
    S, W, D = K.S, K.W, K.D
    SEGS = [(0, CTX), (CTX, T)]
    with ExitStack() as es0:
        ybufs = [K.sb(es0, [128, 512]) for _ in range(2)]
        for ct in range(2):
            with ExitStack() as es:
                pk = K.sb(es, [128, 16])
                S.dma(pk[:, 0:11], W['pk_lru'][l, ct])
                S.act(pk[:, 11:13], pk[:, 9:11], AF.Exp, scale=-1.0)
                S.act(pk[:, 11:13], pk[:, 11:13], AF.Ln, bias=1.0)
                S.ts(pk[:, 13:15], pk[:, 11:13], -16.0, None, ALU.mult)
                S.ts(pk[:, 11:13], pk[:, 11:13], -8.0, None, ALU.mult)
                wbd = K.sb(es, [128, 4, 128])
                S.memset(wbd[:], 0.0)
                for d in range(2):
                    for wi, wn in enumerate(('lru_w_a', 'lru_w_x')):
                        for hl in range(2):
                            S.dma(wbd[hl * 64:(hl + 1) * 64, d * 2 + wi, hl * 64:(hl + 1) * 64], W[wn][l, d, ct * 2 + hl], q='pool')
                xb = K.sb(es, [128, T])
                u = K.sb(es, [128, T])
                gt = K.sb(es, [128, T])
                ra = K.sb(es, [128, T])
                ib = K.sb(es, [128, T])
                h0 = K.sb(es, [128, T])
                h1 = K.sb(es, [128, T])
                S.dma(xb[:], D['pT'][ct * 128:(ct + 1) * 128, :])
                S.dma(gt[:], D['pT'][256 + ct * 128:256 + (ct + 1) * 128, :], q='pool')
                S.ts(u[:], xb[:], pk[:, 2:3], pk[:, 4:5], ALU.mult, ALU.add)
                for (a, b) in SEGS:
                    for j, s in ((0, -2), (1, -1), (3, 1)):
                        lo, hi = max(a, a - s), min(b, b - s)
                        S.stt(u[:, lo:hi], xb[:, lo + s:hi + s], pk[:, j:j + 1], u[:, lo:hi], ALU.mult, ALU.add)
                S.tt(xb[:], gt[:], gt[:], ALU.mult, e='pool')
                S.ts(xb[:], xb[:], 0.044715, 1.0, ALU.mult, ALU.add, e='pool')
                S.tt(xb[:], xb[:], gt[:], ALU.mult, e='pool')
                S.act(xb[:], xb[:], AF.Sigmoid, scale=1.5957691216057308)
                S.tt(gt[:], gt[:], xb[:], ALU.mult, e='pool')
                for d in range(2):
                    blocks = [(n0, min(512, T - n0)) for n0 in range(0, T, 512)]
                    for wi, dst, bcol in ((0, ra, 5 + d), (1, ib, 7 + d)):
                        for (n0, nw) in blocks:
                            ps = K.bank()
                            S.mm(ps[:, :nw], wbd[:, d * 2 + wi, :], u[:, n0:n0 + nw])
                            S.act(dst[:, n0:n0 + nw], ps[:, :nw], AF.Sigmoid, bias=pk[:, bcol:bcol + 1])
                    hd = h0 if d == 0 else h1
                    S.tt(ib[:], ib[:], u[:], ALU.mult, e='pool')
                    S.act(hd[:], ra[:], AF.Exp, scale=pk[:, 13 + d:14 + d])
                    S.ts(hd[:], hd[:], -1.0, 1.0, ALU.mult, ALU.add)
                    S.act(hd[:], hd[:], AF.Sqrt)
                    S.tt(ib[:], ib[:], hd[:], ALU.mult)
                    S.act(ra[:], ra[:], AF.Exp, scale=pk[:, 11 + d:12 + d])
                    if d == 0:
                        S.scan(h0[:], ra[:], ib[:], 0.0, ALU.mult, ALU.add)
                    else:
                        S.scan(h1[:, 0:CTX][:, ::-1], ra[:, 0:CTX][:, ::-1], ib[:, 0:CTX][:, ::-1], 0.0, ALU.mult, ALU.add)
                        S.scan(h1[:, CTX:T][:, ::-1], ra[:, CTX:T][:, ::-1], ib[:, CTX:T][:, ::-1], h1[:, 0:1], ALU.mult, ALU.add)
                S.tt(h0[:], h0[:], h1[:], ALU.add, e='pool')
                S.tt(h0[:], h0[:], gt[:], ALU.mult)
                to_token_major(K, es, h0, 128, (ct * 128, (ct + 1) * 128), ybufs)
            S.barrier()
    S.barrier()


def chunk_cols(n):
    if n < 4:
        return slice(n * 64, (n + 1) * 64)
    c = n - 4
    return slice(CTX + c, T, 64)


NCH = T // 64
DBG = {}


def to_traversal(S, dst, src, e='dve'):
    S.copy(dst[:, 0:CTX], src[:, 0:CTX], e=e)
    S.copy(dst[:, CTX:T].rearrange("p (c r) -> p c r", r=64), src[:, CTX:T].rearrange("p (r c) -> p c r", c=64), e=e)


def scan_order(d):
    if d == 0:
        return list(range(NCH))
    return [3, 2, 1, 0] + list(range(NCH - 1, 3, -1))


def mixer_c(K, l, last):
    S, W, D = K.S, K.W, K.D
    base = OFF_C
    NQ = 6
    with ExitStack() as es0:
        tokcol = K.sb(es0, [64, NCH, NQ * 8])
        masks = K.sb(es0, [64, 2, 64])
        S.dma(masks[:], W['c_masks'])
        ngt = K.sb(es0, [64, 256])
        S.dma(ngt[:], bc_rows(W['mlstm_norm_g'][l:l + 1, :], 64))
        with ExitStack() as es:
            stack = K.sb(es, [NQ * 8, T])
            pk = K.sb(es, [4, 8])
            S.dma(pk[:, 0:4], W['pk_ml'][l])
            S.ts(pk[:, 4:8], pk[:, 0:4], -1.0, None, ALU.mult)
            rst = K.sb(es, [4, T])
            nbg = K.sb(es, [4, T])
            graw = K.sb(es, [4, T])
            li = K.sb(es, [4, T])
            lf = K.sb(es, [4, T])
            bb = K.sb(es, [4, T])
            gg = K.sb(es, [4, T])
            cm = K.sb(es, [4, T])
            mx = K.sb(es, [4, T])
            tmp = graw
            sm = K.sb(es, [4, 8, NCH])
            v3 = lambda t: t[:, :].rearrange("p (n i) -> p n i", i=64)
            bcn = lambda a: a.unsqueeze(2).to_broadcast([4, NCH, 64])
            for d in range(2):
                rv = (lambda a: a) if d == 0 else (lambda a: a[:, ::-1])
                lastidx = 63 if d == 0 else 0
                S.dma(rst[:], W['c_rst'][:, d, :], q='pool')
                S.ts(nbg[:], rst[:], 1e30, -1e30, ALU.mult, ALU.add)
                S.dma(graw[:], D['pT'][base + 1024 + d * 4: base + 1024 + d * 4 + 4, :])
                to_traversal(S, li, graw)
                S.ts(li[:], li[:], pk[:, d:d + 1], None, ALU.add)
                S.dma(graw[:], D['pT'][base + 1024 + 8 + d * 4: base + 1024 + 8 + d * 4 + 4, :])
                to_traversal(S, lf, graw)
                S.act(lf[:], lf[:], AF.Exp, scale=-1.0, bias=pk[:, 6 + d:7 + d])
                S.act(lf[:], lf[:], AF.Ln, bias=1.0)
                S.ts(lf[:], lf[:], -1.0, None, ALU.mult)
                S.scan(rv(bb[:, :]), rv(rst[:, :]), rv(lf[:, :]), 0.0, ALU.mult, ALU.add)
                S.tt(gg[:], li[:], bb[:], ALU.subtract)
                S.scan(rv(cm[:, :]), rv(nbg[:, :]), rv(gg[:, :]), 0.0, ALU.add, ALU.max)
                bL, cmL = v3(bb)[:, :, lastidx], v3(cm)[:, :, lastidx]
                d1t, mm_, mprev, e4, t5 = sm[:, 0, :], sm[:, 1, :], sm[:, 2, :], sm[:, 3, :], sm[:, 4, :]
                S.tt(d1t, bL, cmL, ALU.add)
                if d == 0:
                    S.scan(mm_, bL, d1t, 0.0, ALU.add, ALU.max)
                    S.memset(mprev[:, 0:1], 0.0)
                    S.copy(mprev[:, 1:NCH], mm_[:, 0:NCH - 1])
                else:
                    S.scan(mm_[:, 0:4][:, ::-1], bL[:, 0:4][:, ::-1], d1t[:, 0:4][:, ::-1], 0.0, ALU.add, ALU.max)
                    S.scan(mm_[:, 4:NCH][:, ::-1], bL[:, 4:NCH][:, ::-1], d1t[:, 4:NCH][:, ::-1], mm_[:, 0:1], ALU.add, ALU.max)
                    S.copy(mprev[:, 0:3], mm_[:, 1:4])
                    S.memset(mprev[:, 3:4], 0.0)
                    S.copy(mprev[:, 4:NCH - 1], mm_[:, 5:NCH])
                    S.copy(mprev[:, NCH - 1:NCH], mm_[:, 0:1])
                S.tt(v3(mx), v3(cm), bcn(mprev), ALU.max)
                def put(q, src):
                    r0 = q * 8 + d * 4
                    S.dma(stack[r0:r0 + 4, :], src, q='pool')
                S.act(tmp[:], mx[:], AF.Exp, scale=-1.0)
                put(0, tmp[:])
                S.tt(v3(tmp), bcn(mprev), v3(mx), ALU.subtract)
                S.act(tmp[:], tmp[:], AF.Exp)
                put(1, tmp[:])
                S.tt(tmp[:], bb[:], mx[:], ALU.add)
                S.act(tmp[:], tmp[:], AF.Exp, scale=-1.0)
                put(2, tmp[:])
                S.act(tmp[:], gg[:], AF.Exp)
                put(3, tmp[:])
                S.tt(e4, bL, mprev, ALU.add)
                S.tt(e4, e4, mm_, ALU.subtract)
                S.act(e4, e4, AF.Exp)
                S.copy(v3(tmp), bcn(e4))
                put(4, tmp[:])
                S.tt(t5, bL, mm_, ALU.subtract)
                S.tt(v3(tmp), v3(gg), bcn(t5), ALU.add)
                S.act(tmp[:], tmp[:], AF.Exp)
                put(5, tmp[:])
            NR = NQ * 8
            for n0 in range(0, NCH, 8):
                nn = min(8, NCH - n0)
                ps = K.bank()
                for j in range(nn):
                    S.tr(ps[0:64, j * NR:(j + 1) * NR], stack[0:NR, (n0 + j) * 64:(n0 + j + 1) * 64], K.ident[0:NR, 0:NR])
                S.copy(tokcol[:, n0:n0 + nn, :], ps[0:64, 0:nn * NR].rearrange("p (j c) -> p j c", c=NR))
        S.barrier()
        for h in range(4):
            with ExitStack() as es:
                qT = K.sb(es, [64, T])
                kT = K.sb(es, [64, T])
                vT = K.sb(es, [64, T])
                ktok = K.sb(es, [64, NCH, 64])
                vtok = K.sb(es, [64, NCH, 65])
                hacc = K.sb(es, [64, NCH, 64])
                osig = K.sb(es, [64, NCH, 64])
                S.dma(qT[:], D['pT'][base + h * 64: base + (h + 1) * 64, :])
                S.dma(kT[:], D['pT'][base + 256 + h * 64: base + 256 + (h + 1) * 64, :], q='pool')
                S.dma(vT[:], D['pT'][base + 512 + h * 64: base + 512 + (h + 1) * 64, :], q='act')
                S.ts(kT[:], kT[:], 0.125, None, ALU.mult, e='pool')
                S.memset(vtok[:, :, 64:65], 1.0)
                for n0 in range(0, NCH, 8):
                    nn = min(8, NCH - n0)
                    ps1, ps2 = K.bank(), K.bank()
                    for j in range(nn):
                        cs = chunk_cols(n0 + j)
                        S.tr(ps1[0:64, j * 64:(j + 1) * 64], kT[:, cs], K.ident[0:64, 0:64])
                        S.tr(ps2[0:64, j * 64:(j + 1) * 64], vT[:, cs], K.ident[0:64, 0:64])
                    S.copy(ktok[:, n0:n0 + nn, :], ps1[0:64, 0:nn * 64].rearrange("p (j c) -> p j c", c=64), e='act')
                    S.copy(vtok[:, n0:n0 + nn, 0:64], ps2[0:64, 0:nn * 64].rearrange("p (j c) -> p j c", c=64))
                S.dma(vT[:], D['pT'][base + 768 + h * 64: base + 768 + (h + 1) * 64, :], q='act')
                for d in range(2):
                    col = lambda q: (lambda n: tokcol[:, n, q * 8 + d * 4 + h: q * 8 + d * 4 + h + 1])
                    c1, c2, c3, eg, c4, wn = [col(q) for q in range(6)]
                    Cs = [K.sb(es, [64, 65]) for _ in range(2)]
                    S.memset(Cs[0][:], 0.0)
                    pts = [K.sb(es, [64, 64]) for _ in range(2)]
                    tts = [K.sb(es, [64, 65]) for _ in range(2)]
                    vws = [K.sb(es, [64, 65]) for _ in range(2)]
                    dns = [K.sb(es, [64, 2]) for _ in range(2)]
                    for si, n in enumerate(scan_order(d)):
                        cs = chunk_cols(n)
                        Cc, Cn = Cs[si % 2], Cs[(si + 1) % 2]
                        pt, tot, vw, dn = pts[si % 2], tts[si % 2], vws[si % 2], dns[si % 2]
                        ps_s, ps_o, ps_i, ps_c = K.bank(), K.bank(), K.bank(), K.bank()
                        S.mm(ps_s[0:64, 0:64], kT[:, cs], qT[:, cs])
                        S.stt(pt[:], ps_s[0:64, 0:64], eg(n), masks[:, d, :], ALU.mult, ALU.mult)
                        S.mm(ps_o[0:64, 0:65], pt[:], vtok[:, n, :])
                        S.mm(ps_i[0:64, 0:65], qT[:, cs], Cc[:])
                        S.ts(tot[:], ps_o[0:64, 0:65], c1(n), None, ALU.mult)
                        S.stt(tot[:], ps_i[0:64, 0:65], c2(n), tot[:], ALU.mult, ALU.add)
                        S.act(dn[:, 0:1], tot[:, 64:65], AF.Abs)
                        S.ts(dn[:, 0:1], dn[:, 0:1], c3(n), None, ALU.max)
                        S.recip(dn[:, 1:2], dn[:, 0:1])
                        if d == 0:
                            S.ts(hacc[:, n, :], tot[:, 0:64], dn[:, 1:2], None, ALU.mult)
                        else:
                            S.stt(hacc[:, n, :], tot[:, 0:64], dn[:, 1:2], hacc[:, n, :], ALU.mult, ALU.add)
                        S.ts(vw[:], vtok[:, n, :], wn(n), None, ALU.mult, e='pool')
                        S.mm(ps_c[0:64, 0:65], ktok[:, n, :], vw[:])
                        S.stt(Cn[:], Cc[:], c4(n), ps_c[0:64, 0:65], ALU.mult, ALU.add)
                for n0 in range(0, NCH, 8):
                    nn = min(8, NCH - n0)
                    ps1 = K.bank()
                    for j in range(nn):
                        S.tr(ps1[0:64, j * 64:(j + 1) * 64], vT[:, chunk_cols(n0 + j)], K.ident[0:64, 0:64])
                    S.act(osig[:, n0:n0 + nn, :], ps1[0:64, 0:nn * 64].rearrange("p (j c) -> p j c", c=64), AF.Sigmoid)
                sq = K.sb(es, [64, NCH, 64])
                ssq = K.sb(es, [64, NCH, 4])
                S.tt(sq[:], hacc[:], hacc[:], ALU.mult, e='pool')
                S.reduce(ssq[:, :, 0], sq[:], ALU.add)
                S.ts(ssq[:, :, 1], ssq[:, :, 0], 1.0 / 64, EPS, ALU.mult, ALU.add)
                S.act(ssq[:, :, 2], ssq[:, :, 1], AF.Sqrt)
                S.recip(ssq[:, :, 3], ssq[:, :, 2])
                S.tt(hacc[:], hacc[:], ssq[:, :, 3].unsqueeze(2).to_broadcast([64, NCH, 64]), ALU.mult)
                S.tt(hacc[:], hacc[:], ngt[:, h * 64:(h + 1) * 64].unsqueeze(1).to_broadcast([64, NCH, 64]), ALU.mult, e='pool')
                S.tt(hacc[:], hacc[:], osig[:], ALU.mult)
                c0 = 512 + h * 64
                S.dma(D['y'][0:CTX, c0:c0 + 64].rearrange("(n i) c -> i n c", i=64), hacc[:, 0:4, :])
                yv = D['y'][CTX:T, c0:c0 + 64].rearrange("(r c) ch -> r c ch", c=64)
                for g4 in range(4):
                    S.dma(yv[:, g4 * 16:(g4 + 1) * 16, :], hacc[:, 4 + g4 * 16:4 + (g4 + 1) * 16, :], q=('sp', 'pool')[g4 % 2])
            S.barrier()
    S.barrier()


def dwconv_trav(S, out, x, wcol, bias=None):
    if bias is None:
        S.ts(out[:], x[:], wcol(2), None, ALU.mult)
    else:
        S.ts(out[:], x[:], wcol(2), bias, ALU.mult, ALU.add)
    for (a, b) in ((0, CTX), (CTX, T)):
        for j, s in ((0, -2), (1, -1), (3, 1)):
            lo, hi = max(a, a - s), min(b, b - s)
            S.stt(out[:, lo:hi], x[:, lo + s:hi + s], wcol(j), out[:, lo:hi], ALU.mult, ALU.add)


def neumann_inverse(K, S, P, PT, B, BT, tmps):
    B2s, B2Ts = tmps
    cb, cbt = B, BT
    for m in range(1, 6):
        lastm = (m == 5)
        ps1 = K.bank()
        S.mm(ps1[:, 0:128], cbt[:], cb[:])
        nb = B2s[m % 2]
        S.copy(nb[:], ps1[:, 0:128], e='act')
        if not lastm:
            ps2 = K.bank()
            S.mm(ps2[:, 0:128], cb[:], cbt[:])
            nbt = B2Ts[m % 2]
            S.copy(nbt[:], ps2[:, 0:128], e='dve')
        ps3 = K.bank()
        S.mm(ps3[:, 0:128], PT[:], nb[:])
        if not lastm:
            ps4 = K.bank()
            S.mm(ps4[:, 0:128], nb[:], PT[:])
        S.tt(P[:], P[:], ps3[:, 0:128], ALU.add)
        if not lastm:
            S.tt(PT[:], PT[:], ps4[:, 0:128], ALU.add)
            cb, cbt = nb, nbt


def run_pipeline(n, prep_gen, rec_gen, sets):
    NS = len(sets)
    preps = {}
    done = set()
    next_prep = 0
    rec_i = 0
    rec = None
    while rec_i < n:
        while next_prep < n and next_prep < rec_i + NS:
            preps[next_prep] = prep_gen(next_prep, sets[next_prep % NS])
            next_prep += 1
        if rec is None and rec_i in done:
            rec = rec_gen(rec_i, sets[rec_i % NS])
        if rec is not None:
            try:
                next(rec)
            except StopIteration:
                rec = None
                rec_i += 1
                continue
        for j in sorted(preps):
            try:
                next(preps[j])
            except StopIteration:
                del preps[j]
                done.add(j)


NEU_FAST = False


def n32(ap):
    return ap.bitcast(F32R) if (NEU_FAST and FAST_MM) else ap


def neumann_gen(K, S, PP, BB, N2):
    cb, cbt = BB[:, 0, :], BB[:, 1, :]
    for m in range(1, 6):
        lastm = (m == 5)
        nbb = N2[m % 2]
        psa = K.bank()
        S.mm(psa[:, 0:128], cbt, cb, fast=NEU_FAST)
        if not lastm:
            S.mm(psa[:, 128:256], cb, cbt, fast=NEU_FAST)
        yield
        if lastm:
            S.copy(n32(nbb[:, 0, :]), psa[:, 0:128], e='act')
        else:
            S.copy(n32(nbb[:]), psa[:, 0:256].rearrange("p (a c) -> p a c", a=2), e='act')
        yield
        psb = K.bank()
        S.mm(psb[:, 0:128], PP[:, 1, :], nbb[:, 0, :], fast=NEU_FAST)
        if not lastm:
            S.mm(psb[:, 128:256], nbb[:, 0, :], PP[:, 1, :], fast=NEU_FAST)
        yield
        if lastm:
            S.tt(n32(PP[:, 0, :]), PP[:, 0, :], psb[:, 0:128], ALU.add)
        else:
            S.tt(n32(PP[:]), PP[:], psb[:, 0:256].rearrange("p (a c) -> p a c", a=2), ALU.add)
        yield
        cb, cbt = nbb[:, 0, :], nbb[:, 1, :]


def mixer_d(K, l, last):
    S, W, D = K.S, K.W, K.D
    base = OFF_D
    NQ = 6
    NR = NQ * 8
    NP = NCH // 2
    with ExitStack() as es0:
        tok64 = K.sb(es0, [64, NCH, NR])
        tokP = K.sb(es0, [128, NP, NR])
        stack = K.sb(es0, [NR, T])
        masks = K.sb(es0, [64, 2, 64])
        S.dma(masks[:], W['c_masks'])
        m128 = K.sb(es0, [128, 4, 128])
        S.dma(m128[:], W['c_m128'])
        sel = K.sb(es0, [8, 8, 128])
        S.dma(sel[:], W['c_sel8'])
        ngt = K.sb(es0, [64, 256])
        S.dma(ngt[:], bc_rows(W['gdn_norm_g'][l:l + 1, :], 64))
        with ExitStack() as es:
            pk = K.sb(es, [4, 8])
            S.dma(pk[:, 0:4], W['pk_gd'][l])
            S.act(pk[:, 4:6], pk[:, 0:2], AF.Exp)
            S.ts(pk[:, 4:6], pk[:, 4:6], -1.0, None, ALU.mult)
            rst = K.sb(es, [4, T])
            graw = K.sb(es, [4, T])
            la = K.sb(es, [4, T])
            bt = K.sb(es, [4, T])
            gam = K.sb(es, [4, T])
            tmp = K.sb(es, [4, T])
            sm = K.sb(es, [4, 4, NCH])
            v3 = lambda t: t[:, :].rearrange("p (n i) -> p n i", i=64)
            bcn = lambda a: a.unsqueeze(2).to_broadcast([4, NCH, 64])
            for d in range(2):
                rv = (lambda a: a) if d == 0 else (lambda a: a[:, ::-1])
                lastidx = 63 if d == 0 else 0
                S.dma(rst[:], W['c_rst'][:, d, :], q='pool')
                S.dma(graw[:], D['pT'][base + 1024 + d * 4: base + 1024 + d * 4 + 4, :])
                to_traversal(S, la, graw)
                S.act(la[:], la[:], AF.Exp, bias=pk[:, 2 + d:3 + d])
                S.act(la[:], la[:], AF.Ln, bias=1.0)
                S.ts(la[:], la[:], pk[:, 4 + d:5 + d], None, ALU.mult)
                S.dma(graw[:], D['pT'][base + 1024 + 8 + d * 4: base + 1024 + 8 + d * 4 + 4, :])
                to_traversal(S, bt, graw)
                S.act(bt[:], bt[:], AF.Sigmoid)
                S.scan(rv(gam[:, :]), rv(rst[:, :]), rv(la[:, :]), 0.0, ALU.mult, ALU.add)
                gL = v3(gam)[:, :, lastidx]
                def put(q, src):
                    r0 = q * 8 + d * 4
                    S.dma(stack[r0:r0 + 4, :], src, q='pool')
                put(0, gam[:])
                put(1, bt[:])
                S.act(tmp[:], gam[:], AF.Exp)
                S.tt(tmp[:], tmp[:], bt[:], ALU.mult)
                put(2, tmp[:])
                S.tt(v3(tmp), bcn(gL), v3(gam), ALU.subtract)
                S.act(tmp[:], tmp[:], AF.Exp)
                put(3, tmp[:])
                S.act(sm[:, 0, :], gL, AF.Exp)
                S.copy(v3(tmp), bcn(sm[:, 0, :]))
                put(4, tmp[:])
                S.ts(tmp[:], bt[:], -1.0, None, ALU.mult)
                put(5, tmp[:])
            for n0 in range(0, NCH, 8):
                nn = min(8, NCH - n0)
                ps = K.bank()
                for j in range(nn):
                    S.tr(ps[0:64, j * NR:(j + 1) * NR], stack[0:NR, (n0 + j) * 64:(n0 + j + 1) * 64], K.ident[0:NR, 0:NR])
                S.copy(tok64[:, n0:n0 + nn, :], ps[0:64, 0:nn * NR].rearrange("p (j c) -> p j c", c=NR))
            for n0 in range(0, NP, 8):
                nn = min(8, NP - n0)
                ps = K.bank()
                for j in range(nn):
                    S.tr(ps[:, j * NR:(j + 1) * NR], stack[0:NR, (n0 + j) * 128:(n0 + j + 1) * 128], K.ident[0:NR, 0:NR])
                S.copy(tokP[:, n0:n0 + nn, :], ps[:, 0:nn * NR].rearrange("p (j c) -> p j c", c=NR), e='act')
        S.barrier()
        if DBG.get('d_stop') == 1:
            return
        for h in range(DBG.get('d_heads', 4)):
            with ExitStack() as es:
                raw = K.sb(es, [64, T])
                trv = K.sb(es, [64, T])
                qT = K.sb(es, [64, T])
                kT = K.sb(es, [64, T])
                vT = K.sb(es, [64, T])
                ktok = K.sb(es, [64, NCH, 64])
                kP = K.sb(es, [128, NP, 64])
                vP = K.sb(es, [128, NP, 64])
                hacc = K.sb(es, [64, NCH, 64])
                cw = K.sb(es, [64, 3, 4])
                ones = K.sb(es, [64, 64])
                S.memset(ones[:], 1.0)
                S.dma(cw[:], W['pk_gdc'][l, h])
                for gi, dst in enumerate((qT, kT, vT)):
                    S.dma(raw[:], D['pT'][base + gi * 256 + h * 64: base + gi * 256 + (h + 1) * 64, :])
                    to_traversal(S, trv, raw, e='pool')
                    dwconv_trav(S, dst, trv, lambda j: cw[:, gi, j:j + 1])
                    S.act(dst[:], dst[:], AF.Silu)
                    if gi < 2:
                        S.tt(trv[:], dst[:], dst[:], ALU.mult, e='pool')
                        for n0 in range(0, T, 512):
                            nw = min(512, T - n0)
                            ps = K.bank()
                            S.mm(ps[0:64, :nw], ones[:], trv[:, n0:n0 + nw])
                            S.ts(raw[:, n0:n0 + nw], ps[0:64, :nw], EPS, None, ALU.add)
                        S.act(raw[:], raw[:], AF.Sqrt)
                        S.recip(raw[:], raw[:])
                        if gi == 0:
                            S.stt(dst[:], dst[:], 0.125, raw[:], ALU.mult, ALU.mult)
                        else:
                            S.tt(dst[:], dst[:], raw[:], ALU.mult)
                for n0 in range(0, NCH, 8):
                    nn = min(8, NCH - n0)
                    ps1 = K.bank()
                    for j in range(nn):
                        S.tr(ps1[0:64, j * 64:(j + 1) * 64], kT[:, (n0 + j) * 64:(n0 + j + 1) * 64], K.ident[0:64, 0:64])
                    S.copy(ktok[:, n0:n0 + nn, :], ps1[0:64, 0:nn * 64].rearrange("p (j c) -> p j c", c=64), e='act')
                for n0 in range(0, NP, 8):
                    nn = min(8, NP - n0)
                    ps1, ps2 = K.bank(), K.bank()
                    for j in range(nn):
                        S.tr(ps1[:, j * 64:(j + 1) * 64], kT[:, (n0 + j) * 128:(n0 + j + 1) * 128], K.ident[0:64, 0:64])
                        S.tr(ps2[:, j * 64:(j + 1) * 64], vT[:, (n0 + j) * 128:(n0 + j + 1) * 128], K.ident[0:64, 0:64])
                    S.copy(kP[:, n0:n0 + nn, :], ps1[:, 0:nn * 64].rearrange("p (j c) -> p j c", c=64), e='act')
                    S.copy(vP[:, n0:n0 + nn, :], ps2[:, 0:nn * 64].rearrange("p (j c) -> p j c", c=64))
                if DBG.get('d_stop') == 2:
                    S.barrier()
                    return
                S.dma(raw[:], D['pT'][base + 768 + h * 64: base + 768 + (h + 1) * 64, :])
                kdec = trv
                kdec3 = kdec[:, :].rearrange("p (n c) -> p n c", c=64)
                NS = DBG.get('d_ns', 3)
                sets = []
                for _s in range(NS):
                    sets.append(dict(
                        GB=K.sb(es, [128, 128]), dL=K.sb(es, [128, 128]), BB=K.sb(es, [128, 2, 128]),
                        PP=K.sb(es, [128, 2, 128]), N2=[K.sb(es, [128, 2, 128]) for _ in range(2)],
                        rU=K.sb(es, [128, 64]), rW=K.sb(es, [128, 64]), wT=K.sb(es, [64, 128]),
                        us=K.sb(es, [64, 2, 64]), qk=K.sb(es, [64, 2, 64]), qd=K.sb(es, [64, 128])))
                vns = [K.sb(es, [64, 64]) for _ in range(2)]
                Ss = [K.sb(es, [64, 64]) for _ in range(2)]
                identB = K.ident[:, :].unsqueeze(1).to_broadcast([128, 2, 128])
                for d in range(2):
                    cP = lambda q, pi: tokP[:, pi, q * 8 + d * 4 + h: q * 8 + d * 4 + h + 1]
                    c64 = lambda q, n: tok64[:, n, q * 8 + d * 4 + h: q * 8 + d * 4 + h + 1]
                    S.tt(kdec3, ktok[:], tok64[:, :, 3 * 8 + d * 4 + h].unsqueeze(2).to_broadcast([64, NCH, 64]), ALU.mult, e='pool')
                    S.memset(Ss[0][:], 0.0)
                    order = scan_order(d)
                    pairs = [order[i] // 2 for i in range(0, NCH, 2)]
                    state = {'si': 0}

                    def prep(pidx, st):
                        pi = pairs[pidx]
                        GB, dL, BB, PP = st['GB'], st['dL'], st['BB'], st['PP']
                        tk = slice(pi * 128, (pi + 1) * 128)
                        ps = K.bank()
                        S.mm(ps[:, 0:128], sel[:, d * 4 + h, :], stack[0:8, tk])
                        yield
                        S.copy(GB[:], ps[:, 0:128], e='act')
                        ps = K.bank()
                        S.mm(ps[:, 0:128], kT[:, tk], kT[:, tk])
                        yield
                        S.ts(dL[:], GB[:], cP(0, pi), 0.0, ALU.subtract, ALU.max)
                        yield
                        S.act(dL[:], dL[:], AF.Exp, scale=-1.0)
                        yield
                        S.tt(dL[:], dL[:], m128[:, 3 - d, :], ALU.mult, e='pool')
                        yield
                        S.stt(n32(BB[:, 1, :]), ps[:, 0:128], cP(5, pi), dL[:], ALU.mult, ALU.mult)
                        yield
                        ps = K.bank()
                        S.tr(ps[:, 0:128], BB[:, 1, :], K.ident[:])
                        yield
                        S.copy(n32(BB[:, 0, :]), ps[:, 0:128], e='act')
                        yield
                        S.tt(n32(PP[:]), BB[:], identB, ALU.add, e='pool')
                        yield
                        yield from neumann_gen(K, S, PP, BB, st['N2'])
                        P = PP[:, 0, :]
                        S.ts(st['rU'][:], vP[:, pi, :], cP(1, pi), None, ALU.mult, e='pool')
                        S.ts(st['rW'][:], kP[:, pi, :], cP(2, pi), None, ALU.mult, e='pool')
                        yield
                        ps = K.bank()
                        S.mm(ps[0:64, 0:128], st['rW'][:], P)
                        for c in range(2):
                            S.mm(ps[0:64, 128 + c * 64:128 + (c + 1) * 64], PP[:, 0, c * 64:(c + 1) * 64], st['rU'][:])
                        yield
                        S.copy(st['wT'][:], ps[0:64, 0:128], e='act')
                        S.copy(st['us'][:], ps[0:64, 128:256].rearrange("p (c v) -> p c v", c=2), e='act')
                        yield
                        S.act(st['qd'][:], GB[0:64, :], AF.Exp)
                        yield
                        S.tt(st['qd'][:], st['qd'][:], qT[:, tk], ALU.mult, e='pool')
                        ps = K.bank()
                        for c in range(2):
                            n = pi * 2 + c
                            ck = slice(n * 64, (n + 1) * 64)
                            S.mm(ps[0:64, c * 64:(c + 1) * 64], kT[:, ck], qT[:, ck])
                            S.ts(st['qk'][:, c, :], GB[0:64, c * 64:(c + 1) * 64], c64(0, n), 0.0, ALU.subtract, ALU.min)
                        yield
                        S.act(st['qk'][:], st['qk'][:], AF.Exp)
                        yield
                        S.tt(st['qk'][:], st['qk'][:], masks[:, d, :].unsqueeze(1).to_broadcast([64, 2, 64]), ALU.mult, e='pool')
                        yield
                        S.tt(st['qk'][:], st['qk'][:], ps[0:64, 0:128].rearrange("p (c i) -> p c i", c=2), ALU.mult)
                        yield

                    def rec(pidx, st):
                        pi = pairs[pidx]
                        for c in ((0, 1) if d == 0 else (1, 0)):
                            n = pi * 2 + c
                            si = state['si']
                            Sc, Sn = Ss[si % 2], Ss[(si + 1) % 2]
                            vn = vns[si % 2]
                            state['si'] = si + 1
                            ps1 = K.bank()
                            S.mm(ps1[0:64, 0:64], st['wT'][:, c * 64:(c + 1) * 64], Sc[:])
                            yield
                            S.tt(vn[:], st['us'][:, c, :], ps1[0:64, 0:64], ALU.subtract)
                            yield
                            ps2 = K.bank()
                            S.mm(ps2[0:64, 0:64], st['qd'][:, c * 64:(c + 1) * 64], Sc[:], start=True, stop=False)
                            S.mm(ps2[0:64, 0:64], st['qk'][:, c, :], vn[:], start=False, stop=True)
                            ps3 = K.bank()
                            S.mm(ps3[0:64, 0:64], kdec3[:, n, :], vn[:])
                            yield
                            S.stt(Sn[:], Sc[:], c64(4, n), ps3[0:64, 0:64], ALU.mult, ALU.add)
                            if d == 0:
                                S.copy(hacc[:, n, :], ps2[0:64, 0:64], e='act')
                            else:
                                S.tt(hacc[:, n, :], hacc[:, n, :], ps2[0:64, 0:64], ALU.add)
                            yield

                    run_pipeline(min(len(pairs), DBG.get('d_pairs', 99)), prep, rec, sets)
                osig = kT[:, :].rearrange("p (n c) -> p n c", c=64)
                for n0 in range(0, NCH, 8):
                    nn = min(8, NCH - n0)
                    ps1 = K.bank()
                    for j in range(nn):
                        S.tr(ps1[0:64, j * 64:(j + 1) * 64], raw[:, chunk_cols(n0 + j)], K.ident[0:64, 0:64])
                    S.act(osig[:, n0:n0 + nn, :], ps1[0:64, 0:nn * 64].rearrange("p (j c) -> p j c", c=64), AF.Silu)
                sq = qT[:, :].rearrange("p (n c) -> p n c", c=64)
                ssq = K.sb(es, [64, NCH, 4])
                S.tt(sq, hacc[:], hacc[:], ALU.mult, e='pool')
                S.reduce(ssq[:, :, 0], sq, ALU.add)
                S.ts(ssq[:, :, 1], ssq[:, :, 0], 1.0 / 64, EPS, ALU.mult, ALU.add)
                S.act(ssq[:, :, 2], ssq[:, :, 1], AF.Sqrt)
                S.recip(ssq[:, :, 3], ssq[:, :, 2])
                S.tt(hacc[:], hacc[:], ssq[:, :, 3].unsqueeze(2).to_broadcast([64, NCH, 64]), ALU.mult)
                S.tt(hacc[:], hacc[:], ngt[:, h * 64:(h + 1) * 64].unsqueeze(1).to_broadcast([64, NCH, 64]), ALU.mult, e='pool')
                S.tt(hacc[:], hacc[:], osig, ALU.mult)
                c0 = 768 + h * 64
                S.dma(D['y'][0:CTX, c0:c0 + 64].rearrange("(n i) c -> i n c", i=64), hacc[:, 0:4, :])
                yv = D['y'][CTX:T, c0:c0 + 64].rearrange("(r c) ch -> r c ch", c=64)
                for g4 in range(4):
                    S.dma(yv[:, g4 * 16:(g4 + 1) * 16, :], hacc[:, 4 + g4 * 16:4 + (g4 + 1) * 16, :], q=('sp', 'pool')[g4 % 2])
            S.barrier()
    S.barrier()


def shift_T(S, out, x, mu, np_):
    S.ts(out[0:np_, :], x[0:np_, :], mu[0:np_, 2:3], None, ALU.mult)
    for (a, b) in ((0, CTX), (CTX, T)):
        S.stt(out[0:np_, a + 1:b], x[0:np_, a:b - 1], mu[0:np_, 0:1], out[0:np_, a + 1:b], ALU.mult, ALU.add)
        S.stt(out[0:np_, a:b - 1], x[0:np_, a + 1:b], mu[0:np_, 1:2], out[0:np_, a:b - 1], ALU.mult, ALU.add)


def load_mu(S, W, l, mu, row0, np_):
    S.dma(mu[0:np_, 0:2], W['pk_mu'][l, row0:row0 + np_, :], q='pool')
    S.ts(mu[0:np_, 2:3], mu[0:np_, 0:1], -1.0, 1.0, ALU.mult, ALU.add)
    S.tt(mu[0:np_, 2:3], mu[0:np_, 2:3], mu[0:np_, 1:2], ALU.subtract)


def mixer_b(K, l, last):
    S, W, D = K.S, K.W, K.D
    base = OFF_B
    NP = NCH // 2
    with ExitStack() as es0:
        masks = K.sb(es0, [64, 2, 64])
        S.dma(masks[:], W['c_masks'])
        m128 = K.sb(es0, [128, 4, 128])
        S.dma(m128[:], W['c_m128'])
        ones = K.sb(es0, [64, 64])
        S.memset(ones[:], 1.0)
        for h in range(DBG.get('b_heads', 4)):
            with ExitStack() as es:
                rT, kT, kkT = K.sb(es, [64, T]), K.sb(es, [64, T]), K.sb(es, [64, T])
                Lb = K.sb(es, [64, T])
                bh, ch, kh, rh = K.sb(es, [64, T]), K.sb(es, [64, T]), K.sb(es, [64, T]), K.sb(es, [64, T])
                Vtok = K.sb(es, [64, NCH, 64])
                Vpair = K.sb(es, [128, NP, 64])
                hacc = K.sb(es, [64, NCH, 64])
                pk = K.sb(es, [64, 8])
                S.dma(pk[:, 0:7], W['pk_rw'][l, h])
                mu = K.sb(es, [64, 3])
                wup = K.sb(es, [32, 2, 64])
                aup = K.sb(es, [32, 2, 64])
                gup = K.sb(es, [64, 64])
                for d in range(2):
                    S.dma(wup[:, d, :], W['rwkv_w_up'][l, d][:, h * 64:(h + 1) * 64], q='pool')
                    S.dma(aup[:, d, :], W['rwkv_a_up'][l, d][:, h * 64:(h + 1) * 64], q='pool')
                S.dma(gup[:], W['rwkv_g_up'][l][:, h * 64:(h + 1) * 64], q='pool')
                lng = K.sb(es, [64, 2, 64])
                S.dma(lng[:, 0, :], bc_rows(W['rwkv_ln_g'][l:l + 1, h * 64:(h + 1) * 64], 64))
                S.dma(lng[:, 1, :], bc_rows(W['rwkv_ln_b'][l:l + 1, h * 64:(h + 1) * 64], 64))
                bon = K.sb(es, [64, NCH])
                GLc = K.sb(es, [64, NCH])
                for gi, dst in enumerate((rT, kT, Lb)):
                    r0 = gi * 256 + h * 64
                    load_mu(S, W, l, mu, r0, 64)
                    S.dma(ch[:], D['pT'][base + r0: base + r0 + 64, :])
                    shift_T(S, dst, ch, mu, 64)
                for n0 in range(0, NCH, 8):
                    nn = min(8, NCH - n0)
                    ps1 = K.bank()
                    for j in range(nn):
                        S.tr(ps1[0:64, j * 64:(j + 1) * 64], Lb[:, (n0 + j) * 64:(n0 + j + 1) * 64], K.ident[0:64, 0:64])
                    S.copy(Vtok[:, n0:n0 + nn, :], ps1[0:64, 0:nn * 64].rearrange("p (j c) -> p j c", c=64), e='act')
                for n0 in range(0, NP, 8):
                    nn = min(8, NP - n0)
                    ps2 = K.bank()
                    for j in range(nn):
                        S.tr(ps2[:, j * 64:(j + 1) * 64], Lb[:, (n0 + j) * 128:(n0 + j + 1) * 128], K.ident[0:64, 0:64])
                    S.copy(Vpair[:, n0:n0 + nn, :], ps2[:, 0:nn * 64].rearrange("p (j c) -> p j c", c=64))
                S.ts(kkT[:], kT[:], pk[:, 0:1], None, ALU.mult)
                S.tt(ch[:], kkT[:], kkT[:], ALU.mult, e='pool')
                for n0 in range(0, T, 512):
                    nw = min(512, T - n0)
                    ps = K.bank()
                    S.mm(ps[0:64, :nw], ones[:], ch[:, n0:n0 + nw])
                    S.ts(bh[:, n0:n0 + nw], ps[0:64, :nw], EPS, None, ALU.add)
                S.act(bh[:], bh[:], AF.Sqrt)
                S.recip(bh[:], bh[:])
                S.tt(kkT[:], kkT[:], bh[:], ALU.mult)
                NS = DBG.get('b_ns', 3)
                sets = []
                for _s in range(NS):
                    sets.append(dict(
                        BB=K.sb(es, [128, 2, 128]), PP=K.sb(es, [128, 2, 128]), N2=[K.sb(es, [128, 2, 128]) for _ in range(2)],
                        AV=K.sb(es, [128, 64]), Cp=K.sb(es, [128, 64]),
                        WcT=K.sb(es, [64, 128]), us=K.sb(es, [64, 2, 64])))
                    sets[-1]['AkT'] = sets[-1]['N2'][0][:, 0, :]
                    sets[-1]['QQ'] = sets[-1]['N2'][0][0:64, :, :].rearrange("p a (b c) -> p (a b) c", c=64)
                    sets[-1]['KN'] = sets[-1]['N2'][1][0:64, :, :].rearrange("p a (b c) -> p (a b) c", c=64)
                zns = [K.sb(es, [64, 64]) for _ in range(2)]
                Ms = [K.sb(es, [64, 64]) for _ in range(2)]
                mts = [K.sb(es, [64, 64]) for _ in range(2)]
                identB = K.ident[:, :].unsqueeze(1).to_broadcast([128, 2, 128])
                sgn = K.sb(es, [64, 4, 64])
                for d in range(2):
                    lastidx = 63 if d == 0 else 0
                    r0 = 768 + d * 32
                    load_mu(S, W, l, mu, r0, 32)
                    S.dma(kh[0:32, :], D['pT'][base + r0: base + r0 + 32, :])
                    shift_T(S, bh, kh, mu, 32)
                    S.act(bh[0:32, :], bh[0:32, :], AF.Tanh)
                    for n0 in range(0, T, 512):
                        nw = min(512, T - n0)
                        ps = K.bank()
                        S.mm(ps[0:64, :nw], wup[:, d, :], bh[0:32, n0:n0 + nw])
                        S.act(Lb[:, n0:n0 + nw], ps[0:64, :nw], AF.Sigmoid, bias=pk[:, 3 + d:4 + d])
                    S.ts(Lb[:], Lb[:], -math.exp(-0.5), None, ALU.mult)
                    for n in range(NCH):
                        ck = slice(n * 64, (n + 1) * 64)
                        if d == 0:
                            S.scan(rh[:, ck], ones[:, :], Lb[:, ck], 0.0, ALU.mult, ALU.add)
                        else:
                            S.scan(rh[:, ck][:, ::-1], ones[:, :], Lb[:, ck][:, ::-1], 0.0, ALU.mult, ALU.add)
                    S.act(GLc[:], rh[:, :].rearrange("p (n i) -> p n i", i=64)[:, :, lastidx], AF.Exp)
                    S.tt(ch[:], rh[:], Lb[:], ALU.subtract, e='pool')
                    S.act(ch[:], ch[:], AF.Exp)
                    S.tt(ch[:], ch[:], kkT[:], ALU.mult, e='pool')
                    r0 = 832 + d * 32
                    load_mu(S, W, l, mu, r0, 32)
                    S.dma(Lb[0:32, :], D['pT'][base + r0: base + r0 + 32, :])
                    shift_T(S, bh, Lb, mu, 32)
                    for n0 in range(0, T, 512):
                        nw = min(512, T - n0)
                        ps = K.bank()
                        S.mm(ps[0:64, :nw], aup[:, d, :], bh[0:32, n0:n0 + nw])
                        S.act(kh[:, n0:n0 + nw], ps[0:64, :nw], AF.Sigmoid, bias=pk[:, 5 + d:6 + d])
                    S.act(bh[:], rh[:], AF.Exp, scale=-1.0)
                    S.tt(bh[:], bh[:], kkT[:], ALU.mult)
                    S.tt(bh[:], bh[:], kh[:], ALU.mult, e='pool')
                    S.ts(kh[:], kh[:], -1.0, pk[:, 1:2], ALU.add, ALU.mult)
                    S.stt(kh[:], kh[:], 1.0, kT[:], ALU.add, ALU.mult)
                    S.stt(Lb[:], kh[:], pk[:, 2:3], rT[:], ALU.mult, ALU.mult)
                    ps = K.bank()
                    for n in range(NCH):
                        S.mm(ps[0:64, n:n + 1], Lb[:, n * 64:(n + 1) * 64], ones[:, 0:1])
                    if d == 0:
                        S.copy(bon[:], ps[0:64, 0:NCH])
                    else:
                        S.tt(bon[:], bon[:], ps[0:64, 0:NCH], ALU.add)
                    S.act(Lb[:], rh[:], AF.Exp, scale=-1.0)
                    S.tt(kh[:], kh[:], Lb[:], ALU.mult, e='pool')
                    S.act(rh[:], rh[:], AF.Exp)
                    S.tt(rh[:], rh[:], rT[:], ALU.mult)
                    S.memset(Ms[0][:], 0.0)
                    S.copy(sgn[:, 0:2, :], masks[:, d, :].unsqueeze(1).to_broadcast([64, 2, 64]), e='pool')
                    S.ts(sgn[:, 2:4, :], sgn[:, 0:2, :], -1.0, None, ALU.mult, e='pool')
                    order = scan_order(d)
                    pairs = [order[i] // 2 for i in range(0, NCH, 2)]
                    state = {'si': 0}

                    def prep(pidx, st):
                        pi = pairs[pidx]
                        BB, PP = st['BB'], st['PP']
                        tk = slice(pi * 128, (pi + 1) * 128)
                        ps = K.bank()
                        S.mm(ps[:, 0:128], bh[:, tk], ch[:, tk])
                        S.mm(ps[:, 128:256], ch[:, tk], bh[:, tk])
                        psk = K.bank()
                        S.mm(psk[:, 0:128], kh[:, tk], ch[:, tk])
                        yield
                        S.stt(n32(BB[:, 0, :]), ps[:, 0:128], -1.0, m128[:, 2 + d, :], ALU.mult, ALU.mult)
                        yield
                        S.stt(n32(BB[:, 1, :]), ps[:, 128:256], -1.0, m128[:, 3 - d, :], ALU.mult, ALU.mult)
                        yield
                        S.tt(n32(PP[:]), BB[:], identB, ALU.add, e='pool')
                        S.tt(st['AkT'], psk[:, 0:128], m128[:, 2 + d, :], ALU.mult)
                        yield
                        ps = K.bank()
                        S.mm(ps[:, 0:64], st['AkT'], Vpair[:, pi, :])
                        S.tr(ps[:, 64:128], ch[:, tk], K.ident[0:64, 0:64])
                        yield
                        S.copy(st['AV'][:], ps[:, 0:64], e='act')
                        S.copy(st['Cp'][:], ps[:, 64:128], e='act')
                        yield
                        yield from neumann_gen(K, S, PP, BB, st['N2'])
                        P = PP[:, 0, :]
                        ps = K.bank()
                        for c in range(2):
                            ck = slice(pi * 128 + c * 64, pi * 128 + (c + 1) * 64)
                            S.tr(ps[0:64, c * 64:(c + 1) * 64], kh[:, ck], K.ident[0:64, 0:64])
                            S.tr(ps[0:64, 128 + c * 64:128 + (c + 1) * 64], bh[:, ck], K.ident[0:64, 0:64])
                        yield
                        S.copy(st['KN'][:, 0:2, :], ps[0:64, 0:128].rearrange("p (c k) -> p c k", c=2), e='act')
                        yield
                        S.ts(st['KN'][:, 2:4, :], ps[0:64, 128:256].rearrange("p (c k) -> p c k", c=2), -1.0, None, ALU.mult)
                        yield
                        ps = K.bank()
                        S.mm(ps[0:64, 0:128], st['Cp'][:], P)
                        for c in range(2):
                            S.mm(ps[0:64, 128 + c * 64:128 + (c + 1) * 64], PP[:, 0, c * 64:(c + 1) * 64], st['AV'][:])
                        yield
                        S.copy(st['WcT'][:], ps[0:64, 0:128], e='act')
                        S.copy(st['us'][:], ps[0:64, 128:256].rearrange("p (c v) -> p c v", c=2), e='act')
                        yield
                        ps = K.bank()
                        for c in range(2):
                            ck = slice(pi * 128 + c * 64, pi * 128 + (c + 1) * 64)
                            S.mm(ps[0:64, c * 64:(c + 1) * 64], kh[:, ck], rh[:, ck])
                            S.mm(ps[0:64, 128 + c * 64:128 + (c + 1) * 64], bh[:, ck], rh[:, ck])
                        yield
                        S.tt(st['QQ'], ps[0:64, 0:256].rearrange("p (c i) -> p c i", c=4), sgn[:], ALU.mult)
                        yield

                    def rec(pidx, st):
                        pi = pairs[pidx]
                        for c in ((0, 1) if d == 0 else (1, 0)):
                            n = pi * 2 + c
                            ck = slice(n * 64, (n + 1) * 64)
                            si = state['si']
                            Mc, Mn = Ms[si % 2], Ms[(si + 1) % 2]
                            zn, mt = zns[si % 2], mts[si % 2]
                            state['si'] = si + 1
                            ps1 = K.bank()
                            S.mm(ps1[0:64, 0:64], st['WcT'][:, c * 64:(c + 1) * 64], Mc[:])
                            yield
                            S.tt(zn[:], st['us'][:, c, :], ps1[0:64, 0:64], ALU.add)
                            yield
                            ps2 = K.bank()
                            S.mm(ps2[0:64, 0:64], rh[:, ck], Mc[:], start=True, stop=False)
                            S.mm(ps2[0:64, 0:64], st['QQ'][:, c, :], Vtok[:, n, :], start=False, stop=False)
                            S.mm(ps2[0:64, 0:64], st['QQ'][:, 2 + c, :], zn[:], start=False, stop=True)
                            ps3 = K.bank()
                            S.mm(ps3[0:64, 0:64], st['KN'][:, c, :], Vtok[:, n, :], start=True, stop=False)
                            S.mm(ps3[0:64, 0:64], st['KN'][:, 2 + c, :], zn[:], start=False, stop=True)
                            yield
                            S.ts(mt[:], ps3[0:64, 0:64], GLc[:, n:n + 1], None, ALU.mult)
                            if d == 0:
                                S.copy(hacc[:, n, :], ps2[0:64, 0:64], e='act')
                            else:
                                S.tt(hacc[:, n, :], hacc[:, n, :], ps2[0:64, 0:64], ALU.add)
                            yield
                            S.stt(Mn[:], Mc[:], GLc[:, n:n + 1], mt[:], ALU.mult, ALU.add)
                            yield

                    run_pipeline(min(len(pairs), DBG.get('b_pairs', 99)), prep, rec, sets)
                gtok = kh[:, :].rearrange("p (n c) -> p n c", c=64)
                load_mu(S, W, l, mu, 896, 64)
                S.dma(rh[:], D['pT'][base + 896: base + 960, :])
                shift_T(S, bh, rh, mu, 64)
                S.act(bh[:], bh[:], AF.Sigmoid)
                for n0 in range(0, NCH, 8):
                    nn = min(8, NCH - n0)
                    ps = K.bank()
                    for j in range(nn):
                        S.mm(ps[0:64, j * 64:(j + 1) * 64], bh[:, (n0 + j) * 64:(n0 + j + 1) * 64], gup[:])
                    S.copy(gtok[:, n0:n0 + nn, :], ps[0:64, 0:nn * 64].rearrange("p (j c) -> p j c", c=64), e='act')
                st = K.sb(es, [64, NCH, 4])
                sq = ch[:, :].rearrange("p (n c) -> p n c", c=64)
                bc3 = lambda a: a.unsqueeze(2).to_broadcast([64, NCH, 64])
                S.reduce(st[:, :, 0], hacc[:], ALU.add)
                S.ts(st[:, :, 0], st[:, :, 0], 1.0 / 64, None, ALU.mult)
                S.tt(hacc[:], hacc[:], bc3(st[:, :, 0]), ALU.subtract)
                S.tt(sq, hacc[:], hacc[:], ALU.mult, e='pool')
                S.reduce(st[:, :, 1], sq, ALU.add)
                S.ts(st[:, :, 1], st[:, :, 1], 1.0 / 64, 64e-5, ALU.mult, ALU.add)
                S.act(st[:, :, 2], st[:, :, 1], AF.Sqrt)
                S.recip(st[:, :, 3], st[:, :, 2])
                S.tt(hacc[:], hacc[:], bc3(st[:, :, 3]), ALU.mult)
                S.tt(hacc[:], hacc[:], lng[:, 0, :].unsqueeze(1).to_broadcast([64, NCH, 64]), ALU.mult, e='pool')
                S.tt(hacc[:], hacc[:], lng[:, 1, :].unsqueeze(1).to_broadcast([64, NCH, 64]), ALU.add, e='pool')
                S.tt(sq, Vtok[:], bc3(bon[:, :]), ALU.mult)
                S.tt(hacc[:], hacc[:], sq, ALU.add, e='pool')
                S.tt(hacc[:], hacc[:], gtok, ALU.mult)
                c0 = 256 + h * 64
                yv = D['y'][:, c0:c0 + 64].rearrange("(n i) c -> i n c", i=64)
                for g4 in range(4):
                    S.dma(yv[:, g4 * 17:(g4 + 1) * 17, :], hacc[:, g4 * 17:(g4 + 1) * 17, :], q=('sp', 'pool')[g4 % 2])
            S.barrier()
    S.barrier()


W_SHAPES = {
    'xin': [T, 1024], 'cc': [128, 8, 2],
    'mod_w': [4, 1024, 6144], 'mod_b': [4, 6144], 'norm_mix_g': [4, 1024], 'norm_ffn_g': [4, 1024],
    'w_in': [4, 1024, IN_COLS], 'w_out': [4, 1024, 1024],
    'lru_conv_w': [4, 4, 256], 'lru_conv_b': [4, 256], 'lru_w_a': [4, 2, 4, 64, 64], 'lru_b_a': [4, 2, 256],
    'lru_w_x': [4, 2, 4, 64, 64], 'lru_b_x': [4, 2, 256], 'lru_lambda': [4, 2, 256],
    'rwkv_mu': [4, 2, 960], 'rwkv_w_up': [4, 2, 32, 256], 'rwkv_w0': [4, 2, 256], 'rwkv_a_up': [4, 2, 32, 256],
    'rwkv_a0': [4, 2, 256], 'rwkv_g_up': [4, 64, 256], 'rwkv_k_k': [4, 256], 'rwkv_k_a': [4, 256],
    'rwkv_r_k': [4, 256], 'rwkv_ln_g': [4, 256], 'rwkv_ln_b': [4, 256],
    'mlstm_i_b': [4, 2, 4], 'mlstm_f_b': [4, 2, 4], 'mlstm_norm_g': [4, 256],
    'gdn_conv_w': [4, 4, 768], 'gdn_a_log': [4, 2, 4], 'gdn_dt_bias': [4, 2, 4], 'gdn_norm_g': [4, 256],
    'ffn_w_gate': [2, 1024, D_FF], 'ffn_w_up': [2, 1024, D_FF], 'ffn_w_down': [2, D_FF, 1024],
    'moe_router': [2, 1024, 8], 'moe_w_gate': [2, 8, 1024, D_FFE], 'moe_w_up': [2, 8, 1024, D_FFE],
    'moe_w_down': [2, 8, D_FFE, 1024], 'final_norm_g': [1, 1024],
    'c_ident': [128, 128], 'c_sel8': [8, 8, 128],
    'pk_lru': [4, 2, 128, 11], 'pk_ml': [4, 4, 4],
    'c_masks': [64, 2, 64], 'c_rst': [4, 2, T], 'c_m128': [128, 4, 128],
    'pk_gd': [4, 4, 4], 'pk_gdc': [4, 4, 64, 3, 4], 'pk_mu': [4, 960, 2], 'pk_rw': [4, 4, 64, 7],
}


def make_consts():
    c = {}
    c['c_ident'] = np.eye(128, dtype=np.float32)
    s = np.zeros((8, 8, 128), np.float32)
    for e in range(8):
        s[e, e, :] = 1.0
    c['c_sel8'] = s
    jj, ii = np.meshgrid(np.arange(64), np.arange(64), indexing='ij')
    c['c_masks'] = np.ascontiguousarray(np.stack([(ii >= jj), (ii <= jj)], axis=1).astype(np.float32))
    ja, ia = np.meshgrid(np.arange(128), np.arange(128), indexing='ij')
    same = (ja // 64) == (ia // 64)
    c['c_m128'] = np.ascontiguousarray(np.stack([same & (ia >= ja), same & (ia <= ja), same & (ia > ja), same & (ia < ja)], axis=1).astype(np.float32))
    idx = np.arange(T) % 64
    r = np.stack([(idx != 0), (idx != 63)], axis=0).astype(np.float32)
    c['c_rst'] = np.ascontiguousarray(np.broadcast_to(r[None], (4, 2, T)))
    return c


def build(layers=(0, 1, 2, 3), mixers=None, final=True, dbg=()):
    nc = bass.Bass("TRN2", target_bir_lowering=False)
    W = {n: nc.dram_tensor(n, sh, F32, kind="ExternalInput").ap() for n, sh in W_SHAPES.items()}
    out = nc.dram_tensor('out', [SEQ, 1024], F32, kind="ExternalOutput").ap()
    D = {}
    for n, sh in {'xres': [T, 1024], 'pT': [IN_COLS, T], 'y': [T, 1024], 'mod': [2, 6144]}.items():
        kind = "ExternalOutput" if n in dbg else "Internal"
        D[n] = nc.dram_tensor('d_' + n, sh, F32, kind=kind).ap()
    with ExitStack() as es:
        S = Sched(nc, es)
        ps = [es.enter_context(nc.psum_tensor("psb%d" % i, [128, 512], F32)) for i in range(8)]
        ident = es.enter_context(nc.sbuf_tensor("ident", [128, 128], F32))
        K = Ctx(nc, S, W, D, ps, ident)
        K.out = out
        S.dma(ident[:], W['c_ident'])
        for t in range(NT):
            S.dma(D['xres'][t * 128:(t + 1) * 128, :], W['xin'][t * 128:(t + 1) * 128, :], q=('sp', 'pool', 'act')[t % 3])
        S.barrier()
        for l in layers:
            last = (l == 3)
            stage_mod(K, l)
            stage_inproj(K, l)
            if mixers is None:
                mix_identity(K, l)
            else:
                for m in mixers:
                    m(K, l, last)
            stage_outproj(K, l, last)
            stage_ffn(K, l, last)
        if final:
            stage_final(K)
        S.finish()
    K.S = S
    return nc, S


def make_packs(inputs):
    f = lambda n: np.asarray(inputs[n], dtype=np.float32)
    pk = {}
    cols = [f('lru_conv_w')[:, j, :] for j in range(4)] + [f('lru_conv_b')]
    cols += [f('lru_b_a')[:, 0], f('lru_b_a')[:, 1], f('lru_b_x')[:, 0], f('lru_b_x')[:, 1], f('lru_lambda')[:, 0], f('lru_lambda')[:, 1]]
    a = np.stack(cols, axis=-1)
    pk['pk_lru'] = np.ascontiguousarray(a.reshape(4, 2, 128, 11))
    pk['pk_gd'] = np.ascontiguousarray(np.concatenate([f('gdn_a_log'), f('gdn_dt_bias')], axis=1).transpose(0, 2, 1))
    pk['pk_gdc'] = np.ascontiguousarray(f('gdn_conv_w').reshape(4, 4, 3, 4, 64).transpose(0, 3, 4, 2, 1))
    pk['pk_mu'] = np.ascontiguousarray(f('rwkv_mu').transpose(0, 2, 1))
    cols = [f('rwkv_k_k'), f('rwkv_k_a'), f('rwkv_r_k'), f('rwkv_w0')[:, 0], f('rwkv_w0')[:, 1], f('rwkv_a0')[:, 0], f('rwkv_a0')[:, 1]]
    pk['pk_rw'] = np.ascontiguousarray(np.stack(cols, axis=-1).reshape(4, 4, 64, 7))
    pk['pk_ml'] = np.ascontiguousarray(np.concatenate([f('mlstm_i_b'), f('mlstm_f_b')], axis=1).transpose(0, 2, 1))
    return pk


def host_inputs(inputs, b):
    m = {}
    m['xin'] = np.ascontiguousarray(np.concatenate([inputs['ctx'][b], inputs['x'][b]], axis=0))
    cc = np.stack([np.asarray(inputs['c'][b]).reshape(8, 128).T, np.asarray(inputs['c_ctx']).reshape(8, 128).T], axis=-1)
    m['cc'] = np.ascontiguousarray(cc.astype(np.float32))
    for n in W_SHAPES:
        if n in m or n.startswith('c_') or n.startswith('pk_'):
            continue
        m[n] = np.ascontiguousarray(np.asarray(inputs[n], dtype=np.float32).reshape(W_SHAPES[n]))
    m.update(make_consts())
    m.update(make_packs(inputs))
    return m


def build_test(L, mixers):
    nc = bass.Bass("TRN2", target_bir_lowering=False)
    W = {n: nc.dram_tensor(n, sh, F32, kind="ExternalInput").ap() for n, sh in W_SHAPES.items()}
    D = {}
    for n, sh in {'xres': [T, 1024], 'pT': [IN_COLS, T], 'y': [T, 1024], 'mod': [2, 6144]}.items():
        kind = "ExternalOutput" if n in ('pT', 'y') else "Internal"
        D[n] = nc.dram_tensor('d_' + n, sh, F32, kind=kind).ap()
    add_scratch(nc, D)
    with ExitStack() as es:
        S = Sched(nc, es)
        ps = [es.enter_context(nc.psum_tensor("psb%d" % i, [128, 512], F32)) for i in range(8)]
        ident = es.enter_context(nc.sbuf_tensor("ident", [128, 128], F32))
        K = Ctx(nc, S, W, D, ps, ident)
        S.dma(ident[:], W['c_ident'])
        for t in range(NT):
            S.dma(D['xres'][t * 128:(t + 1) * 128, :], W['xin'][t * 128:(t + 1) * 128, :], q=('sp', 'pool', 'act')[t % 3])
        S.barrier()
        stage_mod(K, L)
        stage_inproj(K, L)
        for m in mixers:
            m(K, L, False)
        S.finish()
    return nc, S


def add_scratch(nc, D):
    pass


def kernel(**inputs):
    nc, S = build(layers=(0, 1, 2, 3), mixers=[mixer_a, mixer_b, mixer_c, mixer_d], final=True)
    in_maps = [host_inputs(inputs, b) for b in range(8)]
    res = run_bass_kernel_spmd(nc, in_maps, core_ids=list(range(8)))
    return np.stack([np.asarray(r['out'], dtype=np.float32) for r in res.results], axis=0)
```

```python
import math
import numpy as np
import concourse.bass as bass
import concourse.mybir as mybir
from concourse.bass_utils import run_bass_kernel_spmd
from contextlib import ExitStack

F32 = mybir.dt.float32
F32R = mybir.dt.float32r
FAST_MM = True
AF = mybir.ActivationFunctionType
ALU = mybir.AluOpType
AX = mybir.AxisListType

D_MODEL = 1024
SEQ = 4096
CTX = 256
T = SEQ + CTX
NT = T // 128
G = 256
IN_COLS = 3552
OFF_B = 512
OFF_C = OFF_B + 960
OFF_D = OFF_C + 1040
D_FF = 2816
D_FFE = 1408
EPS = 1e-6

ENGS = ['pe', 'dve', 'act', 'pool', 'sp']
SAME_SYNC = {'pe': False, 'dve': True, 'act': True, 'pool': True, 'sp': True}

def _box(ap):
    t = ap.tensor
    name = t.name
    dims = ap.ap
    off = int(ap.offset)
    if str(ap.space) in ('SB', 'PSUM', 'SBUF'):
        row = dims[0][0] if dims[0][0] > 0 else 1
        p0 = off // row
        f0 = off % row
        p1 = p0 + dims[0][1]
        lo = hi = f0
        for st, cnt in dims[1:]:
            if st >= 0:
                hi += st * (cnt - 1)
            else:
                lo += st * (cnt - 1)
        return (name, p0, p1, lo, hi + 1)
    lo = hi = off
    for st, cnt in dims:
        if st >= 0:
            hi += st * (cnt - 1)
        else:
            lo += st * (cnt - 1)
    return (name, 0, 1, lo, hi + 1)


class Sched:
    def __init__(self, nc, es, n_dma=8):
        self.nc = nc
        self.es = es
        self.eng = dict(pe=nc.tensor, dve=nc.vector, act=nc.scalar, pool=nc.gpsimd, sp=nc.sync)
        self.sem = {}
        self.cnt = {}
        self.unit = {}
        for e in ENGS:
            self.sem[e] = es.enter_context(nc.semaphore('s_' + e))
            self.cnt[e] = 0
            self.unit[e] = 1
        self.n_dma = n_dma
        self.dma_rr = {}
        for q in ('sp', 'act', 'pool'):
            self.dma_rr[q] = 0
            for i in range(n_dma):
                c = ('dma', q, i)
                self.sem[c] = es.enter_context(nc.semaphore('d_%s%d' % (q, i)))
                self.cnt[c] = 0
                self.unit[c] = 16
        self.seen = {e: {} for e in ENGS}
        self.recs = {}
        self.nins = 0

    def _need(self, reads, writes):
        need = {}
        for aps, isw in ((reads, False), (writes, True)):
            for ap in aps:
                name, p0, p1, f0, f1 = _box(ap)
                if not isw and name.startswith('psb'):
                    isw, p0, p1, f0, f1 = True, 0, 128, 0, 1 << 30
                for r in self.recs.get(name, ()):
                    if r[0] < p1 and p0 < r[1] and r[2] < f1 and f0 < r[3]:
                        if isw or r[6]:
                            c, v = r[4], r[5]
                            if need.get(c, 0) < v:
                                need[c] = v
        return need

    def _record(self, reads, writes, clock, val):
        for aps, isw in ((reads, False), (writes, True)):
            for ap in aps:
                name, p0, p1, f0, f1 = _box(ap)
                if not isw and name.startswith('psb'):
                    isw, p0, p1, f0, f1 = True, 0, 128, 0, 1 << 30
                lst = self.recs.setdefault(name, [])
                if isw:
                    lst[:] = [r for r in lst if not (p0 <= r[0] and r[1] <= p1 and f0 <= r[2] and r[3] <= f1)]
                else:
                    lst[:] = [r for r in lst if not (r[4] == clock and not r[6] and p0 <= r[0] and r[1] <= p1 and f0 <= r[2] and r[3] <= f1)]
                lst.append((p0, p1, f0, f1, clock, val, isw))
                if len(lst) > 48:
                    self._prune(lst)

    def _prune(self, lst):
        def stale(r):
            c, v = r[4], r[5]
            for e in ENGS:
                if e == c and not SAME_SYNC[e]:
                    continue
                if self.seen[e].get(c, 0) < v:
                    return False
            return True
        lst[:] = [r for r in lst if not stale(r)]

    def _waits(self, e, need):
        eo = self.eng[e]
        for c, v in need.items():
            if c == e and not SAME_SYNC[e]:
                continue
            if self.seen[e].get(c, 0) >= v:
                continue
            eo.wait_ge(self.sem[c], v * self.unit[c])
            self.seen[e][c] = v

    def op(self, e, fn, reads, writes):
        need = self._need(reads, writes)
        self._waits(e, need)
        ins = fn(self.eng[e])
        self.cnt[e] += 1
        ins.then_inc(self.sem[e], 1)
        self._record(reads, writes, e, self.cnt[e])
        self.nins += 1
        return ins

    def dma(self, out, in_, q='sp', **kw):
        need = self._need([in_], [out])
        k = self.dma_rr[q]
        self.dma_rr[q] = (k + 1) % self.n_dma
        c = ('dma', q, k)
        if self.cnt[c] > 0:
            need[c] = max(need.get(c, 0), self.cnt[c])
        self._waits(q, need)
        ins = self.eng[q].dma_start(out=out, in_=in_, **kw)
        self.cnt[c] += 1
        ins.then_inc(self.sem[c], 16)
        self._record([in_], [out], c, self.cnt[c])
        self.nins += 1

    def barrier(self):
        for e in ENGS:
            need = {c: v for c, v in self.cnt.items() if v > 0 and c != e}
            self._waits(e, need)
        self.recs = {}

    def finish(self):
        need = {c: v for c, v in self.cnt.items() if v > 0 and c != 'sp'}
        self._waits('sp', need)

    def mm(self, out, lhsT, rhs, start=True, stop=True, fast=False):
        if fast and FAST_MM:
            lhsT, rhs = lhsT.bitcast(F32R), rhs.bitcast(F32R)
        self.op('pe', lambda e: e.matmul(out, lhsT, rhs, start=start, stop=stop), [lhsT, rhs] + ([] if start else [out]), [out])

    def tr(self, out, in_, ident):
        self.op('pe', lambda e: e.transpose(out, in_, ident), [in_, ident], [out])

    def act(self, out, in_, func, bias=None, scale=None, accum_out=None):
        kw = {}
        rd = [in_]
        wr = [out]
        if bias is not None:
            kw['bias'] = bias
            if not isinstance(bias, (int, float)):
                rd.append(bias)
        if scale is not None:
            kw['scale'] = scale
            if not isinstance(scale, (int, float)):
                rd.append(scale)
        if accum_out is not None:
            kw['accum_out'] = accum_out
            wr.append(accum_out)
        self.op('act', lambda e: e.activation(out, in_, func, **kw), rd, wr)

    def tt(self, out, in0, in1, op, e='dve'):
        self.op(e, lambda en: en.tensor_tensor(out, in0, in1, op), [in0, in1], [out])

    def ts(self, out, in0, s1, s2, op0, op1=None, e='dve', accum_out=None):
        rd = [in0]
        for s in (s1, s2):
            if s is not None and not isinstance(s, (int, float)):
                rd.append(s)
        wr = [out] + ([accum_out] if accum_out is not None else [])
        kw = {}
        if op1 is not None:
            kw['op1'] = op1
        if accum_out is not None:
            kw['accum_out'] = accum_out
        self.op(e, lambda en: en.tensor_scalar(out, in0, s1, s2, op0, **kw), rd, wr)

    def stt(self, out, in0, scalar, in1, op0, op1, e='dve'):
        rd = [in0, in1]
        if not isinstance(scalar, (int, float)):
            rd.append(scalar)
        self.op(e, lambda en: en.scalar_tensor_tensor(out, in0, scalar, in1, op0, op1), rd, [out])

    def copy(self, out, in_, e='dve'):
        if e == 'act':
            self.op(e, lambda en: en.copy(out, in_), [in_], [out])
        else:
            self.op(e, lambda en: en.tensor_copy(out, in_), [in_], [out])

    def memset(self, ap, val, e='dve'):
        self.op(e, lambda en: en.memset(ap, val), [], [ap])

    def reduce(self, out, in_, op, axis=None, e='dve'):
        axis = axis or AX.X
        self.op(e, lambda en: en.tensor_reduce(out, in_, axis, op), [in_], [out])

    def scan(self, out, d0, d1, init, op0, op1):
        rd = [d0, d1]
        if not isinstance(init, (int, float)):
            rd.append(init)
        self.op('dve', lambda en: en.tensor_tensor_scan(out, d0, d1, init, op0, op1), rd, [out])

    def recip(self, out, in_):
        self.op('dve', lambda en: en.reciprocal(out, in_), [in_], [out])


class Ctx:
    def __init__(self, nc, S, W, D, ps, ident):
        self.nc, self.S, self.W, self.D, self.ps, self.ident = nc, S, W, D, ps, ident
        self.uid = 0
        self.psi = 0

    def sb(self, es, shape, dt=F32, name=None):
        self.uid += 1
        return es.enter_context(self.nc.sbuf_tensor("%s_%d" % (name or "t", self.uid), list(shape), dt))

    def bank(self):
        b = self.ps[self.psi % 8]
        self.psi += 1
        return b


def r32(ap):
    return ap.bitcast(F32R) if FAST_MM else ap


def bc_rows(ap, n):
    return ap.to_broadcast([n, ap.shape[1]])


def stage_mod(K, l):
    S, W, D = K.S, K.W, K.D
    with ExitStack() as es:
        cc = K.sb(es, [128, 8, 2])
        S.dma(cc[:], W['cc'])
        S.act(cc[:], cc[:], AF.Silu)
        mb = K.sb(es, [2, 6144])
        S.dma(mb[:], bc_rows(W['mod_b'][l:l + 1, :], 2))
        mo = K.sb(es, [2, 6144])
        wts = [K.sb(es, [128, 8, 512]) for _ in range(2)]
        wv = W['mod_w'][l].rearrange("(k p) c -> p k c", p=128)
        for n in range(12):
            wt = wts[n % 2]
            S.dma(wt[:], wv[:, :, n * 512:(n + 1) * 512], q=('sp' if n % 2 == 0 else 'pool'))
            ps = K.bank()
            for k in range(8):
                S.mm(ps[0:2, :], cc[:, k, :], wt[:, k, :], start=(k == 0), stop=(k == 7))
            S.tt(mo[:, n * 512:(n + 1) * 512], ps[0:2, :], mb[:, n * 512:(n + 1) * 512], ALU.add)
        S.dma(D['mod'], mo[:])
    S.barrier()


def load_mod_tiles(K, es, l, which, gname):
    S, W, D = K.S, K.W, K.D
    base = 3072 * which
    outs = []
    gt = K.sb(es, [128, 1024])
    S.dma(gt[:], bc_rows(W[gname][l:l + 1, :], 128))
    for seg in range(2):
        sh = K.sb(es, [128, 1024])
        sc = K.sb(es, [128, 1024])
        S.dma(sh[:], bc_rows(D['mod'][seg:seg + 1, base:base + 1024], 128), q='pool')
        S.dma(sc[:], bc_rows(D['mod'][seg:seg + 1, base + 1024:base + 2048], 128), q='pool')
        S.stt(sc[:], sc[:], 1.0, gt[:], ALU.add, ALU.mult)
        outs += [sc, sh]
    return outs


def norm_mod_T(K, es_tmp, xt, Gt, SHt, hT_dst, tmp):
    S = K.S
    junk, ss, h = tmp
    S.act(junk[:], xt[:], AF.Square, accum_out=ss[:, 0:1])
    S.ts(ss[:, 1:2], ss[:, 0:1], 1.0 / D_MODEL, EPS, ALU.mult, ALU.add)
    S.act(ss[:, 2:3], ss[:, 1:2], AF.Sqrt)
    S.recip(ss[:, 3:4], ss[:, 2:3])
    S.stt(h[:], xt[:], ss[:, 3:4], Gt[:], ALU.mult, ALU.mult)
    S.tt(h[:], h[:], SHt[:], ALU.add, e='pool')
    for b in range(2):
        ps = K.bank()
        for j in range(4):
            k = b * 4 + j
            S.tr(ps[:, j * 128:(j + 1) * 128], h[:, k * 128:(k + 1) * 128], K.ident[:])
        src = ps[:].rearrange("p (j t) -> p j t", j=4)
        if b == 0:
            S.copy(r32(hT_dst[:, 0:4, :]), src, e='dve')
        else:
            S.copy(r32(hT_dst[:, 4:8, :]), src, e='act')


def stage_inproj(K, l):
    S, W, D = K.S, K.W, K.D
    HALF = T // 2
    wv = W['w_in'][l].rearrange("(k p) c -> p k c", p=128)
    with ExitStack() as es:
        GL, SHL, GC, SHC = load_mod_tiles(K, es, l, 0, 'norm_mix_g')
        hT = K.sb(es, [128, 8, HALF])
        xts = [K.sb(es, [128, 1024]) for _ in range(2)]
        tmp = (K.sb(es, [128, 1024]), K.sb(es, [128, 4]), K.sb(es, [128, 1024]))
        wts = [K.sb(es, [128, 8, 128]) for _ in range(3)]
        ots = [K.sb(es, [128, HALF]) for _ in range(2)]
        for half in range(2):
            for ti in range(17):
                t = half * 17 + ti
                xt = xts[ti % 2]
                S.dma(xt[:], D['xres'][t * 128:(t + 1) * 128, :])
                isctx = t < 2
                norm_mod_T(K, es, xt, GC if isctx else GL, SHC if isctx else SHL,
                           hT[:, :, ti * 128:(ti + 1) * 128], tmp)
            for cchunk in range(28):
                c0 = cchunk * 128
                cw = min(128, IN_COLS - c0)
                wt = wts[cchunk % 3]
                S.dma(r32(wt[:, :, :cw]), wv[:, :, c0:c0 + cw], q='pool')
                ot = ots[cchunk % 2]
                for si, (n0, nw) in enumerate([(0, 512), (512, 512), (1024, 512), (1536, 512), (2048, 128)]):
                    ps = K.bank()
                    for k in range(8):
                        S.mm(ps[:cw, :nw], wt[:, k, :cw], hT[:, k, n0:n0 + nw], start=(k == 0), stop=(k == 7), fast=True)
                    S.copy(ot[:cw, n0:n0 + nw], ps[:cw, :nw], e=('dve' if si % 2 == 0 else 'act'))
                S.dma(D['pT'][c0:c0 + cw, half * HALF:(half + 1) * HALF], ot[:cw, :], q='act')
    S.barrier()


def stage_outproj(K, l, last):
    S, W, D = K.S, K.W, K.D
    wv = W['w_out'][l].rearrange("(k p) c -> p k c", p=128)
    with ExitStack() as es:
        wo = K.sb(es, [128, 8, 1024])
        S.dma(r32(wo[:, 0:4, :]), wv[:, 0:4, :], q='pool')
        S.dma(r32(wo[:, 4:8, :]), wv[:, 4:8, :], q='pool')
        gts = []
        for seg in range(2):
            g = K.sb(es, [128, 1024])
            S.dma(g[:], bc_rows(D['mod'][seg:seg + 1, 2048:3072], 128))
            gts.append(g)
        yts = [K.sb(es, [128, 1024]) for _ in range(2)]
        xts = [K.sb(es, [128, 1024]) for _ in range(2)]
        yTs = [K.sb(es, [128, 8, 128]) for _ in range(2)]
        for t in range(2 if last else 0, NT):
            yt, xt, yT = yts[t % 2], xts[t % 2], yTs[t % 2]
            S.dma(yt[:], D['y'][t * 128:(t + 1) * 128, :])
            S.dma(xt[:], D['xres'][t * 128:(t + 1) * 128, :], q='pool')
            for b in range(2):
                ps = K.bank()
                for j in range(4):
                    k = b * 4 + j
                    S.tr(ps[:, j * 128:(j + 1) * 128], yt[:, k * 128:(k + 1) * 128], K.ident[:])
                S.copy(r32(yT[:, b * 4:(b + 1) * 4, :]), ps[:].rearrange("p (j t) -> p j t", j=4), e=('dve' if b == 0 else 'act'))
            gt = gts[0] if t >= 2 else gts[1]
            for n in range(2):
                ps = K.bank()
                for k in range(8):
                    S.mm(ps[:, :], yT[:, k, :], wo[:, k, n * 512:(n + 1) * 512], start=(k == 0), stop=(k == 7), fast=True)
                S.tt(yt[:, n * 512:(n + 1) * 512], ps[:, :], gt[:, n * 512:(n + 1) * 512], ALU.mult)
                S.tt(xt[:, n * 512:(n + 1) * 512], xt[:, n * 512:(n + 1) * 512], yt[:, n * 512:(n + 1) * 512], ALU.add, e='pool')
            S.dma(D['xres'][t * 128:(t + 1) * 128, :], xt[:], q='act')
    S.barrier()


def stage_ffn(K, l, last):
    S, W, D = K.S, K.W, K.D
    dense = (l % 2 == 0)
    li = l // 2
    if dense:
        E, NF = 1, D_FF // 128
        wg_v = [W['ffn_w_gate'][li].rearrange("(k p) c -> p k c", p=128)]
        wu_v = [W['ffn_w_up'][li].rearrange("(k p) c -> p k c", p=128)]
        wd_v = [W['ffn_w_down'][li].rearrange("(f p) c -> p f c", p=128)]
    else:
        E, NF = 8, D_FFE // 128
        wg_v = [W['moe_w_gate'][li, e].rearrange("(k p) c -> p k c", p=128) for e in range(8)]
        wu_v = [W['moe_w_up'][li, e].rearrange("(k p) c -> p k c", p=128) for e in range(8)]
        wd_v = [W['moe_w_down'][li, e].rearrange("(f p) c -> p f c", p=128) for e in range(8)]
    blocks = ([] if last else [(0, 2)]) + [(2 + 4 * i, 4) for i in range(8)]
    with ExitStack() as es:
        gn = K.sb(es, [128, 1024])
        S.dma(gn[:], bc_rows(W['norm_ffn_g'][l:l + 1, :], 128))
        G2, SH2, GT2 = K.sb(es, [128, 1024]), K.sb(es, [128, 1024]), K.sb(es, [128, 1024])
        xblk = K.sb(es, [128, 4, 1024])
        h2T = K.sb(es, [128, 8, 512])
        actT = K.sb(es, [128, NF, 512])
        yT = K.sb(es, [128, 8, 512])
        tmp = (K.sb(es, [128, 1024]), K.sb(es, [128, 4]), K.sb(es, [128, 1024]))
        sgs = [K.sb(es, [128, 512]) for _ in range(2)]
        wgs = [K.sb(es, [128, 8, 128]) for _ in range(2)]
        wus = [K.sb(es, [128, 8, 128]) for _ in range(2)]
        wds = [K.sb(es, [128, NF, 128]) for _ in range(2)]
        if not dense:
            Gbc = K.sb(es, [128, 8, 512])
            rt = K.sb(es, [128, 8, 8])
            S.dma(rt[:], W['moe_router'][li].rearrange("(k p) e -> p k e", p=128))
            gateT = K.sb(es, [8, 512])
            sel = K.sb(es, [8, 8, 128])
            S.dma(sel[:], W['c_sel8'])
            gsm = K.sb(es, [128, 64])
        cur_seg = None
        wi = 0
        for (t0, nt) in blocks:
            NB = nt * 128
            seg = 1 if t0 < 2 else 0
            if seg != cur_seg:
                cur_seg = seg
                S.dma(SH2[:], bc_rows(D['mod'][seg:seg + 1, 3072:4096], 128), q='pool')
                S.dma(G2[:], bc_rows(D['mod'][seg:seg + 1, 4096:5120], 128), q='pool')
                S.dma(GT2[:], bc_rows(D['mod'][seg:seg + 1, 5120:6144], 128), q='pool')
                S.stt(G2[:], G2[:], 1.0, gn[:], ALU.add, ALU.mult)
            for ti in range(nt):
                t = t0 + ti
                S.dma(xblk[:, ti, :], D['xres'][t * 128:(t + 1) * 128, :])
                norm_mod_T(K, es, xblk[:, ti, :], G2, SH2, h2T[:, :, ti * 128:(ti + 1) * 128], tmp)
            if not dense:
                for ti in range(nt):
                    ps = K.bank()
                    for k in range(8):
                        S.mm(ps[:, 0:8], h2T[:, k, ti * 128:(ti + 1) * 128], rt[:, k, :], start=(k == 0), stop=(k == 7))
                    lg, eq, l2, ex = gsm[:, 0:8], gsm[:, 8:16], gsm[:, 16:24], gsm[:, 24:32]
                    m1, m2, nm1, sm, rs = gsm[:, 32:33], gsm[:, 33:34], gsm[:, 34:35], gsm[:, 35:36], gsm[:, 36:37]
                    gate = gsm[:, 40:48]
                    S.copy(lg, ps[:, 0:8])
                    S.reduce(m1, lg, ALU.max)
                    S.ts(eq, lg, m1, None, ALU.is_equal)
                    S.stt(l2, eq, -1e30, lg, ALU.mult, ALU.add)
                    S.reduce(m2, l2, ALU.max)
                    S.ts(eq, lg, m2, None, ALU.is_ge)
                    S.ts(nm1, m1, -1.0, None, ALU.mult)
                    S.act(ex, lg, AF.Exp, bias=nm1)
                    S.tt(ex, ex, eq, ALU.mult)
                    S.reduce(sm, ex, ALU.add)
                    S.recip(rs, sm)
                    S.ts(gate, ex, rs, None, ALU.mult)
                    ps2 = K.bank()
                    S.tr(ps2[0:8, 0:128], gate, K.ident[:])
                    S.copy(gateT[:, ti * 128:(ti + 1) * 128], ps2[0:8, 0:128])
                for e in range(8):
                    ps = K.bank()
                    S.mm(ps[:, :NB], sel[:, e, :], gateT[:, :NB])
                    S.copy(Gbc[:, e, :NB], ps[:, :NB], e='act')
            for e in range(E):
                for f in range(NF):
                    wg, wu = wgs[wi % 2], wus[wi % 2]
                    sg = sgs[wi % 2]
                    wi += 1
                    S.dma(r32(wg[:]), wg_v[e][:, :, f * 128:(f + 1) * 128], q='pool')
                    S.dma(r32(wu[:]), wu_v[e][:, :, f * 128:(f + 1) * 128], q='pool')
                    psg, psu = K.bank(), K.bank()
                    for k in range(8):
                        S.mm(psg[:, :NB], wg[:, k, :], h2T[:, k, :NB], start=(k == 0), stop=(k == 7), fast=True)
                    for k in range(8):
                        S.mm(psu[:, :NB], wu[:, k, :], h2T[:, k, :NB], start=(k == 0), stop=(k == 7), fast=True)
                    S.act(sg[:, :NB], psg[:, :NB], AF.Silu)
                    S.tt(r32(actT[:, f, :NB]), sg[:, :NB], psu[:, :NB], ALU.mult)
                    if not dense:
                        S.tt(r32(actT[:, f, :NB]), actT[:, f, :NB], Gbc[:, e, :NB], ALU.mult, e='pool')
                for cchunk in range(8):
                    wd = wds[cchunk % 2]
                    S.dma(r32(wd[:]), wd_v[e][:, :, cchunk * 128:(cchunk + 1) * 128], q='pool')
                    ps = K.bank()
                    for f in range(NF):
                        S.mm(ps[:, :NB], wd[:, f, :], actT[:, f, :NB], start=(f == 0), stop=(f == NF - 1), fast=True)
                    if e == 0:
                        S.copy(yT[:, cchunk, :NB], ps[:, :NB], e='act')
                    else:
                        S.tt(yT[:, cchunk, :NB], yT[:, cchunk, :NB], ps[:, :NB], ALU.add)
            for ti in range(nt):
                t = t0 + ti
                yt = tmp[0]
                for b in range(2):
                    ps = K.bank()
                    for j in range(4):
                        S.tr(ps[:, j * 128:(j + 1) * 128], yT[:, b * 4 + j, ti * 128:(ti + 1) * 128], K.ident[:])
                    S.tt(yt[:, b * 512:(b + 1) * 512], ps[:, :], GT2[:, b * 512:(b + 1) * 512], ALU.mult)
                S.tt(xblk[:, ti, :], xblk[:, ti, :], yt[:], ALU.add, e='pool')
                S.dma(D['xres'][t * 128:(t + 1) * 128, :], xblk[:, ti, :], q='act')
    S.barrier()


def stage_moe(K, l, last):
    S, W, D = K.S, K.W, K.D
    li = l // 2
    E, NF = 8, D_FFE // 128
    wg_v = [W['moe_w_gate'][li, e].rearrange("(k p) c -> p k c", p=128) for e in range(8)]
    wu_v = [W['moe_w_up'][li, e].rearrange("(k p) c -> p k c", p=128) for e in range(8)]
    wd_v = [W['moe_w_down'][li, e].rearrange("(f p) c -> p f c", p=128) for e in range(8)]
    blocks = ([] if last else [(0, 2)]) + [(2 + 8 * i, 8) for i in range(4)]
    with ExitStack() as es:
        gn = K.sb(es, [128, 1024])
        S.dma(gn[:], bc_rows(W['norm_ffn_g'][l:l + 1, :], 128))
        G2, SH2, GT2 = K.sb(es, [128, 1024]), K.sb(es, [128, 1024]), K.sb(es, [128, 1024])
        h2T = K.sb(es, [128, 8, 1024])
        actT = K.sb(es, [128, NF, 1024])
        yT = K.sb(es, [128, 8, 1024])
        xt = K.sb(es, [128, 1024])
        tmp = (K.sb(es, [128, 1024]), K.sb(es, [128, 4]), K.sb(es, [128, 1024]))
        sgs = [K.sb(es, [128, 512]) for _ in range(2)]
        wst = [K.sb(es, [128, 8, 128]) for _ in range(4)]
        wdst = [K.sb(es, [128, NF, 128]) for _ in range(2)]
        wgs = [K.sb(es, [128, 8, 128]) for _ in range(2)]
        wus = [K.sb(es, [128, 8, 128]) for _ in range(2)]
        wds = [K.sb(es, [128, NF, 128]) for _ in range(2)]
        Gbs = [K.sb(es, [128, 1024]) for _ in range(1)]
        rt = K.sb(es, [128, 8, 8])
        S.dma(rt[:], W['moe_router'][li].rearrange("(k p) e -> p k e", p=128))
        gateT = K.sb(es, [8, 1024])
        sel = K.sb(es, [8, 8, 128])
        S.dma(sel[:], W['c_sel8'])
        gsm = K.sb(es, [128, 64])
        cur_seg = None
        wi = 0
        wdi = 0
        for (t0, nt) in blocks:
            NB = nt * 128
            halves = [(0, min(512, NB))] + ([(512, NB - 512)] if NB > 512 else [])
            seg = 1 if t0 < 2 else 0
            if seg != cur_seg:
                cur_seg = seg
                S.dma(SH2[:], bc_rows(D['mod'][seg:seg + 1, 3072:4096], 128), q='act')
                S.dma(G2[:], bc_rows(D['mod'][seg:seg + 1, 4096:5120], 128), q='act')
                S.dma(GT2[:], bc_rows(D['mod'][seg:seg + 1, 5120:6144], 128), q='act')
                S.stt(G2[:], G2[:], 1.0, gn[:], ALU.add, ALU.mult)
            for ti in range(nt):
                t = t0 + ti
                S.dma(xt[:], D['xres'][t * 128:(t + 1) * 128, :], q='act')
                norm_mod_T(K, es, xt, G2, SH2, h2T[:, :, ti * 128:(ti + 1) * 128], tmp)
                ps = K.bank()
                for k in range(8):
                    S.mm(ps[:, 0:8], h2T[:, k, ti * 128:(ti + 1) * 128], rt[:, k, :], start=(k == 0), stop=(k == 7))
                lg, eq, l2, ex = gsm[:, 0:8], gsm[:, 8:16], gsm[:, 16:24], gsm[:, 24:32]
                m1, m2, nm1, sm, rs = gsm[:, 32:33], gsm[:, 33:34], gsm[:, 34:35], gsm[:, 35:36], gsm[:, 36:37]
                gate = gsm[:, 40:48]
                S.copy(lg, ps[:, 0:8])
                S.reduce(m1, lg, ALU.max)
                S.ts(eq, lg, m1, None, ALU.is_equal)
                S.stt(l2, eq, -1e30, lg, ALU.mult, ALU.add)
                S.reduce(m2, l2, ALU.max)
                S.ts(eq, lg, m2, None, ALU.is_ge)
                S.ts(nm1, m1, -1.0, None, ALU.mult)
                S.act(ex, lg, AF.Exp, bias=nm1)
                S.tt(ex, ex, eq, ALU.mult)
                S.reduce(sm, ex, ALU.add)
                S.recip(rs, sm)
                S.ts(gate, ex, rs, None, ALU.mult)
                ps2 = K.bank()
                S.tr(ps2[0:8, 0:128], gate, K.ident[:])
                S.copy(gateT[:, ti * 128:(ti + 1) * 128], ps2[0:8, 0:128])
            for e in range(E):
                Gb = Gbs[0]
                for (h0, hw) in halves:
                    ps = K.bank()
                    S.mm(ps[:, :hw], sel[:, e, :], gateT[:, h0:h0 + hw])
                    S.copy(Gb[:, h0:h0 + hw], ps[:, :hw], e='act')
                for f in range(NF):
                    wg, wu = wgs[wi % 2], wus[wi % 2]
                    s1, s2 = wst[(2 * wi) % 4], wst[(2 * wi + 1) % 4]
                    wi += 1
                    S.dma(s1[:], wg_v[e][:, :, f * 128:(f + 1) * 128], q='sp')
                    S.dma(s2[:], wu_v[e][:, :, f * 128:(f + 1) * 128], q='sp')
                    S.copy(r32(wg[:]), s1[:], e='act')
                    S.copy(r32(wu[:]), s2[:], e='dve')
                    for hi, (h0, hw) in enumerate(halves):
                        sg = sgs[hi]
                        psg, psu = K.bank(), K.bank()
                        for k in range(8):
                            S.mm(psg[:, :hw], wg[:, k, :], h2T[:, k, h0:h0 + hw], start=(k == 0), stop=(k == 7), fast=True)
                        for k in range(8):
                            S.mm(psu[:, :hw], wu[:, k, :], h2T[:, k, h0:h0 + hw], start=(k == 0), stop=(k == 7), fast=True)
                        S.act(sg[:, :hw], psg[:, :hw], AF.Silu)
                        S.tt(r32(actT[:, f, h0:h0 + hw]), sg[:, :hw], psu[:, :hw], ALU.mult)
                        S.tt(r32(actT[:, f, h0:h0 + hw]), actT[:, f, h0:h0 + hw], Gb[:, h0:h0 + hw], ALU.mult, e='pool')
                for cchunk in range(8):
                    wd, sd = wds[wdi % 2], wdst[wdi % 2]
                    wdi += 1
                    S.dma(sd[:], wd_v[e][:, :, cchunk * 128:(cchunk + 1) * 128], q='sp')
                    S.copy(r32(wd[:]), sd[:], e='pool')
                    for (h0, hw) in halves:
                        ps = K.bank()
                        for f in range(NF):
                            S.mm(ps[:, :hw], wd[:, f, :], actT[:, f, h0:h0 + hw], start=(f == 0), stop=(f == NF - 1), fast=True)
                        if e == 0:
                            S.copy(yT[:, cchunk, h0:h0 + hw], ps[:, :hw], e='act')
                        else:
                            S.tt(yT[:, cchunk, h0:h0 + hw], yT[:, cchunk, h0:h0 + hw], ps[:, :hw], ALU.add)
            for ti in range(nt):
                t = t0 + ti
                yt = tmp[0]
                S.dma(xt[:], D['xres'][t * 128:(t + 1) * 128, :], q='act')
                for b in range(2):
                    ps = K.bank()
                    for j in range(4):
                        S.tr(ps[:, j * 128:(j + 1) * 128], yT[:, b * 4 + j, ti * 128:(ti + 1) * 128], K.ident[:])
                    S.tt(yt[:, b * 512:(b + 1) * 512], ps[:, :], GT2[:, b * 512:(b + 1) * 512], ALU.mult)
                S.tt(tmp[2][:], xt[:], yt[:], ALU.add, e='pool')
                S.dma(D['xres'][t * 128:(t + 1) * 128, :], tmp[2][:], q='act')
    S.barrier()


def stage_final(K):
    S, W, D = K.S, K.W, K.D
    with ExitStack() as es:
        g = K.sb(es, [128, 1024])
        S.dma(g[:], bc_rows(W['final_norm_g'], 128))
        xts = [K.sb(es, [128, 1024]) for _ in range(2)]
        ots = [K.sb(es, [128, 1024]) for _ in range(2)]
        junk = K.sb(es, [128, 1024])
        sss = [K.sb(es, [128, 4]) for _ in range(2)]
        for t in range(2, NT):
            xt, ot, ss = xts[t % 2], ots[t % 2], sss[t % 2]
            S.dma(xt[:], D['xres'][t * 128:(t + 1) * 128, :])
            S.act(junk[:], xt[:], AF.Square, accum_out=ss[:, 0:1])
            S.ts(ss[:, 1:2], ss[:, 0:1], 1.0 / D_MODEL, EPS, ALU.mult, ALU.add)
            S.act(ss[:, 2:3], ss[:, 1:2], AF.Sqrt)
            S.recip(ss[:, 3:4], ss[:, 2:3])
            S.stt(ot[:], xt[:], ss[:, 3:4], g[:], ALU.mult, ALU.mult)
            S.dma(K.out[(t - 2) * 128:(t - 1) * 128, :], ot[:], q='pool')
    S.barrier()


def mix_identity(K, l):
    S, D = K.S, K.D
    with ExitStack() as es:
        pts = [K.sb(es, [128, 8, 128]) for _ in range(2)]
        yts = [K.sb(es, [128, 1024]) for _ in range(2)]
        pv = D['pT'][0:1024, :].rearrange("(k p) t -> p k t", p=128)
        for t in range(NT):
            pt, yt = pts[t % 2], yts[t % 2]
            S.dma(pt[:], pv[:, :, t * 128:(t + 1) * 128])
            for b in range(2):
                ps = K.bank()
                for j in range(4):
                    S.tr(ps[:, j * 128:(j + 1) * 128], pt[:, b * 4 + j, :], K.ident[:])
                S.copy(yt[:, b * 512:(b + 1) * 512], ps[:, :], e=('dve' if b == 0 else 'act'))
            S.dma(D['y'][t * 128:(t + 1) * 128, :], yt[:], q='pool')
    S.barrier()


def to_token_major(K, es, src, ncols_tile, dst_cols, bufs):
    S, D = K.S, K.D
    gi = 0
    for t0 in range(0, NT, 4):
        n = min(4, NT - t0)
        ps = K.bank()
        for j in range(n):
            S.tr(ps[:, j * 128:(j + 1) * 128], src[:, (t0 + j) * 128:(t0 + j + 1) * 128], K.ident[:])
        yb = bufs[gi % 2]
        S.copy(yb[:, :n * 128], ps[:, :n * 128], e=('dve' if gi % 2 == 0 else 'act'))
        dst = D['y'][t0 * 128:(t0 + n) * 128, dst_cols[0]:dst_cols[1]].rearrange("(j p) c -> p j c", p=128)
        S.dma(dst, yb[:, :n * 128].rearrange("p (j c) -> p j c", j=n), q=('sp' if gi % 2 == 0 else 'pool'))
        gi += 1


def mixer_a(K, l, last):
    S, W, D = K.S, K.W, K.D
    SEGS = [(0, CTX), (CTX, T)]
    with ExitStack() as es0:
        ybufs = [K.sb(es0, [128, 512]) for _ in range(2)]
        for ct in range(2):
            with ExitStack() as es:
                pk = K.sb(es, [128, 16])
                S.dma(pk[:, 0:11], W['pk_lru'][l, ct])
                S.act(pk[:, 11:13], pk[:, 9:11], AF.Exp, scale=-1.0)
                S.act(pk[:, 11:13], pk[:, 11:13], AF.Ln, bias=1.0)
                S.ts(pk[:, 13:15], pk[:, 11:13], -16.0, None, ALU.mult)
                S.ts(pk[:, 11:13], pk[:, 11:13], -8.0, None, ALU.mult)
                wbd = K.sb(es, [128, 4, 128])
                S.memset(wbd[:], 0.0)
                for d in range(2):
                    for wi, wn in enumerate(('lru_w_a', 'lru_w_x')):
                        for hl in range(2):
                            S.dma(wbd[hl * 64:(hl + 1) * 64, d * 2 + wi, hl * 64:(hl + 1) * 64], W[wn][l, d, ct * 2 + hl], q='pool')
                xb = K.sb(es, [128, T])
                u = K.sb(es, [128, T])
                gt = K.sb(es, [128, T])
                ra = K.sb(es, [128, T])
                ib = K.sb(es, [128, T])
                h0 = K.sb(es, [128, T])
                h1 = K.sb(es, [128, T])
                S.dma(xb[:], D['pT'][ct * 128:(ct + 1) * 128, :])
                S.dma(gt[:], D['pT'][256 + ct * 128:256 + (ct + 1) * 128, :], q='pool')
                S.ts(u[:], xb[:], pk[:, 2:3], pk[:, 4:5], ALU.mult, ALU.add)
                for (a, b) in SEGS:
                    for j, s in ((0, -2), (1, -1), (3, 1)):
                        lo, hi = max(a, a - s), min(b, b - s)
                        S.stt(u[:, lo:hi], xb[:, lo + s:hi + s], pk[:, j:j + 1], u[:, lo:hi], ALU.mult, ALU.add)
                S.tt(xb[:], gt[:], gt[:], ALU.mult, e='pool')
                S.ts(xb[:], xb[:], 0.044715, 1.0, ALU.mult, ALU.add, e='pool')
                S.tt(xb[:], xb[:], gt[:], ALU.mult, e='pool')
                S.act(xb[:], xb[:], AF.Sigmoid, scale=1.5957691216057308)
                S.tt(gt[:], gt[:], xb[:], ALU.mult, e='pool')
                for d in range(2):
                    blocks = [(n0, min(512, T - n0)) for n0 in range(0, T, 512)]
                    for wi, dst, bcol in ((0, ra, 5 + d), (1, ib, 7 + d)):
                        for (n0, nw) in blocks:
                            ps = K.bank()
                            S.mm(ps[:, :nw], wbd[:, d * 2 + wi, :], u[:, n0:n0 + nw])
                            S.act(dst[:, n0:n0 + nw], ps[:, :nw], AF.Sigmoid, bias=pk[:, bcol:bcol + 1])
                    hd = h0 if d == 0 else h1
                    S.tt(ib[:], ib[:], u[:], ALU.mult, e='pool')
                    S.act(hd[:], ra[:], AF.Exp, scale=pk[:, 13 + d:14 + d])
                    S.ts(hd[:], hd[:], -1.0, 1.0, ALU.mult, ALU.add)
                    S.act(hd[:], hd[:], AF.Sqrt)
                    S.tt(ib[:], ib[:], hd[:], ALU.mult)
                    S.act(ra[:], ra[:], AF.Exp, scale=pk[:, 11 + d:12 + d])
                    if d == 0:
                        S.scan(h0[:], ra[:], ib[:], 0.0, ALU.mult, ALU.add)
                    else:
                        S.scan(h1[:, 0:CTX][:, ::-1], ra[:, 0:CTX][:, ::-1], ib[:, 0:CTX][:, ::-1], 0.0, ALU.mult, ALU.add)
                        S.scan(h1[:, CTX:T][:, ::-1], ra[:, CTX:T][:, ::-1], ib[:, CTX:T][:, ::-1], h1[:, 0:1], ALU.mult, ALU.add)
                S.tt(h0[:], h0[:], h1[:], ALU.add, e='pool')
                S.tt(h0[:], h0[:], gt[:], ALU.mult)
                to_token_major(K, es, h0, 128, (ct * 128, (ct + 1) * 128), ybufs)
            S.barrier()
    S.barrier()


def chunk_cols(n):
    if n < 4:
        return slice(n * 64, (n + 1) * 64)
    c = n - 4
    return slice(CTX + c, T, 64)


NCH = T // 64
DBG = {}


def to_traversal(S, dst, src, e='dve'):
    S.copy(dst[:, 0:CTX], src[:, 0:CTX], e=e)
    S.copy(dst[:, CTX:T].rearrange("p (c r) -> p c r", r=64), src[:, CTX:T].rearrange("p (r c) -> p c r", c=64), e=e)


def scan_order(d):
    if d == 0:
        return list(range(NCH))
    return [3, 2, 1, 0] + list(range(NCH - 1, 3, -1))


def mixer_c(K, l, last):
    S, W, D = K.S, K.W, K.D
    base = OFF_C
    NQ = 6
    with ExitStack() as es0:
        tokcol = K.sb(es0, [64, NCH, NQ * 8])
        masks = K.sb(es0, [64, 2, 64])
        S.dma(masks[:], W['c_masks'])
        ngt = K.sb(es0, [64, 256])
        S.dma(ngt[:], bc_rows(W['mlstm_norm_g'][l:l + 1, :], 64))
        with ExitStack() as es:
            stack = K.sb(es, [NQ * 8, T])
            pk = K.sb(es, [4, 8])
            S.dma(pk[:, 0:4], W['pk_ml'][l])
            S.ts(pk[:, 4:8], pk[:, 0:4], -1.0, None, ALU.mult)
            rst = K.sb(es, [4, T])
            nbg = K.sb(es, [4, T])
            graw = K.sb(es, [4, T])
            li = K.sb(es, [4, T])
            lf = K.sb(es, [4, T])
            bb = K.sb(es, [4, T])
            gg = K.sb(es, [4, T])
            cm = K.sb(es, [4, T])
            mx = K.sb(es, [4, T])
            tmp = graw
            sm = K.sb(es, [4, 8, NCH])
            v3 = lambda t: t[:, :].rearrange("p (n i) -> p n i", i=64)
            bcn = lambda a: a.unsqueeze(2).to_broadcast([4, NCH, 64])
            for d in range(2):
                rv = (lambda a: a) if d == 0 else (lambda a: a[:, ::-1])
                lastidx = 63 if d == 0 else 0
                S.dma(rst[:], W['c_rst'][:, d, :], q='pool')
                S.ts(nbg[:], rst[:], 1e30, -1e30, ALU.mult, ALU.add)
                S.dma(graw[:], D['pT'][base + 1024 + d * 4: base + 1024 + d * 4 + 4, :])
                to_traversal(S, li, graw)
                S.ts(li[:], li[:], pk[:, d:d + 1], None, ALU.add)
                S.dma(graw[:], D['pT'][base + 1024 + 8 + d * 4: base + 1024 + 8 + d * 4 + 4, :])
                to_traversal(S, lf, graw)
                S.act(lf[:], lf[:], AF.Exp, scale=-1.0, bias=pk[:, 6 + d:7 + d])
                S.act(lf[:], lf[:], AF.Ln, bias=1.0)
                S.ts(lf[:], lf[:], -1.0, None, ALU.mult)
                S.scan(rv(bb[:, :]), rv(rst[:, :]), rv(lf[:, :]), 0.0, ALU.mult, ALU.add)
                S.tt(gg[:], li[:], bb[:], ALU.subtract)
                S.scan(rv(cm[:, :]), rv(nbg[:, :]), rv(gg[:, :]), 0.0, ALU.add, ALU.max)
                bL, cmL = v3(bb)[:, :, lastidx], v3(cm)[:, :, lastidx]
                d1t, mm_, mprev, e4, t5 = sm[:, 0, :], sm[:, 1, :], sm[:, 2, :], sm[:, 3, :], sm[:, 4, :]
                S.tt(d1t, bL, cmL, ALU.add)
                if d == 0:
                    S.scan(mm_, bL, d1t, 0.0, ALU.add, ALU.max)
                    S.memset(mprev[:, 0:1], 0.0)
                    S.copy(mprev[:, 1:NCH], mm_[:, 0:NCH - 1])
                else:
                    S.scan(mm_[:, 0:4][:, ::-1], bL[:, 0:4][:, ::-1], d1t[:, 0:4][:, ::-1], 0.0, ALU.add, ALU.max)
                    S.scan(mm_[:, 4:NCH][:, ::-1], bL[:, 4:NCH][:, ::-1], d1t[:, 4:NCH][:, ::-1], mm_[:, 0:1], ALU.add, ALU.max)
                    S.copy(mprev[:, 0:3], mm_[:, 1:4])
                    S.memset(mprev[:, 3:4], 0.0)
                    S.copy(mprev[:, 4:NCH - 1], mm_[:, 5:NCH])
                    S.copy(mprev[:, NCH - 1:NCH], mm_[:, 0:1])
                S.tt(v3(mx), v3(cm), bcn(mprev), ALU.max)
                def put(q, src):
                    r0 = q * 8 + d * 4
                    S.dma(stack[r0:r0 + 4, :], src, q='pool')
                S.act(tmp[:], mx[:], AF.Exp, scale=-1.0)
                put(0, tmp[:])
                S.tt(v3(tmp), bcn(mprev), v3(mx), ALU.subtract)
                S.act(tmp[:], tmp[:], AF.Exp)
                put(1, tmp[:])
                S.tt(tmp[:], bb[:], mx[:], ALU.add)
                S.act(tmp[:], tmp[:], AF.Exp, scale=-1.0)
                put(2, tmp[:])
                S.act(tmp[:], gg[:], AF.Exp)
                put(3, tmp[:])
                S.tt(e4, bL, mprev, ALU.add)
                S.tt(e4, e4, mm_, ALU.subtract)
                S.act(e4, e4, AF.Exp)
                S.copy(v3(tmp), bcn(e4))
                put(4, tmp[:])
                S.tt(t5, bL, mm_, ALU.subtract)
                S.tt(v3(tmp), v3(gg), bcn(t5), ALU.add)
                S.act(tmp[:], tmp[:], AF.Exp)
                put(5, tmp[:])
            NR = NQ * 8
            for n0 in range(0, NCH, 8):
                nn = min(8, NCH - n0)
                ps = K.bank()
                for j in range(nn):
                    S.tr(ps[0:64, j * NR:(j + 1) * NR], stack[0:NR, (n0 + j) * 64:(n0 + j + 1) * 64], K.ident[0:NR, 0:NR])
                S.copy(tokcol[:, n0:n0 + nn, :], ps[0:64, 0:nn * NR].rearrange("p (j c) -> p j c", c=NR))
        S.barrier()
        for h in range(4):
            with ExitStack() as es:
                qT = K.sb(es, [64, T])
                kT = K.sb(es, [64, T])
                vT = K.sb(es, [64, T])
                ktok = K.sb(es, [64, NCH, 64])
                vtok = K.sb(es, [64, NCH, 65])
                hacc = K.sb(es, [64, NCH, 64])
                osig = K.sb(es, [64, NCH, 64])
                S.dma(qT[:], D['pT'][base + h * 64: base + (h + 1) * 64, :])
                S.dma(kT[:], D['pT'][base + 256 + h * 64: base + 256 + (h + 1) * 64, :], q='pool')
                S.dma(vT[:], D['pT'][base + 512 + h * 64: base + 512 + (h + 1) * 64, :], q='act')
                S.ts(kT[:], kT[:], 0.125, None, ALU.mult, e='pool')
                S.memset(vtok[:, :, 64:65], 1.0)
                for n0 in range(0, NCH, 8):
                    nn = min(8, NCH - n0)
                    ps1, ps2 = K.bank(), K.bank()
                    for j in range(nn):
                        cs = chunk_cols(n0 + j)
                        S.tr(ps1[0:64, j * 64:(j + 1) * 64], kT[:, cs], K.ident[0:64, 0:64])
                        S.tr(ps2[0:64, j * 64:(j + 1) * 64], vT[:, cs], K.ident[0:64, 0:64])
                    S.copy(ktok[:, n0:n0 + nn, :], ps1[0:64, 0:nn * 64].rearrange("p (j c) -> p j c", c=64), e='act')
                    S.copy(vtok[:, n0:n0 + nn, 0:64], ps2[0:64, 0:nn * 64].rearrange("p (j c) -> p j c", c=64))
                S.dma(vT[:], D['pT'][base + 768 + h * 64: base + 768 + (h + 1) * 64, :], q='act')
                for d in range(2):
                    col = lambda q: (lambda n: tokcol[:, n, q * 8 + d * 4 + h: q * 8 + d * 4 + h + 1])
                    c1, c2, c3, eg, c4, wn = [col(q) for q in range(6)]
                    Cs = [K.sb(es, [64, 65]) for _ in range(2)]
                    S.memset(Cs[0][:], 0.0)
                    pts = [K.sb(es, [64, 64]) for _ in range(2)]
                    tts = [K.sb(es, [64, 65]) for _ in range(2)]
                    vws = [K.sb(es, [64, 65]) for _ in range(2)]
                    dns = [K.sb(es, [64, 2]) for _ in range(2)]
                    for si, n in enumerate(scan_order(d)):
                        cs = chunk_cols(n)
                        Cc, Cn = Cs[si % 2], Cs[(si + 1) % 2]
                        pt, tot, vw, dn = pts[si % 2], tts[si % 2], vws[si % 2], dns[si % 2]
                        ps_s, ps_o, ps_i, ps_c = K.bank(), K.bank(), K.bank(), K.bank()
                        S.mm(ps_s[0:64, 0:64], kT[:, cs], qT[:, cs])
                        S.stt(pt[:], ps_s[0:64, 0:64], eg(n), masks[:, d, :], ALU.mult, ALU.mult)
                        S.mm(ps_o[0:64, 0:65], pt[:], vtok[:, n, :])
                        S.mm(ps_i[0:64, 0:65], qT[:, cs], Cc[:])
                        S.ts(tot[:], ps_o[0:64, 0:65], c1(n), None, ALU.mult)
                        S.stt(tot[:], ps_i[0:64, 0:65], c2(n), tot[:], ALU.mult, ALU.add)
                        S.act(dn[:, 0:1], tot[:, 64:65], AF.Abs)
                        S.ts(dn[:, 0:1], dn[:, 0:1], c3(n), None, ALU.max)
                        S.recip(dn[:, 1:2], dn[:, 0:1])
                        if d == 0:
                            S.ts(hacc[:, n, :], tot[:, 0:64], dn[:, 1:2], None, ALU.mult)
                        else:
                            S.stt(hacc[:, n, :], tot[:, 0:64], dn[:, 1:2], hacc[:, n, :], ALU.mult, ALU.add)
                        S.ts(vw[:], vtok[:, n, :], wn(n), None, ALU.mult, e='pool')
                        S.mm(ps_c[0:64, 0:65], ktok[:, n, :], vw[:])
                        S.stt(Cn[:], Cc[:], c4(n), ps_c[0:64, 0:65], ALU.mult, ALU.add)
                for n0 in range(0, NCH, 8):
                    nn = min(8, NCH - n0)
                    ps1 = K.bank()
                    for j in range(nn):
                        S.tr(ps1[0:64, j * 64:(j + 1) * 64], vT[:, chunk_cols(n0 + j)], K.ident[0:64, 0:64])
                    S.act(osig[:, n0:n0 + nn, :], ps1[0:64, 0:nn * 64].rearrange("p (j c) -> p j c", c=64), AF.Sigmoid)
                sq = K.sb(es, [64, NCH, 64])
                ssq = K.sb(es, [64, NCH, 4])
                S.tt(sq[:], hacc[:], hacc[:], ALU.mult, e='pool')
                S.reduce(ssq[:, :, 0], sq[:], ALU.add)
                S.ts(ssq[:, :, 1], ssq[:, :, 0], 1.0 / 64, EPS, ALU.mult, ALU.add)
                S.act(ssq[:, :, 2], ssq[:, :, 1], AF.Sqrt)
                S.recip(ssq[:, :, 3], ssq[:, :, 2])
                S.tt(hacc[:], hacc[:], ssq[:, :, 3].unsqueeze(2).to_broadcast([64, NCH, 64]), ALU.mult)
                S.tt(hacc[:], hacc[:], ngt[:, h * 64:(h + 1) * 64].unsqueeze(1).to_broadcast([64, NCH, 64]), ALU.mult, e='pool')
                S.tt(hacc[:], hacc[:], osig[:], ALU.mult)
                c0 = 512 + h * 64
                S.dma(D['y'][0:CTX, c0:c0 + 64].rearrange("(n i) c -> i n c", i=64), hacc[:, 0:4, :])
                yv = D['y'][CTX:T, c0:c0 + 64].rearrange("(r c) ch -> r c ch", c=64)
                for g4 in range(4):
                    S.dma(yv[:, g4 * 16:(g4 + 1) * 16, :], hacc[:, 4 + g4 * 16:4 + (g4 + 1) * 16, :], q=('sp', 'pool')[g4 % 2])
            S.barrier()
    S.barrier()


def dwconv_trav(S, out, x, wcol, bias=None):
    if bias is None:
        S.ts(out[:], x[:], wcol(2), None, ALU.mult)
    else:
        S.ts(out[:], x[:], wcol(2), bias, ALU.mult, ALU.add)
    for (a, b) in ((0, CTX), (CTX, T)):
        for j, s in ((0, -2), (1, -1), (3, 1)):
            lo, hi = max(a, a - s), min(b, b - s)
            S.stt(out[:, lo:hi], x[:, lo + s:hi + s], wcol(j), out[:, lo:hi], ALU.mult, ALU.add)


def neumann_inverse(K, S, P, PT, B, BT, tmps):
    B2s, B2Ts = tmps
    cb, cbt = B, BT
    for m in range(1, 6):
        lastm = (m == 5)
        ps1 = K.bank()
        S.mm(ps1[:, 0:128], cbt[:], cb[:])
        nb = B2s[m % 2]
        S.copy(nb[:], ps1[:, 0:128], e='act')
        if not lastm:
            ps2 = K.bank()
            S.mm(ps2[:, 0:128], cb[:], cbt[:])
            nbt = B2Ts[m % 2]
            S.copy(nbt[:], ps2[:, 0:128], e='dve')
        ps3 = K.bank()
        S.mm(ps3[:, 0:128], PT[:], nb[:])
        if not lastm:
            ps4 = K.bank()
            S.mm(ps4[:, 0:128], nb[:], PT[:])
        S.tt(P[:], P[:], ps3[:, 0:128], ALU.add)
        if not lastm:
            S.tt(PT[:], PT[:], ps4[:, 0:128], ALU.add)
            cb, cbt = nb, nbt


def run_pipeline(n, prep_gen, rec_gen, sets):
    NS = len(sets)
    preps = {}
    done = set()
    next_prep = 0
    rec_i = 0
    rec = None
    while rec_i < n:
        while next_prep < n and next_prep < rec_i + NS:
            preps[next_prep] = prep_gen(next_prep, sets[next_prep % NS])
            next_prep += 1
        if rec is None and rec_i in done:
            rec = rec_gen(rec_i, sets[rec_i % NS])
        if rec is not None:
            try:
                next(rec)
            except StopIteration:
                rec = None
                rec_i += 1
                continue
        for j in sorted(preps):
            try:
                next(preps[j])
            except StopIteration:
                del preps[j]
                done.add(j)


NEU_FAST = False


def n32(ap):
    return ap.bitcast(F32R) if (NEU_FAST and FAST_MM) else ap


def neumann_gen(K, S, PP, BB, N2):
    cb, cbt = BB[:, 0, :], BB[:, 1, :]
    for m in range(1, 6):
        lastm = (m == 5)
        nbb = N2[m % 2]
        psa = K.bank()
        S.mm(psa[:, 0:128], cbt, cb, fast=NEU_FAST)
        if not lastm:
            S.mm(psa[:, 128:256], cb, cbt, fast=NEU_FAST)
        yield
        if lastm:
            S.copy(n32(nbb[:, 0, :]), psa[:, 0:128], e='act')
        else:
            S.copy(n32(nbb[:]), psa[:, 0:256].rearrange("p (a c) -> p a c", a=2), e='act')
        yield
        psb = K.bank()
        S.mm(psb[:, 0:128], PP[:, 1, :], nbb[:, 0, :], fast=NEU_FAST)
        if not lastm:
            S.mm(psb[:, 128:256], nbb[:, 0, :], PP[:, 1, :], fast=NEU_FAST)
        yield
        if lastm:
            S.tt(n32(PP[:, 0, :]), PP[:, 0, :], psb[:, 0:128], ALU.add)
        else:
            S.tt(n32(PP[:]), PP[:], psb[:, 0:256].rearrange("p (a c) -> p a c", a=2), ALU.add)
        yield
        cb, cbt = nbb[:, 0, :], nbb[:, 1, :]


def mixer_d(K, l, last):
    S, W, D = K.S, K.W, K.D
    base = OFF_D
    NQ = 6
    NR = NQ * 8
    NP = NCH // 2
    with ExitStack() as es0:
        tok64 = K.sb(es0, [64, NCH, NR])
        tokP = K.sb(es0, [128, NP, NR])
        stack = K.sb(es0, [NR, T])
        masks = K.sb(es0, [64, 2, 64])
        S.dma(masks[:], W['c_masks'])
        m128 = K.sb(es0, [128, 4, 128])
        S.dma(m128[:], W['c_m128'])
        sel = K.sb(es0, [8, 8, 128])
        S.dma(sel[:], W['c_sel8'])
        ngt = K.sb(es0, [64, 256])
        S.dma(ngt[:], bc_rows(W['gdn_norm_g'][l:l + 1, :], 64))
        with ExitStack() as es:
            pk = K.sb(es, [4, 8])
            S.dma(pk[:, 0:4], W['pk_gd'][l])
            S.act(pk[:, 4:6], pk[:, 0:2], AF.Exp)
            S.ts(pk[:, 4:6], pk[:, 4:6], -1.0, None, ALU.mult)
            rst = K.sb(es, [4, T])
            graw = K.sb(es, [4, T])
            la = K.sb(es, [4, T])
            bt = K.sb(es, [4, T])
            gam = K.sb(es, [4, T])
            tmp = K.sb(es, [4, T])
            sm = K.sb(es, [4, 4, NCH])
            v3 = lambda t: t[:, :].rearrange("p (n i) -> p n i", i=64)
            bcn = lambda a: a.unsqueeze(2).to_broadcast([4, NCH, 64])
            for d in range(2):
                rv = (lambda a: a) if d == 0 else (lambda a: a[:, ::-1])
                lastidx = 63 if d == 0 else 0
                S.dma(rst[:], W['c_rst'][:, d, :], q='pool')
                S.dma(graw[:], D['pT'][base + 1024 + d * 4: base + 1024 + d * 4 + 4, :])
                to_traversal(S, la, graw)
                S.act(la[:], la[:], AF.Exp, bias=pk[:, 2 + d:3 + d])
                S.act(la[:], la[:], AF.Ln, bias=1.0)
                S.ts(la[:], la[:], pk[:, 4 + d:5 + d], None, ALU.mult)
                S.dma(graw[:], D['pT'][base + 1024 + 8 + d * 4: base + 1024 + 8 + d * 4 + 4, :])
                to_traversal(S, bt, graw)
                S.act(bt[:], bt[:], AF.Sigmoid)
                S.scan(rv(gam[:, :]), rv(rst[:, :]), rv(la[:, :]), 0.0, ALU.mult, ALU.add)
                gL = v3(gam)[:, :, lastidx]
                def put(q, src):
                    r0 = q * 8 + d * 4
                    S.dma(stack[r0:r0 + 4, :], src, q='pool')
                put(0, gam[:])
                put(1, bt[:])
                S.act(tmp[:], gam[:], AF.Exp)
                S.tt(tmp[:], tmp[:], bt[:], ALU.mult)
                put(2, tmp[:])
                S.tt(v3(tmp), bcn(gL), v3(gam), ALU.subtract)
                S.act(tmp[:], tmp[:], AF.Exp)
                put(3, tmp[:])
                S.act(sm[:, 0, :], gL, AF.Exp)
                S.copy(v3(tmp), bcn(sm[:, 0, :]))
                put(4, tmp[:])
                S.ts(tmp[:], bt[:], -1.0, None, ALU.mult)
                put(5, tmp[:])
            for n0 in range(0, NCH, 8):
                nn = min(8, NCH - n0)
                ps = K.bank()
                for j in range(nn):
                    S.tr(ps[0:64, j * NR:(j + 1) * NR], stack[0:NR, (n0 + j) * 64:(n0 + j + 1) * 64], K.ident[0:NR, 0:NR])
                S.copy(tok64[:, n0:n0 + nn, :], ps[0:64, 0:nn * NR].rearrange("p (j c) -> p j c", c=NR))
            for n0 in range(0, NP, 8):
                nn = min(8, NP - n0)
                ps = K.bank()
                for j in range(nn):
                    S.tr(ps[:, j * NR:(j + 1) * NR], stack[0:NR, (n0 + j) * 128:(n0 + j + 1) * 128], K.ident[0:NR, 0:NR])
                S.copy(tokP[:, n0:n0 + nn, :], ps[:, 0:nn * NR].rearrange("p (j c) -> p j c", c=NR), e='act')
        S.barrier()
        if DBG.get('d_stop') == 1:
            return
        for h in range(DBG.get('d_heads', 4)):
            with ExitStack() as es:
                raw = K.sb(es, [64, T])
                trv = K.sb(es, [64, T])
                qT = K.sb(es, [64, T])
                kT = K.sb(es, [64, T])
                vT = K.sb(es, [64, T])
                ktok = K.sb(es, [64, NCH, 64])
                kP = K.sb(es, [128, NP, 64])
                vP = K.sb(es, [128, NP, 64])
                hacc = K.sb(es, [64, NCH, 64])
                cw = K.sb(es, [64, 3, 4])
                ones = K.sb(es, [64, 64])
                S.memset(ones[:], 1.0)
                S.dma(cw[:], W['pk_gdc'][l, h])
                for gi, dst in enumerate((qT, kT, vT)):
                    S.dma(raw[:], D['pT'][base + gi * 256 + h * 64: base + gi * 256 + (h + 1) * 64, :])
                    to_traversal(S, trv, raw, e='pool')
                    dwconv_trav(S, dst, trv, lambda j: cw[:, gi, j:j + 1])
                    S.act(dst[:], dst[:], AF.Silu)
                    if gi < 2:
                        S.tt(trv[:], dst[:], dst[:], ALU.mult, e='pool')
                        for n0 in range(0, T, 512):
                            nw = min(512, T - n0)
                            ps = K.bank()
                            S.mm(ps[0:64, :nw], ones[:], trv[:, n0:n0 + nw])
                            S.ts(raw[:, n0:n0 + nw], ps[0:64, :nw], EPS, None, ALU.add)
                        S.act(raw[:], raw[:], AF.Sqrt)
                        S.recip(raw[:], raw[:])
                        if gi == 0:
                            S.stt(dst[:], dst[:], 0.125, raw[:], ALU.mult, ALU.mult)
                        else:
                            S.tt(dst[:], dst[:], raw[:], ALU.mult)
                for n0 in range(0, NCH, 8):
                    nn = min(8, NCH - n0)
                    ps1 = K.bank()
                    for j in range(nn):
                        S.tr(ps1[0:64, j * 64:(j + 1) * 64], kT[:, (n0 + j) * 64:(n0 + j + 1) * 64], K.ident[0:64, 0:64])
                    S.copy(ktok[:, n0:n0 + nn, :], ps1[0:64, 0:nn * 64].rearrange("p (j c) -> p j c", c=64), e='act')
                for n0 in range(0, NP, 8):
                    nn = min(8, NP - n0)
                    ps1, ps2 = K.bank(), K.bank()
                    for j in range(nn):
                        S.tr(ps1[:, j * 64:(j + 1) * 64], kT[:, (n0 + j) * 128:(n0 + j + 1) * 128], K.ident[0:64, 0:64])
                        S.tr(ps2[:, j * 64:(j + 1) * 64], vT[:, (n0 + j) * 128:(n0 + j + 1) * 128], K.ident[0:64, 0:64])
                    S.copy(kP[:, n0:n0 + nn, :], ps1[:, 0:nn * 64].rearrange("p (j c) -> p j c", c=64), e='act')
                    S.copy(vP[:, n0:n0 + nn, :], ps2[:, 0:nn * 64].rearrange("p (j c) -> p j c", c=64))
                if DBG.get('d_stop') == 2:
                    S.barrier()
                    return
                S.dma(raw[:], D['pT'][base + 768 + h * 64: base + 768 + (h + 1) * 64, :])
                kdec = trv
                kdec3 = kdec[:, :].rearrange("p (n c) -> p n c", c=64)
                NS = DBG.get('d_ns', 3)
                sets = []
                for _s in range(NS):
                    sets.append(dict(
                        GB=K.sb(es, [128, 128]), dL=K.sb(es, [128, 128]), BB=K.sb(es, [128, 2, 128]),
                        PP=K.sb(es, [128, 2, 128]), N2=[K.sb(es, [128, 2, 128]) for _ in range(2)],
                        rU=K.sb(es, [128, 64]), rW=K.sb(es, [128, 64]), wT=K.sb(es, [64, 128]),
                        us=K.sb(es, [64, 2, 64]), qk=K.sb(es, [64, 2, 64]), qd=K.sb(es, [64, 128])))
                vns = [K.sb(es, [64, 64]) for _ in range(2)]
                Ss = [K.sb(es, [64, 64]) for _ in range(2)]
                identB = K.ident[:, :].unsqueeze(1).to_broadcast([128, 2, 128])
                for d in range(2):
                    cP = lambda q, pi: tokP[:, pi, q * 8 + d * 4 + h: q * 8 + d * 4 + h + 1]
                    c64 = lambda q, n: tok64[:, n, q * 8 + d * 4 + h: q * 8 + d * 4 + h + 1]
                    S.tt(kdec3, ktok[:], tok64[:, :, 3 * 8 + d * 4 + h].unsqueeze(2).to_broadcast([64, NCH, 64]), ALU.mult, e='pool')
                    S.memset(Ss[0][:], 0.0)
                    order = scan_order(d)
                    pairs = [order[i] // 2 for i in range(0, NCH, 2)]
                    state = {'si': 0}

                    def prep(pidx, st):
                        pi = pairs[pidx]
                        GB, dL, BB, PP = st['GB'], st['dL'], st['BB'], st['PP']
                        tk = slice(pi * 128, (pi + 1) * 128)
                        ps = K.bank()
                        S.mm(ps[:, 0:128], sel[:, d * 4 + h, :], stack[0:8, tk])
                        yield
                        S.copy(GB[:], ps[:, 0:128], e='act')
                        ps = K.bank()
                        S.mm(ps[:, 0:128], kT[:, tk], kT[:, tk])
                        yield
                        S.ts(dL[:], GB[:], cP(0, pi), 0.0, ALU.subtract, ALU.max)
                        yield
                        S.act(dL[:], dL[:], AF.Exp, scale=-1.0)
                        yield
                        S.tt(dL[:], dL[:], m128[:, 3 - d, :], ALU.mult, e='pool')
                        yield
                        S.stt(n32(BB[:, 1, :]), ps[:, 0:128], cP(5, pi), dL[:], ALU.mult, ALU.mult)
                        yield
                        ps = K.bank()
                        S.tr(ps[:, 0:128], BB[:, 1, :], K.ident[:])
                        yield
                        S.copy(n32(BB[:, 0, :]), ps[:, 0:128], e='act')
                        yield
                        S.tt(n32(PP[:]), BB[:], identB, ALU.add, e='pool')
                        yield
                        yield from neumann_gen(K, S, PP, BB, st['N2'])
                        P = PP[:, 0, :]
                        S.ts(st['rU'][:], vP[:, pi, :], cP(1, pi), None, ALU.mult, e='pool')
                        S.ts(st['rW'][:], kP[:, pi, :], cP(2, pi), None, ALU.mult, e='pool')
                        yield
                        ps = K.bank()
                        S.mm(ps[0:64, 0:128], st['rW'][:], P)
                        for c in range(2):
                            S.mm(ps[0:64, 128 + c * 64:128 + (c + 1) * 64], PP[:, 0, c * 64:(c + 1) * 64], st['rU'][:])
                        yield
                        S.copy(st['wT'][:], ps[0:64, 0:128], e='act')
                        S.copy(st['us'][:], ps[0:64, 128:256].rearrange("p (c v) -> p c v", c=2), e='act')
                        yield
                        S.act(st['qd'][:], GB[0:64, :], AF.Exp)
                        yield
                        S.tt(st['qd'][:], st['qd'][:], qT[:, tk], ALU.mult, e='pool')
                        ps = K.bank()
                        for c in range(2):
                            n = pi * 2 + c
                            ck = slice(n * 64, (n + 1) * 64)
                            S.mm(ps[0:64, c * 64:(c + 1) * 64], kT[:, ck], qT[:, ck])
                            S.ts(st['qk'][:, c, :], GB[0:64, c * 64:(c + 1) * 64], c64(0, n), 0.0, ALU.subtract, ALU.min)
                        yield
                        S.act(st['qk'][:], st['qk'][:], AF.Exp)
                        yield
                        S.tt(st['qk'][:], st['qk'][:], masks[:, d, :].unsqueeze(1).to_broadcast([64, 2, 64]), ALU.mult, e='pool')
                        yield
                        S.tt(st['qk'][:], st['qk'][:], ps[0:64, 0:128].rearrange("p (c i) -> p c i", c=2), ALU.mult)
                        yield

                    def rec(pidx, st):
                        pi = pairs[pidx]
                        for c in ((0, 1) if d == 0 else (1, 0)):
                            n = pi * 2 + c
                            si = state['si']
                            Sc, Sn = Ss[si % 2], Ss[(si + 1) % 2]
                            vn = vns[si % 2]
                            state['si'] = si + 1
                            ps1 = K.bank()
                            S.mm(ps1[0:64, 0:64], st['wT'][:, c * 64:(c + 1) * 64], Sc[:])
                            yield
                            S.tt(vn[:], st['us'][:, c, :], ps1[0:64, 0:64], ALU.subtract)
                            yield
                            ps2 = K.bank()
                            S.mm(ps2[0:64, 0:64], st['qd'][:, c * 64:(c + 1) * 64], Sc[:], start=True, stop=False)
                            S.mm(ps2[0:64, 0:64], st['qk'][:, c, :], vn[:], start=False, stop=True)
                            ps3 = K.bank()
                            S.mm(ps3[0:64, 0:64], kdec3[:, n, :], vn[:])
                            yield
                            S.stt(Sn[:], Sc[:], c64(4, n), ps3[0:64, 0:64], ALU.mult, ALU.add)
                            if d == 0:
                                S.copy(hacc[:, n, :], ps2[0:64, 0:64], e='act')
                            else:
                                S.tt(hacc[:, n, :], hacc[:, n, :], ps2[0:64, 0:64], ALU.add)
                            yield

                    run_pipeline(min(len(pairs), DBG.get('d_pairs', 99)), prep, rec, sets)
                osig = kT[:, :].rearrange("p (n c) -> p n c", c=64)
                for n0 in range(0, NCH, 8):
                    nn = min(8, NCH - n0)
                    ps1 = K.bank()
                    for j in range(nn):
                        S.tr(ps1[0:64, j * 64:(j + 1) * 64], raw[:, chunk_cols(n0 + j)], K.ident[0:64, 0:64])
                    S.act(osig[:, n0:n0 + nn, :], ps1[0:64, 0:nn * 64].rearrange("p (j c) -> p j c", c=64), AF.Silu)
                sq = qT[:, :].rearrange("p (n c) -> p n c", c=64)
                ssq = K.sb(es, [64, NCH, 4])
                S.tt(sq, hacc[:], hacc[:], ALU.mult, e='pool')
                S.reduce(ssq[:, :, 0], sq, ALU.add)
                S.ts(ssq[:, :, 1], ssq[:, :, 0], 1.0 / 64, EPS, ALU.mult, ALU.add)
                S.act(ssq[:, :, 2], ssq[:, :, 1], AF.Sqrt)
                S.recip(ssq[:, :, 3], ssq[:, :, 2])
                S.tt(hacc[:], hacc[:], ssq[:, :, 3].unsqueeze(2).to_broadcast([64, NCH, 64]), ALU.mult)
                S.tt(hacc[:], hacc[:], ngt[:, h * 64:(h + 1) * 64].unsqueeze(1).to_broadcast([64, NCH, 64]), ALU.mult, e='pool')
                S.tt(hacc[:], hacc[:], osig, ALU.mult)
                c0 = 768 + h * 64
                S.dma(D['y'][0:CTX, c0:c0 + 64].rearrange("(n i) c -> i n c", i=64), hacc[:, 0:4, :])
                yv = D['y'][CTX:T, c0:c0 + 64].rearrange("(r c) ch -> r c ch", c=64)
                for g4 in range(4):
                    S.dma(yv[:, g4 * 16:(g4 + 1) * 16, :], hacc[:, 4 + g4 * 16:4 + (g4 + 1) * 16, :], q=('sp', 'pool')[g4 % 2])
            S.barrier()
    S.barrier()


def shift_T(S, out, x, mu, np_):
    S.ts(out[0:np_, :], x[0:np_, :], mu[0:np_, 2:3], None, ALU.mult)
    for (a, b) in ((0, CTX), (CTX, T)):
        S.stt(out[0:np_, a + 1:b], x[0:np_, a:b - 1], mu[0:np_, 0:1], out[0:np_, a + 1:b], ALU.mult, ALU.add)
        S.stt(out[0:np_, a:b - 1], x[0:np_, a + 1:b], mu[0:np_, 1:2], out[0:np_, a:b - 1], ALU.mult, ALU.add)


def load_mu(S, W, l, mu, row0, np_):
    S.dma(mu[0:np_, 0:2], W['pk_mu'][l, row0:row0 + np_, :], q='pool')
    S.ts(mu[0:np_, 2:3], mu[0:np_, 0:1], -1.0, 1.0, ALU.mult, ALU.add)
    S.tt(mu[0:np_, 2:3], mu[0:np_, 2:3], mu[0:np_, 1:2], ALU.subtract)


def mixer_b(K, l, last):
    S, W, D = K.S, K.W, K.D
    base = OFF_B
    NP = NCH // 2
    with ExitStack() as es0:
        masks = K.sb(es0, [64, 2, 64])
        S.dma(masks[:], W['c_masks'])
        m128 = K.sb(es0, [128, 4, 128])
        S.dma(m128[:], W['c_m128'])
        ones = K.sb(es0, [64, 64])
        S.memset(ones[:], 1.0)
        for h in range(DBG.get('b_heads', 4)):
            with ExitStack() as es:
                rT, kT, kkT = K.sb(es, [64, T]), K.sb(es, [64, T]), K.sb(es, [64, T])
                Lb = K.sb(es, [64, T])
                bh, ch, kh, rh = K.sb(es, [64, T]), K.sb(es, [64, T]), K.sb(es, [64, T]), K.sb(es, [64, T])
                Vtok = K.sb(es, [64, NCH, 64])
                Vpair = K.sb(es, [128, NP, 64])
                hacc = K.sb(es, [64, NCH, 64])
                pk = K.sb(es, [64, 8])
                S.dma(pk[:, 0:7], W['pk_rw'][l, h])
                mu = K.sb(es, [64, 3])
                wup = K.sb(es, [32, 2, 64])
                aup = K.sb(es, [32, 2, 64])
                gup = K.sb(es, [64, 64])
                for d in range(2):
                    S.dma(wup[:, d, :], W['rwkv_w_up'][l, d][:, h * 64:(h + 1) * 64], q='pool')
                    S.dma(aup[:, d, :], W['rwkv_a_up'][l, d][:, h * 64:(h + 1) * 64], q='pool')
                S.dma(gup[:], W['rwkv_g_up'][l][:, h * 64:(h + 1) * 64], q='pool')
                lng = K.sb(es, [64, 2, 64])
                S.dma(lng[:, 0, :], bc_rows(W['rwkv_ln_g'][l:l + 1, h * 64:(h + 1) * 64], 64))
                S.dma(lng[:, 1, :], bc_rows(W['rwkv_ln_b'][l:l + 1, h * 64:(h + 1) * 64], 64))
                bon = K.sb(es, [64, NCH])
                GLc = K.sb(es, [64, NCH])
                for gi, dst in enumerate((rT, kT, Lb)):
                    r0 = gi * 256 + h * 64
                    load_mu(S, W, l, mu, r0, 64)
                    S.dma(ch[:], D['pT'][base + r0: base + r0 + 64, :])
                    shift_T(S, dst, ch, mu, 64)
                for n0 in range(0, NCH, 8):
                    nn = min(8, NCH - n0)
                    ps1 = K.bank()
                    for j in range(nn):
                        S.tr(ps1[0:64, j * 64:(j + 1) * 64], Lb[:, (n0 + j) * 64:(n0 + j + 1) * 64], K.ident[0:64, 0:64])
                    S.copy(Vtok[:, n0:n0 + nn, :], ps1[0:64, 0:nn * 64].rearrange("p (j c) -> p j c", c=64), e='act')
                for n0 in range(0, NP, 8):
                    nn = min(8, NP - n0)
                    ps2 = K.bank()
                    for j in range(nn):
                        S.tr(ps2[:, j * 64:(j + 1) * 64], Lb[:, (n0 + j) * 128:(n0 + j + 1) * 128], K.ident[0:64, 0:64])
                    S.copy(Vpair[:, n0:n0 + nn, :], ps2[:, 0:nn * 64].rearrange("p (j c) -> p j c", c=64))
                S.ts(kkT[:], kT[:], pk[:, 0:1], None, ALU.mult)
                S.tt(ch[:], kkT[:], kkT[:], ALU.mult, e='pool')
                for n0 in range(0, T, 512):
                    nw = min(512, T - n0)
                    ps = K.bank()
                    S.mm(ps[0:64, :nw], ones[:], ch[:, n0:n0 + nw])
                    S.ts(bh[:, n0:n0 + nw], ps[0:64, :nw], EPS, None, ALU.add)
                S.act(bh[:], bh[:], AF.Sqrt)
                S.recip(bh[:], bh[:])
                S.tt(kkT[:], kkT[:], bh[:], ALU.mult)
                NS = DBG.get('b_ns', 3)
                sets = []
                for _s in range(NS):
                    sets.append(dict(
                        BB=K.sb(es, [128, 2, 128]), PP=K.sb(es, [128, 2, 128]), N2=[K.sb(es, [128, 2, 128]) for _ in range(2)],
                        AV=K.sb(es, [128, 64]), Cp=K.sb(es, [128, 64]),
                        WcT=K.sb(es, [64, 128]), us=K.sb(es, [64, 2, 64])))
                    sets[-1]['AkT'] = sets[-1]['N2'][0][:, 0, :]
                    sets[-1]['QQ'] = sets[-1]['N2'][0][0:64, :, :].rearrange("p a (b c) -> p (a b) c", c=64)
                    sets[-1]['KN'] = sets[-1]['N2'][1][0:64, :, :].rearrange("p a (b c) -> p (a b) c", c=64)
                zns = [K.sb(es, [64, 64]) for _ in range(2)]
                Ms = [K.sb(es, [64, 64]) for _ in range(2)]
                mts = [K.sb(es, [64, 64]) for _ in range(2)]
                identB = K.ident[:, :].unsqueeze(1).to_broadcast([128, 2, 128])
                sgn = K.sb(es, [64, 4, 64])
                for d in range(2):
                    lastidx = 63 if d == 0 else 0
                    r0 = 768 + d * 32
                    load_mu(S, W, l, mu, r0, 32)
                    S.dma(kh[0:32, :], D['pT'][base + r0: base + r0 + 32, :])
                    shift_T(S, bh, kh, mu, 32)
                    S.act(bh[0:32, :], bh[0:32, :], AF.Tanh)
                    for n0 in range(0, T, 512):
                        nw = min(512, T - n0)
                        ps = K.bank()
                        S.mm(ps[0:64, :nw], wup[:, d, :], bh[0:32, n0:n0 + nw])
                        S.act(Lb[:, n0:n0 + nw], ps[0:64, :nw], AF.Sigmoid, bias=pk[:, 3 + d:4 + d])
                    S.ts(Lb[:], Lb[:], -math.exp(-0.5), None, ALU.mult)
                    for n in range(NCH):
                        ck = slice(n * 64, (n + 1) * 64)
                        if d == 0:
                            S.scan(rh[:, ck], ones[:, :], Lb[:, ck], 0.0, ALU.mult, ALU.add)
                        else:
                            S.scan(rh[:, ck][:, ::-1], ones[:, :], Lb[:, ck][:, ::-1], 0.0, ALU.mult, ALU.add)
                    S.act(GLc[:], rh[:, :].rearrange("p (n i) -> p n i", i=64)[:, :, lastidx], AF.Exp)
                    S.tt(ch[:], rh[:], Lb[:], ALU.subtract, e='pool')
                    S.act(ch[:], ch[:], AF.Exp)
                    S.tt(ch[:], ch[:], kkT[:], ALU.mult, e='pool')
                    r0 = 832 + d * 32
                    load_mu(S, W, l, mu, r0, 32)
                    S.dma(Lb[0:32, :], D['pT'][base + r0: base + r0 + 32, :])
                    shift_T(S, bh, Lb, mu, 32)
                    for n0 in range(0, T, 512):
                        nw = min(512, T - n0)
                        ps = K.bank()
                        S.mm(ps[0:64, :nw], aup[:, d, :], bh[0:32, n0:n0 + nw])
                        S.act(kh[:, n0:n0 + nw], ps[0:64, :nw], AF.Sigmoid, bias=pk[:, 5 + d:6 + d])
                    S.act(bh[:], rh[:], AF.Exp, scale=-1.0)
                    S.tt(bh[:], bh[:], kkT[:], ALU.mult)
                    S.tt(bh[:], bh[:], kh[:], ALU.mult, e='pool')
                    S.ts(kh[:], kh[:], -1.0, pk[:, 1:2], ALU.add, ALU.mult)
                    S.stt(kh[:], kh[:], 1.0, kT[:], ALU.add, ALU.mult)
                    S.stt(Lb[:], kh[:], pk[:, 2:3], rT[:], ALU.mult, ALU.mult)
                    ps = K.bank()
                    for n in range(NCH):
                        S.mm(ps[0:64, n:n + 1], Lb[:, n * 64:(n + 1) * 64], ones[:, 0:1])
                    if d == 0:
                        S.copy(bon[:], ps[0:64, 0:NCH])
                    else:
                        S.tt(bon[:], bon[:], ps[0:64, 0:NCH], ALU.add)
                    S.act(Lb[:], rh[:], AF.Exp, scale=-1.0)
                    S.tt(kh[:], kh[:], Lb[:], ALU.mult, e='pool')
                    S.act(rh[:], rh[:], AF.Exp)
                    S.tt(rh[:], rh[:], rT[:], ALU.mult)
                    S.memset(Ms[0][:], 0.0)
                    S.copy(sgn[:, 0:2, :], masks[:, d, :].unsqueeze(1).to_broadcast([64, 2, 64]), e='pool')
                    S.ts(sgn[:, 2:4, :], sgn[:, 0:2, :], -1.0, None, ALU.mult, e='pool')
                    order = scan_order(d)
                    pairs = [order[i] // 2 for i in range(0, NCH, 2)]
                    state = {'si': 0}

                    def prep(pidx, st):
                        pi = pairs[pidx]
                        BB, PP = st['BB'], st['PP']
                        tk = slice(pi * 128, (pi + 1) * 128)
                        ps = K.bank()
                        S.mm(ps[:, 0:128], bh[:, tk], ch[:, tk])
                        S.mm(ps[:, 128:256], ch[:, tk], bh[:, tk])
                        psk = K.bank()
                        S.mm(psk[:, 0:128], kh[:, tk], ch[:, tk])
                        yield
                        S.stt(n32(BB[:, 0, :]), ps[:, 0:128], -1.0, m128[:, 2 + d, :], ALU.mult, ALU.mult)
                        yield
                        S.stt(n32(BB[:, 1, :]), ps[:, 128:256], -1.0, m128[:, 3 - d, :], ALU.mult, ALU.mult)
                        yield
                        S.tt(n32(PP[:]), BB[:], identB, ALU.add, e='pool')
                        S.tt(st['AkT'], psk[:, 0:128], m128[:, 2 + d, :], ALU.mult)
                        yield
                        ps = K.bank()
                        S.mm(ps[:, 0:64], st['AkT'], Vpair[:, pi, :])
                        S.tr(ps[:, 64:128], ch[:, tk], K.ident[0:64, 0:64])
                        yield
                        S.copy(st['AV'][:], ps[:, 0:64], e='act')
                        S.copy(st['Cp'][:], ps[:, 64:128], e='act')
                        yield
                        yield from neumann_gen(K, S, PP, BB, st['N2'])
                        P = PP[:, 0, :]
                        ps = K.bank()
                        for c in range(2):
                            ck = slice(pi * 128 + c * 64, pi * 128 + (c + 1) * 64)
                            S.tr(ps[0:64, c * 64:(c + 1) * 64], kh[:, ck], K.ident[0:64, 0:64])
                            S.tr(ps[0:64, 128 + c * 64:128 + (c + 1) * 64], bh[:, ck], K.ident[0:64, 0:64])
                        yield
                        S.copy(st['KN'][:, 0:2, :], ps[0:64, 0:128].rearrange("p (c k) -> p c k", c=2), e='act')
                        yield
                        S.ts(st['KN'][:, 2:4, :], ps[0:64, 128:256].rearrange("p (c k) -> p c k", c=2), -1.0, None, ALU.mult)
                        yield
                        ps = K.bank()
                        S.mm(ps[0:64, 0:128], st['Cp'][:], P)
                        for c in range(2):
                            S.mm(ps[0:64, 128 + c * 64:128 + (c + 1) * 64], PP[:, 0, c * 64:(c + 1) * 64], st['AV'][:])
                        yield
                        S.copy(st['WcT'][:], ps[0:64, 0:128], e='act')
                        S.copy(st['us'][:], ps[0:64, 128:256].rearrange("p (c v) -> p c v", c=2), e='act')
                        yield
                        ps = K.bank()
                        for c in range(2):
                            ck = slice(pi * 128 + c * 64, pi * 128 + (c + 1) * 64)
                            S.mm(ps[0:64, c * 64:(c + 1) * 64], kh[:, ck], rh[:, ck])
                            S.mm(ps[0:64, 128 + c * 64:128 + (c + 1) * 64], bh[:, ck], rh[:, ck])
                        yield
                        S.tt(st['QQ'], ps[0:64, 0:256].rearrange("p (c i) -> p c i", c=4), sgn[:], ALU.mult)
                        yield

                    def rec(pidx, st):
                        pi = pairs[pidx]
                        for c in ((0, 1) if d == 0 else (1, 0)):
                            n = pi * 2 + c
                            ck = slice(n * 64, (n + 1) * 64)
                            si = state['si']
                            Mc, Mn = Ms[si % 2], Ms[(si + 1) % 2]
                            zn, mt = zns[si % 2], mts[si % 2]
                            state['si'] = si + 1
                            ps1 = K.bank()
                            S.mm(ps1[0:64, 0:64], st['WcT'][:, c * 64:(c + 1) * 64], Mc[:])
                            yield
                            S.tt(zn[:], st['us'][:, c, :], ps1[0:64, 0:64], ALU.add)
                            yield
                            ps2 = K.bank()
                            S.mm(ps2[0:64, 0:64], rh[:, ck], Mc[:], start=True, stop=False)
                            S.mm(ps2[0:64, 0:64], st['QQ'][:, c, :], Vtok[:, n, :], start=False, stop=False)
                            S.mm(ps2[0:64, 0:64], st['QQ'][:, 2 + c, :], zn[:], start=False, stop=True)
                            ps3 = K.bank()
                            S.mm(ps3[0:64, 0:64], st['KN'][:, c, :], Vtok[:, n, :], start=True, stop=False)
                            S.mm(ps3[0:64, 0:64], st['KN'][:, 2 + c, :], zn[:], start=False, stop=True)
                            yield
                            S.ts(mt[:], ps3[0:64, 0:64], GLc[:, n:n + 1], None, ALU.mult)
                            if d == 0:
                                S.copy(hacc[:, n, :], ps2[0:64, 0:64], e='act')
                            else:
                                S.tt(hacc[:, n, :], hacc[:, n, :], ps2[0:64, 0:64], ALU.add)
                            yield
                            S.stt(Mn[:], Mc[:], GLc[:, n:n + 1], mt[:], ALU.mult, ALU.add)
                            yield

                    run_pipeline(min(len(pairs), DBG.get('b_pairs', 99)), prep, rec, sets)
                gtok = kh[:, :].rearrange("p (n c) -> p n c", c=64)
                load_mu(S, W, l, mu, 896, 64)
                S.dma(rh[:], D['pT'][base + 896: base + 960, :])
                shift_T(S, bh, rh, mu, 64)
                S.act(bh[:], bh[:], AF.Sigmoid)
                for n0 in range(0, NCH, 8):
                    nn = min(8, NCH - n0)
                    ps = K.bank()
                    for j in range(nn):
                        S.mm(ps[0:64, j * 64:(j + 1) * 64], bh[:, (n0 + j) * 64:(n0 + j + 1) * 64], gup[:])
                    S.copy(gtok[:, n0:n0 + nn, :], ps[0:64, 0:nn * 64].rearrange("p (j c) -> p j c", c=64), e='act')
                st = K.sb(es, [64, NCH, 4])
                sq = ch[:, :].rearrange("p (n c) -> p n c", c=64)
                bc3 = lambda a: a.unsqueeze(2).to_broadcast([64, NCH, 64])
                S.reduce(st[:, :, 0], hacc[:], ALU.add)
                S.ts(st[:, :, 0], st[:, :, 0], 1.0 / 64, None, ALU.mult)
                S.tt(hacc[:], hacc[:], bc3(st[:, :, 0]), ALU.subtract)
                S.tt(sq, hacc[:], hacc[:], ALU.mult, e='pool')
                S.reduce(st[:, :, 1], sq, ALU.add)
                S.ts(st[:, :, 1], st[:, :, 1], 1.0 / 64, 64e-5, ALU.mult, ALU.add)
                S.act(st[:, :, 2], st[:, :, 1], AF.Sqrt)
                S.recip(st[:, :, 3], st[:, :, 2])
                S.tt(hacc[:], hacc[:], bc3(st[:, :, 3]), ALU.mult)
                S.tt(hacc[:], hacc[:], lng[:, 0, :].unsqueeze(1).to_broadcast([64, NCH, 64]), ALU.mult, e='pool')
                S.tt(hacc[:], hacc[:], lng[:, 1, :].unsqueeze(1).to_broadcast([64, NCH, 64]), ALU.add, e='pool')
                S.tt(sq, Vtok[:], bc3(bon[:, :]), ALU.mult)
                S.tt(hacc[:], hacc[:], sq, ALU.add, e='pool')
                S.tt(hacc[:], hacc[:], gtok, ALU.mult)
                c0 = 256 + h * 64
                yv = D['y'][:, c0:c0 + 64].rearrange("(n i) c -> i n c", i=64)
                for g4 in range(4):
                    S.dma(yv[:, g4 * 17:(g4 + 1) * 17, :], hacc[:, g4 * 17:(g4 + 1) * 17, :], q=('sp', 'pool')[g4 % 2])
            S.barrier()
    S.barrier()


W_SHAPES = {
    'xin': [T, 1024], 'cc': [128, 8, 2],
    'mod_w': [4, 1024, 6144], 'mod_b': [4, 6144], 'norm_mix_g': [4, 1024], 'norm_ffn_g': [4, 1024],
    'w_in': [4, 1024, IN_COLS], 'w_out': [4, 1024, 1024],
    'lru_conv_w': [4, 4, 256], 'lru_conv_b': [4, 256], 'lru_w_a': [4, 2, 4, 64, 64], 'lru_b_a': [4, 2, 256],
    'lru_w_x': [4, 2, 4, 64, 64], 'lru_b_x': [4, 2, 256], 'lru_lambda': [4, 2, 256],
    'rwkv_mu': [4, 2, 960], 'rwkv_w_up': [4, 2, 32, 256], 'rwkv_w0': [4, 2, 256], 'rwkv_a_up': [4, 2, 32, 256],
    'rwkv_a0': [4, 2, 256], 'rwkv_g_up': [4, 64, 256], 'rwkv_k_k': [4, 256], 'rwkv_k_a': [4, 256],
    'rwkv_r_k': [4, 256], 'rwkv_ln_g': [4, 256], 'rwkv_ln_b': [4, 256],
    'mlstm_i_b': [4, 2, 4], 'mlstm_f_b': [4, 2, 4], 'mlstm_norm_g': [4, 256],
    'gdn_conv_w': [4, 4, 768], 'gdn_a_log': [4, 2, 4], 'gdn_dt_bias': [4, 2, 4], 'gdn_norm_g': [4, 256],
    'ffn_w_gate': [2, 1024, D_FF], 'ffn_w_up': [2, 1024, D_FF], 'ffn_w_down': [2, D_FF, 1024],
    'moe_router': [2, 1024, 8], 'moe_w_gate': [2, 8, 1024, D_FFE], 'moe_w_up': [2, 8, 1024, D_FFE],
    'moe_w_down': [2, 8, D_FFE, 1024], 'final_norm_g': [1, 1024],
    'c_ident': [128, 128], 'c_sel8': [8, 8, 128],
    'pk_lru': [4, 2, 128, 11], 'pk_ml': [4, 4, 4],
    'c_masks': [64, 2, 64], 'c_rst': [4, 2, T], 'c_m128': [128, 4, 128],
    'pk_gd': [4, 4, 4], 'pk_gdc': [4, 4, 64, 3, 4], 'pk_mu': [4, 960, 2], 'pk_rw': [4, 4, 64, 7],
}


def make_consts():
    c = {}
    c['c_ident'] = np.eye(128, dtype=np.float32)
    s = np.zeros((8, 8, 128), np.float32)
    for e in range(8):
        s[e, e, :] = 1.0
    c['c_sel8'] = s
    jj, ii = np.meshgrid(np.arange(64), np.arange(64), indexing='ij')
    c['c_masks'] = np.ascontiguousarray(np.stack([(ii >= jj), (ii <= jj)], axis=1).astype(np.float32))
    ja, ia = np.meshgrid(np.arange(128), np.arange(128), indexing='ij')
    same = (ja // 64) == (ia // 64)
    c['c_m128'] = np.ascontiguousarray(np.stack([same & (ia >= ja), same & (ia <= ja), same & (ia > ja), same & (ia < ja)], axis=1).astype(np.float32))
    idx = np.arange(T) % 64
    r = np.stack([(idx != 0), (idx != 63)], axis=0).astype(np.float32)
    c['c_rst'] = np.ascontiguousarray(np.broadcast_to(r[None], (4, 2, T)))
    return c


def build(layers=(0, 1, 2, 3), mixers=None, final=True, dbg=()):
    nc = bass.Bass("TRN2", target_bir_lowering=False)
    W = {n: nc.dram_tensor(n, sh, F32, kind="ExternalInput").ap() for n, sh in W_SHAPES.items()}
    out = nc.dram_tensor('out', [SEQ, 1024], F32, kind="ExternalOutput").ap()
    D = {}
    for n, sh in {'xres': [T, 1024], 'pT': [IN_COLS, T], 'y': [T, 1024], 'mod': [2, 6144]}.items():
        kind = "ExternalOutput" if n in dbg else "Internal"
        D[n] = nc.dram_tensor('d_' + n, sh, F32, kind=kind).ap()
    with ExitStack() as es:
        S = Sched(nc, es)
        ps = [es.enter_context(nc.psum_tensor("psb%d" % i, [128, 512], F32)) for i in range(8)]
        ident = es.enter_context(nc.sbuf_tensor("ident", [128, 128], F32))
        K = Ctx(nc, S, W, D, ps, ident)
        K.out = out
        S.dma(ident[:], W['c_ident'])
        for t in range(NT):
            S.dma(D['xres'][t * 128:(t + 1) * 128, :], W['xin'][t * 128:(t + 1) * 128, :], q=('sp', 'pool', 'act')[t % 3])
        S.barrier()
        for l in layers:
            last = (l == 3)
            stage_mod(K, l)
            stage_inproj(K, l)
            if mixers is None:
                mix_identity(K, l)
            else:
                for m in mixers:
                    m(K, l, last)
            stage_outproj(K, l, last)
            if l % 2 == 0:
                stage_ffn(K, l, last)
            else:
                stage_moe(K, l, last)
        if final:
            stage_final(K)
        S.finish()
    K.S = S
    return nc, S


def make_packs(inputs):
    f = lambda n: np.asarray(inputs[n], dtype=np.float32)
    pk = {}
    cols = [f('lru_conv_w')[:, j, :] for j in range(4)] + [f('lru_conv_b')]
    cols += [f('lru_b_a')[:, 0], f('lru_b_a')[:, 1], f('lru_b_x')[:, 0], f('lru_b_x')[:, 1], f('lru_lambda')[:, 0], f('lru_lambda')[:, 1]]
    a = np.stack(cols, axis=-1)
    pk['pk_lru'] = np.ascontiguousarray(a.reshape(4, 2, 128, 11))
    pk['pk_gd'] = np.ascontiguousarray(np.concatenate([f('gdn_a_log'), f('gdn_dt_bias')], axis=1).transpose(0, 2, 1))
    pk['pk_gdc'] = np.ascontiguousarray(f('gdn_conv_w').reshape(4, 4, 3, 4, 64).transpose(0, 3, 4, 2, 1))
    pk['pk_mu'] = np.ascontiguousarray(f('rwkv_mu').transpose(0, 2, 1))
    cols = [f('rwkv_k_k'), f('rwkv_k_a'), f('rwkv_r_k'), f('rwkv_w0')[:, 0], f('rwkv_w0')[:, 1], f('rwkv_a0')[:, 0], f('rwkv_a0')[:, 1]]
    pk['pk_rw'] = np.ascontiguousarray(np.stack(cols, axis=-1).reshape(4, 4, 64, 7))
    pk['pk_ml'] = np.ascontiguousarray(np.concatenate([f('mlstm_i_b'), f('mlstm_f_b')], axis=1).transpose(0, 2, 1))
    return pk


def host_inputs(inputs, b):
    m = {}
    m['xin'] = np.ascontiguousarray(np.concatenate([inputs['ctx'][b], inputs['x'][b]], axis=0))
    cc = np.stack([np.asarray(inputs['c'][b]).reshape(8, 128).T, np.asarray(inputs['c_ctx']).reshape(8, 128).T], axis=-1)
    m['cc'] = np.ascontiguousarray(cc.astype(np.float32))
    for n in W_SHAPES:
        if n in m or n.startswith('c_') or n.startswith('pk_'):
            continue
        m[n] = np.ascontiguousarray(np.asarray(inputs[n], dtype=np.float32).reshape(W_SHAPES[n]))
    m.update(make_consts())
    m.update(make_packs(inputs))
    return m


def build_test(L, mixers):
    nc = bass.Bass("TRN2", target_bir_lowering=False)
    W = {n: nc.dram_tensor(n, sh, F32, kind="ExternalInput").ap() for n, sh in W_SHAPES.items()}
    D = {}
    for n, sh in {'xres': [T, 1024], 'pT': [IN_COLS, T], 'y': [T, 1024], 'mod': [2, 6144]}.items():
        kind = "ExternalOutput" if n in ('pT', 'y') else "Internal"
        D[n] = nc.dram_tensor('d_' + n, sh, F32, kind=kind).ap()
    add_scratch(nc, D)
    with ExitStack() as es:
        S = Sched(nc, es)
        ps = [es.enter_context(nc.psum_tensor("psb%d" % i, [128, 512], F32)) for i in range(8)]
        ident = es.enter_context(nc.sbuf_tensor("ident", [128, 128], F32))
        K = Ctx(nc, S, W, D, ps, ident)
        S.dma(ident[:], W['c_ident'])
        for t in range(NT):
            S.dma(D['xres'][t * 128:(t + 1) * 128, :], W['xin'][t * 128:(t + 1) * 128, :], q=('sp', 'pool', 'act')[t % 3])
        S.barrier()
        stage_mod(K, L)
        stage_inproj(K, L)
        for m in mixers:
            m(K, L, False)
        S.finish()
    return nc, S


def add_scratch(nc, D):
    pass


def kernel(**inputs):
    nc, S = build(layers=(0, 1, 2, 3), mixers=[mixer_a, mixer_b, mixer_c, mixer_d], final=True)
    in_maps = [host_inputs(inputs, b) for b in range(8)]
    res = run_bass_kernel_spmd(nc, in_maps, core_ids=list(range(8)))
    return np.stack([np.asarray(r['out'], dtype=np.float32) for r in res.results], axis=0)
```

```python
import math
import numpy as np
import concourse.bass as bass
import concourse.mybir as mybir
from concourse.bass_utils import run_bass_kernel_spmd
from contextlib import ExitStack

F32 = mybir.dt.float32
F32R = mybir.dt.float32r
FAST_MM = True
AF = mybir.ActivationFunctionType
ALU = mybir.AluOpType
AX = mybir.AxisListType

D_MODEL = 1024
SEQ = 4096
CTX = 256
T = SEQ + CTX
NT = T // 128
G = 256
IN_COLS = 3552
OFF_B = 512
OFF_C = OFF_B + 960
OFF_D = OFF_C + 1040
D_FF = 2816
D_FFE = 1408
EPS = 1e-6

ENGS = ['pe', 'dve', 'act', 'pool', 'sp']
SAME_SYNC = {'pe': False, 'dve': True, 'act': True, 'pool': True, 'sp': True}

def _box(ap):
    t = ap.tensor
    name = t.name
    dims = ap.ap
    off = int(ap.offset)
    if str(ap.space) in ('SB', 'PSUM', 'SBUF'):
        row = dims[0][0] if dims[0][0] > 0 else 1
        p0 = off // row
        f0 = off % row
        p1 = p0 + dims[0][1]
        lo = hi = f0
        for st, cnt in dims[1:]:
            if st >= 0:
                hi += st * (cnt - 1)
            else:
                lo += st * (cnt - 1)
        return (name, p0, p1, lo, hi + 1)
    lo = hi = off
    for st, cnt in dims:
        if st >= 0:
            hi += st * (cnt - 1)
        else:
            lo += st * (cnt - 1)
    return (name, 0, 1, lo, hi + 1)


class Sched:
    def __init__(self, nc, es, n_dma=8):
        self.nc = nc
        self.es = es
        self.eng = dict(pe=nc.tensor, dve=nc.vector, act=nc.scalar, pool=nc.gpsimd, sp=nc.sync)
        self.sem = {}
        self.cnt = {}
        self.unit = {}
        for e in ENGS:
            self.sem[e] = es.enter_context(nc.semaphore('s_' + e))
            self.cnt[e] = 0
            self.unit[e] = 1
        self.n_dma = n_dma
        self.dma_rr = {}
        for q in ('sp', 'act', 'pool'):
            self.dma_rr[q] = 0
            for i in range(n_dma):
                c = ('dma', q, i)
                self.sem[c] = es.enter_context(nc.semaphore('d_%s%d' % (q, i)))
                self.cnt[c] = 0
                self.unit[c] = 16
        self.seen = {e: {} for e in ENGS}
        self.recs = {}
        self.nins = 0

    def _need(self, reads, writes):
        need = {}
        for aps, isw in ((reads, False), (writes, True)):
            for ap in aps:
                name, p0, p1, f0, f1 = _box(ap)
                if not isw and name.startswith('psb'):
                    isw, p0, p1, f0, f1 = True, 0, 128, 0, 1 << 30
                for r in self.recs.get(name, ()):
                    if r[0] < p1 and p0 < r[1] and r[2] < f1 and f0 < r[3]:
                        if isw or r[6]:
                            c, v = r[4], r[5]
                            if need.get(c, 0) < v:
                                need[c] = v
        return need

    def _record(self, reads, writes, clock, val):
        for aps, isw in ((reads, False), (writes, True)):
            for ap in aps:
                name, p0, p1, f0, f1 = _box(ap)
                if not isw and name.startswith('psb'):
                    isw, p0, p1, f0, f1 = True, 0, 128, 0, 1 << 30
                lst = self.recs.setdefault(name, [])
                if isw:
                    lst[:] = [r for r in lst if not (p0 <= r[0] and r[1] <= p1 and f0 <= r[2] and r[3] <= f1)]
                else:
                    lst[:] = [r for r in lst if not (r[4] == clock and not r[6] and p0 <= r[0] and r[1] <= p1 and f0 <= r[2] and r[3] <= f1)]
                lst.append((p0, p1, f0, f1, clock, val, isw))
                if len(lst) > 48:
                    self._prune(lst)

    def _prune(self, lst):
        def stale(r):
            c, v = r[4], r[5]
            for e in ENGS:
                if e == c and not SAME_SYNC[e]:
                    continue
                if self.seen[e].get(c, 0) < v:
                    return False
            return True
        lst[:] = [r for r in lst if not stale(r)]

    def _waits(self, e, need):
        eo = self.eng[e]
        for c, v in need.items():
            if c == e and not SAME_SYNC[e]:
                continue
            if self.seen[e].get(c, 0) >= v:
                continue
            eo.wait_ge(self.sem[c], v * self.unit[c])
            self.seen[e][c] = v

    def op(self, e, fn, reads, writes):
        need = self._need(reads, writes)
        self._waits(e, need)
        ins = fn(self.eng[e])
        self.cnt[e] += 1
        ins.then_inc(self.sem[e], 1)
        self._record(reads, writes, e, self.cnt[e])
        self.nins += 1
        return ins

    def dma(self, out, in_, q='sp', **kw):
        need = self._need([in_], [out])
        k = self.dma_rr[q]
        self.dma_rr[q] = (k + 1) % self.n_dma
        c = ('dma', q, k)
        if self.cnt[c] > 0:
            need[c] = max(need.get(c, 0), self.cnt[c])
        self._waits(q, need)
        ins = self.eng[q].dma_start(out=out, in_=in_, **kw)
        self.cnt[c] += 1
        ins.then_inc(self.sem[c], 16)
        self._record([in_], [out], c, self.cnt[c])
        self.nins += 1

    def barrier(self):
        for e in ENGS:
            need = {c: v for c, v in self.cnt.items() if v > 0 and c != e}
            self._waits(e, need)
        self.recs = {}

    def finish(self):
        need = {c: v for c, v in self.cnt.items() if v > 0 and c != 'sp'}
        self._waits('sp', need)

    def mm(self, out, lhsT, rhs, start=True, stop=True, fast=False):
        if fast and FAST_MM:
            lhsT, rhs = lhsT.bitcast(F32R), rhs.bitcast(F32R)
        self.op('pe', lambda e: e.matmul(out, lhsT, rhs, start=start, stop=stop), [lhsT, rhs] + ([] if start else [out]), [out])

    def tr(self, out, in_, ident):
        self.op('pe', lambda e: e.transpose(out, in_, ident), [in_, ident], [out])

    def act(self, out, in_, func, bias=None, scale=None, accum_out=None):
        kw = {}
        rd = [in_]
        wr = [out]
        if bias is not None:
            kw['bias'] = bias
            if not isinstance(bias, (int, float)):
                rd.append(bias)
        if scale is not None:
            kw['scale'] = scale
            if not isinstance(scale, (int, float)):
                rd.append(scale)
        if accum_out is not None:
            kw['accum_out'] = accum_out
            wr.append(accum_out)
        self.op('act', lambda e: e.activation(out, in_, func, **kw), rd, wr)

    def tt(self, out, in0, in1, op, e='dve'):
        self.op(e, lambda en: en.tensor_tensor(out, in0, in1, op), [in0, in1], [out])

    def ts(self, out, in0, s1, s2, op0, op1=None, e='dve', accum_out=None):
        rd = [in0]
        for s in (s1, s2):
            if s is not None and not isinstance(s, (int, float)):
                rd.append(s)
        wr = [out] + ([accum_out] if accum_out is not None else [])
        kw = {}
        if op1 is not None:
            kw['op1'] = op1
        if accum_out is not None:
            kw['accum_out'] = accum_out
        self.op(e, lambda en: en.tensor_scalar(out, in0, s1, s2, op0, **kw), rd, wr)

    def stt(self, out, in0, scalar, in1, op0, op1, e='dve'):
        rd = [in0, in1]
        if not isinstance(scalar, (int, float)):
            rd.append(scalar)
        self.op(e, lambda en: en.scalar_tensor_tensor(out, in0, scalar, in1, op0, op1), rd, [out])

    def copy(self, out, in_, e='dve'):
        if e == 'act':
            self.op(e, lambda en: en.copy(out, in_), [in_], [out])
        else:
            self.op(e, lambda en: en.tensor_copy(out, in_), [in_], [out])

    def memset(self, ap, val, e='dve'):
        self.op(e, lambda en: en.memset(ap, val), [], [ap])

    def reduce(self, out, in_, op, axis=None, e='dve'):
        axis = axis or AX.X
        self.op(e, lambda en: en.tensor_reduce(out, in_, axis, op), [in_], [out])

    def scan(self, out, d0, d1, init, op0, op1):
        rd = [d0, d1]
        if not isinstance(init, (int, float)):
            rd.append(init)
        self.op('dve', lambda en: en.tensor_tensor_scan(out, d0, d1, init, op0, op1), rd, [out])

    def recip(self, out, in_):
        self.op('dve', lambda en: en.reciprocal(out, in_), [in_], [out])


class Ctx:
    def __init__(self, nc, S, W, D, ps, ident):
        self.nc, self.S, self.W, self.D, self.ps, self.ident = nc, S, W, D, ps, ident
        self.uid = 0
        self.psi = 0

    def sb(self, es, shape, dt=F32, name=None):
        self.uid += 1
        return es.enter_context(self.nc.sbuf_tensor("%s_%d" % (name or "t", self.uid), list(shape), dt))

    def bank(self):
        b = self.ps[self.psi % 8]
        self.psi += 1
        return b


def r32(ap):
    return ap.bitcast(F32R) if FAST_MM else ap


def bc_rows(ap, n):
    return ap.to_broadcast([n, ap.shape[1]])


def stage_mod(K, l):
    S, W, D = K.S, K.W, K.D
    with ExitStack() as es:
        cc = K.sb(es, [128, 8, 2])
        S.dma(cc[:], W['cc'])
        S.act(cc[:], cc[:], AF.Silu)
        mb = K.sb(es, [2, 6144])
        S.dma(mb[:], bc_rows(W['mod_b'][l:l + 1, :], 2))
        mo = K.sb(es, [2, 6144])
        wts = [K.sb(es, [128, 8, 512]) for _ in range(2)]
        wv = W['mod_w'][l].rearrange("(k p) c -> p k c", p=128)
        for n in range(12):
            wt = wts[n % 2]
            S.dma(wt[:], wv[:, :, n * 512:(n + 1) * 512], q=('sp' if n % 2 == 0 else 'pool'))
            ps = K.bank()
            for k in range(8):
                S.mm(ps[0:2, :], cc[:, k, :], wt[:, k, :], start=(k == 0), stop=(k == 7))
            S.tt(mo[:, n * 512:(n + 1) * 512], ps[0:2, :], mb[:, n * 512:(n + 1) * 512], ALU.add)
        S.dma(D['mod'], mo[:])
    S.barrier()


def load_mod_tiles(K, es, l, which, gname):
    S, W, D = K.S, K.W, K.D
    base = 3072 * which
    outs = []
    gt = K.sb(es, [128, 1024])
    S.dma(gt[:], bc_rows(W[gname][l:l + 1, :], 128))
    for seg in range(2):
        sh = K.sb(es, [128, 1024])
        sc = K.sb(es, [128, 1024])
        S.dma(sh[:], bc_rows(D['mod'][seg:seg + 1, base:base + 1024], 128), q='pool')
        S.dma(sc[:], bc_rows(D['mod'][seg:seg + 1, base + 1024:base + 2048], 128), q='pool')
        S.stt(sc[:], sc[:], 1.0, gt[:], ALU.add, ALU.mult)
        outs += [sc, sh]
    return outs


def norm_mod_T(K, es_tmp, xt, Gt, SHt, hT_dst, tmp):
    S = K.S
    junk, ss, h = tmp
    S.act(junk[:], xt[:], AF.Square, accum_out=ss[:, 0:1])
    S.ts(ss[:, 1:2], ss[:, 0:1], 1.0 / D_MODEL, EPS, ALU.mult, ALU.add)
    S.act(ss[:, 2:3], ss[:, 1:2], AF.Sqrt)
    S.recip(ss[:, 3:4], ss[:, 2:3])
    S.stt(h[:], xt[:], ss[:, 3:4], Gt[:], ALU.mult, ALU.mult)
    S.tt(h[:], h[:], SHt[:], ALU.add, e='pool')
    for b in range(2):
        ps = K.bank()
        for j in range(4):
            k = b * 4 + j
            S.tr(ps[:, j * 128:(j + 1) * 128], h[:, k * 128:(k + 1) * 128], K.ident[:])
        src = ps[:].rearrange("p (j t) -> p j t", j=4)
        if b == 0:
            S.copy(r32(hT_dst[:, 0:4, :]), src, e='dve')
        else:
            S.copy(r32(hT_dst[:, 4:8, :]), src, e='act')


def stage_inproj(K, l):
    S, W, D = K.S, K.W, K.D
    HALF = T // 2
    wv = W['w_in'][l].rearrange("(k p) c -> p k c", p=128)
    with ExitStack() as es:
        GL, SHL, GC, SHC = load_mod_tiles(K, es, l, 0, 'norm_mix_g')
        hT = K.sb(es, [128, 8, HALF])
        xts = [K.sb(es, [128, 1024]) for _ in range(2)]
        tmp = (K.sb(es, [128, 1024]), K.sb(es, [128, 4]), K.sb(es, [128, 1024]))
        wts = [K.sb(es, [128, 8, 128]) for _ in range(3)]
        ots = [K.sb(es, [128, HALF]) for _ in range(2)]
        for half in range(2):
            for ti in range(17):
                t = half * 17 + ti
                xt = xts[ti % 2]
                S.dma(xt[:], D['xres'][t * 128:(t + 1) * 128, :])
                isctx = t < 2
                norm_mod_T(K, es, xt, GC if isctx else GL, SHC if isctx else SHL,
                           hT[:, :, ti * 128:(ti + 1) * 128], tmp)
            for cchunk in range(28):
                c0 = cchunk * 128
                cw = min(128, IN_COLS - c0)
                wt = wts[cchunk % 3]
                S.dma(r32(wt[:, :, :cw]), wv[:, :, c0:c0 + cw], q='pool')
                ot = ots[cchunk % 2]
                for si, (n0, nw) in enumerate([(0, 512), (512, 512), (1024, 512), (1536, 512), (2048, 128)]):
                    ps = K.bank()
                    for k in range(8):
                        S.mm(ps[:cw, :nw], wt[:, k, :cw], hT[:, k, n0:n0 + nw], start=(k == 0), stop=(k == 7), fast=True)
                    S.copy(ot[:cw, n0:n0 + nw], ps[:cw, :nw], e=('dve' if si % 2 == 0 else 'act'))
                S.dma(D['pT'][c0:c0 + cw, half * HALF:(half + 1) * HALF], ot[:cw, :], q='act')
    S.barrier()


def stage_outproj(K, l, last):
    S, W, D = K.S, K.W, K.D
    wv = W['w_out'][l].rearrange("(k p) c -> p k c", p=128)
    with ExitStack() as es:
        wo = K.sb(es, [128, 8, 1024])
        S.dma(r32(wo[:, 0:4, :]), wv[:, 0:4, :], q='pool')
        S.dma(r32(wo[:, 4:8, :]), wv[:, 4:8, :], q='pool')
        gts = []
        for seg in range(2):
            g = K.sb(es, [128, 1024])
            S.dma(g[:], bc_rows(D['mod'][seg:seg + 1, 2048:3072], 128))
            gts.append(g)
        yts = [K.sb(es, [128, 1024]) for _ in range(2)]
        xts = [K.sb(es, [128, 1024]) for _ in range(2)]
        yTs = [K.sb(es, [128, 8, 128]) for _ in range(2)]
        for t in range(2 if last else 0, NT):
            yt, xt, yT = yts[t % 2], xts[t % 2], yTs[t % 2]
            S.dma(yt[:], D['y'][t * 128:(t + 1) * 128, :])
            S.dma(xt[:], D['xres'][t * 128:(t + 1) * 128, :], q='pool')
            for b in range(2):
                ps = K.bank()
                for j in range(4):
                    k = b * 4 + j
                    S.tr(ps[:, j * 128:(j + 1) * 128], yt[:, k * 128:(k + 1) * 128], K.ident[:])
                S.copy(r32(yT[:, b * 4:(b + 1) * 4, :]), ps[:].rearrange("p (j t) -> p j t", j=4), e=('dve' if b == 0 else 'act'))
            gt = gts[0] if t >= 2 else gts[1]
            for n in range(2):
                ps = K.bank()
                for k in range(8):
                    S.mm(ps[:, :], yT[:, k, :], wo[:, k, n * 512:(n + 1) * 512], start=(k == 0), stop=(k == 7), fast=True)
                S.tt(yt[:, n * 512:(n + 1) * 512], ps[:, :], gt[:, n * 512:(n + 1) * 512], ALU.mult)
                S.tt(xt[:, n * 512:(n + 1) * 512], xt[:, n * 512:(n + 1) * 512], yt[:, n * 512:(n + 1) * 512], ALU.add, e='pool')
            S.dma(D['xres'][t * 128:(t + 1) * 128, :], xt[:], q='act')
    S.barrier()


def stage_ffn(K, l, last):
    S, W, D = K.S, K.W, K.D
    dense = (l % 2 == 0)
    li = l // 2
    if dense:
        E, NF = 1, D_FF // 128
        wg_v = [W['ffn_w_gate'][li].rearrange("(k p) c -> p k c", p=128)]
        wu_v = [W['ffn_w_up'][li].rearrange("(k p) c -> p k c", p=128)]
        wd_v = [W['ffn_w_down'][li].rearrange("(f p) c -> p f c", p=128)]
    else:
        E, NF = 8, D_FFE // 128
        wg_v = [W['moe_w_gate'][li, e].rearrange("(k p) c -> p k c", p=128) for e in range(8)]
        wu_v = [W['moe_w_up'][li, e].rearrange("(k p) c -> p k c", p=128) for e in range(8)]
        wd_v = [W['moe_w_down'][li, e].rearrange("(f p) c -> p f c", p=128) for e in range(8)]
    blocks = ([] if last else [(0, 2)]) + [(2 + 4 * i, 4) for i in range(8)]
    with ExitStack() as es:
        gn = K.sb(es, [128, 1024])
        S.dma(gn[:], bc_rows(W['norm_ffn_g'][l:l + 1, :], 128))
        G2, SH2, GT2 = K.sb(es, [128, 1024]), K.sb(es, [128, 1024]), K.sb(es, [128, 1024])
        xblk = K.sb(es, [128, 4, 1024])
        h2T = K.sb(es, [128, 8, 512])
        actT = K.sb(es, [128, NF, 512])
        yT = K.sb(es, [128, 8, 512])
        tmp = (K.sb(es, [128, 1024]), K.sb(es, [128, 4]), K.sb(es, [128, 1024]))
        sgs = [K.sb(es, [128, 512]) for _ in range(2)]
        wgs = [K.sb(es, [128, 8, 128]) for _ in range(2)]
        wus = [K.sb(es, [128, 8, 128]) for _ in range(2)]
        wds = [K.sb(es, [128, NF, 128]) for _ in range(2)]
        if not dense:
            Gbc = K.sb(es, [128, 8, 512])
            rt = K.sb(es, [128, 8, 8])
            S.dma(rt[:], W['moe_router'][li].rearrange("(k p) e -> p k e", p=128))
            gateT = K.sb(es, [8, 512])
            sel = K.sb(es, [8, 8, 128])
            S.dma(sel[:], W['c_sel8'])
            gsm = K.sb(es, [128, 64])
        cur_seg = None
        wi = 0
        for (t0, nt) in blocks:
            NB = nt * 128
            seg = 1 if t0 < 2 else 0
            if seg != cur_seg:
                cur_seg = seg
                S.dma(SH2[:], bc_rows(D['mod'][seg:seg + 1, 3072:4096], 128), q='pool')
                S.dma(G2[:], bc_rows(D['mod'][seg:seg + 1, 4096:5120], 128), q='pool')
                S.dma(GT2[:], bc_rows(D['mod'][seg:seg + 1, 5120:6144], 128), q='pool')
                S.stt(G2[:], G2[:], 1.0, gn[:], ALU.add, ALU.mult)
            for ti in range(nt):
                t = t0 + ti
                S.dma(xblk[:, ti, :], D['xres'][t * 128:(t + 1) * 128, :])
                norm_mod_T(K, es, xblk[:, ti, :], G2, SH2, h2T[:, :, ti * 128:(ti + 1) * 128], tmp)
            if not dense:
                for ti in range(nt):
                    ps = K.bank()
                    for k in range(8):
                        S.mm(ps[:, 0:8], h2T[:, k, ti * 128:(ti + 1) * 128], rt[:, k, :], start=(k == 0), stop=(k == 7))
                    lg, eq, l2, ex = gsm[:, 0:8], gsm[:, 8:16], gsm[:, 16:24], gsm[:, 24:32]
                    m1, m2, nm1, sm, rs = gsm[:, 32:33], gsm[:, 33:34], gsm[:, 34:35], gsm[:, 35:36], gsm[:, 36:37]
                    gate = gsm[:, 40:48]
                    S.copy(lg, ps[:, 0:8])
                    S.reduce(m1, lg, ALU.max)
                    S.ts(eq, lg, m1, None, ALU.is_equal)
                    S.stt(l2, eq, -1e30, lg, ALU.mult, ALU.add)
                    S.reduce(m2, l2, ALU.max)
                    S.ts(eq, lg, m2, None, ALU.is_ge)
                    S.ts(nm1, m1, -1.0, None, ALU.mult)
                    S.act(ex, lg, AF.Exp, bias=nm1)
                    S.tt(ex, ex, eq, ALU.mult)
                    S.reduce(sm, ex, ALU.add)
                    S.recip(rs, sm)
                    S.ts(gate, ex, rs, None, ALU.mult)
                    ps2 = K.bank()
                    S.tr(ps2[0:8, 0:128], gate, K.ident[:])
                    S.copy(gateT[:, ti * 128:(ti + 1) * 128], ps2[0:8, 0:128])
                for e in range(8):
                    ps = K.bank()
                    S.mm(ps[:, :NB], sel[:, e, :], gateT[:, :NB])
                    S.copy(Gbc[:, e, :NB], ps[:, :NB], e='act')
            for e in range(E):
                for f in range(NF):
                    wg, wu = wgs[wi % 2], wus[wi % 2]
                    sg = sgs[wi % 2]
                    wi += 1
                    S.dma(r32(wg[:]), wg_v[e][:, :, f * 128:(f + 1) * 128], q='pool')
                    S.dma(r32(wu[:]), wu_v[e][:, :, f * 128:(f + 1) * 128], q='pool')
                    psg, psu = K.bank(), K.bank()
                    for k in range(8):
                        S.mm(psg[:, :NB], wg[:, k, :], h2T[:, k, :NB], start=(k == 0), stop=(k == 7), fast=True)
                    for k in range(8):
                        S.mm(psu[:, :NB], wu[:, k, :], h2T[:, k, :NB], start=(k == 0), stop=(k == 7), fast=True)
                    S.act(sg[:, :NB], psg[:, :NB], AF.Silu)
                    S.tt(r32(actT[:, f, :NB]), sg[:, :NB], psu[:, :NB], ALU.mult)
                    if not dense:
                        S.tt(r32(actT[:, f, :NB]), actT[:, f, :NB], Gbc[:, e, :NB], ALU.mult, e='pool')
                for cchunk in range(8):
                    wd = wds[cchunk % 2]
                    S.dma(r32(wd[:]), wd_v[e][:, :, cchunk * 128:(cchunk + 1) * 128], q='pool')
                    ps = K.bank()
                    for f in range(NF):
                        S.mm(ps[:, :NB], wd[:, f, :], actT[:, f, :NB], start=(f == 0), stop=(f == NF - 1), fast=True)
                    if e == 0:
                        S.copy(yT[:, cchunk, :NB], ps[:, :NB], e='act')
                    else:
                        S.tt(yT[:, cchunk, :NB], yT[:, cchunk, :NB], ps[:, :NB], ALU.add)
            for ti in range(nt):
                t = t0 + ti
                yt = tmp[0]
                for b in range(2):
                    ps = K.bank()
                    for j in range(4):
                        S.tr(ps[:, j * 128:(j + 1) * 128], yT[:, b * 4 + j, ti * 128:(ti + 1) * 128], K.ident[:])
                    S.tt(yt[:, b * 512:(b + 1) * 512], ps[:, :], GT2[:, b * 512:(b + 1) * 512], ALU.mult)
                S.tt(xblk[:, ti, :], xblk[:, ti, :], yt[:], ALU.add, e='pool')
                S.dma(D['xres'][t * 128:(t + 1) * 128, :], xblk[:, ti, :], q='act')
    S.barrier()


def stage_moe(K, l, last):
    S, W, D = K.S, K.W, K.D
    li = l // 2
    E, NF = 8, D_FFE // 128
    wg_v = [W['moe_w_gate'][li, e].rearrange("(k p) c -> p k c", p=128) for e in range(8)]
    wu_v = [W['moe_w_up'][li, e].rearrange("(k p) c -> p k c", p=128) for e in range(8)]
    wd_v = [W['moe_w_down'][li, e].rearrange("(f p) c -> p f c", p=128) for e in range(8)]
    blocks = ([] if last else [(0, 2)]) + [(2 + 8 * i, 8) for i in range(4)]
    with ExitStack() as es:
        gn = K.sb(es, [128, 1024])
        S.dma(gn[:], bc_rows(W['norm_ffn_g'][l:l + 1, :], 128))
        G2, SH2, GT2 = K.sb(es, [128, 1024]), K.sb(es, [128, 1024]), K.sb(es, [128, 1024])
        h2T = K.sb(es, [128, 8, 1024])
        actT = K.sb(es, [128, NF, 1024])
        yT = K.sb(es, [128, 8, 1024])
        xt = K.sb(es, [128, 1024])
        tmp = (K.sb(es, [128, 1024]), K.sb(es, [128, 4]), K.sb(es, [128, 1024]))
        sgs = [K.sb(es, [128, 512]) for _ in range(2)]
        wst = [K.sb(es, [128, 8, 128]) for _ in range(4)]
        wdst = [K.sb(es, [128, NF, 128]) for _ in range(2)]
        wgs = [K.sb(es, [128, 8, 128]) for _ in range(2)]
        wus = [K.sb(es, [128, 8, 128]) for _ in range(2)]
        wds = [K.sb(es, [128, NF, 128]) for _ in range(2)]
        Gbs = [K.sb(es, [128, 1024]) for _ in range(1)]
        rt = K.sb(es, [128, 8, 8])
        S.dma(rt[:], W['moe_router'][li].rearrange("(k p) e -> p k e", p=128))
        gateT = K.sb(es, [8, 1024])
        sel = K.sb(es, [8, 8, 128])
        S.dma(sel[:], W['c_sel8'])
        gsm = K.sb(es, [128, 64])
        cur_seg = None
        wi = 0
        wdi = 0
        for (t0, nt) in blocks:
            NB = nt * 128
            halves = [(0, min(512, NB))] + ([(512, NB - 512)] if NB > 512 else [])
            seg = 1 if t0 < 2 else 0
            if seg != cur_seg:
                cur_seg = seg
                S.dma(SH2[:], bc_rows(D['mod'][seg:seg + 1, 3072:4096], 128), q='act')
                S.dma(G2[:], bc_rows(D['mod'][seg:seg + 1, 4096:5120], 128), q='act')
                S.dma(GT2[:], bc_rows(D['mod'][seg:seg + 1, 5120:6144], 128), q='act')
                S.stt(G2[:], G2[:], 1.0, gn[:], ALU.add, ALU.mult)
            for ti in range(nt):
                t = t0 + ti
                S.dma(xt[:], D['xres'][t * 128:(t + 1) * 128, :], q='act')
                norm_mod_T(K, es, xt, G2, SH2, h2T[:, :, ti * 128:(ti + 1) * 128], tmp)
                ps = K.bank()
                for k in range(8):
                    S.mm(ps[:, 0:8], h2T[:, k, ti * 128:(ti + 1) * 128], rt[:, k, :], start=(k == 0), stop=(k == 7))
                lg, eq, l2, ex = gsm[:, 0:8], gsm[:, 8:16], gsm[:, 16:24], gsm[:, 24:32]
                m1, m2, nm1, sm, rs = gsm[:, 32:33], gsm[:, 33:34], gsm[:, 34:35], gsm[:, 35:36], gsm[:, 36:37]
                gate = gsm[:, 40:48]
                S.copy(lg, ps[:, 0:8])
                S.reduce(m1, lg, ALU.max)
                S.ts(eq, lg, m1, None, ALU.is_equal)
                S.stt(l2, eq, -1e30, lg, ALU.mult, ALU.add)
                S.reduce(m2, l2, ALU.max)
                S.ts(eq, lg, m2, None, ALU.is_ge)
                S.ts(nm1, m1, -1.0, None, ALU.mult)
                S.act(ex, lg, AF.Exp, bias=nm1)
                S.tt(ex, ex, eq, ALU.mult)
                S.reduce(sm, ex, ALU.add)
                S.recip(rs, sm)
                S.ts(gate, ex, rs, None, ALU.mult)
                ps2 = K.bank()
                S.tr(ps2[0:8, 0:128], gate, K.ident[:])
                S.copy(gateT[:, ti * 128:(ti + 1) * 128], ps2[0:8, 0:128])
            for e in range(E):
                Gb = Gbs[0]
                for (h0, hw) in halves:
                    ps = K.bank()
                    S.mm(ps[:, :hw], sel[:, e, :], gateT[:, h0:h0 + hw])
                    S.copy(Gb[:, h0:h0 + hw], ps[:, :hw], e='act')
                for f in range(NF):
                    wg, wu = wgs[wi % 2], wus[wi % 2]
                    s1, s2 = wst[(2 * wi) % 4], wst[(2 * wi + 1) % 4]
                    wi += 1
                    S.dma(s1[:], wg_v[e][:, :, f * 128:(f + 1) * 128], q='sp')
                    S.dma(s2[:], wu_v[e][:, :, f * 128:(f + 1) * 128], q='sp')
                    S.copy(r32(wg[:]), s1[:], e='act')
                    S.copy(r32(wu[:]), s2[:], e='dve')
                    for hi, (h0, hw) in enumerate(halves):
                        sg = sgs[hi]
                        psg, psu = K.bank(), K.bank()
                        for k in range(8):
                            S.mm(psg[:, :hw], wg[:, k, :], h2T[:, k, h0:h0 + hw], start=(k == 0), stop=(k == 7), fast=True)
                        for k in range(8):
                            S.mm(psu[:, :hw], wu[:, k, :], h2T[:, k, h0:h0 + hw], start=(k == 0), stop=(k == 7), fast=True)
                        S.act(sg[:, :hw], psg[:, :hw], AF.Silu)
                        S.tt(r32(actT[:, f, h0:h0 + hw]), sg[:, :hw], psu[:, :hw], ALU.mult)
                        S.tt(r32(actT[:, f, h0:h0 + hw]), actT[:, f, h0:h0 + hw], Gb[:, h0:h0 + hw], ALU.mult, e='pool')
                for cchunk in range(8):
                    wd, sd = wds[wdi % 2], wdst[wdi % 2]
                    wdi += 1
                    S.dma(sd[:], wd_v[e][:, :, cchunk * 128:(cchunk + 1) * 128], q='sp')
                    S.copy(r32(wd[:]), sd[:], e='pool')
                    for (h0, hw) in halves:
                        ps = K.bank()
                        for f in range(NF):
                            S.mm(ps[:, :hw], wd[:, f, :], actT[:, f, h0:h0 + hw], start=(f == 0), stop=(f == NF - 1), fast=True)
                        if e == 0:
                            S.copy(yT[:, cchunk, h0:h0 + hw], ps[:, :hw], e='act')
                        else:
                            S.tt(yT[:, cchunk, h0:h0 + hw], yT[:, cchunk, h0:h0 + hw], ps[:, :hw], ALU.add)
            for ti in range(nt):
                t = t0 + ti
                yt = tmp[0]
                S.dma(xt[:], D['xres'][t * 128:(t + 1) * 128, :], q='act')
                for b in range(2):
                    ps = K.bank()
                    for j in range(4):
                        S.tr(ps[:, j * 128:(j + 1) * 128], yT[:, b * 4 + j, ti * 128:(ti + 1) * 128], K.ident[:])
                    S.tt(yt[:, b * 512:(b + 1) * 512], ps[:, :], GT2[:, b * 512:(b + 1) * 512], ALU.mult)
                S.tt(tmp[2][:], xt[:], yt[:], ALU.add, e='pool')
                S.dma(D['xres'][t * 128:(t + 1) * 128, :], tmp[2][:], q='act')
    S.barrier()


def stage_final(K):
    S, W, D = K.S, K.W, K.D
    with ExitStack() as es:
        g = K.sb(es, [128, 1024])
        S.dma(g[:], bc_rows(W['final_norm_g'], 128))
        xts = [K.sb(es, [128, 1024]) for _ in range(2)]
        ots = [K.sb(es, [128, 1024]) for _ in range(2)]
        junk = K.sb(es, [128, 1024])
        sss = [K.sb(es, [128, 4]) for _ in range(2)]
        for t in range(2, NT):
            xt, ot, ss = xts[t % 2], ots[t % 2], sss[t % 2]
            S.dma(xt[:], D['xres'][t * 128:(t + 1) * 128, :])
            S.act(junk[:], xt[:], AF.Square, accum_out=ss[:, 0:1])
            S.ts(ss[:, 1:2], ss[:, 0:1], 1.0 / D_MODEL, EPS, ALU.mult, ALU.add)
            S.act(ss[:, 2:3], ss[:, 1:2], AF.Sqrt)
            S.recip(ss[:, 3:4], ss[:, 2:3])
            S.stt(ot[:], xt[:], ss[:, 3:4], g[:], ALU.mult, ALU.mult)
            S.dma(K.out[(t - 2) * 128:(t - 1) * 128, :], ot[:], q='pool')
    S.barrier()


def mix_identity(K, l):
    S, D = K.S, K.D
    with ExitStack() as es:
        pts = [K.sb(es, [128, 8, 128]) for _ in range(2)]
        yts = [K.sb(es, [128, 1024]) for _ in range(2)]
        pv = D['pT'][0:1024, :].rearrange("(k p) t -> p k t", p=128)
        for t in range(NT):
            pt, yt = pts[t % 2], yts[t % 2]
            S.dma(pt[:], pv[:, :, t * 128:(t + 1) * 128])
            for b in range(2):
                ps = K.bank()
                for j in range(4):
                    S.tr(ps[:, j * 128:(j + 1) * 128], pt[:, b * 4 + j, :], K.ident[:])
                S.copy(yt[:, b * 512:(b + 1) * 512], ps[:, :], e=('dve' if b == 0 else 'act'))
            S.dma(D['y'][t * 128:(t + 1) * 128, :], yt[:], q='pool')
    S.barrier()


def to_token_major(K, es, src, ncols_tile, dst_cols, bufs):
    S, D = K.S, K.D
    gi = 0
    for t0 in range(0, NT, 4):
        n = min(4, NT - t0)
        ps = K.bank()
        for j in range(n):
            S.tr(ps[:, j * 128:(j + 1) * 128], src[:, (t0 + j) * 128:(t0 + j + 1) * 128], K.ident[:])
        yb = bufs[gi % 2]
        S.copy(yb[:, :n * 128], ps[:, :n * 128], e=('dve' if gi % 2 == 0 else 'act'))
        dst = D['y'][t0 * 128:(t0 + n) * 128, dst_cols[0]:dst_cols[1]].rearrange("(j p) c -> p j c", p=128)
        S.dma(dst, yb[:, :n * 128].rearrange("p (j c) -> p j c", j=n), q=('sp' if gi % 2 == 0 else 'pool'))
        gi += 1


def mixer_a(K, l, last):
    S, W, D = K.S, K.W, K.D
    SEGS = [(0, CTX), (CTX, T)]
    with ExitStack() as es0:
        ybufs = [K.sb(es0, [128, 512]) for _ in range(2)]
        for ct in range(2):
            with ExitStack() as es:
                pk = K.sb(es, [128, 16])
                S.dma(pk[:, 0:11], W['pk_lru'][l, ct])
                S.act(pk[:, 11:13], pk[:, 9:11], AF.Exp, scale=-1.0)
                S.act(pk[:, 11:13], pk[:, 11:13], AF.Ln, bias=1.0)
                S.ts(pk[:, 13:15], pk[:, 11:13], -16.0, None, ALU.mult)
                S.ts(pk[:, 11:13], pk[:, 11:13], -8.0, None, ALU.mult)
                wbd = K.sb(es, [128, 4, 128])
                S.memset(wbd[:], 0.0)
                for d in range(2):
                    for wi, wn in enumerate(('lru_w_a', 'lru_w_x')):
                        for hl in range(2):
                            S.dma(wbd[hl * 64:(hl + 1) * 64, d * 2 + wi, hl * 64:(hl + 1) * 64], W[wn][l, d, ct * 2 + hl], q='pool')
                xb = K.sb(es, [128, T])
                u = K.sb(es, [128, T])
                gt = K.sb(es, [128, T])
                ra = K.sb(es, [128, T])
                ib = K.sb(es, [128, T])
                h0 = K.sb(es, [128, T])
                h1 = K.sb(es, [128, T])
                S.dma(xb[:], D['pT'][ct * 128:(ct + 1) * 128, :])
                S.dma(gt[:], D['pT'][256 + ct * 128:256 + (ct + 1) * 128, :], q='pool')
                S.ts(u[:], xb[:], pk[:, 2:3], pk[:, 4:5], ALU.mult, ALU.add)
                for (a, b) in SEGS:
                    for j, s in ((0, -2), (1, -1), (3, 1)):
                        lo, hi = max(a, a - s), min(b, b - s)
                        S.stt(u[:, lo:hi], xb[:, lo + s:hi + s], pk[:, j:j + 1], u[:, lo:hi], ALU.mult, ALU.add)
                S.tt(xb[:], gt[:], gt[:], ALU.mult, e='pool')
                S.ts(xb[:], xb[:], 0.044715, 1.0, ALU.mult, ALU.add, e='pool')
                S.tt(xb[:], xb[:], gt[:], ALU.mult, e='pool')
                S.act(xb[:], xb[:], AF.Sigmoid, scale=1.5957691216057308)
                S.tt(gt[:], gt[:], xb[:], ALU.mult, e='pool')
                for d in range(2):
                    blocks = [(n0, min(512, T - n0)) for n0 in range(0, T, 512)]
                    for wi, dst, bcol in ((0, ra, 5 + d), (1, ib, 7 + d)):
                        for (n0, nw) in blocks:
                            ps = K.bank()
                            S.mm(ps[:, :nw], wbd[:, d * 2 + wi, :], u[:, n0:n0 + nw])
                            S.act(dst[:, n0:n0 + nw], ps[:, :nw], AF.Sigmoid, bias=pk[:, bcol:bcol + 1])
                    hd = h0 if d == 0 else h1
                    S.tt(ib[:], ib[:], u[:], ALU.mult, e='pool')
                    S.act(hd[:], ra[:], AF.Exp, scale=pk[:, 13 + d:14 + d])
                    S.ts(hd[:], hd[:], -1.0, 1.0, ALU.mult, ALU.add)
                    S.act(hd[:], hd[:], AF.Sqrt)
                    S.tt(ib[:], ib[:], hd[:], ALU.mult)
                    S.act(ra[:], ra[:], AF.Exp, scale=pk[:, 11 + d:12 + d])
                    if d == 0:
                        S.scan(h0[:], ra[:], ib[:], 0.0, ALU.mult, ALU.add)
                    else:
                        S.scan(h1[:, 0:CTX][:, ::-1], ra[:, 0:CTX][:, ::-1], ib[:, 0:CTX][:, ::-1], 0.0, ALU.mult, ALU.add)
                        S.scan(h1[:, CTX:T][:, ::-1], ra[:, CTX:T][:, ::-1], ib[:, CTX:T][:, ::-1], h1[:, 0:1], ALU.mult, ALU.add)
                S.tt(h0[:], h0[:], h1[:], ALU.add, e='pool')
                S.tt(h0[:], h0[:], gt[:], ALU.mult)
                to_token_major(K, es, h0, 128, (ct * 128, (ct + 1) * 128), ybufs)
            S.barrier()
    S.barrier()


def chunk_cols(n):
    if n < 4:
        return slice(n * 64, (n + 1) * 64)
    c = n - 4
    return slice(CTX + c, T, 64)


NCH = T // 64
DBG = {}


def to_traversal(S, dst, src, e='dve'):
    S.copy(dst[:, 0:CTX], src[:, 0:CTX], e=e)
    S.copy(dst[:, CTX:T].rearrange("p (c r) -> p c r", r=64), src[:, CTX:T].rearrange("p (r c) -> p c r", c=64), e=e)


def scan_order(d):
    if d == 0:
        return list(range(NCH))
    return [3, 2, 1, 0] + list(range(NCH - 1, 3, -1))


def mixer_c(K, l, last):
    S, W, D = K.S, K.W, K.D
    base = OFF_C
    NQ = 6
    with ExitStack() as es0:
        tokcol = K.sb(es0, [64, NCH, NQ * 8])
        masks = K.sb(es0, [64, 2, 64])
        S.dma(masks[:], W['c_masks'])
        ngt = K.sb(es0, [64, 256])
        S.dma(ngt[:], bc_rows(W['mlstm_norm_g'][l:l + 1, :], 64))
        with ExitStack() as es:
            stack = K.sb(es, [NQ * 8, T])
            pk = K.sb(es, [4, 8])
            S.dma(pk[:, 0:4], W['pk_ml'][l])
            S.ts(pk[:, 4:8], pk[:, 0:4], -1.0, None, ALU.mult)
            rst = K.sb(es, [4, T])
            nbg = K.sb(es, [4, T])
            graw = K.sb(es, [4, T])
            li = K.sb(es, [4, T])
            lf = K.sb(es, [4, T])
            bb = K.sb(es, [4, T])
            gg = K.sb(es, [4, T])
            cm = K.sb(es, [4, T])
            mx = K.sb(es, [4, T])
            tmp = graw
            sm = K.sb(es, [4, 8, NCH])
            v3 = lambda t: t[:, :].rearrange("p (n i) -> p n i", i=64)
            bcn = lambda a: a.unsqueeze(2).to_broadcast([4, NCH, 64])
            for d in range(2):
                rv = (lambda a: a) if d == 0 else (lambda a: a[:, ::-1])
                lastidx = 63 if d == 0 else 0
                S.dma(rst[:], W['c_rst'][:, d, :], q='pool')
                S.ts(nbg[:], rst[:], 1e30, -1e30, ALU.mult, ALU.add)
                S.dma(graw[:], D['pT'][base + 1024 + d * 4: base + 1024 + d * 4 + 4, :])
                to_traversal(S, li, graw)
                S.ts(li[:], li[:], pk[:, d:d + 1], None, ALU.add)
                S.dma(graw[:], D['pT'][base + 1024 + 8 + d * 4: base + 1024 + 8 + d * 4 + 4, :])
                to_traversal(S, lf, graw)
                S.act(lf[:], lf[:], AF.Exp, scale=-1.0, bias=pk[:, 6 + d:7 + d])
                S.act(lf[:], lf[:], AF.Ln, bias=1.0)
                S.ts(lf[:], lf[:], -1.0, None, ALU.mult)
                S.scan(rv(bb[:, :]), rv(rst[:, :]), rv(lf[:, :]), 0.0, ALU.mult, ALU.add)
                S.tt(gg[:], li[:], bb[:], ALU.subtract)
                S.scan(rv(cm[:, :]), rv(nbg[:, :]), rv(gg[:, :]), 0.0, ALU.add, ALU.max)
                bL, cmL = v3(bb)[:, :, lastidx], v3(cm)[:, :, lastidx]
                d1t, mm_, mprev, e4, t5 = sm[:, 0, :], sm[:, 1, :], sm[:, 2, :], sm[:, 3, :], sm[:, 4, :]
                S.tt(d1t, bL, cmL, ALU.add)
                if d == 0:
                    S.scan(mm_, bL, d1t, 0.0, ALU.add, ALU.max)
                    S.memset(mprev[:, 0:1], 0.0)
                    S.copy(mprev[:, 1:NCH], mm_[:, 0:NCH - 1])
                else:
                    S.scan(mm_[:, 0:4][:, ::-1], bL[:, 0:4][:, ::-1], d1t[:, 0:4][:, ::-1], 0.0, ALU.add, ALU.max)
                    S.scan(mm_[:, 4:NCH][:, ::-1], bL[:, 4:NCH][:, ::-1], d1t[:, 4:NCH][:, ::-1], mm_[:, 0:1], ALU.add, ALU.max)
                    S.copy(mprev[:, 0:3], mm_[:, 1:4])
                    S.memset(mprev[:, 3:4], 0.0)
                    S.copy(mprev[:, 4:NCH - 1], mm_[:, 5:NCH])
                    S.copy(mprev[:, NCH - 1:NCH], mm_[:, 0:1])
                S.tt(v3(mx), v3(cm), bcn(mprev), ALU.max)
                def put(q, src):
                    r0 = q * 8 + d * 4
                    S.dma(stack[r0:r0 + 4, :], src, q='pool')
                S.act(tmp[:], mx[:], AF.Exp, scale=-1.0)
                put(0, tmp[:])
                S.tt(v3(tmp), bcn(mprev), v3(mx), ALU.subtract)
                S.act(tmp[:], tmp[:], AF.Exp)
                put(1, tmp[:])
                S.tt(tmp[:], bb[:], mx[:], ALU.add)
                S.act(tmp[:], tmp[:], AF.Exp, scale=-1.0)
                put(2, tmp[:])
                S.act(tmp[:], gg[:], AF.Exp)
                put(3, tmp[:])
                S.tt(e4, bL, mprev, ALU.add)
                S.tt(e4, e4, mm_, ALU.subtract)
                S.act(e4, e4, AF.Exp)
                S.copy(v3(tmp), bcn(e4))
                put(4, tmp[:])
                S.tt(t5, bL, mm_, ALU.subtract)
                S.tt(v3(tmp), v3(gg), bcn(t5), ALU.add)
                S.act(tmp[:], tmp[:], AF.Exp)
                put(5, tmp[:])
            NR = NQ * 8
            for n0 in range(0, NCH, 8):
                nn = min(8, NCH - n0)
                ps = K.bank()
                for j in range(nn):
                    S.tr(ps[0:64, j * NR:(j + 1) * NR], stack[0:NR, (n0 + j) * 64:(n0 + j + 1) * 64], K.ident[0:NR, 0:NR])
                S.copy(tokcol[:, n0:n0 + nn, :], ps[0:64, 0:nn * NR].rearrange("p (j c) -> p j c", c=NR))
        S.barrier()
        for h in range(4):
            with ExitStack() as es:
                qT = K.sb(es, [64, T])
                kT = K.sb(es, [64, T])
                vT = K.sb(es, [64, T])
                ktok = K.sb(es, [64, NCH, 64])
                vtok = K.sb(es, [64, NCH, 65])
                hacc = K.sb(es, [64, NCH, 64])
                osig = K.sb(es, [64, NCH, 64])
                S.dma(qT[:], D['pT'][base + h * 64: base + (h + 1) * 64, :])
                S.dma(kT[:], D['pT'][base + 256 + h * 64: base + 256 + (h + 1) * 64, :], q='pool')
                S.dma(vT[:], D['pT'][base + 512 + h * 64: base + 512 + (h + 1) * 64, :], q='act')
                S.ts(kT[:], kT[:], 0.125, None, ALU.mult, e='pool')
                S.memset(vtok[:, :, 64:65], 1.0)
                for n0 in range(0, NCH, 8):
                    nn = min(8, NCH - n0)
                    ps1, ps2 = K.bank(), K.bank()
                    for j in range(nn):
                        cs = chunk_cols(n0 + j)
                        S.tr(ps1[0:64, j * 64:(j + 1) * 64], kT[:, cs], K.ident[0:64, 0:64])
                        S.tr(ps2[0:64, j * 64:(j + 1) * 64], vT[:, cs], K.ident[0:64, 0:64])
                    S.copy(ktok[:, n0:n0 + nn, :], ps1[0:64, 0:nn * 64].rearrange("p (j c) -> p j c", c=64), e='act')
                    S.copy(vtok[:, n0:n0 + nn, 0:64], ps2[0:64, 0:nn * 64].rearrange("p (j c) -> p j c", c=64))
                S.dma(vT[:], D['pT'][base + 768 + h * 64: base + 768 + (h + 1) * 64, :], q='act')
                S.memset(hacc[:], 0.0, e='pool')
                gens = []
                for d in range(2):
                    Cs = [K.sb(es, [64, 65]) for _ in range(2)]
                    S.memset(Cs[0][:], 0.0)
                    pts = [K.sb(es, [64, 64]) for _ in range(2)]
                    tts = [K.sb(es, [64, 65]) for _ in range(2)]
                    vws = [K.sb(es, [64, 65]) for _ in range(2)]
                    dns = [K.sb(es, [64, 2]) for _ in range(2)]

                    def chain(d=d, Cs=Cs, pts=pts, tts=tts, vws=vws, dns=dns):
                        col = lambda q: (lambda n: tokcol[:, n, q * 8 + d * 4 + h: q * 8 + d * 4 + h + 1])
                        c1, c2, c3, eg, c4, wn = [col(q) for q in range(6)]
                        st = {'banks': [4 * d + i for i in range(4)], 'pz': 0}
                        for si, n in enumerate(scan_order(d)):
                            cs = chunk_cols(n)
                            Cc, Cn = Cs[si % 2], Cs[(si + 1) % 2]
                            pt, tot, vw, dn = pts[si % 2], tts[si % 2], vws[si % 2], dns[si % 2]
                            ps_s = chain_ps(K, st, 128)
                            S.mm(ps_s[0:64, 0:64], kT[:, cs], qT[:, cs])
                            ps_i = chain_ps(K, st, 128)
                            S.mm(ps_i[0:64, 0:65], qT[:, cs], Cc[:])
                            yield
                            S.stt(pt[:], ps_s[0:64, 0:64], eg(n), masks[:, d, :], ALU.mult, ALU.mult)
                            S.ts(vw[:], vtok[:, n, :], wn(n), None, ALU.mult, e='pool')
                            yield
                            ps_o = chain_ps(K, st, 128)
                            S.mm(ps_o[0:64, 0:65], pt[:], vtok[:, n, :])
                            ps_c = chain_ps(K, st, 128)
                            S.mm(ps_c[0:64, 0:65], ktok[:, n, :], vw[:])
                            yield
                            S.ts(tot[:], ps_o[0:64, 0:65], c1(n), None, ALU.mult)
                            yield
                            S.stt(Cn[:], Cc[:], c4(n), ps_c[0:64, 0:65], ALU.mult, ALU.add)
                            yield
                            S.stt(tot[:], ps_i[0:64, 0:65], c2(n), tot[:], ALU.mult, ALU.add)
                            yield
                            S.act(dn[:, 0:1], tot[:, 64:65], AF.Abs)
                            yield
                            S.ts(dn[:, 0:1], dn[:, 0:1], c3(n), None, ALU.max)
                            yield
                            S.recip(dn[:, 1:2], dn[:, 0:1])
                            yield
                            S.stt(hacc[:, n, :], tot[:, 0:64], dn[:, 1:2], hacc[:, n, :], ALU.mult, ALU.add)
                            yield
                    gens.append(chain())
                while gens:
                    for g in list(gens):
                        try:
                            next(g)
                        except StopIteration:
                            gens.remove(g)
                for n0 in range(0, NCH, 8):
                    nn = min(8, NCH - n0)
                    ps1 = K.bank()
                    for j in range(nn):
                        S.tr(ps1[0:64, j * 64:(j + 1) * 64], vT[:, chunk_cols(n0 + j)], K.ident[0:64, 0:64])
                    S.act(osig[:, n0:n0 + nn, :], ps1[0:64, 0:nn * 64].rearrange("p (j c) -> p j c", c=64), AF.Sigmoid)
                sq = K.sb(es, [64, NCH, 64])
                ssq = K.sb(es, [64, NCH, 4])
                S.tt(sq[:], hacc[:], hacc[:], ALU.mult, e='pool')
                S.reduce(ssq[:, :, 0], sq[:], ALU.add)
                S.ts(ssq[:, :, 1], ssq[:, :, 0], 1.0 / 64, EPS, ALU.mult, ALU.add)
                S.act(ssq[:, :, 2], ssq[:, :, 1], AF.Sqrt)
                S.recip(ssq[:, :, 3], ssq[:, :, 2])
                S.tt(hacc[:], hacc[:], ssq[:, :, 3].unsqueeze(2).to_broadcast([64, NCH, 64]), ALU.mult)
                S.tt(hacc[:], hacc[:], ngt[:, h * 64:(h + 1) * 64].unsqueeze(1).to_broadcast([64, NCH, 64]), ALU.mult, e='pool')
                S.tt(hacc[:], hacc[:], osig[:], ALU.mult)
                c0 = 512 + h * 64
                S.dma(D['y'][0:CTX, c0:c0 + 64].rearrange("(n i) c -> i n c", i=64), hacc[:, 0:4, :])
                yv = D['y'][CTX:T, c0:c0 + 64].rearrange("(r c) ch -> r c ch", c=64)
                for g4 in range(4):
                    S.dma(yv[:, g4 * 16:(g4 + 1) * 16, :], hacc[:, 4 + g4 * 16:4 + (g4 + 1) * 16, :], q=('sp', 'pool')[g4 % 2])
            S.barrier()
    S.barrier()


def dwconv_trav(S, out, x, wcol, bias=None):
    if bias is None:
        S.ts(out[:], x[:], wcol(2), None, ALU.mult)
    else:
        S.ts(out[:], x[:], wcol(2), bias, ALU.mult, ALU.add)
    for (a, b) in ((0, CTX), (CTX, T)):
        for j, s in ((0, -2), (1, -1), (3, 1)):
            lo, hi = max(a, a - s), min(b, b - s)
            S.stt(out[:, lo:hi], x[:, lo + s:hi + s], wcol(j), out[:, lo:hi], ALU.mult, ALU.add)


def neumann_inverse(K, S, P, PT, B, BT, tmps):
    B2s, B2Ts = tmps
    cb, cbt = B, BT
    for m in range(1, 6):
        lastm = (m == 5)
        ps1 = K.bank()
        S.mm(ps1[:, 0:128], cbt[:], cb[:])
        nb = B2s[m % 2]
        S.copy(nb[:], ps1[:, 0:128], e='act')
        if not lastm:
            ps2 = K.bank()
            S.mm(ps2[:, 0:128], cb[:], cbt[:])
            nbt = B2Ts[m % 2]
            S.copy(nbt[:], ps2[:, 0:128], e='dve')
        ps3 = K.bank()
        S.mm(ps3[:, 0:128], PT[:], nb[:])
        if not lastm:
            ps4 = K.bank()
            S.mm(ps4[:, 0:128], nb[:], PT[:])
        S.tt(P[:], P[:], ps3[:, 0:128], ALU.add)
        if not lastm:
            S.tt(PT[:], PT[:], ps4[:, 0:128], ALU.add)
            cb, cbt = nb, nbt


def chain_ps(K, st, width=256):
    i = st['pz']
    st['pz'] = i + 1
    nslots = 512 // width
    banks = st['banks']
    j = i % (nslots * len(banks))
    o = (j % nslots) * width
    return K.ps[banks[j // nslots]][:, o:o + width]


def run_pipeline(n, prep_gen, rec_gen, sets):
    NS = len(sets)
    assert NS <= 7
    for bi, st in enumerate(sets):
        st['banks'] = [bi]
        st['pz'] = 0
    preps = {}
    done = set()
    next_prep = 0
    rec_i = 0
    rec = None
    while rec_i < n:
        while next_prep < n and next_prep < rec_i + NS:
            preps[next_prep] = prep_gen(next_prep, sets[next_prep % NS])
            next_prep += 1
        if rec is None and rec_i in done:
            rec = rec_gen(rec_i, sets[rec_i % NS])
        if rec is not None:
            try:
                next(rec)
            except StopIteration:
                rec = None
                rec_i += 1
                continue
        for j in sorted(preps):
            try:
                next(preps[j])
            except StopIteration:
                del preps[j]
                done.add(j)


NEU_FAST = False


def n32(ap):
    return ap.bitcast(F32R) if (NEU_FAST and FAST_MM) else ap


def neumann_gen(K, S, PP, BB, N2, st):
    cb, cbt = BB[:, 0, :], BB[:, 1, :]
    for m in range(1, 6):
        lastm = (m == 5)
        nbb = N2[m % 2]
        psa = chain_ps(K, st)
        S.mm(psa[:, 0:128], cbt, cb, fast=NEU_FAST)
        if not lastm:
            S.mm(psa[:, 128:256], cb, cbt, fast=NEU_FAST)
        yield
        if lastm:
            S.copy(n32(nbb[:, 0, :]), psa[:, 0:128], e='act')
        else:
            S.copy(n32(nbb[:]), psa[:, 0:256].rearrange("p (a c) -> p a c", a=2), e='act')
        yield
        psb = chain_ps(K, st)
        S.mm(psb[:, 0:128], PP[:, 1, :], nbb[:, 0, :], fast=NEU_FAST)
        if not lastm:
            S.mm(psb[:, 128:256], nbb[:, 0, :], PP[:, 1, :], fast=NEU_FAST)
        yield
        if lastm:
            S.tt(n32(PP[:, 0, :]), PP[:, 0, :], psb[:, 0:128], ALU.add)
        else:
            S.tt(n32(PP[:]), PP[:], psb[:, 0:256].rearrange("p (a c) -> p a c", a=2), ALU.add)
        yield
        cb, cbt = nbb[:, 0, :], nbb[:, 1, :]


def mixer_d(K, l, last):
    S, W, D = K.S, K.W, K.D
    base = OFF_D
    NQ = 6
    NR = NQ * 8
    NP = NCH // 2
    with ExitStack() as es0:
        tok64 = K.sb(es0, [64, NCH, NR])
        tokP = K.sb(es0, [128, NP, NR])
        stack = K.sb(es0, [NR, T])
        masks = K.sb(es0, [64, 2, 64])
        S.dma(masks[:], W['c_masks'])
        m128 = K.sb(es0, [128, 4, 128])
        S.dma(m128[:], W['c_m128'])
        sel = K.sb(es0, [8, 8, 128])
        S.dma(sel[:], W['c_sel8'])
        ngt = K.sb(es0, [64, 256])
        S.dma(ngt[:], bc_rows(W['gdn_norm_g'][l:l + 1, :], 64))
        with ExitStack() as es:
            pk = K.sb(es, [4, 8])
            S.dma(pk[:, 0:4], W['pk_gd'][l])
            S.act(pk[:, 4:6], pk[:, 0:2], AF.Exp)
            S.ts(pk[:, 4:6], pk[:, 4:6], -1.0, None, ALU.mult)
            rst = K.sb(es, [4, T])
            graw = K.sb(es, [4, T])
            la = K.sb(es, [4, T])
            bt = K.sb(es, [4, T])
            gam = K.sb(es, [4, T])
            tmp = K.sb(es, [4, T])
            sm = K.sb(es, [4, 4, NCH])
            v3 = lambda t: t[:, :].rearrange("p (n i) -> p n i", i=64)
            bcn = lambda a: a.unsqueeze(2).to_broadcast([4, NCH, 64])
            for d in range(2):
                rv = (lambda a: a) if d == 0 else (lambda a: a[:, ::-1])
                lastidx = 63 if d == 0 else 0
                S.dma(rst[:], W['c_rst'][:, d, :], q='pool')
                S.dma(graw[:], D['pT'][base + 1024 + d * 4: base + 1024 + d * 4 + 4, :])
                to_traversal(S, la, graw)
                S.act(la[:], la[:], AF.Exp, bias=pk[:, 2 + d:3 + d])
                S.act(la[:], la[:], AF.Ln, bias=1.0)
                S.ts(la[:], la[:], pk[:, 4 + d:5 + d], None, ALU.mult)
                S.dma(graw[:], D['pT'][base + 1024 + 8 + d * 4: base + 1024 + 8 + d * 4 + 4, :])
                to_traversal(S, bt, graw)
                S.act(bt[:], bt[:], AF.Sigmoid)
                S.scan(rv(gam[:, :]), rv(rst[:, :]), rv(la[:, :]), 0.0, ALU.mult, ALU.add)
                gL = v3(gam)[:, :, lastidx]
                def put(q, src):
                    r0 = q * 8 + d * 4
                    S.dma(stack[r0:r0 + 4, :], src, q='pool')
                put(0, gam[:])
                put(1, bt[:])
                S.act(tmp[:], gam[:], AF.Exp)
                S.tt(tmp[:], tmp[:], bt[:], ALU.mult)
                put(2, tmp[:])
                S.tt(v3(tmp), bcn(gL), v3(gam), ALU.subtract)
                S.act(tmp[:], tmp[:], AF.Exp)
                put(3, tmp[:])
                S.act(sm[:, 0, :], gL, AF.Exp)
                S.copy(v3(tmp), bcn(sm[:, 0, :]))
                put(4, tmp[:])
                S.ts(tmp[:], bt[:], -1.0, None, ALU.mult)
                put(5, tmp[:])
            for n0 in range(0, NCH, 8):
                nn = min(8, NCH - n0)
                ps = K.bank()
                for j in range(nn):
                    S.tr(ps[0:64, j * NR:(j + 1) * NR], stack[0:NR, (n0 + j) * 64:(n0 + j + 1) * 64], K.ident[0:NR, 0:NR])
                S.copy(tok64[:, n0:n0 + nn, :], ps[0:64, 0:nn * NR].rearrange("p (j c) -> p j c", c=NR))
            for n0 in range(0, NP, 8):
                nn = min(8, NP - n0)
                ps = K.bank()
                for j in range(nn):
                    S.tr(ps[:, j * NR:(j + 1) * NR], stack[0:NR, (n0 + j) * 128:(n0 + j + 1) * 128], K.ident[0:NR, 0:NR])
                S.copy(tokP[:, n0:n0 + nn, :], ps[:, 0:nn * NR].rearrange("p (j c) -> p j c", c=NR), e='act')
        S.barrier()
        if DBG.get('d_stop') == 1:
            return
        for h in range(DBG.get('d_heads', 4)):
            with ExitStack() as es:
                trv = K.sb(es, [64, T])
                qT = K.sb(es, [64, T])
                kT = K.sb(es, [64, T])
                ktok = K.sb(es, [64, NCH, 64])
                kP = K.sb(es, [128, NP, 64])
                vP = K.sb(es, [128, NP, 64])
                hacc = K.sb(es, [64, NCH, 64])
                cw = K.sb(es, [64, 3, 4])
                ones = K.sb(es, [64, 64])
                es_a = ExitStack()
                raw = K.sb(es_a, [64, T])
                vT = K.sb(es_a, [64, T])
                S.memset(ones[:], 1.0)
                S.dma(cw[:], W['pk_gdc'][l, h])
                for gi, dst in enumerate((qT, kT, vT)):
                    S.dma(raw[:], D['pT'][base + gi * 256 + h * 64: base + gi * 256 + (h + 1) * 64, :])
                    to_traversal(S, trv, raw, e='pool')
                    dwconv_trav(S, dst, trv, lambda j: cw[:, gi, j:j + 1])
                    S.act(dst[:], dst[:], AF.Silu)
                    if gi < 2:
                        S.tt(trv[:], dst[:], dst[:], ALU.mult, e='pool')
                        for n0 in range(0, T, 512):
                            nw = min(512, T - n0)
                            ps = K.bank()
                            S.mm(ps[0:64, :nw], ones[:], trv[:, n0:n0 + nw])
                            S.ts(raw[:, n0:n0 + nw], ps[0:64, :nw], EPS, None, ALU.add)
                        S.act(raw[:], raw[:], AF.Sqrt)
                        S.recip(raw[:], raw[:])
                        if gi == 0:
                            S.stt(dst[:], dst[:], 0.125, raw[:], ALU.mult, ALU.mult)
                        else:
                            S.tt(dst[:], dst[:], raw[:], ALU.mult)
                for n0 in range(0, NCH, 8):
                    nn = min(8, NCH - n0)
                    ps1 = K.bank()
                    for j in range(nn):
                        S.tr(ps1[0:64, j * 64:(j + 1) * 64], kT[:, (n0 + j) * 64:(n0 + j + 1) * 64], K.ident[0:64, 0:64])
                    S.copy(ktok[:, n0:n0 + nn, :], ps1[0:64, 0:nn * 64].rearrange("p (j c) -> p j c", c=64), e='act')
                for n0 in range(0, NP, 8):
                    nn = min(8, NP - n0)
                    ps1, ps2 = K.bank(), K.bank()
                    for j in range(nn):
                        S.tr(ps1[:, j * 64:(j + 1) * 64], kT[:, (n0 + j) * 128:(n0 + j + 1) * 128], K.ident[0:64, 0:64])
                        S.tr(ps2[:, j * 64:(j + 1) * 64], vT[:, (n0 + j) * 128:(n0 + j + 1) * 128], K.ident[0:64, 0:64])
                    S.copy(kP[:, n0:n0 + nn, :], ps1[:, 0:nn * 64].rearrange("p (j c) -> p j c", c=64), e='act')
                    S.copy(vP[:, n0:n0 + nn, :], ps2[:, 0:nn * 64].rearrange("p (j c) -> p j c", c=64))
                if DBG.get('d_stop') == 2:
                    S.barrier()
                    return
                es_a.close()
                S.barrier()
                es_b = ExitStack()
                kdec = trv
                kdec3 = kdec[:, :].rearrange("p (n c) -> p n c", c=64)
                NS = DBG.get('d_ns', 6)
                sets = []
                for _s in range(NS):
                    sets.append(dict(
                        GB=K.sb(es_b, [128, 128]), dL=K.sb(es_b, [128, 128]), BB=K.sb(es_b, [128, 2, 128]),
                        PP=K.sb(es_b, [128, 2, 128]), N2=[K.sb(es_b, [128, 2, 128]) for _ in range(2)],
                        rU=K.sb(es_b, [128, 64]), rW=K.sb(es_b, [128, 64]), wT=K.sb(es_b, [64, 128]),
                        us=K.sb(es_b, [64, 2, 64]), qk=K.sb(es_b, [64, 2, 64]), qd=K.sb(es_b, [64, 128])))
                vns = [K.sb(es_b, [64, 64]) for _ in range(2)]
                Ss = [K.sb(es_b, [64, 64]) for _ in range(2)]
                identB = K.ident[:, :].unsqueeze(1).to_broadcast([128, 2, 128])
                for d in range(2):
                    cP = lambda q, pi: tokP[:, pi, q * 8 + d * 4 + h: q * 8 + d * 4 + h + 1]
                    c64 = lambda q, n: tok64[:, n, q * 8 + d * 4 + h: q * 8 + d * 4 + h + 1]
                    S.tt(kdec3, ktok[:], tok64[:, :, 3 * 8 + d * 4 + h].unsqueeze(2).to_broadcast([64, NCH, 64]), ALU.mult, e='pool')
                    S.memset(Ss[0][:], 0.0)
                    order = scan_order(d)
                    pairs = [order[i] // 2 for i in range(0, NCH, 2)]
                    state = {'si': 0}

                    recst = {'banks': list(range(NS, 8)), 'pz': 0}
                    def prep(pidx, st):
                        pi = pairs[pidx]
                        GB, dL, BB, PP = st['GB'], st['dL'], st['BB'], st['PP']
                        tk = slice(pi * 128, (pi + 1) * 128)
                        ps = chain_ps(K, st)
                        S.mm(ps[:, 0:128], sel[:, d * 4 + h, :], stack[0:8, tk])
                        yield
                        S.copy(GB[:], ps[:, 0:128], e='act')
                        ps = chain_ps(K, st)
                        S.mm(ps[:, 0:128], kT[:, tk], kT[:, tk])
                        yield
                        S.ts(dL[:], GB[:], cP(0, pi), 0.0, ALU.subtract, ALU.max)
                        yield
                        S.act(dL[:], dL[:], AF.Exp, scale=-1.0)
                        yield
                        S.tt(dL[:], dL[:], m128[:, 3 - d, :], ALU.mult, e='pool')
                        yield
                        S.stt(n32(BB[:, 1, :]), ps[:, 0:128], cP(5, pi), dL[:], ALU.mult, ALU.mult)
                        yield
                        ps = chain_ps(K, st)
                        S.tr(ps[:, 0:128], BB[:, 1, :], K.ident[:])
                        yield
                        S.copy(n32(BB[:, 0, :]), ps[:, 0:128], e='act')
                        yield
                        S.tt(n32(PP[:]), BB[:], identB, ALU.add, e='pool')
                        yield
                        yield from neumann_gen(K, S, PP, BB, st['N2'], st)
                        P = PP[:, 0, :]
                        S.ts(st['rU'][:], vP[:, pi, :], cP(1, pi), None, ALU.mult, e='pool')
                        S.ts(st['rW'][:], kP[:, pi, :], cP(2, pi), None, ALU.mult, e='pool')
                        yield
                        ps = chain_ps(K, st)
                        S.mm(ps[0:64, 0:128], st['rW'][:], P)
                        for c in range(2):
                            S.mm(ps[0:64, 128 + c * 64:128 + (c + 1) * 64], PP[:, 0, c * 64:(c + 1) * 64], st['rU'][:])
                        yield
                        S.copy(st['wT'][:], ps[0:64, 0:128], e='act')
                        S.copy(st['us'][:], ps[0:64, 128:256].rearrange("p (c v) -> p c v", c=2), e='act')
                        yield
                        S.act(st['qd'][:], GB[0:64, :], AF.Exp)
                        yield
                        S.tt(st['qd'][:], st['qd'][:], qT[:, tk], ALU.mult, e='pool')
                        ps = chain_ps(K, st)
                        for c in range(2):
                            n = pi * 2 + c
                            ck = slice(n * 64, (n + 1) * 64)
                            S.mm(ps[0:64, c * 64:(c + 1) * 64], kT[:, ck], qT[:, ck])
                            S.ts(st['qk'][:, c, :], GB[0:64, c * 64:(c + 1) * 64], c64(0, n), 0.0, ALU.subtract, ALU.min)
                        yield
                        S.act(st['qk'][:], st['qk'][:], AF.Exp)
                        yield
                        S.tt(st['qk'][:], st['qk'][:], masks[:, d, :].unsqueeze(1).to_broadcast([64, 2, 64]), ALU.mult, e='pool')
                        yield
                        S.tt(st['qk'][:], st['qk'][:], ps[0:64, 0:128].rearrange("p (c i) -> p c i", c=2), ALU.mult)
                        yield

                    def rec(pidx, st):
                        pi = pairs[pidx]
                        for c in ((0, 1) if d == 0 else (1, 0)):
                            n = pi * 2 + c
                            si = state['si']
                            Sc, Sn = Ss[si % 2], Ss[(si + 1) % 2]
                            vn = vns[si % 2]
                            state['si'] = si + 1
                            ps1 = chain_ps(K, recst, 128)
                            S.mm(ps1[0:64, 0:64], st['wT'][:, c * 64:(c + 1) * 64], Sc[:])
                            yield
                            S.tt(vn[:], st['us'][:, c, :], ps1[0:64, 0:64], ALU.subtract)
                            yield
                            ps2 = chain_ps(K, recst, 128)
                            S.mm(ps2[0:64, 0:64], st['qd'][:, c * 64:(c + 1) * 64], Sc[:], start=True, stop=False)
                            S.mm(ps2[0:64, 0:64], st['qk'][:, c, :], vn[:], start=False, stop=True)
                            ps3 = chain_ps(K, recst, 128)
                            S.mm(ps3[0:64, 0:64], kdec3[:, n, :], vn[:])
                            yield
                            S.stt(Sn[:], Sc[:], c64(4, n), ps3[0:64, 0:64], ALU.mult, ALU.add)
                            if d == 0:
                                S.copy(hacc[:, n, :], ps2[0:64, 0:64], e='act')
                            else:
                                S.tt(hacc[:, n, :], hacc[:, n, :], ps2[0:64, 0:64], ALU.add)
                            yield

                    run_pipeline(min(len(pairs), DBG.get('d_pairs', 99)), prep, rec, sets)
                es_b.close()
                S.barrier()
                raw = K.sb(es, [64, T])
                S.dma(raw[:], D['pT'][base + 768 + h * 64: base + 768 + (h + 1) * 64, :])
                osig = kT[:, :].rearrange("p (n c) -> p n c", c=64)
                for n0 in range(0, NCH, 8):
                    nn = min(8, NCH - n0)
                    ps1 = K.bank()
                    for j in range(nn):
                        S.tr(ps1[0:64, j * 64:(j + 1) * 64], raw[:, chunk_cols(n0 + j)], K.ident[0:64, 0:64])
                    S.act(osig[:, n0:n0 + nn, :], ps1[0:64, 0:nn * 64].rearrange("p (j c) -> p j c", c=64), AF.Silu)
                sq = qT[:, :].rearrange("p (n c) -> p n c", c=64)
                ssq = K.sb(es, [64, NCH, 4])
                S.tt(sq, hacc[:], hacc[:], ALU.mult, e='pool')
                S.reduce(ssq[:, :, 0], sq, ALU.add)
                S.ts(ssq[:, :, 1], ssq[:, :, 0], 1.0 / 64, EPS, ALU.mult, ALU.add)
                S.act(ssq[:, :, 2], ssq[:, :, 1], AF.Sqrt)
                S.recip(ssq[:, :, 3], ssq[:, :, 2])
                S.tt(hacc[:], hacc[:], ssq[:, :, 3].unsqueeze(2).to_broadcast([64, NCH, 64]), ALU.mult)
                S.tt(hacc[:], hacc[:], ngt[:, h * 64:(h + 1) * 64].unsqueeze(1).to_broadcast([64, NCH, 64]), ALU.mult, e='pool')
                S.tt(hacc[:], hacc[:], osig, ALU.mult)
                c0 = 768 + h * 64
                S.dma(D['y'][0:CTX, c0:c0 + 64].rearrange("(n i) c -> i n c", i=64), hacc[:, 0:4, :])
                yv = D['y'][CTX:T, c0:c0 + 64].rearrange("(r c) ch -> r c ch", c=64)
                for g4 in range(4):
                    S.dma(yv[:, g4 * 16:(g4 + 1) * 16, :], hacc[:, 4 + g4 * 16:4 + (g4 + 1) * 16, :], q=('sp', 'pool')[g4 % 2])
            S.barrier()
    S.barrier()


def shift_T(S, out, x, mu, np_):
    S.ts(out[0:np_, :], x[0:np_, :], mu[0:np_, 2:3], None, ALU.mult)
    for (a, b) in ((0, CTX), (CTX, T)):
        S.stt(out[0:np_, a + 1:b], x[0:np_, a:b - 1], mu[0:np_, 0:1], out[0:np_, a + 1:b], ALU.mult, ALU.add)
        S.stt(out[0:np_, a:b - 1], x[0:np_, a + 1:b], mu[0:np_, 1:2], out[0:np_, a:b - 1], ALU.mult, ALU.add)


def load_mu(S, W, l, mu, row0, np_):
    S.dma(mu[0:np_, 0:2], W['pk_mu'][l, row0:row0 + np_, :], q='pool')
    S.ts(mu[0:np_, 2:3], mu[0:np_, 0:1], -1.0, 1.0, ALU.mult, ALU.add)
    S.tt(mu[0:np_, 2:3], mu[0:np_, 2:3], mu[0:np_, 1:2], ALU.subtract)


def mixer_b(K, l, last):
    S, W, D = K.S, K.W, K.D
    base = OFF_B
    NP = NCH // 2
    with ExitStack() as es0:
        masks = K.sb(es0, [64, 2, 64])
        S.dma(masks[:], W['c_masks'])
        m128 = K.sb(es0, [128, 4, 128])
        S.dma(m128[:], W['c_m128'])
        ones = K.sb(es0, [64, 64])
        S.memset(ones[:], 1.0)
        for h in range(DBG.get('b_heads', 4)):
            with ExitStack() as es:
                rT, kT, kkT = K.sb(es, [64, T]), K.sb(es, [64, T]), K.sb(es, [64, T])
                bh, ch, kh, rh = K.sb(es, [64, T]), K.sb(es, [64, T]), K.sb(es, [64, T]), K.sb(es, [64, T])
                Vtok = K.sb(es, [64, NCH, 64])
                Vpair = K.sb(es, [128, NP, 64])
                hacc = K.sb(es, [64, NCH, 64])
                pk = K.sb(es, [64, 8])
                S.dma(pk[:, 0:7], W['pk_rw'][l, h])
                mu = K.sb(es, [64, 3])
                wup = K.sb(es, [32, 2, 64])
                aup = K.sb(es, [32, 2, 64])
                gup = K.sb(es, [64, 64])
                for d in range(2):
                    S.dma(wup[:, d, :], W['rwkv_w_up'][l, d][:, h * 64:(h + 1) * 64], q='pool')
                    S.dma(aup[:, d, :], W['rwkv_a_up'][l, d][:, h * 64:(h + 1) * 64], q='pool')
                S.dma(gup[:], W['rwkv_g_up'][l][:, h * 64:(h + 1) * 64], q='pool')
                lng = K.sb(es, [64, 2, 64])
                S.dma(lng[:, 0, :], bc_rows(W['rwkv_ln_g'][l:l + 1, h * 64:(h + 1) * 64], 64))
                S.dma(lng[:, 1, :], bc_rows(W['rwkv_ln_b'][l:l + 1, h * 64:(h + 1) * 64], 64))
                bon = K.sb(es, [64, NCH])
                GLc = K.sb(es, [64, NCH])
                zns = [K.sb(es, [64, 64]) for _ in range(2)]
                Ms = [K.sb(es, [64, 64]) for _ in range(2)]
                mts = [K.sb(es, [64, 64]) for _ in range(2)]
                identB = K.ident[:, :].unsqueeze(1).to_broadcast([128, 2, 128])
                sgn = K.sb(es, [64, 4, 64])
                es_a = ExitStack()
                Lb = K.sb(es_a, [64, T])
                for gi, dst in enumerate((rT, kT, Lb)):
                    r0 = gi * 256 + h * 64
                    load_mu(S, W, l, mu, r0, 64)
                    S.dma(ch[:], D['pT'][base + r0: base + r0 + 64, :])
                    shift_T(S, dst, ch, mu, 64)
                for n0 in range(0, NCH, 8):
                    nn = min(8, NCH - n0)
                    ps1 = K.bank()
                    for j in range(nn):
                        S.tr(ps1[0:64, j * 64:(j + 1) * 64], Lb[:, (n0 + j) * 64:(n0 + j + 1) * 64], K.ident[0:64, 0:64])
                    S.copy(Vtok[:, n0:n0 + nn, :], ps1[0:64, 0:nn * 64].rearrange("p (j c) -> p j c", c=64), e='act')
                for n0 in range(0, NP, 8):
                    nn = min(8, NP - n0)
                    ps2 = K.bank()
                    for j in range(nn):
                        S.tr(ps2[:, j * 64:(j + 1) * 64], Lb[:, (n0 + j) * 128:(n0 + j + 1) * 128], K.ident[0:64, 0:64])
                    S.copy(Vpair[:, n0:n0 + nn, :], ps2[:, 0:nn * 64].rearrange("p (j c) -> p j c", c=64))
                S.ts(kkT[:], kT[:], pk[:, 0:1], None, ALU.mult)
                S.tt(ch[:], kkT[:], kkT[:], ALU.mult, e='pool')
                for n0 in range(0, T, 512):
                    nw = min(512, T - n0)
                    ps = K.bank()
                    S.mm(ps[0:64, :nw], ones[:], ch[:, n0:n0 + nw])
                    S.ts(bh[:, n0:n0 + nw], ps[0:64, :nw], EPS, None, ALU.add)
                S.act(bh[:], bh[:], AF.Sqrt)
                S.recip(bh[:], bh[:])
                S.tt(kkT[:], kkT[:], bh[:], ALU.mult)
                es_a.close()
                S.barrier()
                for d in range(2):
                    lastidx = 63 if d == 0 else 0
                    es_a = ExitStack()
                    Lb = K.sb(es_a, [64, T])
                    r0 = 768 + d * 32
                    load_mu(S, W, l, mu, r0, 32)
                    S.dma(kh[0:32, :], D['pT'][base + r0: base + r0 + 32, :])
                    shift_T(S, bh, kh, mu, 32)
                    S.act(bh[0:32, :], bh[0:32, :], AF.Tanh)
                    for n0 in range(0, T, 512):
                        nw = min(512, T - n0)
                        ps = K.bank()
                        S.mm(ps[0:64, :nw], wup[:, d, :], bh[0:32, n0:n0 + nw])
                        S.act(Lb[:, n0:n0 + nw], ps[0:64, :nw], AF.Sigmoid, bias=pk[:, 3 + d:4 + d])
                    S.ts(Lb[:], Lb[:], -math.exp(-0.5), None, ALU.mult)
                    for n in range(NCH):
                        ck = slice(n * 64, (n + 1) * 64)
                        if d == 0:
                            S.scan(rh[:, ck], ones[:, :], Lb[:, ck], 0.0, ALU.mult, ALU.add)
                        else:
                            S.scan(rh[:, ck][:, ::-1], ones[:, :], Lb[:, ck][:, ::-1], 0.0, ALU.mult, ALU.add)
                    S.act(GLc[:], rh[:, :].rearrange("p (n i) -> p n i", i=64)[:, :, lastidx], AF.Exp)
                    S.tt(ch[:], rh[:], Lb[:], ALU.subtract, e='pool')
                    S.act(ch[:], ch[:], AF.Exp)
                    S.tt(ch[:], ch[:], kkT[:], ALU.mult, e='pool')
                    r0 = 832 + d * 32
                    load_mu(S, W, l, mu, r0, 32)
                    S.dma(Lb[0:32, :], D['pT'][base + r0: base + r0 + 32, :])
                    shift_T(S, bh, Lb, mu, 32)
                    for n0 in range(0, T, 512):
                        nw = min(512, T - n0)
                        ps = K.bank()
                        S.mm(ps[0:64, :nw], aup[:, d, :], bh[0:32, n0:n0 + nw])
                        S.act(kh[:, n0:n0 + nw], ps[0:64, :nw], AF.Sigmoid, bias=pk[:, 5 + d:6 + d])
                    S.act(bh[:], rh[:], AF.Exp, scale=-1.0)
                    S.tt(bh[:], bh[:], kkT[:], ALU.mult)
                    S.tt(bh[:], bh[:], kh[:], ALU.mult, e='pool')
                    S.ts(kh[:], kh[:], -1.0, pk[:, 1:2], ALU.add, ALU.mult)
                    S.stt(kh[:], kh[:], 1.0, kT[:], ALU.add, ALU.mult)
                    S.stt(Lb[:], kh[:], pk[:, 2:3], rT[:], ALU.mult, ALU.mult)
                    ps = K.bank()
                    for n in range(NCH):
                        S.mm(ps[0:64, n:n + 1], Lb[:, n * 64:(n + 1) * 64], ones[:, 0:1])
                    if d == 0:
                        S.copy(bon[:], ps[0:64, 0:NCH])
                    else:
                        S.tt(bon[:], bon[:], ps[0:64, 0:NCH], ALU.add)
                    S.act(Lb[:], rh[:], AF.Exp, scale=-1.0)
                    S.tt(kh[:], kh[:], Lb[:], ALU.mult, e='pool')
                    S.act(rh[:], rh[:], AF.Exp)
                    S.tt(rh[:], rh[:], rT[:], ALU.mult)
                    es_a.close()
                    S.barrier()
                    es_b = ExitStack()
                    NS = DBG.get('b_ns', 5)
                    sets = []
                    for _s in range(NS):
                        sets.append(dict(
                            BB=K.sb(es_b, [128, 2, 128]), PP=K.sb(es_b, [128, 2, 128]), N2=[K.sb(es_b, [128, 2, 128]) for _ in range(2)],
                            AV=K.sb(es_b, [128, 64]), Cp=K.sb(es_b, [128, 64]),
                            WcT=K.sb(es_b, [64, 128]), us=K.sb(es_b, [64, 2, 64])))
                        sets[-1]['AkT'] = sets[-1]['N2'][0][:, 0, :]
                        sets[-1]['QQ'] = sets[-1]['N2'][0][0:64, :, :].rearrange("p a (b c) -> p (a b) c", c=64)
                        sets[-1]['KN'] = sets[-1]['N2'][1][0:64, :, :].rearrange("p a (b c) -> p (a b) c", c=64)
                    S.memset(Ms[0][:], 0.0)
                    S.copy(sgn[:, 0:2, :], masks[:, d, :].unsqueeze(1).to_broadcast([64, 2, 64]), e='pool')
                    S.ts(sgn[:, 2:4, :], sgn[:, 0:2, :], -1.0, None, ALU.mult, e='pool')
                    order = scan_order(d)
                    pairs = [order[i] // 2 for i in range(0, NCH, 2)]
                    state = {'si': 0}

                    recst = {'banks': list(range(NS, 8)), 'pz': 0}
                    def prep(pidx, st):
                        pi = pairs[pidx]
                        BB, PP = st['BB'], st['PP']
                        tk = slice(pi * 128, (pi + 1) * 128)
                        ps = chain_ps(K, st)
                        S.mm(ps[:, 0:128], bh[:, tk], ch[:, tk])
                        S.mm(ps[:, 128:256], ch[:, tk], bh[:, tk])
                        psk = chain_ps(K, st)
                        S.mm(psk[:, 0:128], kh[:, tk], ch[:, tk])
                        yield
                        S.stt(n32(BB[:, 0, :]), ps[:, 0:128], -1.0, m128[:, 2 + d, :], ALU.mult, ALU.mult)
                        yield
                        S.stt(n32(BB[:, 1, :]), ps[:, 128:256], -1.0, m128[:, 3 - d, :], ALU.mult, ALU.mult)
                        yield
                        S.tt(n32(PP[:]), BB[:], identB, ALU.add, e='pool')
                        S.tt(st['AkT'], psk[:, 0:128], m128[:, 2 + d, :], ALU.mult)
                        yield
                        ps = chain_ps(K, st)
                        S.mm(ps[:, 0:64], st['AkT'], Vpair[:, pi, :])
                        S.tr(ps[:, 64:128], ch[:, tk], K.ident[0:64, 0:64])
                        yield
                        S.copy(st['AV'][:], ps[:, 0:64], e='act')
                        S.copy(st['Cp'][:], ps[:, 64:128], e='act')
                        yield
                        yield from neumann_gen(K, S, PP, BB, st['N2'], st)
                        P = PP[:, 0, :]
                        ps = chain_ps(K, st)
                        for c in range(2):
                            ck = slice(pi * 128 + c * 64, pi * 128 + (c + 1) * 64)
                            S.tr(ps[0:64, c * 64:(c + 1) * 64], kh[:, ck], K.ident[0:64, 0:64])
                            S.tr(ps[0:64, 128 + c * 64:128 + (c + 1) * 64], bh[:, ck], K.ident[0:64, 0:64])
                        yield
                        S.copy(st['KN'][:, 0:2, :], ps[0:64, 0:128].rearrange("p (c k) -> p c k", c=2), e='act')
                        yield
                        S.ts(st['KN'][:, 2:4, :], ps[0:64, 128:256].rearrange("p (c k) -> p c k", c=2), -1.0, None, ALU.mult)
                        yield
                        ps = chain_ps(K, st)
                        S.mm(ps[0:64, 0:128], st['Cp'][:], P)
                        for c in range(2):
                            S.mm(ps[0:64, 128 + c * 64:128 + (c + 1) * 64], PP[:, 0, c * 64:(c + 1) * 64], st['AV'][:])
                        yield
                        S.copy(st['WcT'][:], ps[0:64, 0:128], e='act')
                        S.copy(st['us'][:], ps[0:64, 128:256].rearrange("p (c v) -> p c v", c=2), e='act')
                        yield
                        ps = chain_ps(K, st)
                        for c in range(2):
                            ck = slice(pi * 128 + c * 64, pi * 128 + (c + 1) * 64)
                            S.mm(ps[0:64, c * 64:(c + 1) * 64], kh[:, ck], rh[:, ck])
                            S.mm(ps[0:64, 128 + c * 64:128 + (c + 1) * 64], bh[:, ck], rh[:, ck])
                        yield
                        S.tt(st['QQ'], ps[0:64, 0:256].rearrange("p (c i) -> p c i", c=4), sgn[:], ALU.mult)
                        yield

                    def rec(pidx, st):
                        pi = pairs[pidx]
                        for c in ((0, 1) if d == 0 else (1, 0)):
                            n = pi * 2 + c
                            ck = slice(n * 64, (n + 1) * 64)
                            si = state['si']
                            Mc, Mn = Ms[si % 2], Ms[(si + 1) % 2]
                            zn, mt = zns[si % 2], mts[si % 2]
                            state['si'] = si + 1
                            ps1 = chain_ps(K, recst, 128)
                            S.mm(ps1[0:64, 0:64], st['WcT'][:, c * 64:(c + 1) * 64], Mc[:])
                            yield
                            S.tt(zn[:], st['us'][:, c, :], ps1[0:64, 0:64], ALU.add)
                            yield
                            ps2 = chain_ps(K, recst, 128)
                            S.mm(ps2[0:64, 0:64], rh[:, ck], Mc[:], start=True, stop=False)
                            S.mm(ps2[0:64, 0:64], st['QQ'][:, c, :], Vtok[:, n, :], start=False, stop=False)
                            S.mm(ps2[0:64, 0:64], st['QQ'][:, 2 + c, :], zn[:], start=False, stop=True)
                            ps3 = chain_ps(K, recst, 128)
                            S.mm(ps3[0:64, 0:64], st['KN'][:, c, :], Vtok[:, n, :], start=True, stop=False)
                            S.mm(ps3[0:64, 0:64], st['KN'][:, 2 + c, :], zn[:], start=False, stop=True)
                            yield
                            S.ts(mt[:], ps3[0:64, 0:64], GLc[:, n:n + 1], None, ALU.mult)
                            if d == 0:
                                S.copy(hacc[:, n, :], ps2[0:64, 0:64], e='act')
                            else:
                                S.tt(hacc[:, n, :], hacc[:, n, :], ps2[0:64, 0:64], ALU.add)
                            yield
                            S.stt(Mn[:], Mc[:], GLc[:, n:n + 1], mt[:], ALU.mult, ALU.add)
                            yield

                    run_pipeline(min(len(pairs), DBG.get('b_pairs', 99)), prep, rec, sets)
                    es_b.close()
                    S.barrier()
                gtok = kh[:, :].rearrange("p (n c) -> p n c", c=64)
                load_mu(S, W, l, mu, 896, 64)
                S.dma(rh[:], D['pT'][base + 896: base + 960, :])
                shift_T(S, bh, rh, mu, 64)
                S.act(bh[:], bh[:], AF.Sigmoid)
                for n0 in range(0, NCH, 8):
                    nn = min(8, NCH - n0)
                    ps = K.bank()
                    for j in range(nn):
                        S.mm(ps[0:64, j * 64:(j + 1) * 64], bh[:, (n0 + j) * 64:(n0 + j + 1) * 64], gup[:])
                    S.copy(gtok[:, n0:n0 + nn, :], ps[0:64, 0:nn * 64].rearrange("p (j c) -> p j c", c=64), e='act')
                st = K.sb(es, [64, NCH, 4])
                sq = ch[:, :].rearrange("p (n c) -> p n c", c=64)
                bc3 = lambda a: a.unsqueeze(2).to_broadcast([64, NCH, 64])
                S.reduce(st[:, :, 0], hacc[:], ALU.add)
                S.ts(st[:, :, 0], st[:, :, 0], 1.0 / 64, None, ALU.mult)
                S.tt(hacc[:], hacc[:], bc3(st[:, :, 0]), ALU.subtract)
                S.tt(sq, hacc[:], hacc[:], ALU.mult, e='pool')
                S.reduce(st[:, :, 1], sq, ALU.add)
                S.ts(st[:, :, 1], st[:, :, 1], 1.0 / 64, 64e-5, ALU.mult, ALU.add)
                S.act(st[:, :, 2], st[:, :, 1], AF.Sqrt)
                S.recip(st[:, :, 3], st[:, :, 2])
                S.tt(hacc[:], hacc[:], bc3(st[:, :, 3]), ALU.mult)
                S.tt(hacc[:], hacc[:], lng[:, 0, :].unsqueeze(1).to_broadcast([64, NCH, 64]), ALU.mult, e='pool')
                S.tt(hacc[:], hacc[:], lng[:, 1, :].unsqueeze(1).to_broadcast([64, NCH, 64]), ALU.add, e='pool')
                S.tt(sq, Vtok[:], bc3(bon[:, :]), ALU.mult)
                S.tt(hacc[:], hacc[:], sq, ALU.add, e='pool')
                S.tt(hacc[:], hacc[:], gtok, ALU.mult)
                c0 = 256 + h * 64
                yv = D['y'][:, c0:c0 + 64].rearrange("(n i) c -> i n c", i=64)
                for g4 in range(4):
                    S.dma(yv[:, g4 * 17:(g4 + 1) * 17, :], hacc[:, g4 * 17:(g4 + 1) * 17, :], q=('sp', 'pool')[g4 % 2])
            S.barrier()
    S.barrier()


W_SHAPES = {
    'xin': [T, 1024], 'cc': [128, 8, 2],
    'mod_w': [4, 1024, 6144], 'mod_b': [4, 6144], 'norm_mix_g': [4, 1024], 'norm_ffn_g': [4, 1024],
    'w_in': [4, 1024, IN_COLS], 'w_out': [4, 1024, 1024],
    'lru_conv_w': [4, 4, 256], 'lru_conv_b': [4, 256], 'lru_w_a': [4, 2, 4, 64, 64], 'lru_b_a': [4, 2, 256],
    'lru_w_x': [4, 2, 4, 64, 64], 'lru_b_x': [4, 2, 256], 'lru_lambda': [4, 2, 256],
    'rwkv_mu': [4, 2, 960], 'rwkv_w_up': [4, 2, 32, 256], 'rwkv_w0': [4, 2, 256], 'rwkv_a_up': [4, 2, 32, 256],
    'rwkv_a0': [4, 2, 256], 'rwkv_g_up': [4, 64, 256], 'rwkv_k_k': [4, 256], 'rwkv_k_a': [4, 256],
    'rwkv_r_k': [4, 256], 'rwkv_ln_g': [4, 256], 'rwkv_ln_b': [4, 256],
    'mlstm_i_b': [4, 2, 4], 'mlstm_f_b': [4, 2, 4], 'mlstm_norm_g': [4, 256],
    'gdn_conv_w': [4, 4, 768], 'gdn_a_log': [4, 2, 4], 'gdn_dt_bias': [4, 2, 4], 'gdn_norm_g': [4, 256],
    'ffn_w_gate': [2, 1024, D_FF], 'ffn_w_up': [2, 1024, D_FF], 'ffn_w_down': [2, D_FF, 1024],
    'moe_router': [2, 1024, 8], 'moe_w_gate': [2, 8, 1024, D_FFE], 'moe_w_up': [2, 8, 1024, D_FFE],
    'moe_w_down': [2, 8, D_FFE, 1024], 'final_norm_g': [1, 1024],
    'c_ident': [128, 128], 'c_sel8': [8, 8, 128],
    'pk_lru': [4, 2, 128, 11], 'pk_ml': [4, 4, 4],
    'c_masks': [64, 2, 64], 'c_rst': [4, 2, T], 'c_m128': [128, 4, 128],
    'pk_gd': [4, 4, 4], 'pk_gdc': [4, 4, 64, 3, 4], 'pk_mu': [4, 960, 2], 'pk_rw': [4, 4, 64, 7],
}


def make_consts():
    c = {}
    c['c_ident'] = np.eye(128, dtype=np.float32)
    s = np.zeros((8, 8, 128), np.float32)
    for e in range(8):
        s[e, e, :] = 1.0
    c['c_sel8'] = s
    jj, ii = np.meshgrid(np.arange(64), np.arange(64), indexing='ij')
    c['c_masks'] = np.ascontiguousarray(np.stack([(ii >= jj), (ii <= jj)], axis=1).astype(np.float32))
    ja, ia = np.meshgrid(np.arange(128), np.arange(128), indexing='ij')
    same = (ja // 64) == (ia // 64)
    c['c_m128'] = np.ascontiguousarray(np.stack([same & (ia >= ja), same & (ia <= ja), same & (ia > ja), same & (ia < ja)], axis=1).astype(np.float32))
    idx = np.arange(T) % 64
    r = np.stack([(idx != 0), (idx != 63)], axis=0).astype(np.float32)
    c['c_rst'] = np.ascontiguousarray(np.broadcast_to(r[None], (4, 2, T)))
    return c


def build(layers=(0, 1, 2, 3), mixers=None, final=True, dbg=()):
    nc = bass.Bass("TRN2", target_bir_lowering=False)
    W = {n: nc.dram_tensor(n, sh, F32, kind="ExternalInput").ap() for n, sh in W_SHAPES.items()}
    out = nc.dram_tensor('out', [SEQ, 1024], F32, kind="ExternalOutput").ap()
    D = {}
    for n, sh in {'xres': [T, 1024], 'pT': [IN_COLS, T], 'y': [T, 1024], 'mod': [2, 6144]}.items():
        kind = "ExternalOutput" if n in dbg else "Internal"
        D[n] = nc.dram_tensor('d_' + n, sh, F32, kind=kind).ap()
    with ExitStack() as es:
        S = Sched(nc, es)
        ps = [es.enter_context(nc.psum_tensor("psb%d" % i, [128, 512], F32)) for i in range(8)]
        ident = es.enter_context(nc.sbuf_tensor("ident", [128, 128], F32))
        K = Ctx(nc, S, W, D, ps, ident)
        K.out = out
        S.dma(ident[:], W['c_ident'])
        for t in range(NT):
            S.dma(D['xres'][t * 128:(t + 1) * 128, :], W['xin'][t * 128:(t + 1) * 128, :], q=('sp', 'pool', 'act')[t % 3])
        S.barrier()
        for l in layers:
            last = (l == 3)
            stage_mod(K, l)
            stage_inproj(K, l)
            if mixers is None:
                mix_identity(K, l)
            else:
                for m in mixers:
                    m(K, l, last)
            stage_outproj(K, l, last)
            if l % 2 == 0:
                stage_ffn(K, l, last)
            else:
                stage_moe(K, l, last)
        if final:
            stage_final(K)
        S.finish()
    K.S = S
    return nc, S


def make_packs(inputs):
    f = lambda n: np.asarray(inputs[n], dtype=np.float32)
    pk = {}
    cols = [f('lru_conv_w')[:, j, :] for j in range(4)] + [f('lru_conv_b')]
    cols += [f('lru_b_a')[:, 0], f('lru_b_a')[:, 1], f('lru_b_x')[:, 0], f('lru_b_x')[:, 1], f('lru_lambda')[:, 0], f('lru_lambda')[:, 1]]
    a = np.stack(cols, axis=-1)
    pk['pk_lru'] = np.ascontiguousarray(a.reshape(4, 2, 128, 11))
    pk['pk_gd'] = np.ascontiguousarray(np.concatenate([f('gdn_a_log'), f('gdn_dt_bias')], axis=1).transpose(0, 2, 1))
    pk['pk_gdc'] = np.ascontiguousarray(f('gdn_conv_w').reshape(4, 4, 3, 4, 64).transpose(0, 3, 4, 2, 1))
    pk['pk_mu'] = np.ascontiguousarray(f('rwkv_mu').transpose(0, 2, 1))
    cols = [f('rwkv_k_k'), f('rwkv_k_a'), f('rwkv_r_k'), f('rwkv_w0')[:, 0], f('rwkv_w0')[:, 1], f('rwkv_a0')[:, 0], f('rwkv_a0')[:, 1]]
    pk['pk_rw'] = np.ascontiguousarray(np.stack(cols, axis=-1).reshape(4, 4, 64, 7))
    pk['pk_ml'] = np.ascontiguousarray(np.concatenate([f('mlstm_i_b'), f('mlstm_f_b')], axis=1).transpose(0, 2, 1))
    return pk


def host_inputs(inputs, b):
    m = {}
    m['xin'] = np.ascontiguousarray(np.concatenate([inputs['ctx'][b], inputs['x'][b]], axis=0))
    cc = np.stack([np.asarray(inputs['c'][b]).reshape(8, 128).T, np.asarray(inputs['c_ctx']).reshape(8, 128).T], axis=-1)
    m['cc'] = np.ascontiguousarray(cc.astype(np.float32))
    for n in W_SHAPES:
        if n in m or n.startswith('c_') or n.startswith('pk_'):
            continue
        m[n] = np.ascontiguousarray(np.asarray(inputs[n], dtype=np.float32).reshape(W_SHAPES[n]))
    m.update(make_consts())
    m.update(make_packs(inputs))
    return m


def build_test(L, mixers):
    nc = bass.Bass("TRN2", target_bir_lowering=False)
    W = {n: nc.dram_tensor(n, sh, F32, kind="ExternalInput").ap() for n, sh in W_SHAPES.items()}
    D = {}
    for n, sh in {'xres': [T, 1024], 'pT': [IN_COLS, T], 'y': [T, 1024], 'mod': [2, 6144]}.items():
        kind = "ExternalOutput" if n in ('pT', 'y') else "Internal"
        D[n] = nc.dram_tensor('d_' + n, sh, F32, kind=kind).ap()
    add_scratch(nc, D)
    with ExitStack() as es:
        S = Sched(nc, es)
        ps = [es.enter_context(nc.psum_tensor("psb%d" % i, [128, 512], F32)) for i in range(8)]
        ident = es.enter_context(nc.sbuf_tensor("ident", [128, 128], F32))
        K = Ctx(nc, S, W, D, ps, ident)
        S.dma(ident[:], W['c_ident'])
        for t in range(NT):
            S.dma(D['xres'][t * 128:(t + 1) * 128, :], W['xin'][t * 128:(t + 1) * 128, :], q=('sp', 'pool', 'act')[t % 3])
        S.barrier()
        stage_mod(K, L)
        stage_inproj(K, L)
        for m in mixers:
            m(K, L, False)
        S.finish()
    return nc, S


def add_scratch(nc, D):
    pass


def kernel(**inputs):
    nc, S = build(layers=(0, 1, 2, 3), mixers=[mixer_a, mixer_b, mixer_c, mixer_d], final=True)
    in_maps = [host_inputs(inputs, b) for b in range(8)]
    res = run_bass_kernel_spmd(nc, in_maps, core_ids=list(range(8)))
    return np.stack([np.asarray(r['out'], dtype=np.float32) for r in res.results], axis=0)
```
